# Optimizing a Trainium2 kernel written in Bass

```python
import jax, jax.numpy as jnp
from jax import lax
import numpy as np

D_MODEL = 2048
BATCH = 4
SEQ = 4096
DEPTH = 2

GRID_W = 64
NA_HEADS = 16
NA_HEAD_DIM = 64
NA_WIDTH = NA_HEADS * NA_HEAD_DIM
NA_KH_MAX = 8
NA_KW = 16
MLA_HEADS = 8
MLA_NOPE = 128
MLA_ROPE = 64
MLA_V = 128
MLA_Q_LORA = 448
MLA_KV_LORA = 160
MLA_WIDTH = MLA_HEADS * MLA_V
ROPE_THETA = 10000.0
Q_BLOCK = 128
FNET_GROUPS = 4
FNET_GROUP_DIM = 256
FNET_WIDTH = FNET_GROUPS * FNET_GROUP_DIM
N_BRANCHES = 3
BRANCH_WIDTH = NA_WIDTH
IN_SPLITS = (3 * NA_WIDTH, MLA_Q_LORA, MLA_KV_LORA, MLA_ROPE, FNET_WIDTH, N_BRANCHES * D_MODEL)
IN_COLS = sum(IN_SPLITS)
N_EXPERTS = 16
EXPERT_FF = 2048
CAPACITY_FACTOR = 2
RMS_EPS = 1e-6
NEG_INF = -1e30

kernel_name = "hybrid_na_mla_fnet_ecmoe_encoder"


def rmsnorm(x, g):
    xf = x.astype(jnp.float32)
    y = xf * lax.rsqrt(jnp.mean(xf * xf, axis=-1, keepdims=True) + RMS_EPS)
    return (y * g.astype(jnp.float32)).astype(x.dtype)


def rope_tables(seq):
    pos = jnp.arange(seq, dtype=jnp.float32)
    inv = 1.0 / (ROPE_THETA ** (jnp.arange(0, MLA_ROPE, 2, dtype=jnp.float32) / MLA_ROPE))
    ang = pos[:, None] * inv[None, :]
    return jnp.cos(ang), jnp.sin(ang)


def apply_rope(x, cos, sin):
    xf = x.astype(jnp.float32)
    x1, x2 = jnp.split(xf, 2, axis=-1)
    return jnp.concatenate([x1 * cos - x2 * sin, x2 * cos + x1 * sin], axis=-1).astype(x.dtype)


def neighbourhood_attention(q, k, v, rpb):
    B, S, H, dh = q.shape
    rows = S // GRID_W
    kh = min(NA_KH_MAX, rows)
    qg = q.reshape(B, rows, GRID_W, H, dh)
    kg = k.reshape(B, rows, GRID_W, H, dh)
    vg = v.reshape(B, rows, GRID_W, H, dh)
    cols = np.arange(GRID_W)
    col_start = np.clip(cols - NA_KW // 2, 0, GRID_W - NA_KW)
    col_valid = (cols[None, :] >= col_start[:, None]) & (cols[None, :] < col_start[:, None] + NA_KW)
    col_idx = np.clip(cols[None, :] - cols[:, None] + NA_KW - 1, 0, 2 * NA_KW - 2)
    scale = NA_HEAD_DIM ** -0.5

    def row_block(args):
        r, qr = args
        start = jnp.clip(r - kh // 2, 0, rows - kh)
        kb = lax.dynamic_slice_in_dim(kg, start, kh, axis=1)
        vb = lax.dynamic_slice_in_dim(vg, start, kh, axis=1)
        dr = start + jnp.arange(kh) - r + NA_KH_MAX - 1
        bias = rpb[:, dr[None, :, None], col_idx[:, None, :]]
        s = jnp.einsum('bqhd,bijhd->bhqij', qr, kb).astype(jnp.float32) * scale + bias.astype(jnp.float32)
        s = jnp.where(col_valid[:, None, :], s, NEG_INF)
        p = jax.nn.softmax(s.reshape(B, H, GRID_W, kh * GRID_W), axis=-1)
        p = p.reshape(B, H, GRID_W, kh, GRID_W).astype(v.dtype)
        return jnp.einsum('bhqij,bijhd->bqhd', p, vb)

    out = lax.map(row_block, (jnp.arange(rows), jnp.moveaxis(qg, 1, 0)))
    return jnp.moveaxis(out, 0, 1).reshape(B, S, H * dh)


def latent_attention(c_q, c_kv, k_rope, w_uq, q_norm, w_ukv, kv_norm, cos, sin):
    B, S, _ = c_q.shape
    q = (rmsnorm(c_q, q_norm) @ w_uq).reshape(B, S, MLA_HEADS, MLA_NOPE + MLA_ROPE)
    q_nope, q_pe = q[..., :MLA_NOPE], q[..., MLA_NOPE:]
    q_pe = apply_rope(q_pe, cos[:, None, :], sin[:, None, :])
    kv = (rmsnorm(c_kv, kv_norm) @ w_ukv).reshape(B, S, MLA_HEADS, MLA_NOPE + MLA_V)
    k_nope, v = kv[..., :MLA_NOPE], kv[..., MLA_NOPE:]
    k_pe = apply_rope(k_rope, cos, sin)
    nb = S // Q_BLOCK
    scale = (MLA_NOPE + MLA_ROPE) ** -0.5

    def to_blocks(t):
        return jnp.moveaxis(t.reshape(B, nb, Q_BLOCK, *t.shape[2:]), 1, 0)

    def q_block(args):
        qn, qp = args
        s = (jnp.einsum('bqhd,bkhd->bhqk', qn, k_nope)
             + jnp.einsum('bqhr,bkr->bhqk', qp, k_pe)).astype(jnp.float32) * scale
        p = jax.nn.softmax(s, axis=-1).astype(v.dtype)
        return jnp.einsum('bhqk,bkhd->bqhd', p, v)

    out = lax.map(q_block, (to_blocks(q_nope), to_blocks(q_pe)))
    return jnp.moveaxis(out, 0, 1).reshape(B, S, MLA_HEADS * MLA_V)


def fourier_mix(u):
    B, S, _ = u.shape
    g = u.reshape(B, S, FNET_GROUPS, FNET_GROUP_DIM).astype(jnp.float32)
    f = jnp.fft.fft2(g, axes=(1, 3), norm='ortho').real
    return f.astype(u.dtype).reshape(B, S, FNET_WIDTH)


def hybrid_mixer(xn, w_in, b_gate, w_uq, q_norm, w_ukv, kv_norm, rpb, w_branch, w_o, cos, sin):
    B, S, _ = xn.shape
    proj = xn @ w_in
    offsets = [int(o) for o in np.cumsum(IN_SPLITS)[:-1]]
    qkv_na, c_q, c_kv, k_rope, u_f, gate_logits = jnp.split(proj, offsets, axis=-1)
    q_na, k_na, v_na = [t.reshape(B, S, NA_HEADS, NA_HEAD_DIM) for t in jnp.split(qkv_na, 3, axis=-1)]
    y_na = neighbourhood_attention(q_na, k_na, v_na, rpb)
    y_mla = latent_attention(c_q, c_kv, k_rope, w_uq, q_norm, w_ukv, kv_norm, cos, sin)
    y_f = fourier_mix(u_f)
    gates = jax.nn.sigmoid((gate_logits + b_gate).astype(jnp.float32)).astype(xn.dtype)
    gates = gates.reshape(B, S, N_BRANCHES, D_MODEL)
    merged = (gates[:, :, 0] * (y_na @ w_branch[0])
              + gates[:, :, 1] * (y_mla @ w_branch[1])
              + gates[:, :, 2] * (y_f @ w_branch[2]))
    return merged @ w_o


def expert_choice_ffn(xn, w_router, w_g, w_u, w_d):
    B, S, D = xn.shape
    cap = CAPACITY_FACTOR * S // N_EXPERTS
    aff = jax.nn.softmax((xn @ w_router).astype(jnp.float32), axis=-1)
    gate, idx = lax.top_k(jnp.swapaxes(aff, 1, 2), cap)
    xe = jax.vmap(lambda xb, ib: xb[ib])(xn, idx)
    h = jax.nn.silu(jnp.einsum('becd,edf->becf', xe, w_g)) * jnp.einsum('becd,edf->becf', xe, w_u)
    ye = jnp.einsum('becf,efd->becd', h, w_d) * gate[..., None].astype(xn.dtype)
    return jax.vmap(lambda yb, ib: jnp.zeros((S, D), yb.dtype).at[ib.reshape(-1)].add(yb.reshape(-1, D)))(ye, idx)


def setup_inputs(seed: int = 0) -> dict:
    key = jax.random.key(seed)
    ks = jax.random.split(key, 20)

    def w(k, shape, fan_in):
        return jax.random.normal(k, shape, jnp.float32) * fan_in ** -0.5

    def gain(k, shape):
        return 1.0 + 0.05 * jax.random.normal(k, shape, jnp.float32)

    return {
        'x': jax.random.normal(ks[0], (BATCH, SEQ, D_MODEL), jnp.float32),
        'w_in': w(ks[1], (DEPTH, D_MODEL, IN_COLS), D_MODEL),
        'b_gate': 0.01 * jax.random.normal(ks[2], (DEPTH, N_BRANCHES * D_MODEL), jnp.float32),
        'w_uq': w(ks[3], (DEPTH, MLA_Q_LORA, MLA_HEADS * (MLA_NOPE + MLA_ROPE)), MLA_Q_LORA),
        'q_norm': gain(ks[4], (DEPTH, MLA_Q_LORA)),
        'w_ukv': w(ks[5], (DEPTH, MLA_KV_LORA, MLA_HEADS * (MLA_NOPE + MLA_V)), MLA_KV_LORA),
        'kv_norm': gain(ks[6], (DEPTH, MLA_KV_LORA)),
        'na_rpb': 0.1 * jax.random.normal(ks[7], (DEPTH, NA_HEADS, 2 * NA_KH_MAX - 1, 2 * NA_KW - 1), jnp.float32),
        'w_branch': w(ks[8], (DEPTH, N_BRANCHES, BRANCH_WIDTH, D_MODEL), BRANCH_WIDTH),
        'w_o': w(ks[9], (DEPTH, D_MODEL, D_MODEL), D_MODEL),
        'norm_mix': gain(ks[10], (DEPTH, D_MODEL)),
        'norm_moe': gain(ks[11], (DEPTH, D_MODEL)),
        'w_router': w(ks[12], (DEPTH, D_MODEL, N_EXPERTS), D_MODEL),
        'w_exp_gate': w(ks[13], (DEPTH, N_EXPERTS, D_MODEL, EXPERT_FF), D_MODEL),
        'w_exp_up': w(ks[14], (DEPTH, N_EXPERTS, D_MODEL, EXPERT_FF), D_MODEL),
        'w_exp_down': w(ks[15], (DEPTH, N_EXPERTS, EXPERT_FF, D_MODEL), EXPERT_FF),
        'norm_final': gain(ks[16], (D_MODEL,)),
    }


def reference(x, w_in, b_gate, w_uq, q_norm, w_ukv, kv_norm, na_rpb, w_branch, w_o,
              norm_mix, norm_moe, w_router, w_exp_gate, w_exp_up, w_exp_down, norm_final):
    cos, sin = rope_tables(x.shape[1])
    h = x
    for l in range(DEPTH):
        h = h + hybrid_mixer(rmsnorm(h, norm_mix[l]), w_in[l], b_gate[l], w_uq[l], q_norm[l],
                             w_ukv[l], kv_norm[l], na_rpb[l], w_branch[l], w_o[l], cos, sin)
        h = h + expert_choice_ffn(rmsnorm(h, norm_moe[l]), w_router[l], w_exp_gate[l],
                                  w_exp_up[l], w_exp_down[l])
    return rmsnorm(h, norm_final)
```

```python
import numpy as np
import concourse.bass as bass
import concourse.mybir as mybir
from concourse.bass_utils import run_bass_kernel_spmd

F32 = mybir.dt.float32
BF16 = mybir.dt.bfloat16
I32 = mybir.dt.int32
U32 = mybir.dt.uint32
U16 = mybir.dt.uint16
AF = mybir.ActivationFunctionType
ALU = mybir.AluOpType
AX = mybir.AxisListType


class Buf:
    __slots__ = ("name", "w", "r")

    def __init__(self, name=""):
        self.name = name
        self.w = []
        self.r = []


def _merge(tokens):
    d = {}
    for s, v in tokens:
        if d.get(s, 0) < v:
            d[s] = v
    return d


class Sched:
    ENG = ("pe", "act", "dve", "pool", "sp")

    def __init__(self, nc, es, n_dma_sems=40, rot=30000):
        self.nc = nc
        self.es = es
        self.rot = rot
        self.lists = {e: [] for e in self.ENG}
        self.sems = []
        self.cur = {}
        self.known = {e: {} for e in self.ENG}
        for e in self.ENG:
            self.cur[e] = [self._new_sem("e_" + e), 0]
        self.dma_pool = [[self._new_sem("d%d" % i), 0] for i in range(n_dma_sems)]
        self.dma_next = 0
        self.n_ops = 0

    def _new_sem(self, name):
        h = self.es.enter_context(self.nc.semaphore(name + "_%d" % len(self.sems)))
        self.sems.append(h)
        return len(self.sems) - 1

    def op(self, eng, fn, reads=(), writes=(), dma=False):
        deps = []
        for b in reads:
            deps += b.w
        for b in writes:
            deps += b.w
            deps += b.r
        tok_extra = None
        if dma:
            slot = self.dma_pool[self.dma_next]
            self.dma_next = (self.dma_next + 1) % len(self.dma_pool)
            if slot[1] > 0:
                deps.append((slot[0], 16 * slot[1]))
            slot[1] += 1
            token = (slot[0], 16 * slot[1])
            inc = (slot[0], 16)
        else:
            c = self.cur[eng]
            if c[1] >= self.rot:
                c[0] = self._new_sem("e_" + eng)
                c[1] = 0
            c[1] += 1
            token = (c[0], c[1])
            inc = (c[0], 1)
        need = _merge(deps)
        kn = self.known[eng]
        waits = []
        own = self.cur[eng][0]
        for s, v in need.items():
            if eng == "pe" and s == own and not dma:
                continue
            if kn.get(s, 0) >= v:
                continue
            kn[s] = v
            waits.append((s, v))
        self.lists[eng].append((waits, fn, inc))
        for b in writes:
            b.w = [token]
            b.r = []
        for b in reads:
            if b in writes:
                continue
            m = _merge(b.r + [token])
            b.r = list(m.items())
        self.n_ops += 1
        return token

    def wait_all(self, eng, bufs):
        deps = []
        for b in bufs:
            deps += b.w
        need = _merge(deps)
        waits = [(s, v) for s, v in need.items()]
        self.lists[eng].append((waits, None, None))

    def emit(self):
        nc = self.nc
        sems = self.sems
        lists = self.lists

        def run(engname, e):
            for waits, fn, inc in lists[engname]:
                for s, v in waits:
                    e.wait_ge(sems[s], v)
                if fn is not None:
                    ins = fn(e)
                    ins.then_inc(sems[inc[0]], inc[1])

        with nc.Block() as block:
            @block.tensor
            def _(e):
                run("pe", e)

            @block.scalar
            def _(e):
                run("act", e)

            @block.vector
            def _(e):
                run("dve", e)

            @block.gpsimd
            def _(e):
                run("pool", e)

            @block.sync
            def _(e):
                run("sp", e)


import contextlib

D = 2048
T = 4096
INC = 10912
EPS = 1e-6


_UID = [0]


def U(name):
    _UID[0] += 1
    return "%s_u%d" % (name, _UID[0])


class Rot:
    def __init__(self, nc, st, name, n, shape, dtype, psum=False):
        self.slots = []
        for i in range(n):
            if psum:
                t = st.enter_context(nc.psum_tensor(U("%s%d" % (name, i)), shape, dtype))
            else:
                t = st.enter_context(nc.sbuf_tensor(U("%s%d") % (name, i), shape, dtype))
            self.slots.append((t, Buf(name + str(i))))
        self.i = 0

    def nxt(self):
        s = self.slots[self.i]
        self.i = (self.i + 1) % len(self.slots)
        return s


class Ctx:
    def __init__(self, nc, es, debug_outs=()):
        self.nc = nc
        self.es = es
        self.S = Sched(nc, es)
        self.debug_outs = set(debug_outs)
        self.dbufs = {}
        self.cast_i = 0

    def dram(self, name, shape, dtype):
        kind = "ExternalOutput" if name in self.debug_outs else "Internal"
        t = self.nc.dram_tensor(name, list(shape), dtype, kind=kind).ap()
        return t

    def buf(self, key):
        if key not in self.dbufs:
            self.dbufs[key] = Buf(str(key))
        return self.dbufs[key]

    def end_stage(self):
        S = self.S
        waits = [(s[0], 16 * s[1]) for s in S.dma_pool if s[1] > 0]
        for e in ("sp", "pool", "act"):
            S.lists[e].append((list(waits), None, None))
        S.emit()
        S.lists = {e: [] for e in S.ENG}


def load_consts(C, st, ident_f):
    nc, S = C.nc, C.S
    idf = st.enter_context(nc.sbuf_tensor(U("idf"), [128, 128], F32))
    idb = st.enter_context(nc.sbuf_tensor(U("idb"), [128, 128], BF16))
    bf = Buf("idf")
    bb = Buf("idb")
    S.op("sp", lambda e: e.dma_start(out=idf[:], in_=ident_f[:, :]), writes=[bf], dma=True)
    S.op("dve", lambda e: e.tensor_copy(out=idb[:], in_=idf[:]), reads=[bf], writes=[bb])
    return idf, bf, idb, bb


def rms_tile(C, pools, src_ap, gain_t, gain_b, out_t, out_b, width, src_reads):
    nc, S = C.nc, C.S
    ht, hb = pools["h"].nxt()
    S.op("sp", lambda e: e.dma_start(out=ht[:, :width], in_=src_ap), reads=src_reads, writes=[hb], dma=True)
    jt, jb = pools["junk"].nxt()
    st_, sb_ = pools["stat"].nxt()
    S.op("act", lambda e: e.activation(out=jt[:, :width], in_=ht[:, :width], func=AF.Square, accum_out=st_[:, 0:1]),
         reads=[hb], writes=[jb, sb_])
    S.op("act", lambda e: e.activation(out=st_[:, 1:2], in_=st_[:, 0:1], func=AF.Sqrt, scale=1.0 / width, bias=pools["eps"][0][:, 0:1]),
         reads=[sb_, pools["eps"][1]], writes=[sb_])
    S.op("dve", lambda e: e.reciprocal(out=st_[:, 2:3], in_=st_[:, 1:2]), reads=[sb_], writes=[sb_])
    S.op("dve", lambda e: e.scalar_tensor_tensor(out=out_t, in0=ht[:, :width], scalar=st_[:, 2:3], in1=gain_t[:, :width],
                                                op0=ALU.mult, op1=ALU.mult),
         reads=[hb, sb_, gain_b], writes=[out_b])
    return ht, hb


def norm_pools(C, st, width=D, nh=2):
    nc, S = C.nc, C.S
    pools = {
        "h": Rot(nc, st, "nh", nh, [128, width], F32),
        "junk": Rot(nc, st, "nj", 1, [128, width], BF16),
        "stat": Rot(nc, st, "ns", 4, [128, 4], F32),
    }
    eps_t = st.enter_context(nc.sbuf_tensor(U("epsT"), [128, 1], F32))
    eb = Buf("eps")
    S.op("dve", lambda e: e.memset(eps_t[:], EPS), writes=[eb])
    pools["eps"] = (eps_t, eb)
    return pools


def load_gain(C, st, name, vec_ap, width):
    nc, S = C.nc, C.S
    g = st.enter_context(nc.sbuf_tensor(U(name), [128, width], F32))
    gb = Buf(name)
    S.op("sp", lambda e: e.dma_start(out=g[:], in_=vec_ap.partition_broadcast(128)), writes=[gb], dma=True)
    return g, gb


def pipeline(n, prep, compute, depth=1):
    for i in range(min(depth, n)):
        prep(i)
    for i in range(n):
        if i + depth < n:
            prep(i + depth)
        compute(i)


CAST_PATTERN = {"default": ("act", "dve"), "moe": ("act", "dve", "act", "dve", "pool")}


def cast_op(C, out_ap, in_ap, reads, writes, pattern="default"):
    S = C.S
    pat = CAST_PATTERN[pattern]
    eng = pat[C.cast_i % len(pat)]
    C.cast_i += 1
    if eng == "dve":
        S.op("dve", lambda e: e.tensor_copy(out=out_ap, in_=in_ap), reads=reads, writes=writes)
    elif eng == "act":
        S.op("act", lambda e: e.copy(out=out_ap, in_=in_ap), reads=reads, writes=writes)
    else:
        S.op("pool", lambda e: e.tensor_copy(out=out_ap, in_=in_ap), reads=reads, writes=writes)


def stage_inproj(C, h_ap, h_buf, gain_ap, w_in, b_gate, ident_f, sc):
    nc, S = C.nc, C.S
    HALF = 2048
    with contextlib.ExitStack() as st:
        idf, idfb, idb, idbb = load_consts(C, st, ident_f)
        pools = norm_pools(C, st)
        gain_t, gain_b = load_gain(C, st, "gain1", gain_ap, D)
        xs_pool = Rot(nc, st, "xs", 2, [128, D], BF16)
        xnT = st.enter_context(nc.sbuf_tensor(U("xnT"), [128, 16, HALF], BF16))
        xnT_b = [Buf("xnT%d" % i) for i in range(16)]
        wst = Rot(nc, st, "wst", 3, [128, 16, 128], F32)
        wbf = Rot(nc, st, "wbf", 3, [128, 16, 128], BF16)
        ost_b = Rot(nc, st, "ostb", 2, [128, HALF], BF16)
        ost_f = Rot(nc, st, "ostf", 2, [128, HALF], F32)
        bg = st.enter_context(nc.sbuf_tensor(U("bg"), [128, 48], F32))
        bgb = Buf("bg")
        S.op("sp", lambda e: e.dma_start(out=bg[:], in_=b_gate.rearrange("(c p) -> p c", p=128), allow_slow_non_contiguous=True), writes=[bgb], dma=True)
        pmm = Rot(nc, st, "pmm", 6, [128, 512], F32, psum=True)
        ptr = Rot(nc, st, "ptr", 2, [128, 8, 128], BF16, psum=True)

        chunks = []
        for i in range(16):
            chunks.append((i * 128, 128, "F", "qkT", i * 128))
        for i in range(8):
            chunks.append((2048 + i * 128, 128, "T", "vna", i * 128))
        c0 = 3072
        r0 = 0
        while r0 < 672:
            m = min(128, 672 - r0)
            chunks.append((c0 + r0, m, "C", "cT", r0))
            r0 += m
        for i in range(8):
            chunks.append((3744 + i * 128, 128, "F", "ufT", i * 128))
        for i in range(48):
            chunks.append((4768 + i * 128, 128, "G", "gT", i * 128))

        w_v = w_in
        for half in range(T // HALF):
            t0 = half * HALF
            for tt in range(16):
                xs_t, xs_b = xs_pool.nxt()
                rms_tile(C, pools, h_ap[t0 + tt * 128: t0 + (tt + 1) * 128, :], gain_t, gain_b, xs_t[:], xs_b, D, [h_buf])
                for g in range(2):
                    pt, pb = ptr.nxt()
                    for j in range(8):
                        kc = g * 8 + j
                        S.op("pe", lambda e, pt=pt, j=j, kc=kc, xs_t=xs_t: e.transpose(out=pt[:, j, :], in_=xs_t[:, kc * 128:(kc + 1) * 128], identity=idb[:]),
                             reads=[xs_b, idbb], writes=[pb])
                    S.op("dve", lambda e, pt=pt, g=g, tt=tt: e.tensor_copy(out=xnT[:, g * 8:(g + 1) * 8, tt * 128:(tt + 1) * 128], in_=pt[:]),
                         reads=[pb], writes=[xnT_b[tt]])
            wjob = {}

            def prep(i):
                (c0, m, mode, dst, dr0) = chunks[i]
                ws_t, ws_b = wst.nxt()
                S.op("sp", lambda e: e.dma_start(out=ws_t[:, :, :m], in_=w_v[:, c0:c0 + m].rearrange("(kc p) n -> p kc n", p=128)),
                     writes=[ws_b], dma=True)
                wb_t, wb_b = wbf.nxt()
                cast_op(C, wb_t[:, :, :m], ws_t[:, :, :m], [ws_b], [wb_b])
                wjob[i] = (wb_t, wb_b)

            def compute(i, t0=t0):
                (c0, m, mode, dst, dr0) = chunks[i]
                wb_t, wb_b = wjob.pop(i)
                if mode != "T":
                    if mode == "C":
                        o_t, o_b = ost_f.nxt()
                    else:
                        o_t, o_b = ost_b.nxt()
                    for tb in range(4):
                        p_t, p_b = pmm.nxt()
                        for kc in range(16):
                            S.op("pe", lambda e, p_t=p_t, kc=kc, tb=tb: e.matmul(p_t[:m, :], lhsT=wb_t[:, kc, :m], rhs=xnT[:, kc, tb * 512:(tb + 1) * 512], start=(kc == 0), stop=(kc == 15)),
                                 reads=[wb_b] + xnT_b[tb * 4:(tb + 1) * 4], writes=[p_b])
                        if mode == "G":
                            gi = dr0 // 128
                            S.op("act", lambda e, p_t=p_t, tb=tb, gi=gi: e.activation(out=o_t[:, tb * 512:(tb + 1) * 512], in_=p_t[:, :], func=AF.Sigmoid, bias=bg[:, gi:gi + 1]),
                                 reads=[p_b, bgb], writes=[o_b])
                        else:
                            S.op("dve", lambda e, p_t=p_t, tb=tb: e.tensor_copy(out=o_t[:m, tb * 512:(tb + 1) * 512], in_=p_t[:m, :]),
                                 reads=[p_b], writes=[o_b])
                    S.op("sp", lambda e: e.dma_start(out=sc[dst][dr0:dr0 + m, t0:t0 + HALF], in_=o_t[:m, :]),
                         reads=[o_b], writes=[C.buf(dst)], dma=True)
                else:
                    o_t, o_b = ost_b.nxt()
                    for g4 in range(4):
                        p_t, p_b = pmm.nxt()
                        for q in range(4):
                            tt = g4 * 4 + q
                            for kc in range(16):
                                S.op("pe", lambda e, p_t=p_t, kc=kc, tt=tt, q=q: e.matmul(p_t[:, q * 128:(q + 1) * 128], lhsT=xnT[:, kc, tt * 128:(tt + 1) * 128], rhs=wb_t[:, kc, :], start=(kc == 0), stop=(kc == 15)),
                                     reads=[wb_b, xnT_b[tt]], writes=[p_b])
                        S.op("dve", lambda e, p_t=p_t, g4=g4: e.tensor_copy(out=o_t[:, g4 * 512:(g4 + 1) * 512], in_=p_t[:, :]),
                             reads=[p_b], writes=[o_b])
                    S.op("sp", lambda e: e.dma_start(
                        out=sc["vna"][t0:t0 + HALF, dr0:dr0 + 128].rearrange("(tt p) c -> p tt c", p=128),
                        in_=o_t[:, :].rearrange("p (tt c) -> p tt c", c=128)),
                        reads=[o_b], writes=[C.buf("vna")], dma=True)

            pipeline(len(chunks), prep, compute, depth=2)
        C.end_stage()


def fm_rmsnorm(C, st, name, src, src_buf, row0, nrows, gain_ap, onesf, onesf_b, eps, pmm, out_t, out_b, cin, csq, rs):
    nc, S = C.nc, C.S
    nch = (nrows + 127) // 128
    gcol = st.enter_context(nc.sbuf_tensor(U(name + "g"), [128, nch], F32))
    gb = Buf(name + "g")
    for c in range(nch):
        ksz = min(128, nrows - c * 128)
        S.op("sp", lambda e, c=c, ksz=ksz: e.dma_start(out=gcol[:ksz, c:c + 1], in_=gain_ap[c * 128:c * 128 + ksz].rearrange("(p o) -> p o", o=1)),
             writes=[gb], dma=True)
    for tb in range(T // 512):
        ci, cib = cin.nxt()
        cs, csb = csq.nxt()
        for c in range(nch):
            ksz = min(128, nrows - c * 128)
            S.op("sp", lambda e, ci=ci, c=c, ksz=ksz, tb=tb: e.dma_start(out=ci[:ksz, c, :], in_=src[row0 + c * 128: row0 + c * 128 + ksz, tb * 512:(tb + 1) * 512]),
                 reads=[src_buf], writes=[cib], dma=True)
        p_t, p_b = pmm.nxt()
        for c in range(nch):
            ksz = min(128, nrows - c * 128)
            S.op("act", lambda e, ci=ci, cs=cs, c=c, ksz=ksz: e.activation(out=cs[:ksz, c, :], in_=ci[:ksz, c, :], func=AF.Square),
                 reads=[cib], writes=[csb])
            S.op("pe", lambda e, p_t=p_t, cs=cs, c=c, ksz=ksz: e.matmul(p_t[:, :], lhsT=onesf[:ksz, :], rhs=cs[:ksz, c, :], start=(c == 0), stop=(c == nch - 1)),
                 reads=[csb, onesf_b], writes=[p_b])
        r_t, r_b = rs.nxt()
        S.op("act", lambda e, r_t=r_t, p_t=p_t: e.activation(out=r_t[:, :], in_=p_t[:, :], func=AF.Sqrt, scale=1.0 / nrows, bias=eps[0][:, 0:1]),
             reads=[p_b, eps[1]], writes=[r_b])
        S.op("dve", lambda e, r_t=r_t: e.reciprocal(out=r_t[:, :], in_=r_t[:, :]), reads=[r_b], writes=[r_b])
        for c in range(nch):
            ksz = min(128, nrows - c * 128)
            S.op("dve", lambda e, ci=ci, r_t=r_t, c=c, ksz=ksz, tb=tb: e.scalar_tensor_tensor(
                out=out_t[:ksz, c, tb * 512:(tb + 1) * 512], in0=ci[:ksz, c, :], scalar=gcol[:ksz, c:c + 1], in1=r_t[:ksz, :], op0=ALU.mult, op1=ALU.mult),
                reads=[cib, r_b, gb], writes=[out_b])


def stage_mla(C, sc, w_uq, q_norm, w_ukv, kv_norm, cos2T, sin2T, ymlaT):
    nc, S = C.nc, C.S
    cT = sc["cT"]
    cTb = C.buf("cT")
    scale = 192.0 ** -0.5
    QCH = [(0, 128), (128, 128), (256, 128), (384, 64)]
    KCH = [(0, 128), (128, 32)]
    with contextlib.ExitStack() as st:
        onesf = st.enter_context(nc.sbuf_tensor(U("onesf"), [128, 128], F32))
        onesb = st.enter_context(nc.sbuf_tensor(U("onesb"), [128, 128], BF16))
        of_b, ob_b = Buf("onesf"), Buf("onesb")
        S.op("dve", lambda e: e.memset(onesf[:], 1.0), writes=[of_b])
        S.op("dve", lambda e: e.memset(onesb[:], 1.0), writes=[ob_b])
        eps_t = st.enter_context(nc.sbuf_tensor(U("epsT"), [128, 1], F32))
        eb = Buf("eps")
        S.op("dve", lambda e: e.memset(eps_t[:], EPS), writes=[eb])
        pmm = Rot(nc, st, "pmm", 4, [128, 512], F32, psum=True)
        pO = Rot(nc, st, "pO", 2, [128, 512], F32, psum=True)
        pD = Rot(nc, st, "pD", 2, [128, 512], F32, psum=True)
        cqn = st.enter_context(nc.sbuf_tensor(U("cqn"), [128, 4, T], BF16))
        ckvn = st.enter_context(nc.sbuf_tensor(U("ckvn"), [128, 2, T], BF16))
        cqn_b, ckvn_b = Buf("cqn"), Buf("ckvn")
        cin = Rot(nc, st, "fci", 2, [128, 4, 512], F32)
        csq = Rot(nc, st, "fcs", 1, [128, 4, 512], F32)
        rs = Rot(nc, st, "frs", 2, [128, 512], F32)
        fm_rmsnorm(C, st, "nq", cT, cTb, 0, 448, q_norm, onesf, of_b, (eps_t, eb), pmm, cqn, cqn_b, cin, csq, rs)
        fm_rmsnorm(C, st, "nk", cT, cTb, 448, 160, kv_norm, onesf, of_b, (eps_t, eb), pmm, ckvn, ckvn_b, cin, csq, rs)
        kpe = st.enter_context(nc.sbuf_tensor(U("kpe"), [128, T], BF16))
        kpe_b = Buf("kpe")
        S.op("pool", lambda e: e.memset(kpe[64:128, :], 0.0), writes=[kpe_b])
        tmpA = Rot(nc, st, "tmpA", 2, [64, 512], F32)
        tmpB = Rot(nc, st, "tmpB", 2, [64, 512], F32)
        tmpC = Rot(nc, st, "tmpC", 2, [64, 512], F32)
        tmpD = Rot(nc, st, "tmpD", 2, [64, 512], F32)
        cosr = Rot(nc, st, "cosr", 2, [64, 512], F32)
        sinr = Rot(nc, st, "sinr", 2, [64, 512], F32)

        def rope(src_a, src_a_b, src_r, src_r_b, tb, out_ap, out_b):
            ct, cb = cosr.nxt()
            s_t, s_b = sinr.nxt()
            S.op("sp", lambda e: e.dma_start(out=ct[:, :], in_=cos2T[:, tb * 512:(tb + 1) * 512]), writes=[cb], dma=True)
            S.op("sp", lambda e: e.dma_start(out=s_t[:, :], in_=sin2T[:, tb * 512:(tb + 1) * 512]), writes=[s_b], dma=True)
            a_t, a_b = tmpC.nxt()
            b_t, b_b = tmpD.nxt()
            S.op("dve", lambda e: e.tensor_tensor(out=a_t[:, :], in0=src_a, in1=ct[:, :], op=ALU.mult), reads=[src_a_b, cb], writes=[a_b])
            S.op("dve", lambda e: e.tensor_tensor(out=b_t[:, :], in0=src_r, in1=s_t[:, :], op=ALU.mult), reads=[src_r_b, s_b], writes=[b_b])
            S.op("dve", lambda e: e.tensor_tensor(out=out_ap, in0=a_t[:, :], in1=b_t[:, :], op=ALU.add), reads=[a_b, b_b], writes=[out_b])

        for tb in range(T // 512):
            a_t, a_b = tmpA.nxt()
            r_t, r_b = tmpB.nxt()
            S.op("sp", lambda e, a_t=a_t, tb=tb: e.dma_start(out=a_t[:, :], in_=cT[608:672, tb * 512:(tb + 1) * 512]), reads=[cTb], writes=[a_b], dma=True)
            S.op("sp", lambda e, r_t=r_t, tb=tb: e.dma_start(out=r_t[0:32, :], in_=cT[640:672, tb * 512:(tb + 1) * 512]), reads=[cTb], writes=[r_b], dma=True)
            S.op("sp", lambda e, r_t=r_t, tb=tb: e.dma_start(out=r_t[32:64, :], in_=cT[608:640, tb * 512:(tb + 1) * 512]), reads=[cTb], writes=[r_b], dma=True)
            rope(a_t[:, :], a_b, r_t[:, :], r_b, tb, kpe[0:64, tb * 512:(tb + 1) * 512], kpe_b)

        qn = st.enter_context(nc.sbuf_tensor(U("qn"), [128, T], BF16))
        qp = st.enter_context(nc.sbuf_tensor(U("qp"), [128, T], BF16))
        kn = st.enter_context(nc.sbuf_tensor(U("kn"), [128, T], BF16))
        vv = st.enter_context(nc.sbuf_tensor(U("vv"), [128, 32, 128], BF16))
        qn_b, qp_b, kn_b, vv_b = Buf("qn"), Buf("qp"), Buf("kn"), Buf("vv")
        S.op("pool", lambda e: e.memset(qp[64:128, :], 0.0), writes=[qp_b])
        wq_s = st.enter_context(nc.sbuf_tensor(U("wq_s"), [128, 4, 256], F32))
        wq_bf = st.enter_context(nc.sbuf_tensor(U("wq_bf"), [128, 4, 256], BF16))
        wk_s = st.enter_context(nc.sbuf_tensor(U("wk_s"), [128, 2, 256], F32))
        wk_bf = st.enter_context(nc.sbuf_tensor(U("wk_bf"), [128, 2, 256], BF16))
        wq_sb, wq_bb, wk_sb, wk_bb = Buf("wqs"), Buf("wqb"), Buf("wks"), Buf("wkb")
        Et = Rot(nc, st, "Et", 6, [128, 512], BF16)
        accA = Rot(nc, st, "accA", 2, [128, 512], F32)
        accB = Rot(nc, st, "accB", 2, [128, 512], F32)
        rden = Rot(nc, st, "rden", 2, [128, 512], F32)
        yst = Rot(nc, st, "yst", 2, [128, 512], BF16)
        qa = Rot(nc, st, "qa", 2, [64, 512], F32)
        qr = Rot(nc, st, "qr", 2, [64, 512], F32)

        for h in range(8):
            for c, (k0, ksz) in enumerate(QCH):
                S.op("sp", lambda e, c=c, k0=k0, ksz=ksz, h=h: e.dma_start(out=wq_s[:ksz, c, 0:192], in_=w_uq[k0:k0 + ksz, h * 192:(h + 1) * 192]), writes=[wq_sb], dma=True)
                S.op("sp", lambda e, c=c, k0=k0, ksz=ksz, h=h: e.dma_start(out=wq_s[:ksz, c, 192:224], in_=w_uq[k0:k0 + ksz, h * 192 + 160:h * 192 + 192]), writes=[wq_sb], dma=True)
                S.op("sp", lambda e, c=c, k0=k0, ksz=ksz, h=h: e.dma_start(out=wq_s[:ksz, c, 224:256], in_=w_uq[k0:k0 + ksz, h * 192 + 128:h * 192 + 160]), writes=[wq_sb], dma=True)
            for c, (k0, ksz) in enumerate(KCH):
                S.op("sp", lambda e, c=c, k0=k0, ksz=ksz, h=h: e.dma_start(out=wk_s[:ksz, c, :], in_=w_ukv[k0:k0 + ksz, h * 256:(h + 1) * 256]), writes=[wk_sb], dma=True)
            for c, (k0, ksz) in enumerate(QCH):
                S.op("dve", lambda e, c=c, ksz=ksz: e.tensor_copy(out=wq_bf[:ksz, c, :], in_=wq_s[:ksz, c, :]), reads=[wq_sb], writes=[wq_bb])
            for c, (k0, ksz) in enumerate(KCH):
                S.op("dve", lambda e, c=c, ksz=ksz: e.tensor_copy(out=wk_bf[:ksz, c, :], in_=wk_s[:ksz, c, :]), reads=[wk_sb], writes=[wk_bb])
            for tb in range(T // 512):
                tsl = slice(tb * 512, (tb + 1) * 512)
                p_t, p_b = pmm.nxt()
                for c, (k0, ksz) in enumerate(QCH):
                    S.op("pe", lambda e, p_t=p_t, c=c, ksz=ksz, tsl=tsl: e.matmul(p_t[:, :], lhsT=wq_bf[:ksz, c, 0:128], rhs=cqn[:ksz, c, tsl], start=(c == 0), stop=(c == 3)),
                         reads=[wq_bb, cqn_b], writes=[p_b])
                S.op("act", lambda e, p_t=p_t, tsl=tsl: e.copy(out=qn[:, tsl], in_=p_t[:, :]), reads=[p_b], writes=[qn_b])
                p1, p1b = pmm.nxt()
                for c, (k0, ksz) in enumerate(QCH):
                    S.op("pe", lambda e, p1=p1, c=c, ksz=ksz, tsl=tsl: e.matmul(p1[0:64, :], lhsT=wq_bf[:ksz, c, 128:192], rhs=cqn[:ksz, c, tsl], start=(c == 0), stop=(c == 3)),
                         reads=[wq_bb, cqn_b], writes=[p1b])
                p2, p2b = pmm.nxt()
                for c, (k0, ksz) in enumerate(QCH):
                    S.op("pe", lambda e, p2=p2, c=c, ksz=ksz, tsl=tsl: e.matmul(p2[0:64, :], lhsT=wq_bf[:ksz, c, 192:256], rhs=cqn[:ksz, c, tsl], start=(c == 0), stop=(c == 3)),
                         reads=[wq_bb, cqn_b], writes=[p2b])
                qa_t, qa_b = qa.nxt()
                qr_t, qr_b = qr.nxt()
                S.op("act", lambda e, qa_t=qa_t, p1=p1: e.copy(out=qa_t[:, :], in_=p1[0:64, :]), reads=[p1b], writes=[qa_b])
                S.op("act", lambda e, qr_t=qr_t, p2=p2: e.copy(out=qr_t[:, :], in_=p2[0:64, :]), reads=[p2b], writes=[qr_b])
                rope(qa_t[:, :], qa_b, qr_t[:, :], qr_b, tb, qp[0:64, tsl], qp_b)
                p3, p3b = pmm.nxt()
                for c, (k0, ksz) in enumerate(KCH):
                    S.op("pe", lambda e, p3=p3, c=c, ksz=ksz, tsl=tsl: e.matmul(p3[:, :], lhsT=wk_bf[:ksz, c, 0:128], rhs=ckvn[:ksz, c, tsl], start=(c == 0), stop=(c == 1)),
                         reads=[wk_bb, ckvn_b], writes=[p3b])
                S.op("act", lambda e, p3=p3, tsl=tsl: e.copy(out=kn[:, tsl], in_=p3[:, :]), reads=[p3b], writes=[kn_b])
                p4, p4b = pmm.nxt()
                for q4 in range(4):
                    tt = tb * 4 + q4
                    for c, (k0, ksz) in enumerate(KCH):
                        S.op("pe", lambda e, p4=p4, c=c, ksz=ksz, tt=tt, q4=q4: e.matmul(p4[:, q4 * 128:(q4 + 1) * 128], lhsT=ckvn[:ksz, c, tt * 128:(tt + 1) * 128], rhs=wk_bf[:ksz, c, 128:256], start=(c == 0), stop=(c == 1)),
                             reads=[wk_bb, ckvn_b], writes=[p4b])
                S.op("dve", lambda e, p4=p4, tb=tb: e.tensor_copy(out=vv[:, tb * 4:(tb + 1) * 4, :], in_=p4[:, :].rearrange("p (a b) -> p a b", b=128)),
                     reads=[p4b], writes=[vv_b])
            SK = 2
            fr = {}
            acc = {}

            def front(it):
                qb, kc = divmod(it, 32)
                qsl = slice(qb * 512, (qb + 1) * 512)
                ksl = slice(kc * 128, (kc + 1) * 128)
                s_t, s_b = pmm.nxt()
                S.op("pe", lambda e: e.matmul(s_t[:, :], lhsT=kn[:, ksl], rhs=qn[:, qsl], start=True, stop=False), reads=[kn_b, qn_b], writes=[s_b])
                S.op("pe", lambda e: e.matmul(s_t[:, :], lhsT=kpe[:, ksl], rhs=qp[:, qsl], start=False, stop=True), reads=[kpe_b, qp_b], writes=[s_b])
                e_t, e_b = Et.nxt()
                S.op("act", lambda e: e.activation(out=e_t[:, :], in_=s_t[:, :], func=AF.Exp, scale=scale), reads=[s_b], writes=[e_b])
                fr[it] = (e_t, e_b)

            def back(it, h=h):
                qb, kc = divmod(it, 32)
                qsl = slice(qb * 512, (qb + 1) * 512)
                e_t, e_b = fr.pop(it)
                if kc == 0:
                    acc["o"] = pO.nxt()
                    acc["a0"] = accA.nxt()
                    acc["a1"] = accB.nxt()
                o_t, o_b = acc["o"]
                S.op("pe", lambda e: e.matmul(o_t[:, :], lhsT=vv[:, kc, :], rhs=e_t[:, :], start=(kc == 0), stop=(kc == 31)), reads=[vv_b, e_b], writes=[o_b])
                if kc == 0:
                    acc["d"] = pD.nxt()
                d_t, d_b = acc["d"]
                S.op("pe", lambda e: e.matmul(d_t[:, :], lhsT=onesb[:, :], rhs=e_t[:, :], start=(kc == 0), stop=(kc == 31)), reads=[ob_b, e_b], writes=[d_b])
                if kc == 31:
                    rd_t, rd_b = rden.nxt()
                    S.op("dve", lambda e: e.reciprocal(out=rd_t[:, :], in_=d_t[:, :]), reads=[d_b], writes=[rd_b])
                    y_t, y_b = yst.nxt()
                    S.op("dve", lambda e: e.tensor_tensor(out=y_t[:, :], in0=o_t[:, :], in1=rd_t[:, :], op=ALU.mult), reads=[o_b, rd_b], writes=[y_b])
                    S.op("sp", lambda e: e.dma_start(out=ymlaT[h * 128:(h + 1) * 128, qsl], in_=y_t[:, :]), reads=[y_b], writes=[C.buf("ymlaT")], dma=True)

            NIT = (T // 512) * 32
            for it in range(NIT + SK):
                if it < NIT:
                    front(it)
                if it - SK >= 0:
                    back(it - SK)
        C.end_stage()


def stage_na(C, sc, rpbT, maskc, ynaT):
    nc, S = C.nc, C.S
    qkT, vna = sc["qkT"], sc["vna"]
    qk_b, v_b = C.buf("qkT"), C.buf("vna")
    scale = 64.0 ** -0.5
    with contextlib.ExitStack() as st:
        ones = st.enter_context(nc.sbuf_tensor(U("ones"), [128, 64], BF16))
        ones_b = Buf("ones")
        S.op("dve", lambda e: e.memset(ones[:], 1.0), writes=[ones_b])
        mk = st.enter_context(nc.sbuf_tensor(U("mk"), [128, 14 * 64], F32))
        mk_b = Buf("mk")
        S.op("sp", lambda e: e.dma_start(out=mk[:, :], in_=maskc.rearrange("j d q -> j (d q)")), writes=[mk_b], dma=True)
        khp = Rot(nc, st, "kh", 2, [128, T], BF16)
        qhp = Rot(nc, st, "qh", 2, [128, T], BF16)
        vhp = Rot(nc, st, "vh", 2, [128, 63, 128], BF16)
        btp = Rot(nc, st, "bt", 2, [128, 14 * 64], F32)
        tmp = Rot(nc, st, "tmp", 4, [128, 256], F32)
        Ep = Rot(nc, st, "E", 6, [128, 256], BF16)
        rdp = Rot(nc, st, "rd", 2, [128, 512], F32)
        yp = Rot(nc, st, "y", 2, [64, 512], BF16)
        ps = Rot(nc, st, "ps", 5, [128, 256], F32, psum=True)
        po = Rot(nc, st, "po", 3, [128, 512], F32, psum=True)
        SK = 4
        state = {}
        for (kh_, khb_) in khp.slots:
            S.op("pool", lambda e, kh_=kh_: e.memset(kh_[64:128, :], 0.0), writes=[khb_])
        for (qh_, qhb_) in qhp.slots:
            S.op("pool", lambda e, qh_=qh_: e.memset(qh_[64:128, :], 0.0), writes=[qhb_])
        for (vh_, vhb_) in vhp.slots:
            S.op("pool", lambda e, vh_=vh_: e.memset(vh_[:, :, 64:128], 1.0), writes=[vhb_])

        def head_load(h):
            kh, kh_b = khp.nxt()
            qh, qh_b = qhp.nxt()
            vh, vh_b = vhp.nxt()
            bt, bt_b = btp.nxt()
            S.op("sp", lambda e: e.dma_start(out=qh[0:64, :], in_=qkT[h * 64:(h + 1) * 64, :]), reads=[qk_b], writes=[qh_b], dma=True)
            S.op("sp", lambda e: e.dma_start(out=kh[0:64, :], in_=qkT[1024 + h * 64:1024 + (h + 1) * 64, :]), reads=[qk_b], writes=[kh_b], dma=True)
            for g in range(2):
                S.op("sp", lambda e, g=g: e.dma_start(out=vh[:, g * 16:(g + 1) * 16, 0:64], in_=vna[g * 2048:(g + 1) * 2048, h * 64:(h + 1) * 64].rearrange("(t p) c -> p t c", p=128)),
                     reads=[v_b], writes=[vh_b], dma=True)
            S.op("sp", lambda e: e.dma_start(out=vh[:, 32:48, 0:64], in_=vna[64:64 + 2048, h * 64:(h + 1) * 64].rearrange("(t p) c -> p t c", p=128)),
                 reads=[v_b], writes=[vh_b], dma=True)
            S.op("sp", lambda e: e.dma_start(out=vh[:, 48:63, 0:64], in_=vna[64 + 2048:64 + 2048 + 15 * 128, h * 64:(h + 1) * 64].rearrange("(t p) c -> p t c", p=128)),
                 reads=[v_b], writes=[vh_b], dma=True)
            S.op("sp", lambda e: e.dma_start(out=bt[:, :], in_=rpbT[h].rearrange("j d q -> j (d q)")), writes=[bt_b], dma=True)
            S.op("pool", lambda e: e.tensor_tensor(out=bt[:, :], in0=bt[:, :], in1=mk[:, :], op=ALU.add), reads=[mk_b], writes=[bt_b])
            state[h] = (kh, kh_b, qh, qh_b, vh, vh_b, bt, bt_b)

        fr = {}

        def front(it):
            h, r = divmod(it, 64)
            if r == 0:
                head_load(h)
            kh, kh_b, qh, qh_b, vh, vh_b, bt, bt_b = state[h]
            start = min(max(r - 4, 0), 56)
            base = start - r + 7
            s_t, s_b = ps.nxt()
            for c in range(4):
                S.op("pe", lambda e, c=c: e.matmul(s_t[:, c * 64:(c + 1) * 64], lhsT=kh[:, (start + 2 * c) * 64:(start + 2 * c + 2) * 64], rhs=qh[:, r * 64:(r + 1) * 64], start=True, stop=True),
                     reads=[kh_b, qh_b], writes=[s_b])
            t_t, t_b = tmp.nxt()
            S.op("dve", lambda e: e.scalar_tensor_tensor(out=t_t[:, :].rearrange("p (c q) -> p c q", q=64), in0=s_t[:, :].rearrange("p (c q) -> p c q", q=64), scalar=scale,
                                                        in1=bt[:, :].rearrange("p (d q) -> p d q", q=64)[:, base:base + 7:2, :], op0=ALU.mult, op1=ALU.add),
                 reads=[s_b, bt_b], writes=[t_b])
            e_t, e_b = Ep.nxt()
            S.op("act", lambda e: e.activation(out=e_t[:, :], in_=t_t[:, :], func=AF.Exp), reads=[t_b], writes=[e_b])
            fr[it] = (e_t, e_b, start)

        acc = {}

        def back(it):
            h, r = divmod(it, 64)
            kh, kh_b, qh, qh_b, vh, vh_b, bt, bt_b = state[h]
            e_t, e_b, start = fr.pop(it)
            rr = r % 8
            r8 = r // 8
            if rr == 0:
                acc["o"] = po.nxt()
            o_t, o_b = acc["o"]
            for c in range(4):
                g = start + 2 * c
                vi = g // 2 if g % 2 == 0 else 32 + (g - 1) // 2
                S.op("pe", lambda e, c=c, vi=vi: e.matmul(o_t[:, rr * 64:(rr + 1) * 64], lhsT=vh[:, vi, :], rhs=e_t[:, c * 64:(c + 1) * 64], start=(c == 0), stop=(c == 3)),
                     reads=[vh_b, e_b], writes=[o_b])
            if rr == 7:
                rd_t, rd_b = rdp.nxt()
                S.op("dve", lambda e: e.reciprocal(out=rd_t[64:128, :], in_=o_t[64:128, :]), reads=[o_b], writes=[rd_b])
                y_t, y_b = yp.nxt()
                S.op("dve", lambda e: e.tensor_tensor(out=y_t[:, :], in0=o_t[0:64, :], in1=rd_t[64:128, :], op=ALU.mult), reads=[o_b, rd_b], writes=[y_b])
                S.op("sp", lambda e: e.dma_start(out=ynaT[h * 64:(h + 1) * 64, r8 * 512:(r8 + 1) * 512], in_=y_t[:, :]), reads=[y_b], writes=[C.buf("ynaT")], dma=True)

        NIT = 16 * 64
        for it in range(NIT + SK):
            if it < NIT:
                front(it)
            if it - SK >= 0:
                back(it - SK)
        C.end_stage()


def na_host_tables(rpb):
    cols = np.arange(64)
    col_start = np.clip(cols - 8, 0, 64 - 16)
    valid = (cols[None, :] >= col_start[:, None]) & (cols[None, :] < col_start[:, None] + 16)
    col_idx = np.clip(cols[None, :] - cols[:, None] + 15, 0, 30)
    t = rpb[:, :, col_idx]
    t = np.transpose(t, (0, 3, 1, 2))
    rpbT = np.ascontiguousarray(np.concatenate([t[:, :, 0:14, :], t[:, :, 1:15, :]], axis=1)).astype(np.float32)
    m = np.where(valid.T, 0.0, -30000.0).astype(np.float32)
    m2 = np.concatenate([m, m], axis=0)
    maskc = np.ascontiguousarray(np.broadcast_to(m2[:, None, :], (128, 14, 64))).astype(np.float32)
    return rpbT, maskc


def stage_fnet(C, sc, ccs, csT, ssT, yfT):
    nc, S = C.nc, C.S
    ufT = sc["ufT"]
    uf_b = C.buf("ufT")
    SB = 256
    with contextlib.ExitStack() as st:
        ccf = st.enter_context(nc.sbuf_tensor(U("ccf"), [128, 2, 2, 256], F32))
        ccb = st.enter_context(nc.sbuf_tensor(U("ccb"), [128, 2, 2, 256], BF16))
        ccf_b, ccb_b = Buf("ccf"), Buf("ccb")
        for m in range(2):
            S.op("sp", lambda e, m=m: e.dma_start(out=ccf[:, m, :, :], in_=ccs[m].rearrange("(kc p) n -> p kc n", p=128)), writes=[ccf_b], dma=True)
        S.op("dve", lambda e: e.tensor_copy(out=ccb[:], in_=ccf[:]), reads=[ccf_b], writes=[ccb_b])
        ug = st.enter_context(nc.sbuf_tensor(U("ug"), [128, 2, T], BF16))
        ug_b = Buf("ug")
        gcs = st.enter_context(nc.sbuf_tensor(U("gcs"), [128, 2, 32, 256], BF16))
        gcs_b = Buf("gcs")
        csp = Rot(nc, st, "csb", 3, [128, 32, SB], BF16)
        ssp = Rot(nc, st, "ssb", 3, [128, 32, SB], BF16)
        yst = Rot(nc, st, "yst", 2, [128, SB], BF16)
        pa = Rot(nc, st, "pa", 4, [128, 512], F32, psum=True)
        pb = Rot(nc, st, "pb", 4, [128, 512], F32, psum=True)
        for g in range(4):
            S.op("sp", lambda e, g=g: e.dma_start(out=ug[:, :, :], in_=ufT[g * 256:(g + 1) * 256, :].rearrange("(kc p) t -> p kc t", p=128)), reads=[uf_b], writes=[ug_b], dma=True)
            for tt in range(32):
                p_t, p_b = pa.nxt()
                for m in range(2):
                    for kc in range(2):
                        S.op("pe", lambda e, p_t=p_t, m=m, kc=kc, tt=tt: e.matmul(p_t[:, m * 256:(m + 1) * 256], lhsT=ug[:, kc, tt * 128:(tt + 1) * 128], rhs=ccb[:, m, kc, :], start=(kc == 0), stop=(kc == 1)),
                             reads=[ug_b, ccb_b], writes=[p_b])
                S.op("act", lambda e, p_t=p_t, tt=tt: e.copy(out=gcs[:, :, tt, :], in_=p_t[:, :].rearrange("p (m c) -> p m c", m=2)), reads=[p_b], writes=[gcs_b])
            blk = {}

            def prep(sb):
                cs_t, cs_b = csp.nxt()
                ss_t, ss_b = ssp.nxt()
                S.op("sp", lambda e: e.dma_start(out=cs_t[:, :, :], in_=csT[:, sb * SB:(sb + 1) * SB].rearrange("(tt p) n -> p tt n", p=128)), writes=[cs_b], dma=True)
                S.op("sp", lambda e: e.dma_start(out=ss_t[:, :, :], in_=ssT[:, sb * SB:(sb + 1) * SB].rearrange("(tt p) n -> p tt n", p=128)), writes=[ss_b], dma=True)
                blk[sb] = (cs_t, cs_b, ss_t, ss_b)

            def compute(sb, g=g):
                cs_t, cs_b, ss_t, ss_b = blk.pop(sb)
                for half in range(2):
                    p_t, p_b = pb.nxt()
                    for tt in range(32):
                        S.op("pe", lambda e, p_t=p_t, tt=tt, half=half: e.matmul(p_t[:, :SB], lhsT=gcs[:, 0, tt, half * 128:(half + 1) * 128], rhs=cs_t[:, tt, :], start=(tt == 0), stop=False),
                             reads=[gcs_b, cs_b], writes=[p_b])
                        S.op("pe", lambda e, p_t=p_t, tt=tt, half=half: e.matmul(p_t[:, :SB], lhsT=gcs[:, 1, tt, half * 128:(half + 1) * 128], rhs=ss_t[:, tt, :], start=False, stop=(tt == 31)),
                             reads=[gcs_b, ss_b], writes=[p_b])
                    y_t, y_b = yst.nxt()
                    S.op("dve", lambda e, y_t=y_t, p_t=p_t: e.tensor_copy(out=y_t[:, :], in_=p_t[:, :SB]), reads=[p_b], writes=[y_b])
                    S.op("sp", lambda e, y_t=y_t, half=half: e.dma_start(out=yfT[g * 256 + half * 128: g * 256 + (half + 1) * 128, sb * SB:(sb + 1) * SB], in_=y_t[:, :]),
                         reads=[y_b], writes=[C.buf("yfT")], dma=True)

            pipeline(T // SB, prep, compute, depth=2)
        C.end_stage()


def fnet_host_consts():
    import ml_dtypes
    n = np.arange(256, dtype=np.float64)
    a = 2 * np.pi * np.outer(n, n) / 256.0
    ccs = np.stack([np.cos(a) / 16.0, -np.sin(a) / 16.0]).astype(np.float32)
    s = np.arange(T, dtype=np.int64)
    ph = (np.outer(s, s) % T).astype(np.float64) * (2 * np.pi / T)
    csT = (np.cos(ph) / 64.0).astype(np.float32).astype(ml_dtypes.bfloat16)
    ssT = (np.sin(ph) / 64.0).astype(np.float32).astype(ml_dtypes.bfloat16)
    return ccs, csT, ssT


def stage_merge(C, sc, w_branch, mergedT):
    nc, S = C.nc, C.S
    TQ = 1024
    ysrc = [(sc["ynaT"], C.buf("ynaT")), (sc["ymlaT"], C.buf("ymlaT")), (sc["yfT"], C.buf("yfT"))]
    gT, gT_b = sc["gT"], C.buf("gT")
    with contextlib.ExitStack() as st:
        yb = st.enter_context(nc.sbuf_tensor(U("yb"), [128, 24, TQ], BF16))
        yb_b = Buf("yb")
        wst = Rot(nc, st, "wst", 3, [128, 24, 128], F32)
        wbf = Rot(nc, st, "wbf", 3, [128, 24, 128], BF16)
        gtp = Rot(nc, st, "gt", 6, [128, 3, 512], BF16)
        m0p = Rot(nc, st, "m0", 2, [128, 512], F32)
        m1p = Rot(nc, st, "m1", 2, [128, 512], F32)
        m2p = Rot(nc, st, "m2", 2, [128, 512], F32)
        mo = Rot(nc, st, "mo", 2, [128, TQ], BF16)
        pp = Rot(nc, st, "pp", 6, [128, 512], F32, psum=True)
        jobs = [(tq, dc) for tq in range(T // TQ) for dc in range(16)]
        wjob = {}

        def prep(i):
            tq, dc = jobs[i]
            ws_t, ws_b = wst.nxt()
            for b in range(3):
                S.op("sp", lambda e, b=b: e.dma_start(out=ws_t[:, b * 8:(b + 1) * 8, :], in_=w_branch[b, :, dc * 128:(dc + 1) * 128].rearrange("(kc p) n -> p kc n", p=128)),
                     writes=[ws_b], dma=True)
            wb_t, wb_b = wbf.nxt()
            cast_op(C, wb_t[:, :, :], ws_t[:, :, :], [ws_b], [wb_b])
            gts = []
            for tb in range(TQ // 512):
                g_t, g_b = gtp.nxt()
                t0 = tq * TQ
                S.op("sp", lambda e, g_t=g_t, tb=tb, t0=t0: e.dma_start(
                    out=g_t[:, :, :], in_=gT[:, t0 + tb * 512:t0 + (tb + 1) * 512].rearrange("(b c p) t -> c p b t", b=3, p=128)[dc]),
                    reads=[gT_b], writes=[g_b], dma=True)
                gts.append((g_t, g_b))
            wjob[i] = (wb_t, wb_b, gts)

        def compute(i):
            tq, dc = jobs[i]
            t0 = tq * TQ
            if dc == 0:
                for b in range(3):
                    S.op("sp", lambda e, b=b: e.dma_start(out=yb[:, b * 8:(b + 1) * 8, :], in_=ysrc[b][0][:, t0:t0 + TQ].rearrange("(kc p) t -> p kc t", p=128)),
                         reads=[ysrc[b][1]], writes=[yb_b], dma=True)
            wb_t, wb_b, gts = wjob.pop(i)
            o_t, o_b = mo.nxt()
            for tb in range(TQ // 512):
                g_t, g_b = gts[tb]
                ps = []
                for b in range(3):
                    p_t, p_b = pp.nxt()
                    for kc in range(8):
                        S.op("pe", lambda e, p_t=p_t, b=b, kc=kc, tb=tb: e.matmul(p_t[:, :], lhsT=wb_t[:, b * 8 + kc, :], rhs=yb[:, b * 8 + kc, tb * 512:(tb + 1) * 512], start=(kc == 0), stop=(kc == 7)),
                             reads=[wb_b, yb_b], writes=[p_b])
                    ps.append((p_t, p_b))
                ms = []
                for b, mp in enumerate((m0p, m1p, m2p)):
                    m_t, m_b = mp.nxt()
                    S.op("dve", lambda e, m_t=m_t, b=b, g_t=g_t, p_t=ps[b][0]: e.tensor_tensor(out=m_t[:, :], in0=p_t[:, :], in1=g_t[:, b, :], op=ALU.mult),
                         reads=[ps[b][1], g_b], writes=[m_b])
                    ms.append((m_t, m_b))
                S.op("pool", lambda e, a=ms[0][0], b_=ms[1][0]: e.tensor_tensor(out=a[:, :], in0=a[:, :], in1=b_[:, :], op=ALU.add),
                     reads=[ms[1][1]], writes=[ms[0][1]])
                S.op("pool", lambda e, a=ms[0][0], c_=ms[2][0], tb=tb: e.tensor_tensor(out=o_t[:, tb * 512:(tb + 1) * 512], in0=a[:, :], in1=c_[:, :], op=ALU.add),
                     reads=[ms[0][1], ms[2][1]], writes=[o_b])
            S.op("sp", lambda e: e.dma_start(out=mergedT[dc * 128:(dc + 1) * 128, t0:t0 + TQ], in_=o_t[:, :]), reads=[o_b], writes=[C.buf("mergedT")], dma=True)

        pipeline(len(jobs), prep, compute, depth=2)
        C.end_stage()


def stage_outproj(C, mergedT, w_o, h_in, h_in_b, h_out, h_out_b):
    nc, S = C.nc, C.S
    TQ = 1024
    mg_b = C.buf("mergedT")
    with contextlib.ExitStack() as st:
        mt = st.enter_context(nc.sbuf_tensor(U("mt"), [128, 16, TQ], BF16))
        mt_b = Buf("mt")
        hacc = st.enter_context(nc.sbuf_tensor(U("hacc"), [128, 8, D], F32))
        hacc_b = [Buf("hacc%d" % i) for i in range(8)]
        wst = Rot(nc, st, "wst", 3, [128, 16, 128], F32)
        wow = Rot(nc, st, "wow", 2, [128, 16, 512], BF16)
        wo_cur = {}
        pp = Rot(nc, st, "pp", 4, [128, 512], F32, psum=True)
        jobs = [(tq, cc) for tq in range(T // TQ) for cc in range(16)]
        wjob = {}

        def prep(i):
            tq, cc = jobs[i]
            q = cc % 4
            if q == 0:
                wo_cur["t"] = wow.nxt()
            wb_t, wb_b = wo_cur["t"]
            ws_t, ws_b = wst.nxt()
            S.op("sp", lambda e: e.dma_start(out=ws_t[:, :, :], in_=w_o[:, cc * 128:(cc + 1) * 128].rearrange("(kc p) n -> p kc n", p=128)), writes=[ws_b], dma=True)
            cast_op(C, wb_t[:, :, q * 128:(q + 1) * 128], ws_t[:, :, :], [ws_b], [wb_b])
            wjob[i] = (wb_t, wb_b)

        def compute(i):
            tq, cc = jobs[i]
            t0 = tq * TQ
            if cc == 0:
                S.op("sp", lambda e: e.dma_start(out=mt[:, :, :], in_=mergedT[:, t0:t0 + TQ].rearrange("(kc p) t -> p kc t", p=128)), reads=[mg_b], writes=[mt_b], dma=True)
                for tt in range(8):
                    S.op("sp", lambda e, tt=tt: e.dma_start(out=hacc[:, tt, :], in_=h_in[t0 + tt * 128:t0 + (tt + 1) * 128, :]), reads=[h_in_b], writes=[hacc_b[tt]], dma=True)
            wb_t, wb_b = wjob.pop(i)
            if cc % 4 == 3:
                c4 = cc // 4
                for tt in range(8):
                    p_t, p_b = pp.nxt()
                    for kc in range(16):
                        S.op("pe", lambda e, p_t=p_t, kc=kc, tt=tt: e.matmul(p_t[:, :], lhsT=mt[:, kc, tt * 128:(tt + 1) * 128], rhs=wb_t[:, kc, :], start=(kc == 0), stop=(kc == 15)),
                             reads=[wb_b, mt_b], writes=[p_b])
                    S.op("dve", lambda e, p_t=p_t, tt=tt: e.tensor_tensor(out=hacc[:, tt, c4 * 512:(c4 + 1) * 512], in0=hacc[:, tt, c4 * 512:(c4 + 1) * 512], in1=p_t[:, :], op=ALU.add),
                         reads=[p_b], writes=[hacc_b[tt]])
            if cc == 15:
                for tt in range(8):
                    S.op("sp", lambda e, tt=tt: e.dma_start(out=h_out[t0 + tt * 128:t0 + (tt + 1) * 128, :], in_=hacc[:, tt, :]), reads=[hacc_b[tt]], writes=[h_out_b], dma=True)

        pipeline(len(jobs), prep, compute, depth=2)
        C.end_stage()


def stage_route(C, h_ap, h_b, gain_ap, w_router, ident_f, xn2, idx_d, gate_d, NE=16, CAP=512):
    nc, S = C.nc, C.S
    with contextlib.ExitStack() as st:
        idf, idfb, idb, idbb = load_consts(C, st, ident_f)
        pools = norm_pools(C, st, nh=3)
        gain_t, gain_b = load_gain(C, st, "gain2", gain_ap, D)
        wr = st.enter_context(nc.sbuf_tensor(U("wr"), [128, 16, NE], F32))
        wr_b = Buf("wr")
        S.op("sp", lambda e: e.dma_start(out=wr[:, :, :], in_=w_router.rearrange("(kc p) n -> p kc n", p=128)), writes=[wr_b], dma=True)
        xf = Rot(nc, st, "xf", 3, [128, D], F32)
        xb = Rot(nc, st, "xb", 3, [128, D], BF16)
        xT = Rot(nc, st, "xT", 3, [128, 16, 128], F32)
        sm = Rot(nc, st, "sm", 4, [128, 8], F32)
        ex = Rot(nc, st, "ex", 4, [128, NE], F32)
        af = Rot(nc, st, "af", 4, [128, NE], F32)
        affT = st.enter_context(nc.sbuf_tensor(U("affT"), [NE, T], F32))
        affT2 = st.enter_context(nc.sbuf_tensor(U("affT2"), [NE, T], F32))
        affT_b, affT2_b = Buf("affT"), Buf("affT2")
        vals = st.enter_context(nc.sbuf_tensor(U("vals"), [NE, CAP], F32))
        idx = st.enter_context(nc.sbuf_tensor(U("idx"), [NE, CAP], U32))
        vals_b, idx_b = Buf("vals"), Buf("idx")
        ptr = Rot(nc, st, "ptr", 3, [128, 4, 128], F32, psum=True)
        pl = Rot(nc, st, "pl", 3, [128, 512], F32, psum=True)
        pt2 = Rot(nc, st, "pt2", 2, [128, 512], F32, psum=True)
        lg = {}

        def front(tt):
            x_t, x_b = xf.nxt()
            rms_tile(C, pools, h_ap[tt * 128:(tt + 1) * 128, :], gain_t, gain_b, x_t[:], x_b, D, [h_b])
            xb_t, xb_b = xb.nxt()
            S.op("act", lambda e: e.copy(out=xb_t[:, :], in_=x_t[:, :]), reads=[x_b], writes=[xb_b])
            S.op("sp", lambda e: e.dma_start(out=xn2[tt * 128:(tt + 1) * 128, :], in_=xb_t[:, :]), reads=[xb_b], writes=[C.buf("xn2")], dma=True)
            xT_t, xT_b = xT.nxt()
            for g in range(4):
                p_t, p_b = ptr.nxt()
                for j in range(4):
                    kc = g * 4 + j
                    S.op("pe", lambda e, p_t=p_t, j=j, kc=kc: e.transpose(out=p_t[:, j, :], in_=x_t[:, kc * 128:(kc + 1) * 128], identity=idf[:]),
                         reads=[x_b, idfb], writes=[p_b])
                S.op("dve", lambda e, p_t=p_t, g=g: e.tensor_copy(out=xT_t[:, g * 4:(g + 1) * 4, :], in_=p_t[:, :, :]), reads=[p_b], writes=[xT_b])
            l_t, l_b = pl.nxt()
            for kc in range(16):
                S.op("pe", lambda e, kc=kc: e.matmul(l_t[:, :NE], lhsT=xT_t[:, kc, :], rhs=wr[:, kc, :], start=(kc == 0), stop=(kc == 15)),
                     reads=[xT_b, wr_b], writes=[l_b])
            lg[tt] = (l_t, l_b)

        def back(tt):
            l_t, l_b = lg.pop(tt)
            s_t, s_b = sm.nxt()
            S.op("dve", lambda e: e.tensor_reduce(out=s_t[:, 0:1], in_=l_t[:, :NE], axis=AX.X, op=ALU.max, negate=True), reads=[l_b], writes=[s_b])
            e_t, e_b = ex.nxt()
            S.op("act", lambda e: e.activation(out=e_t[:, :], in_=l_t[:, :NE], func=AF.Exp, bias=s_t[:, 0:1], accum_out=s_t[:, 1:2]),
                 reads=[l_b, s_b], writes=[e_b, s_b])
            S.op("dve", lambda e: e.reciprocal(out=s_t[:, 2:3], in_=s_t[:, 1:2]), reads=[s_b], writes=[s_b])
            a_t, a_b = af.nxt()
            S.op("dve", lambda e: e.tensor_scalar(out=a_t[:, :], in0=e_t[:, :], scalar1=s_t[:, 2:3], scalar2=None, op0=ALU.mult), reads=[e_b, s_b], writes=[a_b])
            q_t, q_b = pt2.nxt()
            S.op("pe", lambda e: e.transpose(out=q_t[:NE, :128], in_=a_t[:, :], identity=idf[:]), reads=[a_b, idfb], writes=[q_b])
            S.op("dve", lambda e: e.tensor_copy(out=affT[:, tt * 128:(tt + 1) * 128], in_=q_t[:NE, :128]), reads=[q_b], writes=[affT_b])

        NT = T // 128
        SKR = 2
        for tt in range(NT + SKR):
            if tt < NT:
                front(tt)
            if tt - SKR >= 0:
                back(tt - SKR)
        cur, cur_b, oth, oth_b = affT, affT_b, affT2, affT2_b
        for r in range(CAP // 8):
            S.op("dve", lambda e, cur=cur, r=r: e.max(out=vals[:, r * 8:(r + 1) * 8], in_=cur[:, :]), reads=[cur_b], writes=[vals_b])
            S.op("dve", lambda e, cur=cur, r=r: e.max_index(out=idx[:, r * 8:(r + 1) * 8], in_max=vals[:, r * 8:(r + 1) * 8], in_values=cur[:, :]), reads=[cur_b, vals_b], writes=[idx_b])
            if r < CAP // 8 - 1:
                S.op("dve", lambda e, cur=cur, oth=oth, r=r: e.match_replace(out=oth[:, :], in_to_replace=vals[:, r * 8:(r + 1) * 8], in_values=cur[:, :], imm_value=-1.0),
                     reads=[cur_b, vals_b], writes=[oth_b])
                cur, cur_b, oth, oth_b = oth, oth_b, cur, cur_b
        S.op("sp", lambda e: e.dma_start(out=idx_d[:, :], in_=idx[:, :]), reads=[idx_b], writes=[C.buf("idx_d")], dma=True)
        S.op("sp", lambda e: e.dma_start(out=gate_d[:, :], in_=vals[:, :]), reads=[vals_b], writes=[C.buf("gate_d")], dma=True)
        C.end_stage()


def stage_experts(C, h_ap, h_b, xn2, idx_d, gate_d, w_g, w_u, w_d, ident_f, NE=16, CAP=512):
    nc, S = C.nc, C.S
    NJ = CAP // 128
    with contextlib.ExitStack() as st:
        idf, idfb, idb, idbb = load_consts(C, st, ident_f)
        idxc = st.enter_context(nc.sbuf_tensor(U("idxc"), [128, NE * NJ], U32))
        gatec = st.enter_context(nc.sbuf_tensor(U("gatec"), [128, NE * NJ], F32))
        idxc_b, gatec_b = Buf("idxc"), Buf("gatec")
        S.op("sp", lambda e: e.dma_start(out=idxc[:, :].rearrange("p (e j) -> p e j", j=NJ), in_=idx_d.rearrange("e (j p) -> p e j", p=128), allow_slow_non_contiguous=True),
             reads=[C.buf("idx_d")], writes=[idxc_b], dma=True)
        S.op("sp", lambda e: e.dma_start(out=gatec[:, :].rearrange("p (e j) -> p e j", j=NJ), in_=gate_d.rearrange("e (j p) -> p e j", p=128), allow_slow_non_contiguous=True),
             reads=[C.buf("gate_d")], writes=[gatec_b], dma=True)
        xg = Rot(nc, st, "xg", 4, [128, D], BF16)
        xeTp = Rot(nc, st, "xeT", 2, [128, 16, CAP], BF16)
        hT = st.enter_context(nc.sbuf_tensor(U("hT"), [128, 16, CAP], BF16))
        hT_b = Buf("hT")
        ye = st.enter_context(nc.sbuf_tensor(U("ye"), [128, NJ, D], F32))
        ye_b = [Buf("ye%d" % j) for j in range(NJ)]
        wst = Rot(nc, st, "wst", 3, [128, 16, 128], F32)
        wbf = Rot(nc, st, "wbf", 3, [128, 16, 128], BF16)
        sg = Rot(nc, st, "sg", 2, [128, CAP], F32)
        wdw = Rot(nc, st, "wdw", 2, [128, 16, 512], BF16)
        wd_cur = {}
        ptr = Rot(nc, st, "ptr", 2, [128, 8, 128], BF16, psum=True)
        pg = Rot(nc, st, "pg", 2, [128, 512], F32, psum=True)
        pu = Rot(nc, st, "pu", 2, [128, 512], F32, psum=True)
        pd = Rot(nc, st, "pd", 2, [128, 512], F32, psum=True)
        xn2_b = C.buf("xn2")

        def load_w(src):
            ws_t, ws_b = wst.nxt()
            S.op("sp", lambda e: e.dma_start(out=ws_t[:, :, :], in_=src.rearrange("(kc p) n -> p kc n", p=128)), writes=[ws_b], dma=True)
            wb_t, wb_b = wbf.nxt()
            cast_op(C, wb_t[:, :, :], ws_t[:, :, :], [ws_b], [wb_b])
            return wb_t, wb_b

        xe_of = {}

        def gather(ex):
            xeT, xeT_b = xeTp.nxt()
            xe_of[ex] = (xeT, xeT_b)
            for j in range(NJ):
                col = ex * NJ + j
                x_t, x_b = xg.nxt()
                S.op("pool", lambda e, x_t=x_t, col=col: e.indirect_dma_start(out=x_t[:, :], out_offset=None, in_=xn2[:, :], in_offset=bass.IndirectOffsetOnAxis(ap=idxc[:, col:col + 1], axis=0)),
                     reads=[xn2_b, idxc_b], writes=[x_b], dma=True)
                for g in range(2):
                    p_t, p_b = ptr.nxt()
                    for jj in range(8):
                        kc = g * 8 + jj
                        S.op("pe", lambda e, p_t=p_t, jj=jj, kc=kc, x_t=x_t: e.transpose(out=p_t[:, jj, :], in_=x_t[:, kc * 128:(kc + 1) * 128], identity=idb[:]),
                             reads=[x_b, idbb], writes=[p_b])
                    S.op("dve", lambda e, p_t=p_t, g=g, j=j: e.tensor_copy(out=xeT[:, g * 8:(g + 1) * 8, j * 128:(j + 1) * 128], in_=p_t[:]), reads=[p_b], writes=[xeT_b])

        jobs = []
        for ex in range(NE):
            for fc in range(16):
                jobs += [(ex, "g", fc), (ex, "u", fc)]
            jobs += [(ex, "d", dc) for dc in range(16)]
        wjob = {}
        sil = {}

        def prep(i):
            ex, kind, c = jobs[i]
            if kind == "d":
                q = c % 4
                if q == 0:
                    wd_cur["t"] = wdw.nxt()
                wt, wt_b = wd_cur["t"]
                ws_t, ws_b = wst.nxt()
                S.op("sp", lambda e: e.dma_start(out=ws_t[:, :, :], in_=w_d[ex, :, c * 128:(c + 1) * 128].rearrange("(kc p) n -> p kc n", p=128)), writes=[ws_b], dma=True)
                cast_op(C, wt[:, :, q * 128:(q + 1) * 128], ws_t[:, :, :], [ws_b], [wt_b])
                wjob[i] = (wt, wt_b)
                return
            src = {"g": w_g, "u": w_u}[kind]
            wjob[i] = load_w(src[ex, :, c * 128:(c + 1) * 128])

        def compute(i):
            ex, kind, c = jobs[i]
            if kind == "g":
                xeT, xeT_b = xe_of[ex]
                wg_t, wg_b = wjob.pop(i)
                g_t, g_b = pg.nxt()
                for kc in range(16):
                    S.op("pe", lambda e, kc=kc: e.matmul(g_t[:, :CAP], lhsT=wg_t[:, kc, :], rhs=xeT[:, kc, :], start=(kc == 0), stop=(kc == 15)),
                         reads=[wg_b, xeT_b], writes=[g_b])
                s_t, s_b = sg.nxt()
                S.op("act", lambda e: e.activation(out=s_t[:, :], in_=g_t[:, :CAP], func=AF.Silu), reads=[g_b], writes=[s_b])
                sil[(ex, c)] = (s_t, s_b)
            elif kind == "u":
                xeT, xeT_b = xe_of[ex]
                wu_t, wu_b = wjob.pop(i)
                u_t, u_b = pu.nxt()
                for kc in range(16):
                    S.op("pe", lambda e, kc=kc: e.matmul(u_t[:, :CAP], lhsT=wu_t[:, kc, :], rhs=xeT[:, kc, :], start=(kc == 0), stop=(kc == 15)),
                         reads=[wu_b, xeT_b], writes=[u_b])
                s_t, s_b = sil.pop((ex, c))
                S.op("dve", lambda e: e.tensor_tensor(out=hT[:, c, :], in0=u_t[:, :CAP], in1=s_t[:, :], op=ALU.mult), reads=[u_b, s_b], writes=[hT_b])
            else:
                dc = c
                if dc == 0 and ex + 1 < NE:
                    gather(ex + 1)
                wd_t, wd_b = wjob.pop(i)
                if dc % 4 == 3:
                    d4 = dc // 4
                    for j in range(NJ):
                        col = ex * NJ + j
                        p_t, p_b = pd.nxt()
                        for fc in range(16):
                            S.op("pe", lambda e, p_t=p_t, j=j, fc=fc: e.matmul(p_t[:, :], lhsT=hT[:, fc, j * 128:(j + 1) * 128], rhs=wd_t[:, fc, :], start=(fc == 0), stop=(fc == 15)),
                                 reads=[wd_b, hT_b], writes=[p_b])
                        S.op("act", lambda e, p_t=p_t, j=j, col=col: e.activation(out=ye[:, j, d4 * 512:(d4 + 1) * 512], in_=p_t[:, :], func=AF.Copy, scale=gatec[:, col:col + 1]),
                             reads=[p_b, gatec_b], writes=[ye_b[j]])
                if dc == 15:
                    for j in range(NJ):
                        col = ex * NJ + j
                        S.op("pool", lambda e, j=j, col=col: e.indirect_dma_start(out=h_ap[:, :], out_offset=bass.IndirectOffsetOnAxis(ap=idxc[:, col:col + 1], axis=0), in_=ye[:, j, :], in_offset=None, compute_op=ALU.add),
                             reads=[ye_b[j], idxc_b], writes=[h_b], dma=True)

        gather(0)
        pipeline(len(jobs), prep, compute, depth=2)
        C.end_stage()


def stage_final(C, h_ap, h_b, gain_ap, y_ap, y_b):
    nc, S = C.nc, C.S
    with contextlib.ExitStack() as st:
        pools = norm_pools(C, st)
        gain_t, gain_b = load_gain(C, st, "gainf", gain_ap, D)
        of = Rot(nc, st, "of", 2, [128, D], F32)
        for tt in range(T // 128):
            o_t, o_b = of.nxt()
            rms_tile(C, pools, h_ap[tt * 128:(tt + 1) * 128, :], gain_t, gain_b, o_t[:], o_b, D, [h_b])
            S.op("sp", lambda e, o_t=o_t, tt=tt: e.dma_start(out=y_ap[tt * 128:(tt + 1) * 128, :], in_=o_t[:, :]), reads=[o_b], writes=[y_b], dma=True)
        C.end_stage()


NCORES = 4
NB = 4 // NCORES
DEPTH = 2
_CACHE = {}


def build_program():
    nc = bass.Bass("TRN2", target_bir_lowering=False)
    def inp(name, shape, dt=F32):
        return nc.dram_tensor(name, list(shape), dt, kind="ExternalInput").ap()
    x = inp("x", [NB * T, D])
    w_in = inp("w_in", [DEPTH, D, INC])
    b_gate = inp("b_gate", [DEPTH, 6144])
    w_uq = inp("w_uq", [DEPTH, 448, 1536])
    q_norm = inp("q_norm", [DEPTH, 448])
    w_ukv = inp("w_ukv", [DEPTH, 160, 2048])
    kv_norm = inp("kv_norm", [DEPTH, 160])
    rpbT = inp("rpbT", [DEPTH, 16, 128, 14, 64])
    maskc = inp("maskc", [128, 14, 64])
    w_branch = inp("w_branch", [DEPTH, 3, 1024, 2048])
    w_o = inp("w_o", [DEPTH, D, D])
    norm_mix = inp("norm_mix", [DEPTH, D])
    norm_moe = inp("norm_moe", [DEPTH, D])
    w_router = inp("w_router", [DEPTH, D, 16])
    w_g = inp("w_exp_gate", [DEPTH, 16, D, D])
    w_u = inp("w_exp_up", [DEPTH, 16, D, D])
    w_d = inp("w_exp_down", [DEPTH, 16, D, D])
    norm_final = inp("norm_final", [D])
    ident = inp("ident", [128, 128])
    cos2T = inp("cos2T", [64, T])
    sin2T = inp("sin2T", [64, T])
    ccs = inp("ccs", [2, 256, 256])
    csT = inp("csT", [T, T], BF16)
    ssT = inp("ssT", [T, T], BF16)
    y = nc.dram_tensor("y", [NB * T, D], F32, kind="ExternalOutput").ap()
    with contextlib.ExitStack() as es:
        C = Ctx(nc, es)
        sc = {"qkT": C.dram("qkT", [2048, T], BF16), "vna": C.dram("vna", [T, 1024], BF16), "cT": C.dram("cT", [672, T], F32),
              "ufT": C.dram("ufT", [1024, T], BF16), "gT": C.dram("gT", [6144, T], BF16),
              "ynaT": C.dram("ynaT", [1024, T], BF16), "ymlaT": C.dram("ymlaT", [1024, T], BF16), "yfT": C.dram("yfT", [1024, T], BF16)}
        mergedT = C.dram("mergedT", [2048, T], BF16)
        hA = C.dram("hA", [T, D], F32)
        xn2 = C.dram("xn2", [T, D], BF16)
        idx_d = C.dram("idx_d", [16, 512], U32)
        gate_d = C.dram("gate_d", [16, 512], F32)
        hA_b = C.buf("hA")
        x_b = Buf("x")
        y_b = C.buf("y")
        for b in range(NB):
            xb = x[b * T:(b + 1) * T, :]
            for l in range(DEPTH):
                h_in, h_in_b = (xb, x_b) if l == 0 else (hA, hA_b)
                stage_inproj(C, h_in, h_in_b, norm_mix[l], w_in[l], b_gate[l], ident, sc)
                stage_na(C, sc, rpbT[l], maskc, sc["ynaT"])
                stage_mla(C, sc, w_uq[l], q_norm[l], w_ukv[l], kv_norm[l], cos2T, sin2T, sc["ymlaT"])
                stage_fnet(C, sc, ccs, csT, ssT, sc["yfT"])
                stage_merge(C, sc, w_branch[l], mergedT)
                stage_outproj(C, mergedT, w_o[l], h_in, h_in_b, hA, hA_b)
                stage_route(C, hA, hA_b, norm_moe[l], w_router[l], ident, xn2, idx_d, gate_d)
                stage_experts(C, hA, hA_b, xn2, idx_d, gate_d, w_g[l], w_u[l], w_d[l], ident)
            stage_final(C, hA, hA_b, norm_final, y[b * T:(b + 1) * T, :], y_b)
    return nc


def rope_consts():
    pos = np.arange(T, dtype=np.float32)
    inv = (1.0 / (10000.0 ** (np.arange(0, 64, 2, dtype=np.float32) / 64))).astype(np.float32)
    ang = pos[:, None] * inv[None, :]
    cos, sin = np.cos(ang).astype(np.float32), np.sin(ang).astype(np.float32)
    cos2T = np.ascontiguousarray(np.concatenate([cos, cos], 1).T)
    sin2T = np.ascontiguousarray(np.concatenate([-sin, sin], 1).T)
    return cos2T, sin2T


def kernel(x, w_in, b_gate, w_uq, q_norm, w_ukv, kv_norm, na_rpb, w_branch, w_o,
           norm_mix, norm_moe, w_router, w_exp_gate, w_exp_up, w_exp_down, norm_final):
    f = lambda a: np.ascontiguousarray(np.asarray(a, dtype=np.float32))
    if "nc" not in _CACHE:
        _CACHE["nc"] = build_program()
        cos2T, sin2T = rope_consts()
        ccs, csT, ssT = fnet_host_consts()
        _CACHE["consts"] = dict(cos2T=cos2T, sin2T=sin2T, ccs=ccs, csT=csT, ssT=ssT, ident=np.eye(128, dtype=np.float32))
    nc = _CACHE["nc"]
    na_rpb = f(na_rpb)
    tabs = [na_host_tables(na_rpb[l]) for l in range(DEPTH)]
    rpbT = np.stack([t[0] for t in tabs])
    maskc = tabs[0][1]
    shared = dict(w_in=f(w_in), b_gate=f(b_gate), w_uq=f(w_uq), q_norm=f(q_norm), w_ukv=f(w_ukv), kv_norm=f(kv_norm),
                  rpbT=rpbT, maskc=maskc, w_branch=f(w_branch), w_o=f(w_o), norm_mix=f(norm_mix), norm_moe=f(norm_moe),
                  w_router=f(w_router), w_exp_gate=f(w_exp_gate), w_exp_up=f(w_exp_up), w_exp_down=f(w_exp_down),
                  norm_final=f(norm_final), **_CACHE["consts"])
    xf = f(x).reshape(4 * T, D)
    in_maps = []
    for c in range(NCORES):
        m = dict(shared)
        m["x"] = xf[c * NB * T:(c + 1) * NB * T]
        in_maps.append(m)
    res = run_bass_kernel_spmd(nc, in_maps, core_ids=list(range(NCORES)))
    out = np.concatenate([np.asarray(r["y"], dtype=np.float32) for r in res.results], axis=0)
    return out.reshape(4, T, D)
```

```python
import numpy as np
import concourse.bass as bass
import concourse.mybir as mybir
from concourse.bass_utils import run_bass_kernel_spmd

F32 = mybir.dt.float32
BF16 = mybir.dt.bfloat16
I32 = mybir.dt.int32
U32 = mybir.dt.uint32
U16 = mybir.dt.uint16
AF = mybir.ActivationFunctionType
ALU = mybir.AluOpType
AX = mybir.AxisListType


class Buf:
    __slots__ = ("name", "w", "r")

    def __init__(self, name=""):
        self.name = name
        self.w = []
        self.r = []


def _merge(tokens):
    d = {}
    for s, v in tokens:
        if d.get(s, 0) < v:
            d[s] = v
    return d


class Sched:
    ENG = ("pe", "act", "dve", "pool", "sp")

    def __init__(self, nc, es, n_dma_sems=40, rot=30000):
        self.nc = nc
        self.es = es
        self.rot = rot
        self.lists = {e: [] for e in self.ENG}
        self.sems = []
        self.cur = {}
        self.known = {e: {} for e in self.ENG}
        for e in self.ENG:
            self.cur[e] = [self._new_sem("e_" + e), 0]
        self.dma_pool = [[self._new_sem("d%d" % i), 0] for i in range(n_dma_sems)]
        self.dma_next = 0
        self.n_ops = 0

    def _new_sem(self, name):
        h = self.es.enter_context(self.nc.semaphore(name + "_%d" % len(self.sems)))
        self.sems.append(h)
        return len(self.sems) - 1

    def op(self, eng, fn, reads=(), writes=(), dma=False):
        deps = []
        for b in reads:
            deps += b.w
        for b in writes:
            deps += b.w
            deps += b.r
        tok_extra = None
        if dma:
            slot = self.dma_pool[self.dma_next]
            self.dma_next = (self.dma_next + 1) % len(self.dma_pool)
            if slot[1] > 0:
                deps.append((slot[0], 16 * slot[1]))
            slot[1] += 1
            token = (slot[0], 16 * slot[1])
            inc = (slot[0], 16)
        else:
            c = self.cur[eng]
            if c[1] >= self.rot:
                c[0] = self._new_sem("e_" + eng)
                c[1] = 0
            c[1] += 1
            token = (c[0], c[1])
            inc = (c[0], 1)
        need = _merge(deps)
        kn = self.known[eng]
        waits = []
        own = self.cur[eng][0]
        for s, v in need.items():
            if eng == "pe" and s == own and not dma:
                continue
            if kn.get(s, 0) >= v:
                continue
            kn[s] = v
            waits.append((s, v))
        self.lists[eng].append((waits, fn, inc))
        for b in writes:
            b.w = [token]
            b.r = []
        for b in reads:
            if b in writes:
                continue
            m = _merge(b.r + [token])
            b.r = list(m.items())
        self.n_ops += 1
        return token

    def wait_all(self, eng, bufs):
        deps = []
        for b in bufs:
            deps += b.w
        need = _merge(deps)
        waits = [(s, v) for s, v in need.items()]
        self.lists[eng].append((waits, None, None))

    def emit(self):
        nc = self.nc
        sems = self.sems
        lists = self.lists

        def run(engname, e):
            for waits, fn, inc in lists[engname]:
                for s, v in waits:
                    e.wait_ge(sems[s], v)
                if fn is not None:
                    ins = fn(e)
                    ins.then_inc(sems[inc[0]], inc[1])

        with nc.Block() as block:
            @block.tensor
            def _(e):
                run("pe", e)

            @block.scalar
            def _(e):
                run("act", e)

            @block.vector
            def _(e):
                run("dve", e)

            @block.gpsimd
            def _(e):
                run("pool", e)

            @block.sync
            def _(e):
                run("sp", e)


import contextlib

D = 2048
T = 4096
INC = 10912
EPS = 1e-6


_UID = [0]


def U(name):
    _UID[0] += 1
    return "%s_u%d" % (name, _UID[0])


class Rot:
    def __init__(self, nc, st, name, n, shape, dtype, psum=False):
        self.slots = []
        for i in range(n):
            if psum:
                t = st.enter_context(nc.psum_tensor(U("%s%d" % (name, i)), shape, dtype))
            else:
                t = st.enter_context(nc.sbuf_tensor(U("%s%d") % (name, i), shape, dtype))
            self.slots.append((t, Buf(name + str(i))))
        self.i = 0

    def nxt(self):
        s = self.slots[self.i]
        self.i = (self.i + 1) % len(self.slots)
        return s


class Ctx:
    def __init__(self, nc, es, debug_outs=()):
        self.nc = nc
        self.es = es
        self.S = Sched(nc, es)
        self.debug_outs = set(debug_outs)
        self.dbufs = {}
        self.cast_i = 0

    def dram(self, name, shape, dtype):
        kind = "ExternalOutput" if name in self.debug_outs else "Internal"
        t = self.nc.dram_tensor(name, list(shape), dtype, kind=kind).ap()
        return t

    def buf(self, key):
        if key not in self.dbufs:
            self.dbufs[key] = Buf(str(key))
        return self.dbufs[key]

    def end_stage(self):
        S = self.S
        waits = [(s[0], 16 * s[1]) for s in S.dma_pool if s[1] > 0]
        for e in ("sp", "pool", "act"):
            S.lists[e].append((list(waits), None, None))
        S.emit()
        S.lists = {e: [] for e in S.ENG}


def load_consts(C, st, ident_f):
    nc, S = C.nc, C.S
    idf = st.enter_context(nc.sbuf_tensor(U("idf"), [128, 128], F32))
    idb = st.enter_context(nc.sbuf_tensor(U("idb"), [128, 128], BF16))
    bf = Buf("idf")
    bb = Buf("idb")
    S.op("sp", lambda e: e.dma_start(out=idf[:], in_=ident_f[:, :]), writes=[bf], dma=True)
    S.op("dve", lambda e: e.tensor_copy(out=idb[:], in_=idf[:]), reads=[bf], writes=[bb])
    return idf, bf, idb, bb


def rms_tile(C, pools, src_ap, gain_t, gain_b, out_t, out_b, width, src_reads):
    nc, S = C.nc, C.S
    ht, hb = pools["h"].nxt()
    S.op("sp", lambda e: e.dma_start(out=ht[:, :width], in_=src_ap), reads=src_reads, writes=[hb], dma=True)
    jt, jb = pools["junk"].nxt()
    st_, sb_ = pools["stat"].nxt()
    S.op("act", lambda e: e.activation(out=jt[:, :width], in_=ht[:, :width], func=AF.Square, accum_out=st_[:, 0:1]),
         reads=[hb], writes=[jb, sb_])
    S.op("act", lambda e: e.activation(out=st_[:, 1:2], in_=st_[:, 0:1], func=AF.Sqrt, scale=1.0 / width, bias=pools["eps"][0][:, 0:1]),
         reads=[sb_, pools["eps"][1]], writes=[sb_])
    S.op("dve", lambda e: e.reciprocal(out=st_[:, 2:3], in_=st_[:, 1:2]), reads=[sb_], writes=[sb_])
    S.op("dve", lambda e: e.scalar_tensor_tensor(out=out_t, in0=ht[:, :width], scalar=st_[:, 2:3], in1=gain_t[:, :width],
                                                op0=ALU.mult, op1=ALU.mult),
         reads=[hb, sb_, gain_b], writes=[out_b])
    return ht, hb


def norm_pools(C, st, width=D, nh=2):
    nc, S = C.nc, C.S
    pools = {
        "h": Rot(nc, st, "nh", nh, [128, width], F32),
        "junk": Rot(nc, st, "nj", 1, [128, width], BF16),
        "stat": Rot(nc, st, "ns", 4, [128, 4], F32),
    }
    eps_t = st.enter_context(nc.sbuf_tensor(U("epsT"), [128, 1], F32))
    eb = Buf("eps")
    S.op("dve", lambda e: e.memset(eps_t[:], EPS), writes=[eb])
    pools["eps"] = (eps_t, eb)
    return pools


def load_gain(C, st, name, vec_ap, width):
    nc, S = C.nc, C.S
    g = st.enter_context(nc.sbuf_tensor(U(name), [128, width], F32))
    gb = Buf(name)
    S.op("sp", lambda e: e.dma_start(out=g[:], in_=vec_ap.partition_broadcast(128)), writes=[gb], dma=True)
    return g, gb


def pipeline(n, prep, compute, depth=1):
    for i in range(min(depth, n)):
        prep(i)
    for i in range(n):
        if i + depth < n:
            prep(i + depth)
        compute(i)


CAST_PATTERN = {"default": ("act", "dve"), "moe": ("act", "dve", "act", "dve", "pool")}


def cast_op(C, out_ap, in_ap, reads, writes, pattern="default"):
    S = C.S
    pat = CAST_PATTERN[pattern]
    eng = pat[C.cast_i % len(pat)]
    C.cast_i += 1
    if eng == "dve":
        S.op("dve", lambda e: e.tensor_copy(out=out_ap, in_=in_ap), reads=reads, writes=writes)
    elif eng == "act":
        S.op("act", lambda e: e.copy(out=out_ap, in_=in_ap), reads=reads, writes=writes)
    else:
        S.op("pool", lambda e: e.tensor_copy(out=out_ap, in_=in_ap), reads=reads, writes=writes)


def stage_inproj(C, h_ap, h_buf, gain_ap, w_in, b_gate, ident_f, sc):
    nc, S = C.nc, C.S
    HALF = 2048
    with contextlib.ExitStack() as st:
        idf, idfb, idb, idbb = load_consts(C, st, ident_f)
        pools = norm_pools(C, st)
        gain_t, gain_b = load_gain(C, st, "gain1", gain_ap, D)
        xs_pool = Rot(nc, st, "xs", 2, [128, D], BF16)
        xnT = st.enter_context(nc.sbuf_tensor(U("xnT"), [128, 16, HALF], BF16))
        xnT_b = [Buf("xnT%d" % i) for i in range(16)]
        wst = Rot(nc, st, "wst", 3, [128, 16, 128], F32)
        wbf = Rot(nc, st, "wbf", 3, [128, 16, 128], BF16)
        ost_b = Rot(nc, st, "ostb", 2, [128, HALF], BF16)
        ost_f = Rot(nc, st, "ostf", 2, [128, HALF], F32)
        bg = st.enter_context(nc.sbuf_tensor(U("bg"), [128, 48], F32))
        bgb = Buf("bg")
        S.op("sp", lambda e: e.dma_start(out=bg[:], in_=b_gate.rearrange("(c p) -> p c", p=128), allow_slow_non_contiguous=True), writes=[bgb], dma=True)
        pmm = Rot(nc, st, "pmm", 6, [128, 512], F32, psum=True)
        ptr = Rot(nc, st, "ptr", 2, [128, 8, 128], BF16, psum=True)

        chunks = []
        for i in range(16):
            chunks.append((i * 128, 128, "F", "qkT", i * 128))
        for i in range(8):
            chunks.append((2048 + i * 128, 128, "T", "vna", i * 128))
        c0 = 3072
        r0 = 0
        while r0 < 672:
            m = min(128, 672 - r0)
            chunks.append((c0 + r0, m, "C", "cT", r0))
            r0 += m
        for i in range(8):
            chunks.append((3744 + i * 128, 128, "F", "ufT", i * 128))
        for i in range(48):
            chunks.append((4768 + i * 128, 128, "G", "gT", i * 128))

        w_v = w_in
        for half in range(T // HALF):
            t0 = half * HALF
            for tt in range(16):
                xs_t, xs_b = xs_pool.nxt()
                rms_tile(C, pools, h_ap[t0 + tt * 128: t0 + (tt + 1) * 128, :], gain_t, gain_b, xs_t[:], xs_b, D, [h_buf])
                for g in range(2):
                    pt, pb = ptr.nxt()
                    for j in range(8):
                        kc = g * 8 + j
                        S.op("pe", lambda e, pt=pt, j=j, kc=kc, xs_t=xs_t: e.transpose(out=pt[:, j, :], in_=xs_t[:, kc * 128:(kc + 1) * 128], identity=idb[:]),
                             reads=[xs_b, idbb], writes=[pb])
                    S.op("dve", lambda e, pt=pt, g=g, tt=tt: e.tensor_copy(out=xnT[:, g * 8:(g + 1) * 8, tt * 128:(tt + 1) * 128], in_=pt[:]),
                         reads=[pb], writes=[xnT_b[tt]])
            wjob = {}

            def prep(i):
                (c0, m, mode, dst, dr0) = chunks[i]
                ws_t, ws_b = wst.nxt()
                S.op("sp", lambda e: e.dma_start(out=ws_t[:, :, :m], in_=w_v[:, c0:c0 + m].rearrange("(kc p) n -> p kc n", p=128)),
                     writes=[ws_b], dma=True)
                wb_t, wb_b = wbf.nxt()
                cast_op(C, wb_t[:, :, :m], ws_t[:, :, :m], [ws_b], [wb_b])
                wjob[i] = (wb_t, wb_b)

            def compute(i, t0=t0):
                (c0, m, mode, dst, dr0) = chunks[i]
                wb_t, wb_b = wjob.pop(i)
                if mode != "T":
                    if mode == "C":
                        o_t, o_b = ost_f.nxt()
                    else:
                        o_t, o_b = ost_b.nxt()
                    for tb in range(4):
                        p_t, p_b = pmm.nxt()
                        for kc in range(16):
                            S.op("pe", lambda e, p_t=p_t, kc=kc, tb=tb: e.matmul(p_t[:m, :], lhsT=wb_t[:, kc, :m], rhs=xnT[:, kc, tb * 512:(tb + 1) * 512], start=(kc == 0), stop=(kc == 15)),
                                 reads=[wb_b] + xnT_b[tb * 4:(tb + 1) * 4], writes=[p_b])
                        if mode == "G":
                            gi = dr0 // 128
                            S.op("act", lambda e, p_t=p_t, tb=tb, gi=gi: e.activation(out=o_t[:, tb * 512:(tb + 1) * 512], in_=p_t[:, :], func=AF.Sigmoid, bias=bg[:, gi:gi + 1]),
                                 reads=[p_b, bgb], writes=[o_b])
                        else:
                            S.op("dve", lambda e, p_t=p_t, tb=tb: e.tensor_copy(out=o_t[:m, tb * 512:(tb + 1) * 512], in_=p_t[:m, :]),
                                 reads=[p_b], writes=[o_b])
                    S.op("sp", lambda e: e.dma_start(out=sc[dst][dr0:dr0 + m, t0:t0 + HALF], in_=o_t[:m, :]),
                         reads=[o_b], writes=[C.buf(dst)], dma=True)
                else:
                    o_t, o_b = ost_b.nxt()
                    for g4 in range(4):
                        p_t, p_b = pmm.nxt()
                        for q in range(4):
                            tt = g4 * 4 + q
                            for kc in range(16):
                                S.op("pe", lambda e, p_t=p_t, kc=kc, tt=tt, q=q: e.matmul(p_t[:, q * 128:(q + 1) * 128], lhsT=xnT[:, kc, tt * 128:(tt + 1) * 128], rhs=wb_t[:, kc, :], start=(kc == 0), stop=(kc == 15)),
                                     reads=[wb_b, xnT_b[tt]], writes=[p_b])
                        S.op("dve", lambda e, p_t=p_t, g4=g4: e.tensor_copy(out=o_t[:, g4 * 512:(g4 + 1) * 512], in_=p_t[:, :]),
                             reads=[p_b], writes=[o_b])
                    S.op("sp", lambda e: e.dma_start(
                        out=sc["vna"][t0:t0 + HALF, dr0:dr0 + 128].rearrange("(tt p) c -> p tt c", p=128),
                        in_=o_t[:, :].rearrange("p (tt c) -> p tt c", c=128)),
                        reads=[o_b], writes=[C.buf("vna")], dma=True)

            pipeline(len(chunks), prep, compute, depth=2)
        C.end_stage()


def fm_rmsnorm(C, st, name, src, src_buf, row0, nrows, gain_ap, onesf, onesf_b, eps, pmm, out_t, out_b, cin, csq, rs):
    nc, S = C.nc, C.S
    nch = (nrows + 127) // 128
    gcol = st.enter_context(nc.sbuf_tensor(U(name + "g"), [128, nch], F32))
    gb = Buf(name + "g")
    for c in range(nch):
        ksz = min(128, nrows - c * 128)
        S.op("sp", lambda e, c=c, ksz=ksz: e.dma_start(out=gcol[:ksz, c:c + 1], in_=gain_ap[c * 128:c * 128 + ksz].rearrange("(p o) -> p o", o=1)),
             writes=[gb], dma=True)
    for tb in range(T // 512):
        ci, cib = cin.nxt()
        cs, csb = csq.nxt()
        for c in range(nch):
            ksz = min(128, nrows - c * 128)
            S.op("sp", lambda e, ci=ci, c=c, ksz=ksz, tb=tb: e.dma_start(out=ci[:ksz, c, :], in_=src[row0 + c * 128: row0 + c * 128 + ksz, tb * 512:(tb + 1) * 512]),
                 reads=[src_buf], writes=[cib], dma=True)
        p_t, p_b = pmm.nxt()
        for c in range(nch):
            ksz = min(128, nrows - c * 128)
            S.op("act", lambda e, ci=ci, cs=cs, c=c, ksz=ksz: e.activation(out=cs[:ksz, c, :], in_=ci[:ksz, c, :], func=AF.Square),
                 reads=[cib], writes=[csb])
            S.op("pe", lambda e, p_t=p_t, cs=cs, c=c, ksz=ksz: e.matmul(p_t[:, :], lhsT=onesf[:ksz, :], rhs=cs[:ksz, c, :], start=(c == 0), stop=(c == nch - 1)),
                 reads=[csb, onesf_b], writes=[p_b])
        r_t, r_b = rs.nxt()
        S.op("act", lambda e, r_t=r_t, p_t=p_t: e.activation(out=r_t[:, :], in_=p_t[:, :], func=AF.Sqrt, scale=1.0 / nrows, bias=eps[0][:, 0:1]),
             reads=[p_b, eps[1]], writes=[r_b])
        S.op("dve", lambda e, r_t=r_t: e.reciprocal(out=r_t[:, :], in_=r_t[:, :]), reads=[r_b], writes=[r_b])
        for c in range(nch):
            ksz = min(128, nrows - c * 128)
            S.op("dve", lambda e, ci=ci, r_t=r_t, c=c, ksz=ksz, tb=tb: e.scalar_tensor_tensor(
                out=out_t[:ksz, c, tb * 512:(tb + 1) * 512], in0=ci[:ksz, c, :], scalar=gcol[:ksz, c:c + 1], in1=r_t[:ksz, :], op0=ALU.mult, op1=ALU.mult),
                reads=[cib, r_b, gb], writes=[out_b])


def stage_mla(C, sc, w_uq, q_norm, w_ukv, kv_norm, cos2T, sin2T, ymlaT):
    nc, S = C.nc, C.S
    cT = sc["cT"]
    cTb = C.buf("cT")
    scale = 192.0 ** -0.5
    QCH = [(0, 128), (128, 128), (256, 128), (384, 64)]
    KCH = [(0, 128), (128, 32)]
    with contextlib.ExitStack() as st:
        onesf = st.enter_context(nc.sbuf_tensor(U("onesf"), [128, 128], F32))
        onesb = st.enter_context(nc.sbuf_tensor(U("onesb"), [128, 128], BF16))
        of_b, ob_b = Buf("onesf"), Buf("onesb")
        S.op("dve", lambda e: e.memset(onesf[:], 1.0), writes=[of_b])
        S.op("dve", lambda e: e.memset(onesb[:], 1.0), writes=[ob_b])
        eps_t = st.enter_context(nc.sbuf_tensor(U("epsT"), [128, 1], F32))
        eb = Buf("eps")
        S.op("dve", lambda e: e.memset(eps_t[:], EPS), writes=[eb])
        pmm = Rot(nc, st, "pmm", 4, [128, 512], F32, psum=True)
        pO = Rot(nc, st, "pO", 2, [128, 512], F32, psum=True)
        pD = Rot(nc, st, "pD", 2, [128, 512], F32, psum=True)
        cqn = st.enter_context(nc.sbuf_tensor(U("cqn"), [128, 4, T], BF16))
        ckvn = st.enter_context(nc.sbuf_tensor(U("ckvn"), [128, 2, T], BF16))
        cqn_b, ckvn_b = Buf("cqn"), Buf("ckvn")
        cin = Rot(nc, st, "fci", 2, [128, 4, 512], F32)
        csq = Rot(nc, st, "fcs", 1, [128, 4, 512], F32)
        rs = Rot(nc, st, "frs", 2, [128, 512], F32)
        fm_rmsnorm(C, st, "nq", cT, cTb, 0, 448, q_norm, onesf, of_b, (eps_t, eb), pmm, cqn, cqn_b, cin, csq, rs)
        fm_rmsnorm(C, st, "nk", cT, cTb, 448, 160, kv_norm, onesf, of_b, (eps_t, eb), pmm, ckvn, ckvn_b, cin, csq, rs)
        kpe = st.enter_context(nc.sbuf_tensor(U("kpe"), [128, T], BF16))
        kpe_b = Buf("kpe")
        S.op("pool", lambda e: e.memset(kpe[64:128, :], 0.0), writes=[kpe_b])
        tmpA = Rot(nc, st, "tmpA", 2, [64, 512], F32)
        tmpB = Rot(nc, st, "tmpB", 2, [64, 512], F32)
        tmpC = Rot(nc, st, "tmpC", 2, [64, 512], F32)
        tmpD = Rot(nc, st, "tmpD", 2, [64, 512], F32)
        cosr = Rot(nc, st, "cosr", 2, [64, 512], F32)
        sinr = Rot(nc, st, "sinr", 2, [64, 512], F32)

        def rope(src_a, src_a_b, src_r, src_r_b, tb, out_ap, out_b):
            ct, cb = cosr.nxt()
            s_t, s_b = sinr.nxt()
            S.op("sp", lambda e: e.dma_start(out=ct[:, :], in_=cos2T[:, tb * 512:(tb + 1) * 512]), writes=[cb], dma=True)
            S.op("sp", lambda e: e.dma_start(out=s_t[:, :], in_=sin2T[:, tb * 512:(tb + 1) * 512]), writes=[s_b], dma=True)
            a_t, a_b = tmpC.nxt()
            b_t, b_b = tmpD.nxt()
            S.op("dve", lambda e: e.tensor_tensor(out=a_t[:, :], in0=src_a, in1=ct[:, :], op=ALU.mult), reads=[src_a_b, cb], writes=[a_b])
            S.op("dve", lambda e: e.tensor_tensor(out=b_t[:, :], in0=src_r, in1=s_t[:, :], op=ALU.mult), reads=[src_r_b, s_b], writes=[b_b])
            S.op("dve", lambda e: e.tensor_tensor(out=out_ap, in0=a_t[:, :], in1=b_t[:, :], op=ALU.add), reads=[a_b, b_b], writes=[out_b])

        for tb in range(T // 512):
            a_t, a_b = tmpA.nxt()
            r_t, r_b = tmpB.nxt()
            S.op("sp", lambda e, a_t=a_t, tb=tb: e.dma_start(out=a_t[:, :], in_=cT[608:672, tb * 512:(tb + 1) * 512]), reads=[cTb], writes=[a_b], dma=True)
            S.op("sp", lambda e, r_t=r_t, tb=tb: e.dma_start(out=r_t[0:32, :], in_=cT[640:672, tb * 512:(tb + 1) * 512]), reads=[cTb], writes=[r_b], dma=True)
            S.op("sp", lambda e, r_t=r_t, tb=tb: e.dma_start(out=r_t[32:64, :], in_=cT[608:640, tb * 512:(tb + 1) * 512]), reads=[cTb], writes=[r_b], dma=True)
            rope(a_t[:, :], a_b, r_t[:, :], r_b, tb, kpe[0:64, tb * 512:(tb + 1) * 512], kpe_b)

        qn = st.enter_context(nc.sbuf_tensor(U("qn"), [128, T], BF16))
        qp = st.enter_context(nc.sbuf_tensor(U("qp"), [128, T], BF16))
        kn = st.enter_context(nc.sbuf_tensor(U("kn"), [128, T], BF16))
        vv = st.enter_context(nc.sbuf_tensor(U("vv"), [128, 32, 128], BF16))
        qn_b, qp_b, kn_b, vv_b = Buf("qn"), Buf("qp"), Buf("kn"), Buf("vv")
        S.op("pool", lambda e: e.memset(qp[64:128, :], 0.0), writes=[qp_b])
        wq_s = st.enter_context(nc.sbuf_tensor(U("wq_s"), [128, 4, 256], F32))
        wq_bf = st.enter_context(nc.sbuf_tensor(U("wq_bf"), [128, 4, 256], BF16))
        wk_s = st.enter_context(nc.sbuf_tensor(U("wk_s"), [128, 2, 256], F32))
        wk_bf = st.enter_context(nc.sbuf_tensor(U("wk_bf"), [128, 2, 256], BF16))
        wq_sb, wq_bb, wk_sb, wk_bb = Buf("wqs"), Buf("wqb"), Buf("wks"), Buf("wkb")
        Et = Rot(nc, st, "Et", 6, [128, 512], BF16)
        accA = Rot(nc, st, "accA", 2, [128, 512], F32)
        accB = Rot(nc, st, "accB", 2, [128, 512], F32)
        rden = Rot(nc, st, "rden", 2, [128, 512], F32)
        yst = Rot(nc, st, "yst", 2, [128, 512], BF16)
        qa = Rot(nc, st, "qa", 2, [64, 512], F32)
        qr = Rot(nc, st, "qr", 2, [64, 512], F32)

        for h in range(8):
            for c, (k0, ksz) in enumerate(QCH):
                S.op("sp", lambda e, c=c, k0=k0, ksz=ksz, h=h: e.dma_start(out=wq_s[:ksz, c, 0:192], in_=w_uq[k0:k0 + ksz, h * 192:(h + 1) * 192]), writes=[wq_sb], dma=True)
                S.op("sp", lambda e, c=c, k0=k0, ksz=ksz, h=h: e.dma_start(out=wq_s[:ksz, c, 192:224], in_=w_uq[k0:k0 + ksz, h * 192 + 160:h * 192 + 192]), writes=[wq_sb], dma=True)
                S.op("sp", lambda e, c=c, k0=k0, ksz=ksz, h=h: e.dma_start(out=wq_s[:ksz, c, 224:256], in_=w_uq[k0:k0 + ksz, h * 192 + 128:h * 192 + 160]), writes=[wq_sb], dma=True)
            for c, (k0, ksz) in enumerate(KCH):
                S.op("sp", lambda e, c=c, k0=k0, ksz=ksz, h=h: e.dma_start(out=wk_s[:ksz, c, :], in_=w_ukv[k0:k0 + ksz, h * 256:(h + 1) * 256]), writes=[wk_sb], dma=True)
            for c, (k0, ksz) in enumerate(QCH):
                S.op("dve", lambda e, c=c, ksz=ksz: e.tensor_copy(out=wq_bf[:ksz, c, :], in_=wq_s[:ksz, c, :]), reads=[wq_sb], writes=[wq_bb])
            for c, (k0, ksz) in enumerate(KCH):
                S.op("dve", lambda e, c=c, ksz=ksz: e.tensor_copy(out=wk_bf[:ksz, c, :], in_=wk_s[:ksz, c, :]), reads=[wk_sb], writes=[wk_bb])
            for tb in range(T // 512):
                tsl = slice(tb * 512, (tb + 1) * 512)
                p_t, p_b = pmm.nxt()
                for c, (k0, ksz) in enumerate(QCH):
                    S.op("pe", lambda e, p_t=p_t, c=c, ksz=ksz, tsl=tsl: e.matmul(p_t[:, :], lhsT=wq_bf[:ksz, c, 0:128], rhs=cqn[:ksz, c, tsl], start=(c == 0), stop=(c == 3)),
                         reads=[wq_bb, cqn_b], writes=[p_b])
                S.op("act", lambda e, p_t=p_t, tsl=tsl: e.copy(out=qn[:, tsl], in_=p_t[:, :]), reads=[p_b], writes=[qn_b])
                p1, p1b = pmm.nxt()
                for c, (k0, ksz) in enumerate(QCH):
                    S.op("pe", lambda e, p1=p1, c=c, ksz=ksz, tsl=tsl: e.matmul(p1[0:64, :], lhsT=wq_bf[:ksz, c, 128:192], rhs=cqn[:ksz, c, tsl], start=(c == 0), stop=(c == 3)),
                         reads=[wq_bb, cqn_b], writes=[p1b])
                p2, p2b = pmm.nxt()
                for c, (k0, ksz) in enumerate(QCH):
                    S.op("pe", lambda e, p2=p2, c=c, ksz=ksz, tsl=tsl: e.matmul(p2[0:64, :], lhsT=wq_bf[:ksz, c, 192:256], rhs=cqn[:ksz, c, tsl], start=(c == 0), stop=(c == 3)),
                         reads=[wq_bb, cqn_b], writes=[p2b])
                qa_t, qa_b = qa.nxt()
                qr_t, qr_b = qr.nxt()
                S.op("act", lambda e, qa_t=qa_t, p1=p1: e.copy(out=qa_t[:, :], in_=p1[0:64, :]), reads=[p1b], writes=[qa_b])
                S.op("act", lambda e, qr_t=qr_t, p2=p2: e.copy(out=qr_t[:, :], in_=p2[0:64, :]), reads=[p2b], writes=[qr_b])
                rope(qa_t[:, :], qa_b, qr_t[:, :], qr_b, tb, qp[0:64, tsl], qp_b)
                p3, p3b = pmm.nxt()
                for c, (k0, ksz) in enumerate(KCH):
                    S.op("pe", lambda e, p3=p3, c=c, ksz=ksz, tsl=tsl: e.matmul(p3[:, :], lhsT=wk_bf[:ksz, c, 0:128], rhs=ckvn[:ksz, c, tsl], start=(c == 0), stop=(c == 1)),
                         reads=[wk_bb, ckvn_b], writes=[p3b])
                S.op("act", lambda e, p3=p3, tsl=tsl: e.copy(out=kn[:, tsl], in_=p3[:, :]), reads=[p3b], writes=[kn_b])
                p4, p4b = pmm.nxt()
                for q4 in range(4):
                    tt = tb * 4 + q4
                    for c, (k0, ksz) in enumerate(KCH):
                        S.op("pe", lambda e, p4=p4, c=c, ksz=ksz, tt=tt, q4=q4: e.matmul(p4[:, q4 * 128:(q4 + 1) * 128], lhsT=ckvn[:ksz, c, tt * 128:(tt + 1) * 128], rhs=wk_bf[:ksz, c, 128:256], start=(c == 0), stop=(c == 1)),
                             reads=[wk_bb, ckvn_b], writes=[p4b])
                S.op("dve", lambda e, p4=p4, tb=tb: e.tensor_copy(out=vv[:, tb * 4:(tb + 1) * 4, :], in_=p4[:, :].rearrange("p (a b) -> p a b", b=128)),
                     reads=[p4b], writes=[vv_b])
            SK = 2
            fr = {}
            acc = {}

            def front(it):
                qb, kc = divmod(it, 32)
                qsl = slice(qb * 512, (qb + 1) * 512)
                ksl = slice(kc * 128, (kc + 1) * 128)
                s_t, s_b = pmm.nxt()
                S.op("pe", lambda e: e.matmul(s_t[:, :], lhsT=kn[:, ksl], rhs=qn[:, qsl], start=True, stop=False), reads=[kn_b, qn_b], writes=[s_b])
                S.op("pe", lambda e: e.matmul(s_t[:, :], lhsT=kpe[:, ksl], rhs=qp[:, qsl], start=False, stop=True), reads=[kpe_b, qp_b], writes=[s_b])
                e_t, e_b = Et.nxt()
                S.op("act", lambda e: e.activation(out=e_t[:, :], in_=s_t[:, :], func=AF.Exp, scale=scale), reads=[s_b], writes=[e_b])
                fr[it] = (e_t, e_b)

            def back(it, h=h):
                qb, kc = divmod(it, 32)
                qsl = slice(qb * 512, (qb + 1) * 512)
                e_t, e_b = fr.pop(it)
                if kc == 0:
                    acc["o"] = pO.nxt()
                    acc["a0"] = accA.nxt()
                    acc["a1"] = accB.nxt()
                o_t, o_b = acc["o"]
                S.op("pe", lambda e: e.matmul(o_t[:, :], lhsT=vv[:, kc, :], rhs=e_t[:, :], start=(kc == 0), stop=(kc == 31)), reads=[vv_b, e_b], writes=[o_b])
                if kc == 0:
                    acc["d"] = pD.nxt()
                d_t, d_b = acc["d"]
                S.op("pe", lambda e: e.matmul(d_t[:, :], lhsT=onesb[:, :], rhs=e_t[:, :], start=(kc == 0), stop=(kc == 31)), reads=[ob_b, e_b], writes=[d_b])
                if kc == 31:
                    rd_t, rd_b = rden.nxt()
                    S.op("dve", lambda e: e.reciprocal(out=rd_t[:, :], in_=d_t[:, :]), reads=[d_b], writes=[rd_b])
                    y_t, y_b = yst.nxt()
                    S.op("dve", lambda e: e.tensor_tensor(out=y_t[:, :], in0=o_t[:, :], in1=rd_t[:, :], op=ALU.mult), reads=[o_b, rd_b], writes=[y_b])
                    S.op("sp", lambda e: e.dma_start(out=ymlaT[h * 128:(h + 1) * 128, qsl], in_=y_t[:, :]), reads=[y_b], writes=[C.buf("ymlaT")], dma=True)

            NIT = (T // 512) * 32
            for it in range(NIT + SK):
                if it < NIT:
                    front(it)
                if it - SK >= 0:
                    back(it - SK)
        C.end_stage()


def stage_na(C, sc, rpbT, maskc, ynaT, wprep=None):
    nc, S = C.nc, C.S
    qkT, vna = sc["qkT"], sc["vna"]
    qk_b, v_b = C.buf("qkT"), C.buf("vna")
    scale = 64.0 ** -0.5
    with contextlib.ExitStack() as st:
        ones = st.enter_context(nc.sbuf_tensor(U("ones"), [128, 64], BF16))
        ones_b = Buf("ones")
        S.op("dve", lambda e: e.memset(ones[:], 1.0), writes=[ones_b])
        mk = st.enter_context(nc.sbuf_tensor(U("mk"), [128, 14 * 64], F32))
        mk_b = Buf("mk")
        S.op("sp", lambda e: e.dma_start(out=mk[:, :], in_=maskc.rearrange("j d q -> j (d q)")), writes=[mk_b], dma=True)
        khp = Rot(nc, st, "kh", 2, [128, T], BF16)
        qhp = Rot(nc, st, "qh", 2, [128, T], BF16)
        vhp = Rot(nc, st, "vh", 2, [128, 63, 128], BF16)
        btp = Rot(nc, st, "bt", 2, [128, 14 * 64], F32)
        tmp = Rot(nc, st, "tmp", 4, [128, 256], F32)
        Ep = Rot(nc, st, "E", 6, [128, 256], BF16)
        rdp = Rot(nc, st, "rd", 2, [128, 512], F32)
        yp = Rot(nc, st, "y", 2, [64, 512], BF16)
        ps = Rot(nc, st, "ps", 5, [128, 256], F32, psum=True)
        po = Rot(nc, st, "po", 3, [128, 512], F32, psum=True)
        SK = 4
        state = {}
        if wprep is not None:
            w_branch_, w_o_, wbr_c, wo_c = wprep
            pw_st = Rot(nc, st, "pwst", 2, [128, 24, 128], F32)
            pw_bf = Rot(nc, st, "pwbf", 2, [128, 24, 128], BF16)
            po_st = Rot(nc, st, "post", 2, [128, 16, 128], F32)
            po_bf = Rot(nc, st, "pobf", 2, [128, 16, 128], BF16)

        def wprep_step(dc):
            ws_t, ws_b = pw_st.nxt()
            for b in range(3):
                S.op("sp", lambda e, b=b: e.dma_start(out=ws_t[:, b * 8:(b + 1) * 8, :], in_=w_branch_[b, :, dc * 128:(dc + 1) * 128].rearrange("(kc p) n -> p kc n", p=128)),
                     writes=[ws_b], dma=True)
            wb_t, wb_b = pw_bf.nxt()
            S.op("act", lambda e: e.copy(out=wb_t[:, :, :], in_=ws_t[:, :, :]), reads=[ws_b], writes=[wb_b])
            S.op("sp", lambda e: e.dma_start(out=wbr_c[dc], in_=wb_t[:, :, :].rearrange("p a b -> p (a b)")), reads=[wb_b], writes=[C.buf("wbr_c")], dma=True)
            os_t, os_b = po_st.nxt()
            S.op("sp", lambda e: e.dma_start(out=os_t[:, :, :], in_=w_o_[:, dc * 128:(dc + 1) * 128].rearrange("(kc p) n -> p kc n", p=128)), writes=[os_b], dma=True)
            ob_t, ob_b = po_bf.nxt()
            S.op("act", lambda e: e.copy(out=ob_t[:, :, :], in_=os_t[:, :, :]), reads=[os_b], writes=[ob_b])
            S.op("sp", lambda e: e.dma_start(out=wo_c[dc], in_=ob_t[:, :, :].rearrange("p a b -> p (a b)")), reads=[ob_b], writes=[C.buf("wo_c")], dma=True)
        for (kh_, khb_) in khp.slots:
            S.op("pool", lambda e, kh_=kh_: e.memset(kh_[64:128, :], 0.0), writes=[khb_])
        for (qh_, qhb_) in qhp.slots:
            S.op("pool", lambda e, qh_=qh_: e.memset(qh_[64:128, :], 0.0), writes=[qhb_])
        for (vh_, vhb_) in vhp.slots:
            S.op("pool", lambda e, vh_=vh_: e.memset(vh_[:, :, 64:128], 1.0), writes=[vhb_])

        def head_load(h):
            kh, kh_b = khp.nxt()
            qh, qh_b = qhp.nxt()
            vh, vh_b = vhp.nxt()
            bt, bt_b = btp.nxt()
            S.op("sp", lambda e: e.dma_start(out=qh[0:64, :], in_=qkT[h * 64:(h + 1) * 64, :]), reads=[qk_b], writes=[qh_b], dma=True)
            S.op("sp", lambda e: e.dma_start(out=kh[0:64, :], in_=qkT[1024 + h * 64:1024 + (h + 1) * 64, :]), reads=[qk_b], writes=[kh_b], dma=True)
            for g in range(2):
                S.op("sp", lambda e, g=g: e.dma_start(out=vh[:, g * 16:(g + 1) * 16, 0:64], in_=vna[g * 2048:(g + 1) * 2048, h * 64:(h + 1) * 64].rearrange("(t p) c -> p t c", p=128)),
                     reads=[v_b], writes=[vh_b], dma=True)
            S.op("sp", lambda e: e.dma_start(out=vh[:, 32:48, 0:64], in_=vna[64:64 + 2048, h * 64:(h + 1) * 64].rearrange("(t p) c -> p t c", p=128)),
                 reads=[v_b], writes=[vh_b], dma=True)
            S.op("sp", lambda e: e.dma_start(out=vh[:, 48:63, 0:64], in_=vna[64 + 2048:64 + 2048 + 15 * 128, h * 64:(h + 1) * 64].rearrange("(t p) c -> p t c", p=128)),
                 reads=[v_b], writes=[vh_b], dma=True)
            S.op("sp", lambda e: e.dma_start(out=bt[:, :], in_=rpbT[h].rearrange("j d q -> j (d q)")), writes=[bt_b], dma=True)
            S.op("pool", lambda e: e.tensor_tensor(out=bt[:, :], in0=bt[:, :], in1=mk[:, :], op=ALU.add), reads=[mk_b], writes=[bt_b])
            state[h] = (kh, kh_b, qh, qh_b, vh, vh_b, bt, bt_b)
            if wprep is not None:
                wprep_step(h)

        fr = {}

        def front(it):
            h, r = divmod(it, 64)
            if r == 0:
                head_load(h)
            kh, kh_b, qh, qh_b, vh, vh_b, bt, bt_b = state[h]
            start = min(max(r - 4, 0), 56)
            base = start - r + 7
            s_t, s_b = ps.nxt()
            for c in range(4):
                S.op("pe", lambda e, c=c: e.matmul(s_t[:, c * 64:(c + 1) * 64], lhsT=kh[:, (start + 2 * c) * 64:(start + 2 * c + 2) * 64], rhs=qh[:, r * 64:(r + 1) * 64], start=True, stop=True),
                     reads=[kh_b, qh_b], writes=[s_b])
            t_t, t_b = tmp.nxt()
            S.op("dve", lambda e: e.scalar_tensor_tensor(out=t_t[:, :].rearrange("p (c q) -> p c q", q=64), in0=s_t[:, :].rearrange("p (c q) -> p c q", q=64), scalar=scale,
                                                        in1=bt[:, :].rearrange("p (d q) -> p d q", q=64)[:, base:base + 7:2, :], op0=ALU.mult, op1=ALU.add),
                 reads=[s_b, bt_b], writes=[t_b])
            e_t, e_b = Ep.nxt()
            S.op("act", lambda e: e.activation(out=e_t[:, :], in_=t_t[:, :], func=AF.Exp), reads=[t_b], writes=[e_b])
            fr[it] = (e_t, e_b, start)

        acc = {}

        def back(it):
            h, r = divmod(it, 64)
            kh, kh_b, qh, qh_b, vh, vh_b, bt, bt_b = state[h]
            e_t, e_b, start = fr.pop(it)
            rr = r % 8
            r8 = r // 8
            if rr == 0:
                acc["o"] = po.nxt()
            o_t, o_b = acc["o"]
            for c in range(4):
                g = start + 2 * c
                vi = g // 2 if g % 2 == 0 else 32 + (g - 1) // 2
                S.op("pe", lambda e, c=c, vi=vi: e.matmul(o_t[:, rr * 64:(rr + 1) * 64], lhsT=vh[:, vi, :], rhs=e_t[:, c * 64:(c + 1) * 64], start=(c == 0), stop=(c == 3)),
                     reads=[vh_b, e_b], writes=[o_b])
            if rr == 7:
                rd_t, rd_b = rdp.nxt()
                S.op("dve", lambda e: e.reciprocal(out=rd_t[64:128, :], in_=o_t[64:128, :]), reads=[o_b], writes=[rd_b])
                y_t, y_b = yp.nxt()
                S.op("dve", lambda e: e.tensor_tensor(out=y_t[:, :], in0=o_t[0:64, :], in1=rd_t[64:128, :], op=ALU.mult), reads=[o_b, rd_b], writes=[y_b])
                S.op("sp", lambda e: e.dma_start(out=ynaT[h * 64:(h + 1) * 64, r8 * 512:(r8 + 1) * 512], in_=y_t[:, :]), reads=[y_b], writes=[C.buf("ynaT")], dma=True)

        NIT = 16 * 64
        for it in range(NIT + SK):
            if it < NIT:
                front(it)
            if it - SK >= 0:
                back(it - SK)
        C.end_stage()


def na_host_tables(rpb):
    cols = np.arange(64)
    col_start = np.clip(cols - 8, 0, 64 - 16)
    valid = (cols[None, :] >= col_start[:, None]) & (cols[None, :] < col_start[:, None] + 16)
    col_idx = np.clip(cols[None, :] - cols[:, None] + 15, 0, 30)
    t = rpb[:, :, col_idx]
    t = np.transpose(t, (0, 3, 1, 2))
    rpbT = np.ascontiguousarray(np.concatenate([t[:, :, 0:14, :], t[:, :, 1:15, :]], axis=1)).astype(np.float32)
    m = np.where(valid.T, 0.0, -30000.0).astype(np.float32)
    m2 = np.concatenate([m, m], axis=0)
    maskc = np.ascontiguousarray(np.broadcast_to(m2[:, None, :], (128, 14, 64))).astype(np.float32)
    return rpbT, maskc


def stage_fnet(C, sc, ccs, csT, ssT, yfT):
    nc, S = C.nc, C.S
    ufT = sc["ufT"]
    uf_b = C.buf("ufT")
    SB = 256
    with contextlib.ExitStack() as st:
        ccf = st.enter_context(nc.sbuf_tensor(U("ccf"), [128, 2, 2, 256], F32))
        ccb = st.enter_context(nc.sbuf_tensor(U("ccb"), [128, 2, 2, 256], BF16))
        ccf_b, ccb_b = Buf("ccf"), Buf("ccb")
        for m in range(2):
            S.op("sp", lambda e, m=m: e.dma_start(out=ccf[:, m, :, :], in_=ccs[m].rearrange("(kc p) n -> p kc n", p=128)), writes=[ccf_b], dma=True)
        S.op("dve", lambda e: e.tensor_copy(out=ccb[:], in_=ccf[:]), reads=[ccf_b], writes=[ccb_b])
        ug = st.enter_context(nc.sbuf_tensor(U("ug"), [128, 2, T], BF16))
        ug_b = Buf("ug")
        gcs = st.enter_context(nc.sbuf_tensor(U("gcs"), [128, 2, 32, 256], BF16))
        gcs_b = Buf("gcs")
        csp = Rot(nc, st, "csb", 3, [128, 32, SB], BF16)
        ssp = Rot(nc, st, "ssb", 3, [128, 32, SB], BF16)
        yst = Rot(nc, st, "yst", 2, [128, SB], BF16)
        pa = Rot(nc, st, "pa", 4, [128, 512], F32, psum=True)
        pb = Rot(nc, st, "pb", 4, [128, 512], F32, psum=True)
        for g in range(4):
            S.op("sp", lambda e, g=g: e.dma_start(out=ug[:, :, :], in_=ufT[g * 256:(g + 1) * 256, :].rearrange("(kc p) t -> p kc t", p=128)), reads=[uf_b], writes=[ug_b], dma=True)
            for tt in range(32):
                p_t, p_b = pa.nxt()
                for m in range(2):
                    for kc in range(2):
                        S.op("pe", lambda e, p_t=p_t, m=m, kc=kc, tt=tt: e.matmul(p_t[:, m * 256:(m + 1) * 256], lhsT=ug[:, kc, tt * 128:(tt + 1) * 128], rhs=ccb[:, m, kc, :], start=(kc == 0), stop=(kc == 1)),
                             reads=[ug_b, ccb_b], writes=[p_b])
                S.op("act", lambda e, p_t=p_t, tt=tt: e.copy(out=gcs[:, :, tt, :], in_=p_t[:, :].rearrange("p (m c) -> p m c", m=2)), reads=[p_b], writes=[gcs_b])
            blk = {}

            def prep(sb):
                cs_t, cs_b = csp.nxt()
                ss_t, ss_b = ssp.nxt()
                S.op("sp", lambda e: e.dma_start(out=cs_t[:, :, :], in_=csT[:, sb * SB:(sb + 1) * SB].rearrange("(tt p) n -> p tt n", p=128)), writes=[cs_b], dma=True)
                S.op("sp", lambda e: e.dma_start(out=ss_t[:, :, :], in_=ssT[:, sb * SB:(sb + 1) * SB].rearrange("(tt p) n -> p tt n", p=128)), writes=[ss_b], dma=True)
                blk[sb] = (cs_t, cs_b, ss_t, ss_b)

            def compute(sb, g=g):
                cs_t, cs_b, ss_t, ss_b = blk.pop(sb)
                for half in range(2):
                    p_t, p_b = pb.nxt()
                    for tt in range(32):
                        S.op("pe", lambda e, p_t=p_t, tt=tt, half=half: e.matmul(p_t[:, :SB], lhsT=gcs[:, 0, tt, half * 128:(half + 1) * 128], rhs=cs_t[:, tt, :], start=(tt == 0), stop=False),
                             reads=[gcs_b, cs_b], writes=[p_b])
                        S.op("pe", lambda e, p_t=p_t, tt=tt, half=half: e.matmul(p_t[:, :SB], lhsT=gcs[:, 1, tt, half * 128:(half + 1) * 128], rhs=ss_t[:, tt, :], start=False, stop=(tt == 31)),
                             reads=[gcs_b, ss_b], writes=[p_b])
                    y_t, y_b = yst.nxt()
                    S.op("dve", lambda e, y_t=y_t, p_t=p_t: e.tensor_copy(out=y_t[:, :], in_=p_t[:, :SB]), reads=[p_b], writes=[y_b])
                    S.op("sp", lambda e, y_t=y_t, half=half: e.dma_start(out=yfT[g * 256 + half * 128: g * 256 + (half + 1) * 128, sb * SB:(sb + 1) * SB], in_=y_t[:, :]),
                         reads=[y_b], writes=[C.buf("yfT")], dma=True)

            pipeline(T // SB, prep, compute, depth=2)
        C.end_stage()


def fnet_host_consts():
    import ml_dtypes
    n = np.arange(256, dtype=np.float64)
    a = 2 * np.pi * np.outer(n, n) / 256.0
    ccs = np.stack([np.cos(a) / 16.0, -np.sin(a) / 16.0]).astype(np.float32)
    s = np.arange(T, dtype=np.int64)
    ph = (np.outer(s, s) % T).astype(np.float64) * (2 * np.pi / T)
    csT = (np.cos(ph) / 64.0).astype(np.float32).astype(ml_dtypes.bfloat16)
    ssT = (np.sin(ph) / 64.0).astype(np.float32).astype(ml_dtypes.bfloat16)
    return ccs, csT, ssT


def stage_merge(C, sc, w_branch, mergedT, wbr_c=None):
    nc, S = C.nc, C.S
    TQ = 1024
    ysrc = [(sc["ynaT"], C.buf("ynaT")), (sc["ymlaT"], C.buf("ymlaT")), (sc["yfT"], C.buf("yfT"))]
    gT, gT_b = sc["gT"], C.buf("gT")
    with contextlib.ExitStack() as st:
        yb = st.enter_context(nc.sbuf_tensor(U("yb"), [128, 24, TQ], BF16))
        yb_b = Buf("yb")
        wst = Rot(nc, st, "wst", 3, [128, 24, 128], F32)
        wbf = Rot(nc, st, "wbf", 3, [128, 24, 128], BF16)
        gtp = Rot(nc, st, "gt", 6, [128, 3, 512], BF16)
        m0p = Rot(nc, st, "m0", 2, [128, 512], F32)
        m1p = Rot(nc, st, "m1", 2, [128, 512], F32)
        m2p = Rot(nc, st, "m2", 2, [128, 512], F32)
        mo = Rot(nc, st, "mo", 2, [128, TQ], BF16)
        pp = Rot(nc, st, "pp", 6, [128, 512], F32, psum=True)
        jobs = [(tq, dc) for tq in range(T // TQ) for dc in range(16)]
        wjob = {}

        def prep(i):
            tq, dc = jobs[i]
            wb_t, wb_b = wbf.nxt()
            if wbr_c is not None:
                S.op("sp", lambda e: e.dma_start(out=wb_t[:, :, :].rearrange("p a b -> p (a b)"), in_=wbr_c[dc]), reads=[C.buf("wbr_c")], writes=[wb_b], dma=True)
            else:
                ws_t, ws_b = wst.nxt()
                for b in range(3):
                    S.op("sp", lambda e, b=b: e.dma_start(out=ws_t[:, b * 8:(b + 1) * 8, :], in_=w_branch[b, :, dc * 128:(dc + 1) * 128].rearrange("(kc p) n -> p kc n", p=128)),
                         writes=[ws_b], dma=True)
                cast_op(C, wb_t[:, :, :], ws_t[:, :, :], [ws_b], [wb_b])
            gts = []
            for tb in range(TQ // 512):
                g_t, g_b = gtp.nxt()
                t0 = tq * TQ
                S.op("sp", lambda e, g_t=g_t, tb=tb, t0=t0: e.dma_start(
                    out=g_t[:, :, :], in_=gT[:, t0 + tb * 512:t0 + (tb + 1) * 512].rearrange("(b c p) t -> c p b t", b=3, p=128)[dc]),
                    reads=[gT_b], writes=[g_b], dma=True)
                gts.append((g_t, g_b))
            wjob[i] = (wb_t, wb_b, gts)

        def compute(i):
            tq, dc = jobs[i]
            t0 = tq * TQ
            if dc == 0:
                for b in range(3):
                    S.op("sp", lambda e, b=b: e.dma_start(out=yb[:, b * 8:(b + 1) * 8, :], in_=ysrc[b][0][:, t0:t0 + TQ].rearrange("(kc p) t -> p kc t", p=128)),
                         reads=[ysrc[b][1]], writes=[yb_b], dma=True)
            wb_t, wb_b, gts = wjob.pop(i)
            o_t, o_b = mo.nxt()
            for tb in range(TQ // 512):
                g_t, g_b = gts[tb]
                ps = []
                for b in range(3):
                    p_t, p_b = pp.nxt()
                    for kc in range(8):
                        S.op("pe", lambda e, p_t=p_t, b=b, kc=kc, tb=tb: e.matmul(p_t[:, :], lhsT=wb_t[:, b * 8 + kc, :], rhs=yb[:, b * 8 + kc, tb * 512:(tb + 1) * 512], start=(kc == 0), stop=(kc == 7)),
                             reads=[wb_b, yb_b], writes=[p_b])
                    ps.append((p_t, p_b))
                ms = []
                for b, mp in enumerate((m0p, m1p, m2p)):
                    m_t, m_b = mp.nxt()
                    S.op("dve", lambda e, m_t=m_t, b=b, g_t=g_t, p_t=ps[b][0]: e.tensor_tensor(out=m_t[:, :], in0=p_t[:, :], in1=g_t[:, b, :], op=ALU.mult),
                         reads=[ps[b][1], g_b], writes=[m_b])
                    ms.append((m_t, m_b))
                S.op("pool", lambda e, a=ms[0][0], b_=ms[1][0]: e.tensor_tensor(out=a[:, :], in0=a[:, :], in1=b_[:, :], op=ALU.add),
                     reads=[ms[1][1]], writes=[ms[0][1]])
                S.op("pool", lambda e, a=ms[0][0], c_=ms[2][0], tb=tb: e.tensor_tensor(out=o_t[:, tb * 512:(tb + 1) * 512], in0=a[:, :], in1=c_[:, :], op=ALU.add),
                     reads=[ms[0][1], ms[2][1]], writes=[o_b])
            S.op("sp", lambda e: e.dma_start(out=mergedT[dc * 128:(dc + 1) * 128, t0:t0 + TQ], in_=o_t[:, :]), reads=[o_b], writes=[C.buf("mergedT")], dma=True)

        pipeline(len(jobs), prep, compute, depth=2)
        C.end_stage()


def stage_outproj(C, mergedT, w_o, h_in, h_in_b, h_out, h_out_b, wo_c=None):
    nc, S = C.nc, C.S
    TQ = 1024
    mg_b = C.buf("mergedT")
    with contextlib.ExitStack() as st:
        mt = st.enter_context(nc.sbuf_tensor(U("mt"), [128, 16, TQ], BF16))
        mt_b = Buf("mt")
        hacc = st.enter_context(nc.sbuf_tensor(U("hacc"), [128, 8, D], F32))
        hacc_b = [Buf("hacc%d" % i) for i in range(8)]
        wst = Rot(nc, st, "wst", 3, [128, 16, 128], F32)
        wow = Rot(nc, st, "wow", 2, [128, 16, 512], BF16)
        wo_cur = {}
        pp = Rot(nc, st, "pp", 4, [128, 512], F32, psum=True)
        jobs = [(tq, cc) for tq in range(T // TQ) for cc in range(16)]
        wjob = {}

        def prep(i):
            tq, cc = jobs[i]
            q = cc % 4
            if q == 0:
                wo_cur["t"] = wow.nxt()
            wb_t, wb_b = wo_cur["t"]
            if wo_c is not None:
                S.op("sp", lambda e: e.dma_start(out=wb_t[:, :, q * 128:(q + 1) * 128], in_=wo_c[cc].rearrange("p (a b) -> p a b", b=128)), reads=[C.buf("wo_c")], writes=[wb_b], dma=True)
            else:
                ws_t, ws_b = wst.nxt()
                S.op("sp", lambda e: e.dma_start(out=ws_t[:, :, :], in_=w_o[:, cc * 128:(cc + 1) * 128].rearrange("(kc p) n -> p kc n", p=128)), writes=[ws_b], dma=True)
                cast_op(C, wb_t[:, :, q * 128:(q + 1) * 128], ws_t[:, :, :], [ws_b], [wb_b])
            wjob[i] = (wb_t, wb_b)

        def compute(i):
            tq, cc = jobs[i]
            t0 = tq * TQ
            if cc == 0:
                S.op("sp", lambda e: e.dma_start(out=mt[:, :, :], in_=mergedT[:, t0:t0 + TQ].rearrange("(kc p) t -> p kc t", p=128)), reads=[mg_b], writes=[mt_b], dma=True)
                for tt in range(8):
                    S.op("sp", lambda e, tt=tt: e.dma_start(out=hacc[:, tt, :], in_=h_in[t0 + tt * 128:t0 + (tt + 1) * 128, :]), reads=[h_in_b], writes=[hacc_b[tt]], dma=True)
            wb_t, wb_b = wjob.pop(i)
            if cc % 4 == 3:
                c4 = cc // 4
                for tt in range(8):
                    p_t, p_b = pp.nxt()
                    for kc in range(16):
                        S.op("pe", lambda e, p_t=p_t, kc=kc, tt=tt: e.matmul(p_t[:, :], lhsT=mt[:, kc, tt * 128:(tt + 1) * 128], rhs=wb_t[:, kc, :], start=(kc == 0), stop=(kc == 15)),
                             reads=[wb_b, mt_b], writes=[p_b])
                    S.op("dve", lambda e, p_t=p_t, tt=tt: e.tensor_tensor(out=hacc[:, tt, c4 * 512:(c4 + 1) * 512], in0=hacc[:, tt, c4 * 512:(c4 + 1) * 512], in1=p_t[:, :], op=ALU.add),
                         reads=[p_b], writes=[hacc_b[tt]])
            if cc == 15:
                for tt in range(8):
                    S.op("sp", lambda e, tt=tt: e.dma_start(out=h_out[t0 + tt * 128:t0 + (tt + 1) * 128, :], in_=hacc[:, tt, :]), reads=[hacc_b[tt]], writes=[h_out_b], dma=True)

        pipeline(len(jobs), prep, compute, depth=2)
        C.end_stage()


def stage_route(C, h_ap, h_b, gain_ap, w_router, ident_f, xn2, idx_d, gate_d, NE=16, CAP=512):
    nc, S = C.nc, C.S
    with contextlib.ExitStack() as st:
        idf, idfb, idb, idbb = load_consts(C, st, ident_f)
        pools = norm_pools(C, st, nh=3)
        gain_t, gain_b = load_gain(C, st, "gain2", gain_ap, D)
        wr = st.enter_context(nc.sbuf_tensor(U("wr"), [128, 16, NE], F32))
        wr_b = Buf("wr")
        S.op("sp", lambda e: e.dma_start(out=wr[:, :, :], in_=w_router.rearrange("(kc p) n -> p kc n", p=128)), writes=[wr_b], dma=True)
        xf = Rot(nc, st, "xf", 3, [128, D], F32)
        xb = Rot(nc, st, "xb", 3, [128, D], BF16)
        xT = Rot(nc, st, "xT", 3, [128, 16, 128], F32)
        sm = Rot(nc, st, "sm", 4, [128, 8], F32)
        ex = Rot(nc, st, "ex", 4, [128, NE], F32)
        af = Rot(nc, st, "af", 4, [128, NE], F32)
        affT = st.enter_context(nc.sbuf_tensor(U("affT"), [NE, T], F32))
        affT2 = st.enter_context(nc.sbuf_tensor(U("affT2"), [NE, T], F32))
        affT_b, affT2_b = Buf("affT"), Buf("affT2")
        vals = st.enter_context(nc.sbuf_tensor(U("vals"), [NE, CAP], F32))
        idx = st.enter_context(nc.sbuf_tensor(U("idx"), [NE, CAP], U32))
        vals_b, idx_b = Buf("vals"), Buf("idx")
        ptr = Rot(nc, st, "ptr", 3, [128, 4, 128], F32, psum=True)
        pl = Rot(nc, st, "pl", 3, [128, 512], F32, psum=True)
        pt2 = Rot(nc, st, "pt2", 2, [128, 512], F32, psum=True)
        lg = {}

        def front(tt):
            x_t, x_b = xf.nxt()
            rms_tile(C, pools, h_ap[tt * 128:(tt + 1) * 128, :], gain_t, gain_b, x_t[:], x_b, D, [h_b])
            xb_t, xb_b = xb.nxt()
            S.op("act", lambda e: e.copy(out=xb_t[:, :], in_=x_t[:, :]), reads=[x_b], writes=[xb_b])
            S.op("sp", lambda e: e.dma_start(out=xn2[tt * 128:(tt + 1) * 128, :], in_=xb_t[:, :]), reads=[xb_b], writes=[C.buf("xn2")], dma=True)
            xT_t, xT_b = xT.nxt()
            for g in range(4):
                p_t, p_b = ptr.nxt()
                for j in range(4):
                    kc = g * 4 + j
                    S.op("pe", lambda e, p_t=p_t, j=j, kc=kc: e.transpose(out=p_t[:, j, :], in_=x_t[:, kc * 128:(kc + 1) * 128], identity=idf[:]),
                         reads=[x_b, idfb], writes=[p_b])
                S.op("dve", lambda e, p_t=p_t, g=g: e.tensor_copy(out=xT_t[:, g * 4:(g + 1) * 4, :], in_=p_t[:, :, :]), reads=[p_b], writes=[xT_b])
            l_t, l_b = pl.nxt()
            for kc in range(16):
                S.op("pe", lambda e, kc=kc: e.matmul(l_t[:, :NE], lhsT=xT_t[:, kc, :], rhs=wr[:, kc, :], start=(kc == 0), stop=(kc == 15)),
                     reads=[xT_b, wr_b], writes=[l_b])
            lg[tt] = (l_t, l_b)

        def back(tt):
            l_t, l_b = lg.pop(tt)
            s_t, s_b = sm.nxt()
            S.op("dve", lambda e: e.tensor_reduce(out=s_t[:, 0:1], in_=l_t[:, :NE], axis=AX.X, op=ALU.max, negate=True), reads=[l_b], writes=[s_b])
            e_t, e_b = ex.nxt()
            S.op("act", lambda e: e.activation(out=e_t[:, :], in_=l_t[:, :NE], func=AF.Exp, bias=s_t[:, 0:1], accum_out=s_t[:, 1:2]),
                 reads=[l_b, s_b], writes=[e_b, s_b])
            S.op("dve", lambda e: e.reciprocal(out=s_t[:, 2:3], in_=s_t[:, 1:2]), reads=[s_b], writes=[s_b])
            a_t, a_b = af.nxt()
            S.op("dve", lambda e: e.tensor_scalar(out=a_t[:, :], in0=e_t[:, :], scalar1=s_t[:, 2:3], scalar2=None, op0=ALU.mult), reads=[e_b, s_b], writes=[a_b])
            q_t, q_b = pt2.nxt()
            S.op("pe", lambda e: e.transpose(out=q_t[:NE, :128], in_=a_t[:, :], identity=idf[:]), reads=[a_b, idfb], writes=[q_b])
            S.op("dve", lambda e: e.tensor_copy(out=affT[:, tt * 128:(tt + 1) * 128], in_=q_t[:NE, :128]), reads=[q_b], writes=[affT_b])

        NT = T // 128
        SKR = 2
        for tt in range(NT + SKR):
            if tt < NT:
                front(tt)
            if tt - SKR >= 0:
                back(tt - SKR)
        cur, cur_b, oth, oth_b = affT, affT_b, affT2, affT2_b
        for r in range(CAP // 8):
            S.op("dve", lambda e, cur=cur, r=r: e.max(out=vals[:, r * 8:(r + 1) * 8], in_=cur[:, :]), reads=[cur_b], writes=[vals_b])
            S.op("dve", lambda e, cur=cur, r=r: e.max_index(out=idx[:, r * 8:(r + 1) * 8], in_max=vals[:, r * 8:(r + 1) * 8], in_values=cur[:, :]), reads=[cur_b, vals_b], writes=[idx_b])
            if r < CAP // 8 - 1:
                S.op("dve", lambda e, cur=cur, oth=oth, r=r: e.match_replace(out=oth[:, :], in_to_replace=vals[:, r * 8:(r + 1) * 8], in_values=cur[:, :], imm_value=-1.0),
                     reads=[cur_b, vals_b], writes=[oth_b])
                cur, cur_b, oth, oth_b = oth, oth_b, cur, cur_b
        S.op("sp", lambda e: e.dma_start(out=idx_d[:, :], in_=idx[:, :]), reads=[idx_b], writes=[C.buf("idx_d")], dma=True)
        S.op("sp", lambda e: e.dma_start(out=gate_d[:, :], in_=vals[:, :]), reads=[vals_b], writes=[C.buf("gate_d")], dma=True)
        C.end_stage()


def stage_experts(C, h_ap, h_b, xn2, idx_d, gate_d, w_g, w_u, w_d, ident_f, NE=16, CAP=512):
    nc, S = C.nc, C.S
    NJ = CAP // 128
    with contextlib.ExitStack() as st:
        idf, idfb, idb, idbb = load_consts(C, st, ident_f)
        idxc = st.enter_context(nc.sbuf_tensor(U("idxc"), [128, NE * NJ], U32))
        gatec = st.enter_context(nc.sbuf_tensor(U("gatec"), [128, NE * NJ], F32))
        idxc_b, gatec_b = Buf("idxc"), Buf("gatec")
        S.op("sp", lambda e: e.dma_start(out=idxc[:, :].rearrange("p (e j) -> p e j", j=NJ), in_=idx_d.rearrange("e (j p) -> p e j", p=128), allow_slow_non_contiguous=True),
             reads=[C.buf("idx_d")], writes=[idxc_b], dma=True)
        S.op("sp", lambda e: e.dma_start(out=gatec[:, :].rearrange("p (e j) -> p e j", j=NJ), in_=gate_d.rearrange("e (j p) -> p e j", p=128), allow_slow_non_contiguous=True),
             reads=[C.buf("gate_d")], writes=[gatec_b], dma=True)
        xg = Rot(nc, st, "xg", 4, [128, D], BF16)
        xeTp = Rot(nc, st, "xeT", 2, [128, 16, CAP], BF16)
        hT = st.enter_context(nc.sbuf_tensor(U("hT"), [128, 16, CAP], BF16))
        hT_b = Buf("hT")
        ye = st.enter_context(nc.sbuf_tensor(U("ye"), [128, NJ, D], F32))
        ye_b = [Buf("ye%d" % j) for j in range(NJ)]
        wst = Rot(nc, st, "wst", 3, [128, 16, 128], F32)
        wbf = Rot(nc, st, "wbf", 3, [128, 16, 128], BF16)
        sg = Rot(nc, st, "sg", 2, [128, CAP], F32)
        wdw = Rot(nc, st, "wdw", 2, [128, 16, 512], BF16)
        wd_cur = {}
        ptr = Rot(nc, st, "ptr", 2, [128, 8, 128], BF16, psum=True)
        pg = Rot(nc, st, "pg", 2, [128, 512], F32, psum=True)
        pu = Rot(nc, st, "pu", 2, [128, 512], F32, psum=True)
        pd = Rot(nc, st, "pd", 2, [128, 512], F32, psum=True)
        xn2_b = C.buf("xn2")

        def load_w(src):
            ws_t, ws_b = wst.nxt()
            S.op("sp", lambda e: e.dma_start(out=ws_t[:, :, :], in_=src.rearrange("(kc p) n -> p kc n", p=128)), writes=[ws_b], dma=True)
            wb_t, wb_b = wbf.nxt()
            cast_op(C, wb_t[:, :, :], ws_t[:, :, :], [ws_b], [wb_b])
            return wb_t, wb_b

        xe_of = {}

        def gather(ex):
            xeT, xeT_b = xeTp.nxt()
            xe_of[ex] = (xeT, xeT_b)
            for j in range(NJ):
                col = ex * NJ + j
                x_t, x_b = xg.nxt()
                S.op("pool", lambda e, x_t=x_t, col=col: e.indirect_dma_start(out=x_t[:, :], out_offset=None, in_=xn2[:, :], in_offset=bass.IndirectOffsetOnAxis(ap=idxc[:, col:col + 1], axis=0)),
                     reads=[xn2_b, idxc_b], writes=[x_b], dma=True)
                for g in range(2):
                    p_t, p_b = ptr.nxt()
                    for jj in range(8):
                        kc = g * 8 + jj
                        S.op("pe", lambda e, p_t=p_t, jj=jj, kc=kc, x_t=x_t: e.transpose(out=p_t[:, jj, :], in_=x_t[:, kc * 128:(kc + 1) * 128], identity=idb[:]),
                             reads=[x_b, idbb], writes=[p_b])
                    S.op("dve", lambda e, p_t=p_t, g=g, j=j: e.tensor_copy(out=xeT[:, g * 8:(g + 1) * 8, j * 128:(j + 1) * 128], in_=p_t[:]), reads=[p_b], writes=[xeT_b])

        jobs = []
        for ex in range(NE):
            for fc in range(16):
                jobs += [(ex, "g", fc), (ex, "u", fc)]
            jobs += [(ex, "d", dc) for dc in range(16)]
        wjob = {}
        sil = {}

        def prep(i):
            ex, kind, c = jobs[i]
            if kind == "d":
                q = c % 4
                if q == 0:
                    wd_cur["t"] = wdw.nxt()
                wt, wt_b = wd_cur["t"]
                ws_t, ws_b = wst.nxt()
                S.op("sp", lambda e: e.dma_start(out=ws_t[:, :, :], in_=w_d[ex, :, c * 128:(c + 1) * 128].rearrange("(kc p) n -> p kc n", p=128)), writes=[ws_b], dma=True)
                cast_op(C, wt[:, :, q * 128:(q + 1) * 128], ws_t[:, :, :], [ws_b], [wt_b])
                wjob[i] = (wt, wt_b)
                return
            src = {"g": w_g, "u": w_u}[kind]
            wjob[i] = load_w(src[ex, :, c * 128:(c + 1) * 128])

        def compute(i):
            ex, kind, c = jobs[i]
            if kind == "g":
                xeT, xeT_b = xe_of[ex]
                wg_t, wg_b = wjob.pop(i)
                g_t, g_b = pg.nxt()
                for kc in range(16):
                    S.op("pe", lambda e, kc=kc: e.matmul(g_t[:, :CAP], lhsT=wg_t[:, kc, :], rhs=xeT[:, kc, :], start=(kc == 0), stop=(kc == 15)),
                         reads=[wg_b, xeT_b], writes=[g_b])
                s_t, s_b = sg.nxt()
                S.op("act", lambda e: e.activation(out=s_t[:, :], in_=g_t[:, :CAP], func=AF.Silu), reads=[g_b], writes=[s_b])
                sil[(ex, c)] = (s_t, s_b)
            elif kind == "u":
                xeT, xeT_b = xe_of[ex]
                wu_t, wu_b = wjob.pop(i)
                u_t, u_b = pu.nxt()
                for kc in range(16):
                    S.op("pe", lambda e, kc=kc: e.matmul(u_t[:, :CAP], lhsT=wu_t[:, kc, :], rhs=xeT[:, kc, :], start=(kc == 0), stop=(kc == 15)),
                         reads=[wu_b, xeT_b], writes=[u_b])
                s_t, s_b = sil.pop((ex, c))
                S.op("dve", lambda e: e.tensor_tensor(out=hT[:, c, :], in0=u_t[:, :CAP], in1=s_t[:, :], op=ALU.mult), reads=[u_b, s_b], writes=[hT_b])
            else:
                dc = c
                if dc == 0 and ex + 1 < NE:
                    gather(ex + 1)
                wd_t, wd_b = wjob.pop(i)
                if dc % 4 == 3:
                    d4 = dc // 4
                    for j in range(NJ):
                        col = ex * NJ + j
                        p_t, p_b = pd.nxt()
                        for fc in range(16):
                            S.op("pe", lambda e, p_t=p_t, j=j, fc=fc: e.matmul(p_t[:, :], lhsT=hT[:, fc, j * 128:(j + 1) * 128], rhs=wd_t[:, fc, :], start=(fc == 0), stop=(fc == 15)),
                                 reads=[wd_b, hT_b], writes=[p_b])
                        S.op("act", lambda e, p_t=p_t, j=j, col=col: e.activation(out=ye[:, j, d4 * 512:(d4 + 1) * 512], in_=p_t[:, :], func=AF.Copy, scale=gatec[:, col:col + 1]),
                             reads=[p_b, gatec_b], writes=[ye_b[j]])
                if dc == 15:
                    for j in range(NJ):
                        col = ex * NJ + j
                        S.op("pool", lambda e, j=j, col=col: e.indirect_dma_start(out=h_ap[:, :], out_offset=bass.IndirectOffsetOnAxis(ap=idxc[:, col:col + 1], axis=0), in_=ye[:, j, :], in_offset=None, compute_op=ALU.add),
                             reads=[ye_b[j], idxc_b], writes=[h_b], dma=True)

        gather(0)
        pipeline(len(jobs), prep, compute, depth=2)
        C.end_stage()


def stage_final(C, h_ap, h_b, gain_ap, y_ap, y_b):
    nc, S = C.nc, C.S
    with contextlib.ExitStack() as st:
        pools = norm_pools(C, st)
        gain_t, gain_b = load_gain(C, st, "gainf", gain_ap, D)
        of = Rot(nc, st, "of", 2, [128, D], F32)
        for tt in range(T // 128):
            o_t, o_b = of.nxt()
            rms_tile(C, pools, h_ap[tt * 128:(tt + 1) * 128, :], gain_t, gain_b, o_t[:], o_b, D, [h_b])
            S.op("sp", lambda e, o_t=o_t, tt=tt: e.dma_start(out=y_ap[tt * 128:(tt + 1) * 128, :], in_=o_t[:, :]), reads=[o_b], writes=[y_b], dma=True)
        C.end_stage()


NCORES = 4
NB = 4 // NCORES
DEPTH = 2
_CACHE = {}


def build_program():
    nc = bass.Bass("TRN2", target_bir_lowering=False)
    def inp(name, shape, dt=F32):
        return nc.dram_tensor(name, list(shape), dt, kind="ExternalInput").ap()
    x = inp("x", [NB * T, D])
    w_in = inp("w_in", [DEPTH, D, INC])
    b_gate = inp("b_gate", [DEPTH, 6144])
    w_uq = inp("w_uq", [DEPTH, 448, 1536])
    q_norm = inp("q_norm", [DEPTH, 448])
    w_ukv = inp("w_ukv", [DEPTH, 160, 2048])
    kv_norm = inp("kv_norm", [DEPTH, 160])
    rpbT = inp("rpbT", [DEPTH, 16, 128, 14, 64])
    maskc = inp("maskc", [128, 14, 64])
    w_branch = inp("w_branch", [DEPTH, 3, 1024, 2048])
    w_o = inp("w_o", [DEPTH, D, D])
    norm_mix = inp("norm_mix", [DEPTH, D])
    norm_moe = inp("norm_moe", [DEPTH, D])
    w_router = inp("w_router", [DEPTH, D, 16])
    w_g = inp("w_exp_gate", [DEPTH, 16, D, D])
    w_u = inp("w_exp_up", [DEPTH, 16, D, D])
    w_d = inp("w_exp_down", [DEPTH, 16, D, D])
    norm_final = inp("norm_final", [D])
    ident = inp("ident", [128, 128])
    cos2T = inp("cos2T", [64, T])
    sin2T = inp("sin2T", [64, T])
    ccs = inp("ccs", [2, 256, 256])
    csT = inp("csT", [T, T], BF16)
    ssT = inp("ssT", [T, T], BF16)
    y = nc.dram_tensor("y", [NB * T, D], F32, kind="ExternalOutput").ap()
    with contextlib.ExitStack() as es:
        C = Ctx(nc, es)
        sc = {"qkT": C.dram("qkT", [2048, T], BF16), "vna": C.dram("vna", [T, 1024], BF16), "cT": C.dram("cT", [672, T], F32),
              "ufT": C.dram("ufT", [1024, T], BF16), "gT": C.dram("gT", [6144, T], BF16),
              "ynaT": C.dram("ynaT", [1024, T], BF16), "ymlaT": C.dram("ymlaT", [1024, T], BF16), "yfT": C.dram("yfT", [1024, T], BF16)}
        mergedT = C.dram("mergedT", [2048, T], BF16)
        wbr_c = C.dram("wbr_c", [16, 128, 24 * 128], BF16)
        wo_c = C.dram("wo_c", [16, 128, 16 * 128], BF16)
        hA = C.dram("hA", [T, D], F32)
        xn2 = C.dram("xn2", [T, D], BF16)
        idx_d = C.dram("idx_d", [16, 512], U32)
        gate_d = C.dram("gate_d", [16, 512], F32)
        hA_b = C.buf("hA")
        x_b = Buf("x")
        y_b = C.buf("y")
        for b in range(NB):
            xb = x[b * T:(b + 1) * T, :]
            for l in range(DEPTH):
                h_in, h_in_b = (xb, x_b) if l == 0 else (hA, hA_b)
                stage_inproj(C, h_in, h_in_b, norm_mix[l], w_in[l], b_gate[l], ident, sc)
                stage_na(C, sc, rpbT[l], maskc, sc["ynaT"], wprep=(w_branch[l], w_o[l], wbr_c, wo_c))
                stage_mla(C, sc, w_uq[l], q_norm[l], w_ukv[l], kv_norm[l], cos2T, sin2T, sc["ymlaT"])
                stage_fnet(C, sc, ccs, csT, ssT, sc["yfT"])
                stage_merge(C, sc, w_branch[l], mergedT, wbr_c=wbr_c)
                stage_outproj(C, mergedT, w_o[l], h_in, h_in_b, hA, hA_b, wo_c=wo_c)
                stage_route(C, hA, hA_b, norm_moe[l], w_router[l], ident, xn2, idx_d, gate_d)
                stage_experts(C, hA, hA_b, xn2, idx_d, gate_d, w_g[l], w_u[l], w_d[l], ident)
            stage_final(C, hA, hA_b, norm_final, y[b * T:(b + 1) * T, :], y_b)
    return nc


def rope_consts():
    pos = np.arange(T, dtype=np.float32)
    inv = (1.0 / (10000.0 ** (np.arange(0, 64, 2, dtype=np.float32) / 64))).astype(np.float32)
    ang = pos[:, None] * inv[None, :]
    cos, sin = np.cos(ang).astype(np.float32), np.sin(ang).astype(np.float32)
    cos2T = np.ascontiguousarray(np.concatenate([cos, cos], 1).T)
    sin2T = np.ascontiguousarray(np.concatenate([-sin, sin], 1).T)
    return cos2T, sin2T


def kernel(x, w_in, b_gate, w_uq, q_norm, w_ukv, kv_norm, na_rpb, w_branch, w_o,
           norm_mix, norm_moe, w_router, w_exp_gate, w_exp_up, w_exp_down, norm_final):
    f = lambda a: np.ascontiguousarray(np.asarray(a, dtype=np.float32))
    if "nc" not in _CACHE:
        _CACHE["nc"] = build_program()
        cos2T, sin2T = rope_consts()
        ccs, csT, ssT = fnet_host_consts()
        _CACHE["consts"] = dict(cos2T=cos2T, sin2T=sin2T, ccs=ccs, csT=csT, ssT=ssT, ident=np.eye(128, dtype=np.float32))
    nc = _CACHE["nc"]
    na_rpb = f(na_rpb)
    tabs = [na_host_tables(na_rpb[l]) for l in range(DEPTH)]
    rpbT = np.stack([t[0] for t in tabs])
    maskc = tabs[0][1]
    shared = dict(w_in=f(w_in), b_gate=f(b_gate), w_uq=f(w_uq), q_norm=f(q_norm), w_ukv=f(w_ukv), kv_norm=f(kv_norm),
                  rpbT=rpbT, maskc=maskc, w_branch=f(w_branch), w_o=f(w_o), norm_mix=f(norm_mix), norm_moe=f(norm_moe),
                  w_router=f(w_router), w_exp_gate=f(w_exp_gate), w_exp_up=f(w_exp_up), w_exp_down=f(w_exp_down),
                  norm_final=f(norm_final), **_CACHE["consts"])
    xf = f(x).reshape(4 * T, D)
    in_maps = []
    for c in range(NCORES):
        m = dict(shared)
        m["x"] = xf[c * NB * T:(c + 1) * NB * T]
        in_maps.append(m)
    res = run_bass_kernel_spmd(nc, in_maps, core_ids=list(range(NCORES)))
    out = np.concatenate([np.asarray(r["y"], dtype=np.float32) for r in res.results], axis=0)
    return out.reshape(4, T, D)
```

```python
import numpy as np
import concourse.bass as bass
import concourse.mybir as mybir
from concourse.bass_utils import run_bass_kernel_spmd

F32 = mybir.dt.float32
BF16 = mybir.dt.bfloat16
I32 = mybir.dt.int32
U32 = mybir.dt.uint32
U16 = mybir.dt.uint16
AF = mybir.ActivationFunctionType
ALU = mybir.AluOpType
AX = mybir.AxisListType


class Buf:
    __slots__ = ("name", "w", "r")

    def __init__(self, name=""):
        self.name = name
        self.w = []
        self.r = []


def _merge(tokens):
    d = {}
    for s, v in tokens:
        if d.get(s, 0) < v:
            d[s] = v
    return d


class Sched:
    ENG = ("pe", "act", "dve", "pool", "sp")

    def __init__(self, nc, es, n_dma_sems=40, rot=30000):
        self.nc = nc
        self.es = es
        self.rot = rot
        self.lists = {e: [] for e in self.ENG}
        self.sems = []
        self.cur = {}
        self.known = {e: {} for e in self.ENG}
        for e in self.ENG:
            self.cur[e] = [self._new_sem("e_" + e), 0]
        self.dma_pool = [[self._new_sem("d%d" % i), 0] for i in range(n_dma_sems)]
        self.dma_next = 0
        self.n_ops = 0

    def _new_sem(self, name):
        h = self.es.enter_context(self.nc.semaphore(name + "_%d" % len(self.sems)))
        self.sems.append(h)
        return len(self.sems) - 1

    def op(self, eng, fn, reads=(), writes=(), dma=False):
        deps = []
        for b in reads:
            deps += b.w
        for b in writes:
            deps += b.w
            deps += b.r
        tok_extra = None
        if dma:
            slot = self.dma_pool[self.dma_next]
            self.dma_next = (self.dma_next + 1) % len(self.dma_pool)
            if slot[1] > 0:
                deps.append((slot[0], 16 * slot[1]))
            slot[1] += 1
            token = (slot[0], 16 * slot[1])
            inc = (slot[0], 16)
        else:
            c = self.cur[eng]
            if c[1] >= self.rot:
                c[0] = self._new_sem("e_" + eng)
                c[1] = 0
            c[1] += 1
            token = (c[0], c[1])
            inc = (c[0], 1)
        need = _merge(deps)
        kn = self.known[eng]
        waits = []
        own = self.cur[eng][0]
        for s, v in need.items():
            if eng == "pe" and s == own and not dma:
                continue
            if kn.get(s, 0) >= v:
                continue
            kn[s] = v
            waits.append((s, v))
        self.lists[eng].append((waits, fn, inc))
        for b in writes:
            b.w = [token]
            b.r = []
        for b in reads:
            if b in writes:
                continue
            m = _merge(b.r + [token])
            b.r = list(m.items())
        self.n_ops += 1
        return token

    def wait_all(self, eng, bufs):
        deps = []
        for b in bufs:
            deps += b.w
        need = _merge(deps)
        waits = [(s, v) for s, v in need.items()]
        self.lists[eng].append((waits, None, None))

    def emit(self):
        nc = self.nc
        sems = self.sems
        lists = self.lists

        def run(engname, e):
            for waits, fn, inc in lists[engname]:
                for s, v in waits:
                    e.wait_ge(sems[s], v)
                if fn is not None:
                    ins = fn(e)
                    ins.then_inc(sems[inc[0]], inc[1])

        with nc.Block() as block:
            @block.tensor
            def _(e):
                run("pe", e)

            @block.scalar
            def _(e):
                run("act", e)

            @block.vector
            def _(e):
                run("dve", e)

            @block.gpsimd
            def _(e):
                run("pool", e)

            @block.sync
            def _(e):
                run("sp", e)


import contextlib

D = 2048
T = 4096
INC = 10912
EPS = 1e-6


_UID = [0]


def U(name):
    _UID[0] += 1
    return "%s_u%d" % (name, _UID[0])


class Rot:
    def __init__(self, nc, st, name, n, shape, dtype, psum=False):
        self.slots = []
        for i in range(n):
            if psum:
                t = st.enter_context(nc.psum_tensor(U("%s%d" % (name, i)), shape, dtype))
            else:
                t = st.enter_context(nc.sbuf_tensor(U("%s%d") % (name, i), shape, dtype))
            self.slots.append((t, Buf(name + str(i))))
        self.i = 0

    def nxt(self):
        s = self.slots[self.i]
        self.i = (self.i + 1) % len(self.slots)
        return s


class Ctx:
    def __init__(self, nc, es, debug_outs=()):
        self.nc = nc
        self.es = es
        self.S = Sched(nc, es)
        self.debug_outs = set(debug_outs)
        self.dbufs = {}
        self.cast_i = 0

    def dram(self, name, shape, dtype):
        kind = "ExternalOutput" if name in self.debug_outs else "Internal"
        t = self.nc.dram_tensor(name, list(shape), dtype, kind=kind).ap()
        return t

    def buf(self, key):
        if key not in self.dbufs:
            self.dbufs[key] = Buf(str(key))
        return self.dbufs[key]

    def end_stage(self):
        S = self.S
        waits = [(s[0], 16 * s[1]) for s in S.dma_pool if s[1] > 0]
        for e in ("sp", "pool", "act"):
            S.lists[e].append((list(waits), None, None))
        S.emit()
        S.lists = {e: [] for e in S.ENG}


def load_consts(C, st, ident_f):
    nc, S = C.nc, C.S
    idf = st.enter_context(nc.sbuf_tensor(U("idf"), [128, 128], F32))
    idb = st.enter_context(nc.sbuf_tensor(U("idb"), [128, 128], BF16))
    bf = Buf("idf")
    bb = Buf("idb")
    S.op("sp", lambda e: e.dma_start(out=idf[:], in_=ident_f[:, :]), writes=[bf], dma=True)
    S.op("dve", lambda e: e.tensor_copy(out=idb[:], in_=idf[:]), reads=[bf], writes=[bb])
    return idf, bf, idb, bb


def rms_tile(C, pools, src_ap, gain_t, gain_b, out_t, out_b, width, src_reads):
    nc, S = C.nc, C.S
    ht, hb = pools["h"].nxt()
    S.op("sp", lambda e: e.dma_start(out=ht[:, :width], in_=src_ap), reads=src_reads, writes=[hb], dma=True)
    jt, jb = pools["junk"].nxt()
    st_, sb_ = pools["stat"].nxt()
    S.op("act", lambda e: e.activation(out=jt[:, :width], in_=ht[:, :width], func=AF.Square, accum_out=st_[:, 0:1]),
         reads=[hb], writes=[jb, sb_])
    S.op("act", lambda e: e.activation(out=st_[:, 1:2], in_=st_[:, 0:1], func=AF.Sqrt, scale=1.0 / width, bias=pools["eps"][0][:, 0:1]),
         reads=[sb_, pools["eps"][1]], writes=[sb_])
    S.op("dve", lambda e: e.reciprocal(out=st_[:, 2:3], in_=st_[:, 1:2]), reads=[sb_], writes=[sb_])
    S.op("dve", lambda e: e.scalar_tensor_tensor(out=out_t, in0=ht[:, :width], scalar=st_[:, 2:3], in1=gain_t[:, :width],
                                                op0=ALU.mult, op1=ALU.mult),
         reads=[hb, sb_, gain_b], writes=[out_b])
    return ht, hb


def norm_pools(C, st, width=D, nh=2):
    nc, S = C.nc, C.S
    pools = {
        "h": Rot(nc, st, "nh", nh, [128, width], F32),
        "junk": Rot(nc, st, "nj", 1, [128, width], BF16),
        "stat": Rot(nc, st, "ns", 4, [128, 4], F32),
    }
    eps_t = st.enter_context(nc.sbuf_tensor(U("epsT"), [128, 1], F32))
    eb = Buf("eps")
    S.op("dve", lambda e: e.memset(eps_t[:], EPS), writes=[eb])
    pools["eps"] = (eps_t, eb)
    return pools


def load_gain(C, st, name, vec_ap, width):
    nc, S = C.nc, C.S
    g = st.enter_context(nc.sbuf_tensor(U(name), [128, width], F32))
    gb = Buf(name)
    S.op("sp", lambda e: e.dma_start(out=g[:], in_=vec_ap.partition_broadcast(128)), writes=[gb], dma=True)
    return g, gb


def pipeline(n, prep, compute, depth=1):
    for i in range(min(depth, n)):
        prep(i)
    for i in range(n):
        if i + depth < n:
            prep(i + depth)
        compute(i)


CAST_PATTERN = {"default": ("act", "dve"), "moe": ("act", "dve", "act", "dve", "pool")}


def cast_op(C, out_ap, in_ap, reads, writes, pattern="default"):
    S = C.S
    pat = CAST_PATTERN[pattern]
    eng = pat[C.cast_i % len(pat)]
    C.cast_i += 1
    if eng == "dve":
        S.op("dve", lambda e: e.tensor_copy(out=out_ap, in_=in_ap), reads=reads, writes=writes)
    elif eng == "act":
        S.op("act", lambda e: e.copy(out=out_ap, in_=in_ap), reads=reads, writes=writes)
    else:
        S.op("pool", lambda e: e.tensor_copy(out=out_ap, in_=in_ap), reads=reads, writes=writes)


def stage_inproj(C, h_ap, h_buf, gain_ap, w_in, b_gate, ident_f, sc):
    nc, S = C.nc, C.S
    HALF = 2048
    with contextlib.ExitStack() as st:
        idf, idfb, idb, idbb = load_consts(C, st, ident_f)
        pools = norm_pools(C, st)
        gain_t, gain_b = load_gain(C, st, "gain1", gain_ap, D)
        xs_pool = Rot(nc, st, "xs", 2, [128, D], BF16)
        xnT = st.enter_context(nc.sbuf_tensor(U("xnT"), [128, 16, HALF], BF16))
        xnT_b = [Buf("xnT%d" % i) for i in range(16)]
        wst = Rot(nc, st, "wst", 3, [128, 16, 128], F32)
        wbf = Rot(nc, st, "wbf", 3, [128, 16, 128], BF16)
        ost_b = Rot(nc, st, "ostb", 2, [128, HALF], BF16)
        ost_f = Rot(nc, st, "ostf", 2, [128, HALF], F32)
        bg = st.enter_context(nc.sbuf_tensor(U("bg"), [128, 48], F32))
        bgb = Buf("bg")
        S.op("sp", lambda e: e.dma_start(out=bg[:], in_=b_gate.rearrange("(c p) -> p c", p=128), allow_slow_non_contiguous=True), writes=[bgb], dma=True)
        pmm = Rot(nc, st, "pmm", 6, [128, 512], F32, psum=True)
        ptr = Rot(nc, st, "ptr", 2, [128, 8, 128], BF16, psum=True)

        chunks = []
        for i in range(16):
            chunks.append((i * 128, 128, "F", "qkT", i * 128))
        for i in range(8):
            chunks.append((2048 + i * 128, 128, "T", "vna", i * 128))
        c0 = 3072
        r0 = 0
        while r0 < 672:
            m = min(128, 672 - r0)
            chunks.append((c0 + r0, m, "C", "cT", r0))
            r0 += m
        for i in range(8):
            chunks.append((3744 + i * 128, 128, "F", "ufT", i * 128))
        for i in range(48):
            chunks.append((4768 + i * 128, 128, "G", "gT", i * 128))

        w_v = w_in
        for half in range(T // HALF):
            t0 = half * HALF
            for tt in range(16):
                xs_t, xs_b = xs_pool.nxt()
                rms_tile(C, pools, h_ap[t0 + tt * 128: t0 + (tt + 1) * 128, :], gain_t, gain_b, xs_t[:], xs_b, D, [h_buf])
                for g in range(2):
                    pt, pb = ptr.nxt()
                    for j in range(8):
                        kc = g * 8 + j
                        S.op("pe", lambda e, pt=pt, j=j, kc=kc, xs_t=xs_t: e.transpose(out=pt[:, j, :], in_=xs_t[:, kc * 128:(kc + 1) * 128], identity=idb[:]),
                             reads=[xs_b, idbb], writes=[pb])
                    S.op("dve", lambda e, pt=pt, g=g, tt=tt: e.tensor_copy(out=xnT[:, g * 8:(g + 1) * 8, tt * 128:(tt + 1) * 128], in_=pt[:]),
                         reads=[pb], writes=[xnT_b[tt]])
            wjob = {}

            def prep(i):
                (c0, m, mode, dst, dr0) = chunks[i]
                ws_t, ws_b = wst.nxt()
                S.op("sp", lambda e: e.dma_start(out=ws_t[:, :, :m], in_=w_v[:, c0:c0 + m].rearrange("(kc p) n -> p kc n", p=128)),
                     writes=[ws_b], dma=True)
                wb_t, wb_b = wbf.nxt()
                cast_op(C, wb_t[:, :, :m], ws_t[:, :, :m], [ws_b], [wb_b])
                wjob[i] = (wb_t, wb_b)

            def compute(i, t0=t0):
                (c0, m, mode, dst, dr0) = chunks[i]
                wb_t, wb_b = wjob.pop(i)
                if mode != "T":
                    if mode == "C":
                        o_t, o_b = ost_f.nxt()
                    else:
                        o_t, o_b = ost_b.nxt()
                    for tb in range(4):
                        p_t, p_b = pmm.nxt()
                        for kc in range(16):
                            S.op("pe", lambda e, p_t=p_t, kc=kc, tb=tb: e.matmul(p_t[:m, :], lhsT=wb_t[:, kc, :m], rhs=xnT[:, kc, tb * 512:(tb + 1) * 512], start=(kc == 0), stop=(kc == 15)),
                                 reads=[wb_b] + xnT_b[tb * 4:(tb + 1) * 4], writes=[p_b])
                        if mode == "G":
                            gi = dr0 // 128
                            S.op("act", lambda e, p_t=p_t, tb=tb, gi=gi: e.activation(out=o_t[:, tb * 512:(tb + 1) * 512], in_=p_t[:, :], func=AF.Sigmoid, bias=bg[:, gi:gi + 1]),
                                 reads=[p_b, bgb], writes=[o_b])
                        else:
                            S.op("dve", lambda e, p_t=p_t, tb=tb: e.tensor_copy(out=o_t[:m, tb * 512:(tb + 1) * 512], in_=p_t[:m, :]),
                                 reads=[p_b], writes=[o_b])
                    S.op("sp", lambda e: e.dma_start(out=sc[dst][dr0:dr0 + m, t0:t0 + HALF], in_=o_t[:m, :]),
                         reads=[o_b], writes=[C.buf(dst)], dma=True)
                else:
                    o_t, o_b = ost_b.nxt()
                    for g4 in range(4):
                        p_t, p_b = pmm.nxt()
                        for q in range(4):
                            tt = g4 * 4 + q
                            for kc in range(16):
                                S.op("pe", lambda e, p_t=p_t, kc=kc, tt=tt, q=q: e.matmul(p_t[:, q * 128:(q + 1) * 128], lhsT=xnT[:, kc, tt * 128:(tt + 1) * 128], rhs=wb_t[:, kc, :], start=(kc == 0), stop=(kc == 15)),
                                     reads=[wb_b, xnT_b[tt]], writes=[p_b])
                        S.op("dve", lambda e, p_t=p_t, g4=g4: e.tensor_copy(out=o_t[:, g4 * 512:(g4 + 1) * 512], in_=p_t[:, :]),
                             reads=[p_b], writes=[o_b])
                    S.op("sp", lambda e: e.dma_start(
                        out=sc["vna"][t0:t0 + HALF, dr0:dr0 + 128].rearrange("(tt p) c -> p tt c", p=128),
                        in_=o_t[:, :].rearrange("p (tt c) -> p tt c", c=128)),
                        reads=[o_b], writes=[C.buf("vna")], dma=True)

            pipeline(len(chunks), prep, compute, depth=2)
        C.end_stage()


def fm_rmsnorm(C, st, name, src, src_buf, row0, nrows, gain_ap, onesf, onesf_b, eps, pmm, out_t, out_b, cin, csq, rs):
    nc, S = C.nc, C.S
    nch = (nrows + 127) // 128
    gcol = st.enter_context(nc.sbuf_tensor(U(name + "g"), [128, nch], F32))
    gb = Buf(name + "g")
    for c in range(nch):
        ksz = min(128, nrows - c * 128)
        S.op("sp", lambda e, c=c, ksz=ksz: e.dma_start(out=gcol[:ksz, c:c + 1], in_=gain_ap[c * 128:c * 128 + ksz].rearrange("(p o) -> p o", o=1)),
             writes=[gb], dma=True)
    for tb in range(T // 512):
        ci, cib = cin.nxt()
        cs, csb = csq.nxt()
        for c in range(nch):
            ksz = min(128, nrows - c * 128)
            S.op("sp", lambda e, ci=ci, c=c, ksz=ksz, tb=tb: e.dma_start(out=ci[:ksz, c, :], in_=src[row0 + c * 128: row0 + c * 128 + ksz, tb * 512:(tb + 1) * 512]),
                 reads=[src_buf], writes=[cib], dma=True)
        p_t, p_b = pmm.nxt()
        for c in range(nch):
            ksz = min(128, nrows - c * 128)
            S.op("act", lambda e, ci=ci, cs=cs, c=c, ksz=ksz: e.activation(out=cs[:ksz, c, :], in_=ci[:ksz, c, :], func=AF.Square),
                 reads=[cib], writes=[csb])
            S.op("pe", lambda e, p_t=p_t, cs=cs, c=c, ksz=ksz: e.matmul(p_t[:, :], lhsT=onesf[:ksz, :], rhs=cs[:ksz, c, :], start=(c == 0), stop=(c == nch - 1)),
                 reads=[csb, onesf_b], writes=[p_b])
        r_t, r_b = rs.nxt()
        S.op("act", lambda e, r_t=r_t, p_t=p_t: e.activation(out=r_t[:, :], in_=p_t[:, :], func=AF.Sqrt, scale=1.0 / nrows, bias=eps[0][:, 0:1]),
             reads=[p_b, eps[1]], writes=[r_b])
        S.op("dve", lambda e, r_t=r_t: e.reciprocal(out=r_t[:, :], in_=r_t[:, :]), reads=[r_b], writes=[r_b])
        for c in range(nch):
            ksz = min(128, nrows - c * 128)
            S.op("dve", lambda e, ci=ci, r_t=r_t, c=c, ksz=ksz, tb=tb: e.scalar_tensor_tensor(
                out=out_t[:ksz, c, tb * 512:(tb + 1) * 512], in0=ci[:ksz, c, :], scalar=gcol[:ksz, c:c + 1], in1=r_t[:ksz, :], op0=ALU.mult, op1=ALU.mult),
                reads=[cib, r_b, gb], writes=[out_b])


def stage_mla(C, sc, w_uq, q_norm, w_ukv, kv_norm, cos2T, sin2T, ymlaT):
    nc, S = C.nc, C.S
    cT = sc["cT"]
    cTb = C.buf("cT")
    scale = 192.0 ** -0.5
    QCH = [(0, 128), (128, 128), (256, 128), (384, 64)]
    KCH = [(0, 128), (128, 32)]
    with contextlib.ExitStack() as st:
        onesf = st.enter_context(nc.sbuf_tensor(U("onesf"), [128, 128], F32))
        onesb = st.enter_context(nc.sbuf_tensor(U("onesb"), [128, 128], BF16))
        of_b, ob_b = Buf("onesf"), Buf("onesb")
        S.op("dve", lambda e: e.memset(onesf[:], 1.0), writes=[of_b])
        S.op("dve", lambda e: e.memset(onesb[:], 1.0), writes=[ob_b])
        eps_t = st.enter_context(nc.sbuf_tensor(U("epsT"), [128, 1], F32))
        eb = Buf("eps")
        S.op("dve", lambda e: e.memset(eps_t[:], EPS), writes=[eb])
        pmm = Rot(nc, st, "pmm", 4, [128, 512], F32, psum=True)
        pO = Rot(nc, st, "pO", 2, [128, 512], F32, psum=True)
        pD = Rot(nc, st, "pD", 2, [128, 512], F32, psum=True)
        cqn = st.enter_context(nc.sbuf_tensor(U("cqn"), [128, 4, T], BF16))
        ckvn = st.enter_context(nc.sbuf_tensor(U("ckvn"), [128, 2, T], BF16))
        cqn_b, ckvn_b = Buf("cqn"), Buf("ckvn")
        cin = Rot(nc, st, "fci", 2, [128, 4, 512], F32)
        csq = Rot(nc, st, "fcs", 1, [128, 4, 512], F32)
        rs = Rot(nc, st, "frs", 2, [128, 512], F32)
        fm_rmsnorm(C, st, "nq", cT, cTb, 0, 448, q_norm, onesf, of_b, (eps_t, eb), pmm, cqn, cqn_b, cin, csq, rs)
        fm_rmsnorm(C, st, "nk", cT, cTb, 448, 160, kv_norm, onesf, of_b, (eps_t, eb), pmm, ckvn, ckvn_b, cin, csq, rs)
        kpe = st.enter_context(nc.sbuf_tensor(U("kpe"), [128, T], BF16))
        kpe_b = Buf("kpe")
        S.op("pool", lambda e: e.memset(kpe[64:128, :], 0.0), writes=[kpe_b])
        tmpA = Rot(nc, st, "tmpA", 2, [64, 512], F32)
        tmpB = Rot(nc, st, "tmpB", 2, [64, 512], F32)
        tmpC = Rot(nc, st, "tmpC", 2, [64, 512], F32)
        tmpD = Rot(nc, st, "tmpD", 2, [64, 512], F32)
        cosr = Rot(nc, st, "cosr", 2, [64, 512], F32)
        sinr = Rot(nc, st, "sinr", 2, [64, 512], F32)

        def rope(src_a, src_a_b, src_r, src_r_b, tb, out_ap, out_b):
            ct, cb = cosr.nxt()
            s_t, s_b = sinr.nxt()
            S.op("sp", lambda e: e.dma_start(out=ct[:, :], in_=cos2T[:, tb * 512:(tb + 1) * 512]), writes=[cb], dma=True)
            S.op("sp", lambda e: e.dma_start(out=s_t[:, :], in_=sin2T[:, tb * 512:(tb + 1) * 512]), writes=[s_b], dma=True)
            a_t, a_b = tmpC.nxt()
            b_t, b_b = tmpD.nxt()
            S.op("dve", lambda e: e.tensor_tensor(out=a_t[:, :], in0=src_a, in1=ct[:, :], op=ALU.mult), reads=[src_a_b, cb], writes=[a_b])
            S.op("dve", lambda e: e.tensor_tensor(out=b_t[:, :], in0=src_r, in1=s_t[:, :], op=ALU.mult), reads=[src_r_b, s_b], writes=[b_b])
            S.op("dve", lambda e: e.tensor_tensor(out=out_ap, in0=a_t[:, :], in1=b_t[:, :], op=ALU.add), reads=[a_b, b_b], writes=[out_b])

        for tb in range(T // 512):
            a_t, a_b = tmpA.nxt()
            r_t, r_b = tmpB.nxt()
            S.op("sp", lambda e, a_t=a_t, tb=tb: e.dma_start(out=a_t[:, :], in_=cT[608:672, tb * 512:(tb + 1) * 512]), reads=[cTb], writes=[a_b], dma=True)
            S.op("sp", lambda e, r_t=r_t, tb=tb: e.dma_start(out=r_t[0:32, :], in_=cT[640:672, tb * 512:(tb + 1) * 512]), reads=[cTb], writes=[r_b], dma=True)
            S.op("sp", lambda e, r_t=r_t, tb=tb: e.dma_start(out=r_t[32:64, :], in_=cT[608:640, tb * 512:(tb + 1) * 512]), reads=[cTb], writes=[r_b], dma=True)
            rope(a_t[:, :], a_b, r_t[:, :], r_b, tb, kpe[0:64, tb * 512:(tb + 1) * 512], kpe_b)

        qn = st.enter_context(nc.sbuf_tensor(U("qn"), [128, T], BF16))
        qp = st.enter_context(nc.sbuf_tensor(U("qp"), [128, T], BF16))
        kn = st.enter_context(nc.sbuf_tensor(U("kn"), [128, T], BF16))
        vv = st.enter_context(nc.sbuf_tensor(U("vv"), [128, 32, 128], BF16))
        qn_b, qp_b, kn_b, vv_b = Buf("qn"), Buf("qp"), Buf("kn"), Buf("vv")
        S.op("pool", lambda e: e.memset(qp[64:128, :], 0.0), writes=[qp_b])
        wq_s = st.enter_context(nc.sbuf_tensor(U("wq_s"), [128, 4, 256], F32))
        wq_bf = st.enter_context(nc.sbuf_tensor(U("wq_bf"), [128, 4, 256], BF16))
        wk_s = st.enter_context(nc.sbuf_tensor(U("wk_s"), [128, 2, 256], F32))
        wk_bf = st.enter_context(nc.sbuf_tensor(U("wk_bf"), [128, 2, 256], BF16))
        wq_sb, wq_bb, wk_sb, wk_bb = Buf("wqs"), Buf("wqb"), Buf("wks"), Buf("wkb")
        Et = Rot(nc, st, "Et", 6, [128, 512], BF16)
        accA = Rot(nc, st, "accA", 2, [128, 512], F32)
        accB = Rot(nc, st, "accB", 2, [128, 512], F32)
        rden = Rot(nc, st, "rden", 2, [128, 512], F32)
        yst = Rot(nc, st, "yst", 2, [128, 512], BF16)
        qa = Rot(nc, st, "qa", 2, [64, 512], F32)
        qr = Rot(nc, st, "qr", 2, [64, 512], F32)

        for h in range(8):
            for c, (k0, ksz) in enumerate(QCH):
                S.op("sp", lambda e, c=c, k0=k0, ksz=ksz, h=h: e.dma_start(out=wq_s[:ksz, c, 0:192], in_=w_uq[k0:k0 + ksz, h * 192:(h + 1) * 192]), writes=[wq_sb], dma=True)
                S.op("sp", lambda e, c=c, k0=k0, ksz=ksz, h=h: e.dma_start(out=wq_s[:ksz, c, 192:224], in_=w_uq[k0:k0 + ksz, h * 192 + 160:h * 192 + 192]), writes=[wq_sb], dma=True)
                S.op("sp", lambda e, c=c, k0=k0, ksz=ksz, h=h: e.dma_start(out=wq_s[:ksz, c, 224:256], in_=w_uq[k0:k0 + ksz, h * 192 + 128:h * 192 + 160]), writes=[wq_sb], dma=True)
            for c, (k0, ksz) in enumerate(KCH):
                S.op("sp", lambda e, c=c, k0=k0, ksz=ksz, h=h: e.dma_start(out=wk_s[:ksz, c, :], in_=w_ukv[k0:k0 + ksz, h * 256:(h + 1) * 256]), writes=[wk_sb], dma=True)
            for c, (k0, ksz) in enumerate(QCH):
                S.op("dve", lambda e, c=c, ksz=ksz: e.tensor_copy(out=wq_bf[:ksz, c, :], in_=wq_s[:ksz, c, :]), reads=[wq_sb], writes=[wq_bb])
            for c, (k0, ksz) in enumerate(KCH):
                S.op("dve", lambda e, c=c, ksz=ksz: e.tensor_copy(out=wk_bf[:ksz, c, :], in_=wk_s[:ksz, c, :]), reads=[wk_sb], writes=[wk_bb])
            for tb in range(T // 512):
                tsl = slice(tb * 512, (tb + 1) * 512)
                p_t, p_b = pmm.nxt()
                for c, (k0, ksz) in enumerate(QCH):
                    S.op("pe", lambda e, p_t=p_t, c=c, ksz=ksz, tsl=tsl: e.matmul(p_t[:, :], lhsT=wq_bf[:ksz, c, 0:128], rhs=cqn[:ksz, c, tsl], start=(c == 0), stop=(c == 3)),
                         reads=[wq_bb, cqn_b], writes=[p_b])
                S.op("act", lambda e, p_t=p_t, tsl=tsl: e.copy(out=qn[:, tsl], in_=p_t[:, :]), reads=[p_b], writes=[qn_b])
                p1, p1b = pmm.nxt()
                for c, (k0, ksz) in enumerate(QCH):
                    S.op("pe", lambda e, p1=p1, c=c, ksz=ksz, tsl=tsl: e.matmul(p1[0:64, :], lhsT=wq_bf[:ksz, c, 128:192], rhs=cqn[:ksz, c, tsl], start=(c == 0), stop=(c == 3)),
                         reads=[wq_bb, cqn_b], writes=[p1b])
                p2, p2b = pmm.nxt()
                for c, (k0, ksz) in enumerate(QCH):
                    S.op("pe", lambda e, p2=p2, c=c, ksz=ksz, tsl=tsl: e.matmul(p2[0:64, :], lhsT=wq_bf[:ksz, c, 192:256], rhs=cqn[:ksz, c, tsl], start=(c == 0), stop=(c == 3)),
                         reads=[wq_bb, cqn_b], writes=[p2b])
                qa_t, qa_b = qa.nxt()
                qr_t, qr_b = qr.nxt()
                S.op("act", lambda e, qa_t=qa_t, p1=p1: e.copy(out=qa_t[:, :], in_=p1[0:64, :]), reads=[p1b], writes=[qa_b])
                S.op("act", lambda e, qr_t=qr_t, p2=p2: e.copy(out=qr_t[:, :], in_=p2[0:64, :]), reads=[p2b], writes=[qr_b])
                rope(qa_t[:, :], qa_b, qr_t[:, :], qr_b, tb, qp[0:64, tsl], qp_b)
                p3, p3b = pmm.nxt()
                for c, (k0, ksz) in enumerate(KCH):
                    S.op("pe", lambda e, p3=p3, c=c, ksz=ksz, tsl=tsl: e.matmul(p3[:, :], lhsT=wk_bf[:ksz, c, 0:128], rhs=ckvn[:ksz, c, tsl], start=(c == 0), stop=(c == 1)),
                         reads=[wk_bb, ckvn_b], writes=[p3b])
                S.op("act", lambda e, p3=p3, tsl=tsl: e.copy(out=kn[:, tsl], in_=p3[:, :]), reads=[p3b], writes=[kn_b])
                p4, p4b = pmm.nxt()
                for q4 in range(4):
                    tt = tb * 4 + q4
                    for c, (k0, ksz) in enumerate(KCH):
                        S.op("pe", lambda e, p4=p4, c=c, ksz=ksz, tt=tt, q4=q4: e.matmul(p4[:, q4 * 128:(q4 + 1) * 128], lhsT=ckvn[:ksz, c, tt * 128:(tt + 1) * 128], rhs=wk_bf[:ksz, c, 128:256], start=(c == 0), stop=(c == 1)),
                             reads=[wk_bb, ckvn_b], writes=[p4b])
                S.op("dve", lambda e, p4=p4, tb=tb: e.tensor_copy(out=vv[:, tb * 4:(tb + 1) * 4, :], in_=p4[:, :].rearrange("p (a b) -> p a b", b=128)),
                     reads=[p4b], writes=[vv_b])
            SK = 2
            fr = {}
            acc = {}

            def front(it):
                qb, kc = divmod(it, 32)
                qsl = slice(qb * 512, (qb + 1) * 512)
                ksl = slice(kc * 128, (kc + 1) * 128)
                s_t, s_b = pmm.nxt()
                S.op("pe", lambda e: e.matmul(s_t[:, :], lhsT=kn[:, ksl], rhs=qn[:, qsl], start=True, stop=False), reads=[kn_b, qn_b], writes=[s_b])
                S.op("pe", lambda e: e.matmul(s_t[:, :], lhsT=kpe[:, ksl], rhs=qp[:, qsl], start=False, stop=True), reads=[kpe_b, qp_b], writes=[s_b])
                e_t, e_b = Et.nxt()
                S.op("act", lambda e: e.activation(out=e_t[:, :], in_=s_t[:, :], func=AF.Exp, scale=scale), reads=[s_b], writes=[e_b])
                fr[it] = (e_t, e_b)

            def back(it, h=h):
                qb, kc = divmod(it, 32)
                qsl = slice(qb * 512, (qb + 1) * 512)
                e_t, e_b = fr.pop(it)
                if kc == 0:
                    acc["o"] = pO.nxt()
                    acc["a0"] = accA.nxt()
                    acc["a1"] = accB.nxt()
                o_t, o_b = acc["o"]
                S.op("pe", lambda e: e.matmul(o_t[:, :], lhsT=vv[:, kc, :], rhs=e_t[:, :], start=(kc == 0), stop=(kc == 31)), reads=[vv_b, e_b], writes=[o_b])
                if kc == 0:
                    acc["d"] = pD.nxt()
                d_t, d_b = acc["d"]
                S.op("pe", lambda e: e.matmul(d_t[:, :], lhsT=onesb[:, :], rhs=e_t[:, :], start=(kc == 0), stop=(kc == 31)), reads=[ob_b, e_b], writes=[d_b])
                if kc == 31:
                    rd_t, rd_b = rden.nxt()
                    S.op("dve", lambda e: e.reciprocal(out=rd_t[:, :], in_=d_t[:, :]), reads=[d_b], writes=[rd_b])
                    y_t, y_b = yst.nxt()
                    S.op("dve", lambda e: e.tensor_tensor(out=y_t[:, :], in0=o_t[:, :], in1=rd_t[:, :], op=ALU.mult), reads=[o_b, rd_b], writes=[y_b])
                    S.op("sp", lambda e: e.dma_start(out=ymlaT[h * 128:(h + 1) * 128, qsl], in_=y_t[:, :]), reads=[y_b], writes=[C.buf("ymlaT")], dma=True)

            NIT = (T // 512) * 32
            for it in range(NIT + SK):
                if it < NIT:
                    front(it)
                if it - SK >= 0:
                    back(it - SK)
        C.end_stage()


def stage_na(C, sc, rpbT, maskc, ynaT, wprep=None):
    nc, S = C.nc, C.S
    qkT, vna = sc["qkT"], sc["vna"]
    qk_b, v_b = C.buf("qkT"), C.buf("vna")
    scale = 64.0 ** -0.5
    with contextlib.ExitStack() as st:
        ones = st.enter_context(nc.sbuf_tensor(U("ones"), [128, 64], BF16))
        ones_b = Buf("ones")
        S.op("dve", lambda e: e.memset(ones[:], 1.0), writes=[ones_b])
        mk = st.enter_context(nc.sbuf_tensor(U("mk"), [128, 14 * 64], F32))
        mk_b = Buf("mk")
        S.op("sp", lambda e: e.dma_start(out=mk[:, :], in_=maskc.rearrange("j d q -> j (d q)")), writes=[mk_b], dma=True)
        khp = Rot(nc, st, "kh", 2, [128, T], BF16)
        qhp = Rot(nc, st, "qh", 2, [128, T], BF16)
        vhp = Rot(nc, st, "vh", 2, [128, 63, 128], BF16)
        btp = Rot(nc, st, "bt", 2, [128, 14 * 64], F32)
        btq = Rot(nc, st, "btq", 2, [128, 512], F32)
        tmp = Rot(nc, st, "tmp", 4, [128, 512], F32)
        Ep = Rot(nc, st, "E", 6, [128, 512], BF16)
        rdp = Rot(nc, st, "rd", 2, [128, 512], F32)
        yp = Rot(nc, st, "y", 2, [64, 512], BF16)
        ps = Rot(nc, st, "ps", 5, [128, 512], F32, psum=True)
        po = Rot(nc, st, "po", 3, [128, 512], F32, psum=True)
        SK = 3
        state = {}
        if wprep is not None:
            w_branch_, w_o_, wbr_c, wo_c = wprep
            pw_st = Rot(nc, st, "pwst", 2, [128, 24, 128], F32)
            pw_bf = Rot(nc, st, "pwbf", 2, [128, 24, 128], BF16)
            po_st = Rot(nc, st, "post", 2, [128, 16, 128], F32)
            po_bf = Rot(nc, st, "pobf", 2, [128, 16, 128], BF16)

        def wprep_step(dc):
            ws_t, ws_b = pw_st.nxt()
            for b in range(3):
                S.op("sp", lambda e, b=b: e.dma_start(out=ws_t[:, b * 8:(b + 1) * 8, :], in_=w_branch_[b, :, dc * 128:(dc + 1) * 128].rearrange("(kc p) n -> p kc n", p=128)),
                     writes=[ws_b], dma=True)
            wb_t, wb_b = pw_bf.nxt()
            S.op("act", lambda e: e.copy(out=wb_t[:, :, :], in_=ws_t[:, :, :]), reads=[ws_b], writes=[wb_b])
            S.op("sp", lambda e: e.dma_start(out=wbr_c[dc], in_=wb_t[:, :, :].rearrange("p a b -> p (a b)")), reads=[wb_b], writes=[C.buf("wbr_c")], dma=True)
            os_t, os_b = po_st.nxt()
            S.op("sp", lambda e: e.dma_start(out=os_t[:, :, :], in_=w_o_[:, dc * 128:(dc + 1) * 128].rearrange("(kc p) n -> p kc n", p=128)), writes=[os_b], dma=True)
            ob_t, ob_b = po_bf.nxt()
            S.op("act", lambda e: e.copy(out=ob_t[:, :, :], in_=os_t[:, :, :]), reads=[os_b], writes=[ob_b])
            S.op("sp", lambda e: e.dma_start(out=wo_c[dc], in_=ob_t[:, :, :].rearrange("p a b -> p (a b)")), reads=[ob_b], writes=[C.buf("wo_c")], dma=True)
        for (kh_, khb_) in khp.slots:
            S.op("pool", lambda e, kh_=kh_: e.memset(kh_[64:128, :], 0.0), writes=[khb_])
        for (qh_, qhb_) in qhp.slots:
            S.op("pool", lambda e, qh_=qh_: e.memset(qh_[64:128, :], 0.0), writes=[qhb_])
        for (vh_, vhb_) in vhp.slots:
            S.op("pool", lambda e, vh_=vh_: e.memset(vh_[:, :, 64:128], 1.0), writes=[vhb_])

        def head_load(h):
            kh, kh_b = khp.nxt()
            qh, qh_b = qhp.nxt()
            vh, vh_b = vhp.nxt()
            bt, bt_b = btp.nxt()
            S.op("sp", lambda e: e.dma_start(out=qh[0:64, :], in_=qkT[h * 64:(h + 1) * 64, :]), reads=[qk_b], writes=[qh_b], dma=True)
            S.op("sp", lambda e: e.dma_start(out=kh[0:64, :], in_=qkT[1024 + h * 64:1024 + (h + 1) * 64, :]), reads=[qk_b], writes=[kh_b], dma=True)
            for g in range(2):
                S.op("sp", lambda e, g=g: e.dma_start(out=vh[:, g * 16:(g + 1) * 16, 0:64], in_=vna[g * 2048:(g + 1) * 2048, h * 64:(h + 1) * 64].rearrange("(t p) c -> p t c", p=128)),
                     reads=[v_b], writes=[vh_b], dma=True)
            S.op("sp", lambda e: e.dma_start(out=vh[:, 32:48, 0:64], in_=vna[64:64 + 2048, h * 64:(h + 1) * 64].rearrange("(t p) c -> p t c", p=128)),
                 reads=[v_b], writes=[vh_b], dma=True)
            S.op("sp", lambda e: e.dma_start(out=vh[:, 48:63, 0:64], in_=vna[64 + 2048:64 + 2048 + 15 * 128, h * 64:(h + 1) * 64].rearrange("(t p) c -> p t c", p=128)),
                 reads=[v_b], writes=[vh_b], dma=True)
            S.op("sp", lambda e: e.dma_start(out=bt[:, :], in_=rpbT[h].rearrange("j d q -> j (d q)")), writes=[bt_b], dma=True)
            S.op("pool", lambda e: e.tensor_tensor(out=bt[:, :], in0=bt[:, :], in1=mk[:, :], op=ALU.add), reads=[mk_b], writes=[bt_b])
            bq, bq_b = btq.nxt()
            for a_ in range(2):
                S.op("pool", lambda e, a_=a_: e.tensor_copy(out=bq[:, a_ * 256:(a_ + 1) * 256].rearrange("p (c q) -> p c q", q=64), in_=bt[:, :].rearrange("p (d q) -> p d q", q=64)[:, 3:10:2, :]),
                     reads=[bt_b], writes=[bq_b])
            state[h] = (kh, kh_b, qh, qh_b, vh, vh_b, bt, bt_b, bq, bq_b)
            if wprep is not None:
                wprep_step(h)

        fr = {}
        units = []
        for h_ in range(16):
            for r_ in (0, 1, 2, 3):
                units.append((h_, [r_]))
            for r_ in range(4, 60, 2):
                units.append((h_, [r_, r_ + 1]))
            for r_ in (60, 61, 62, 63):
                units.append((h_, [r_]))

        def front(it):
            h, rows = units[it]
            if rows[0] == 0:
                head_load(h)
            kh, kh_b, qh, qh_b, vh, vh_b, bt, bt_b, bq, bq_b = state[h]
            s_t, s_b = ps.nxt()
            starts = []
            for ri, r in enumerate(rows):
                start = min(max(r - 4, 0), 56)
                starts.append(start)
                for c in range(4):
                    S.op("pe", lambda e, c=c, ri=ri, r=r, start=start: e.matmul(s_t[:, (ri * 4 + c) * 64:(ri * 4 + c + 1) * 64], lhsT=kh[:, (start + 2 * c) * 64:(start + 2 * c + 2) * 64], rhs=qh[:, r * 64:(r + 1) * 64], start=True, stop=True),
                         reads=[kh_b, qh_b], writes=[s_b])
            t_t, t_b = tmp.nxt()
            e_t, e_b = Ep.nxt()
            if len(rows) == 2:
                S.op("dve", lambda e: e.scalar_tensor_tensor(out=t_t[:, :], in0=s_t[:, :], scalar=scale, in1=bq[:, :], op0=ALU.mult, op1=ALU.add),
                     reads=[s_b, bq_b], writes=[t_b])
                S.op("act", lambda e: e.activation(out=e_t[:, :], in_=t_t[:, :], func=AF.Exp), reads=[t_b], writes=[e_b])
            else:
                base = starts[0] - rows[0] + 7
                S.op("dve", lambda e: e.scalar_tensor_tensor(out=t_t[:, 0:256].rearrange("p (c q) -> p c q", q=64), in0=s_t[:, 0:256].rearrange("p (c q) -> p c q", q=64), scalar=scale,
                                                            in1=bt[:, :].rearrange("p (d q) -> p d q", q=64)[:, base:base + 7:2, :], op0=ALU.mult, op1=ALU.add),
                     reads=[s_b, bt_b], writes=[t_b])
                S.op("act", lambda e: e.activation(out=e_t[:, 0:256], in_=t_t[:, 0:256], func=AF.Exp), reads=[t_b], writes=[e_b])
            fr[it] = (e_t, e_b, starts)

        acc = {}

        def back(it):
            h, rows = units[it]
            kh, kh_b, qh, qh_b, vh, vh_b, bt, bt_b, bq, bq_b = state[h]
            e_t, e_b, starts = fr.pop(it)
            for ri, r in enumerate(rows):
                back_row(h, r, ri, starts[ri], e_t, e_b, vh, vh_b)

        def back_row(h, r, ri, start, e_t, e_b, vh, vh_b):
            rr = r % 8
            r8 = r // 8
            if rr == 0:
                acc["o"] = po.nxt()
            o_t, o_b = acc["o"]
            for c in range(4):
                g = start + 2 * c
                vi = g // 2 if g % 2 == 0 else 32 + (g - 1) // 2
                S.op("pe", lambda e, c=c, vi=vi: e.matmul(o_t[:, rr * 64:(rr + 1) * 64], lhsT=vh[:, vi, :], rhs=e_t[:, (ri * 4 + c) * 64:(ri * 4 + c + 1) * 64], start=(c == 0), stop=(c == 3)),
                     reads=[vh_b, e_b], writes=[o_b])
            if rr == 7:
                rd_t, rd_b = rdp.nxt()
                S.op("dve", lambda e: e.reciprocal(out=rd_t[64:128, :], in_=o_t[64:128, :]), reads=[o_b], writes=[rd_b])
                y_t, y_b = yp.nxt()
                S.op("dve", lambda e: e.tensor_tensor(out=y_t[:, :], in0=o_t[0:64, :], in1=rd_t[64:128, :], op=ALU.mult), reads=[o_b, rd_b], writes=[y_b])
                S.op("sp", lambda e: e.dma_start(out=ynaT[h * 64:(h + 1) * 64, r8 * 512:(r8 + 1) * 512], in_=y_t[:, :]), reads=[y_b], writes=[C.buf("ynaT")], dma=True)

        NIT = len(units)
        for it in range(NIT + SK):
            if it < NIT:
                front(it)
            if it - SK >= 0:
                back(it - SK)
        C.end_stage()


def na_host_tables(rpb):
    cols = np.arange(64)
    col_start = np.clip(cols - 8, 0, 64 - 16)
    valid = (cols[None, :] >= col_start[:, None]) & (cols[None, :] < col_start[:, None] + 16)
    col_idx = np.clip(cols[None, :] - cols[:, None] + 15, 0, 30)
    t = rpb[:, :, col_idx]
    t = np.transpose(t, (0, 3, 1, 2))
    rpbT = np.ascontiguousarray(np.concatenate([t[:, :, 0:14, :], t[:, :, 1:15, :]], axis=1)).astype(np.float32)
    m = np.where(valid.T, 0.0, -30000.0).astype(np.float32)
    m2 = np.concatenate([m, m], axis=0)
    maskc = np.ascontiguousarray(np.broadcast_to(m2[:, None, :], (128, 14, 64))).astype(np.float32)
    return rpbT, maskc


def stage_fnet(C, sc, ccs, csT, ssT, yfT):
    nc, S = C.nc, C.S
    ufT = sc["ufT"]
    uf_b = C.buf("ufT")
    SB = 256
    HN = T // 2
    NT2 = HN // 128
    with contextlib.ExitStack() as st:
        ccf = st.enter_context(nc.sbuf_tensor(U("ccf"), [128, 2, 2, 256], F32))
        ccb = st.enter_context(nc.sbuf_tensor(U("ccb"), [128, 2, 2, 256], BF16))
        ccf_b, ccb_b = Buf("ccf"), Buf("ccb")
        for m in range(2):
            S.op("sp", lambda e, m=m: e.dma_start(out=ccf[:, m, :, :], in_=ccs[m].rearrange("(kc p) n -> p kc n", p=128)), writes=[ccf_b], dma=True)
        S.op("dve", lambda e: e.tensor_copy(out=ccb[:], in_=ccf[:]), reads=[ccf_b], writes=[ccb_b])
        ugp = Rot(nc, st, "ug", 2, [128, 2, T], BF16)
        upm = st.enter_context(nc.sbuf_tensor(U("upm"), [128, 2, 2, HN], BF16))
        upm_b = Buf("upm")
        gcs = st.enter_context(nc.sbuf_tensor(U("gcs"), [128, 2, NT2, 256], BF16))
        gcs_b = Buf("gcs")
        e2 = st.enter_context(nc.sbuf_tensor(U("e2"), [128, 256], BF16))
        e2_b = Buf("e2")
        S.op("pool", lambda e: e.memset(e2[:, :], 0.0), writes=[e2_b])
        csp = Rot(nc, st, "csb", 3, [128, NT2, SB], BF16)
        ssp = Rot(nc, st, "ssb", 3, [128, NT2, SB], BF16)
        cxp = Rot(nc, st, "cxb", 3, [128, SB], BF16)
        yst = Rot(nc, st, "yst", 2, [128, SB], BF16)
        pa = Rot(nc, st, "pa", 3, [128, 512], F32, psum=True)
        pb = Rot(nc, st, "pb", 4, [128, 512], F32, psum=True)
        px = Rot(nc, st, "px", 1, [128, 512], F32, psum=True)
        for g in range(4):
            ug, ug_b = ugp.nxt()
            S.op("sp", lambda e, g=g, ug=ug: e.dma_start(out=ug[:, :, :], in_=ufT[g * 256:(g + 1) * 256, :].rearrange("(kc p) t -> p kc t", p=128)), reads=[uf_b], writes=[ug_b], dma=True)
            for kc in range(2):
                S.op("dve", lambda e, kc=kc, ug=ug: e.tensor_tensor(out=upm[:, 0, kc, 1:HN], in0=ug[:, kc, 1:HN], in1=ug[:, kc, T - 1:HN:-1], op=ALU.add), reads=[ug_b], writes=[upm_b])
                S.op("pool", lambda e, kc=kc, ug=ug: e.tensor_tensor(out=upm[:, 1, kc, 1:HN], in0=ug[:, kc, 1:HN], in1=ug[:, kc, T - 1:HN:-1], op=ALU.subtract), reads=[ug_b], writes=[upm_b])
                S.op("dve", lambda e, kc=kc, ug=ug: e.tensor_copy(out=upm[:, 0, kc, 0:1], in_=ug[:, kc, 0:1]), reads=[ug_b], writes=[upm_b])
                S.op("pool", lambda e, kc=kc: e.memset(upm[:, 1, kc, 0:1], 0.0), writes=[upm_b])
            x_t, x_b = px.nxt()
            for kc in range(2):
                S.op("pe", lambda e, kc=kc, ug=ug: e.matmul(x_t[0:1, 0:256], lhsT=ug[:, kc, HN:HN + 1], rhs=ccb[:, 0, kc, :], start=(kc == 0), stop=(kc == 1)),
                     reads=[ug_b, ccb_b], writes=[x_b])
            S.op("act", lambda e: e.copy(out=e2[0:1, :], in_=x_t[0:1, 0:256]), reads=[x_b], writes=[e2_b])
            for tt in range(NT2):
                p_t, p_b = pa.nxt()
                for m in range(2):
                    for kc in range(2):
                        S.op("pe", lambda e, p_t=p_t, m=m, kc=kc, tt=tt: e.matmul(p_t[:, m * 256:(m + 1) * 256], lhsT=upm[:, m, kc, tt * 128:(tt + 1) * 128], rhs=ccb[:, m, kc, :], start=(kc == 0), stop=(kc == 1)),
                             reads=[upm_b, ccb_b], writes=[p_b])
                S.op("act", lambda e, p_t=p_t, tt=tt: e.copy(out=gcs[:, :, tt, :], in_=p_t[:, :].rearrange("p (m c) -> p m c", m=2)), reads=[p_b], writes=[gcs_b])
            blk = {}

            def prep(sb):
                cs_t, cs_b = csp.nxt()
                ss_t, ss_b = ssp.nxt()
                cx_t, cx_b = cxp.nxt()
                S.op("sp", lambda e: e.dma_start(out=cs_t[:, :, :], in_=csT[0:HN, sb * SB:(sb + 1) * SB].rearrange("(tt p) n -> p tt n", p=128)), writes=[cs_b], dma=True)
                S.op("sp", lambda e: e.dma_start(out=ss_t[:, :, :], in_=ssT[0:HN, sb * SB:(sb + 1) * SB].rearrange("(tt p) n -> p tt n", p=128)), writes=[ss_b], dma=True)
                S.op("sp", lambda e: e.dma_start(out=cx_t[:, :], in_=csT[HN:HN + 128, sb * SB:(sb + 1) * SB]), writes=[cx_b], dma=True)
                blk[sb] = (cs_t, cs_b, ss_t, ss_b, cx_t, cx_b)

            def compute(sb, g=g):
                cs_t, cs_b, ss_t, ss_b, cx_t, cx_b = blk.pop(sb)
                for half in range(2):
                    p_t, p_b = pb.nxt()
                    for tt in range(NT2):
                        S.op("pe", lambda e, p_t=p_t, tt=tt, half=half: e.matmul(p_t[:, :SB], lhsT=gcs[:, 0, tt, half * 128:(half + 1) * 128], rhs=cs_t[:, tt, :], start=(tt == 0), stop=False),
                             reads=[gcs_b, cs_b], writes=[p_b])
                        S.op("pe", lambda e, p_t=p_t, tt=tt, half=half: e.matmul(p_t[:, :SB], lhsT=gcs[:, 1, tt, half * 128:(half + 1) * 128], rhs=ss_t[:, tt, :], start=False, stop=False),
                             reads=[gcs_b, ss_b], writes=[p_b])
                    S.op("pe", lambda e, p_t=p_t, half=half: e.matmul(p_t[:, :SB], lhsT=e2[:, half * 128:(half + 1) * 128], rhs=cx_t[:, :], start=False, stop=True),
                         reads=[e2_b, cx_b], writes=[p_b])
                    y_t, y_b = yst.nxt()
                    S.op("dve", lambda e, y_t=y_t, p_t=p_t: e.tensor_copy(out=y_t[:, :], in_=p_t[:, :SB]), reads=[p_b], writes=[y_b])
                    S.op("sp", lambda e, y_t=y_t, half=half: e.dma_start(out=yfT[g * 256 + half * 128: g * 256 + (half + 1) * 128, sb * SB:(sb + 1) * SB], in_=y_t[:, :]),
                         reads=[y_b], writes=[C.buf("yfT")], dma=True)

            pipeline(T // SB, prep, compute, depth=2)
        C.end_stage()


def fnet_host_consts():
    import ml_dtypes
    n = np.arange(256, dtype=np.float64)
    a = 2 * np.pi * np.outer(n, n) / 256.0
    ccs = np.stack([np.cos(a) / 16.0, -np.sin(a) / 16.0]).astype(np.float32)
    s = np.arange(T, dtype=np.int64)
    ph = (np.outer(s, s) % T).astype(np.float64) * (2 * np.pi / T)
    csT = (np.cos(ph) / 64.0).astype(np.float32).astype(ml_dtypes.bfloat16)
    ssT = (np.sin(ph) / 64.0).astype(np.float32).astype(ml_dtypes.bfloat16)
    return ccs, csT, ssT


def stage_merge(C, sc, w_branch, mergedT, wbr_c=None):
    nc, S = C.nc, C.S
    TQ = 1024
    ysrc = [(sc["ynaT"], C.buf("ynaT")), (sc["ymlaT"], C.buf("ymlaT")), (sc["yfT"], C.buf("yfT"))]
    gT, gT_b = sc["gT"], C.buf("gT")
    with contextlib.ExitStack() as st:
        yb = st.enter_context(nc.sbuf_tensor(U("yb"), [128, 24, TQ], BF16))
        yb_b = Buf("yb")
        wst = Rot(nc, st, "wst", 3, [128, 24, 128], F32)
        wbf = Rot(nc, st, "wbf", 3, [128, 24, 128], BF16)
        gtp = Rot(nc, st, "gt", 6, [128, 3, 512], BF16)
        m0p = Rot(nc, st, "m0", 2, [128, 512], F32)
        m1p = Rot(nc, st, "m1", 2, [128, 512], F32)
        m2p = Rot(nc, st, "m2", 2, [128, 512], F32)
        mo = Rot(nc, st, "mo", 2, [128, TQ], BF16)
        pp = Rot(nc, st, "pp", 6, [128, 512], F32, psum=True)
        jobs = [(tq, dc) for tq in range(T // TQ) for dc in range(16)]
        wjob = {}

        def prep(i):
            tq, dc = jobs[i]
            wb_t, wb_b = wbf.nxt()
            if wbr_c is not None:
                S.op("sp", lambda e: e.dma_start(out=wb_t[:, :, :].rearrange("p a b -> p (a b)"), in_=wbr_c[dc]), reads=[C.buf("wbr_c")], writes=[wb_b], dma=True)
            else:
                ws_t, ws_b = wst.nxt()
                for b in range(3):
                    S.op("sp", lambda e, b=b: e.dma_start(out=ws_t[:, b * 8:(b + 1) * 8, :], in_=w_branch[b, :, dc * 128:(dc + 1) * 128].rearrange("(kc p) n -> p kc n", p=128)),
                         writes=[ws_b], dma=True)
                cast_op(C, wb_t[:, :, :], ws_t[:, :, :], [ws_b], [wb_b])
            gts = []
            for tb in range(TQ // 512):
                g_t, g_b = gtp.nxt()
                t0 = tq * TQ
                S.op("sp", lambda e, g_t=g_t, tb=tb, t0=t0: e.dma_start(
                    out=g_t[:, :, :], in_=gT[:, t0 + tb * 512:t0 + (tb + 1) * 512].rearrange("(b c p) t -> c p b t", b=3, p=128)[dc]),
                    reads=[gT_b], writes=[g_b], dma=True)
                gts.append((g_t, g_b))
            wjob[i] = (wb_t, wb_b, gts)

        def compute(i):
            tq, dc = jobs[i]
            t0 = tq * TQ
            if dc == 0:
                for b in range(3):
                    S.op("sp", lambda e, b=b: e.dma_start(out=yb[:, b * 8:(b + 1) * 8, :], in_=ysrc[b][0][:, t0:t0 + TQ].rearrange("(kc p) t -> p kc t", p=128)),
                         reads=[ysrc[b][1]], writes=[yb_b], dma=True)
            wb_t, wb_b, gts = wjob.pop(i)
            o_t, o_b = mo.nxt()
            for tb in range(TQ // 512):
                g_t, g_b = gts[tb]
                ps = []
                for b in range(3):
                    p_t, p_b = pp.nxt()
                    for kc in range(8):
                        S.op("pe", lambda e, p_t=p_t, b=b, kc=kc, tb=tb: e.matmul(p_t[:, :], lhsT=wb_t[:, b * 8 + kc, :], rhs=yb[:, b * 8 + kc, tb * 512:(tb + 1) * 512], start=(kc == 0), stop=(kc == 7)),
                             reads=[wb_b, yb_b], writes=[p_b])
                    ps.append((p_t, p_b))
                ms = []
                for b, mp in enumerate((m0p, m1p, m2p)):
                    m_t, m_b = mp.nxt()
                    S.op("dve", lambda e, m_t=m_t, b=b, g_t=g_t, p_t=ps[b][0]: e.tensor_tensor(out=m_t[:, :], in0=p_t[:, :], in1=g_t[:, b, :], op=ALU.mult),
                         reads=[ps[b][1], g_b], writes=[m_b])
                    ms.append((m_t, m_b))
                S.op("pool", lambda e, a=ms[0][0], b_=ms[1][0]: e.tensor_tensor(out=a[:, :], in0=a[:, :], in1=b_[:, :], op=ALU.add),
                     reads=[ms[1][1]], writes=[ms[0][1]])
                S.op("pool", lambda e, a=ms[0][0], c_=ms[2][0], tb=tb: e.tensor_tensor(out=o_t[:, tb * 512:(tb + 1) * 512], in0=a[:, :], in1=c_[:, :], op=ALU.add),
                     reads=[ms[0][1], ms[2][1]], writes=[o_b])
            S.op("sp", lambda e: e.dma_start(out=mergedT[dc * 128:(dc + 1) * 128, t0:t0 + TQ], in_=o_t[:, :]), reads=[o_b], writes=[C.buf("mergedT")], dma=True)

        pipeline(len(jobs), prep, compute, depth=2)
        C.end_stage()


def stage_outproj(C, mergedT, w_o, h_in, h_in_b, h_out, h_out_b, wo_c=None):
    nc, S = C.nc, C.S
    TQ = 1024
    mg_b = C.buf("mergedT")
    with contextlib.ExitStack() as st:
        mt = st.enter_context(nc.sbuf_tensor(U("mt"), [128, 16, TQ], BF16))
        mt_b = Buf("mt")
        hacc = st.enter_context(nc.sbuf_tensor(U("hacc"), [128, 8, D], F32))
        hacc_b = [Buf("hacc%d" % i) for i in range(8)]
        wst = Rot(nc, st, "wst", 3, [128, 16, 128], F32)
        wow = Rot(nc, st, "wow", 2, [128, 16, 512], BF16)
        wo_cur = {}
        pp = Rot(nc, st, "pp", 4, [128, 512], F32, psum=True)
        jobs = [(tq, cc) for tq in range(T // TQ) for cc in range(16)]
        wjob = {}

        def prep(i):
            tq, cc = jobs[i]
            q = cc % 4
            if q == 0:
                wo_cur["t"] = wow.nxt()
            wb_t, wb_b = wo_cur["t"]
            if wo_c is not None:
                S.op("sp", lambda e: e.dma_start(out=wb_t[:, :, q * 128:(q + 1) * 128], in_=wo_c[cc].rearrange("p (a b) -> p a b", b=128)), reads=[C.buf("wo_c")], writes=[wb_b], dma=True)
            else:
                ws_t, ws_b = wst.nxt()
                S.op("sp", lambda e: e.dma_start(out=ws_t[:, :, :], in_=w_o[:, cc * 128:(cc + 1) * 128].rearrange("(kc p) n -> p kc n", p=128)), writes=[ws_b], dma=True)
                cast_op(C, wb_t[:, :, q * 128:(q + 1) * 128], ws_t[:, :, :], [ws_b], [wb_b])
            wjob[i] = (wb_t, wb_b)

        def compute(i):
            tq, cc = jobs[i]
            t0 = tq * TQ
            if cc == 0:
                S.op("sp", lambda e: e.dma_start(out=mt[:, :, :], in_=mergedT[:, t0:t0 + TQ].rearrange("(kc p) t -> p kc t", p=128)), reads=[mg_b], writes=[mt_b], dma=True)
                for tt in range(8):
                    S.op("sp", lambda e, tt=tt: e.dma_start(out=hacc[:, tt, :], in_=h_in[t0 + tt * 128:t0 + (tt + 1) * 128, :]), reads=[h_in_b], writes=[hacc_b[tt]], dma=True)
            wb_t, wb_b = wjob.pop(i)
            if cc % 4 == 3:
                c4 = cc // 4
                for tt in range(8):
                    p_t, p_b = pp.nxt()
                    for kc in range(16):
                        S.op("pe", lambda e, p_t=p_t, kc=kc, tt=tt: e.matmul(p_t[:, :], lhsT=mt[:, kc, tt * 128:(tt + 1) * 128], rhs=wb_t[:, kc, :], start=(kc == 0), stop=(kc == 15)),
                             reads=[wb_b, mt_b], writes=[p_b])
                    S.op("dve", lambda e, p_t=p_t, tt=tt: e.tensor_tensor(out=hacc[:, tt, c4 * 512:(c4 + 1) * 512], in0=hacc[:, tt, c4 * 512:(c4 + 1) * 512], in1=p_t[:, :], op=ALU.add),
                         reads=[p_b], writes=[hacc_b[tt]])
            if cc == 15:
                for tt in range(8):
                    S.op("sp", lambda e, tt=tt: e.dma_start(out=h_out[t0 + tt * 128:t0 + (tt + 1) * 128, :], in_=hacc[:, tt, :]), reads=[hacc_b[tt]], writes=[h_out_b], dma=True)

        pipeline(len(jobs), prep, compute, depth=2)
        C.end_stage()


def stage_route(C, h_ap, h_b, gain_ap, w_router, ident_f, xn2, idx_d, gate_d, NE=16, CAP=512):
    nc, S = C.nc, C.S
    with contextlib.ExitStack() as st:
        idf, idfb, idb, idbb = load_consts(C, st, ident_f)
        pools = norm_pools(C, st, nh=3)
        gain_t, gain_b = load_gain(C, st, "gain2", gain_ap, D)
        wr = st.enter_context(nc.sbuf_tensor(U("wr"), [128, 16, NE], F32))
        wr_b = Buf("wr")
        S.op("sp", lambda e: e.dma_start(out=wr[:, :, :], in_=w_router.rearrange("(kc p) n -> p kc n", p=128)), writes=[wr_b], dma=True)
        xf = Rot(nc, st, "xf", 3, [128, D], F32)
        xb = Rot(nc, st, "xb", 3, [128, D], BF16)
        xT = Rot(nc, st, "xT", 3, [128, 16, 128], F32)
        sm = Rot(nc, st, "sm", 4, [128, 8], F32)
        ex = Rot(nc, st, "ex", 4, [128, NE], F32)
        af = Rot(nc, st, "af", 4, [128, NE], F32)
        affT = st.enter_context(nc.sbuf_tensor(U("affT"), [NE, T], F32))
        affT2 = st.enter_context(nc.sbuf_tensor(U("affT2"), [NE, T], F32))
        affT_b, affT2_b = Buf("affT"), Buf("affT2")
        vals = st.enter_context(nc.sbuf_tensor(U("vals"), [NE, CAP], F32))
        idx = st.enter_context(nc.sbuf_tensor(U("idx"), [NE, CAP], U32))
        vals_b, idx_b = Buf("vals"), Buf("idx")
        ptr = Rot(nc, st, "ptr", 3, [128, 4, 128], F32, psum=True)
        pl = Rot(nc, st, "pl", 3, [128, 512], F32, psum=True)
        pt2 = Rot(nc, st, "pt2", 2, [128, 512], F32, psum=True)
        lg = {}

        def front(tt):
            x_t, x_b = xf.nxt()
            rms_tile(C, pools, h_ap[tt * 128:(tt + 1) * 128, :], gain_t, gain_b, x_t[:], x_b, D, [h_b])
            xb_t, xb_b = xb.nxt()
            S.op("act", lambda e: e.copy(out=xb_t[:, :], in_=x_t[:, :]), reads=[x_b], writes=[xb_b])
            S.op("sp", lambda e: e.dma_start(out=xn2[tt * 128:(tt + 1) * 128, :], in_=xb_t[:, :]), reads=[xb_b], writes=[C.buf("xn2")], dma=True)
            xT_t, xT_b = xT.nxt()
            for g in range(4):
                p_t, p_b = ptr.nxt()
                for j in range(4):
                    kc = g * 4 + j
                    S.op("pe", lambda e, p_t=p_t, j=j, kc=kc: e.transpose(out=p_t[:, j, :], in_=x_t[:, kc * 128:(kc + 1) * 128], identity=idf[:]),
                         reads=[x_b, idfb], writes=[p_b])
                S.op("dve", lambda e, p_t=p_t, g=g: e.tensor_copy(out=xT_t[:, g * 4:(g + 1) * 4, :], in_=p_t[:, :, :]), reads=[p_b], writes=[xT_b])
            l_t, l_b = pl.nxt()
            for kc in range(16):
                S.op("pe", lambda e, kc=kc: e.matmul(l_t[:, :NE], lhsT=xT_t[:, kc, :], rhs=wr[:, kc, :], start=(kc == 0), stop=(kc == 15)),
                     reads=[xT_b, wr_b], writes=[l_b])
            lg[tt] = (l_t, l_b)

        def back(tt):
            l_t, l_b = lg.pop(tt)
            s_t, s_b = sm.nxt()
            S.op("dve", lambda e: e.tensor_reduce(out=s_t[:, 0:1], in_=l_t[:, :NE], axis=AX.X, op=ALU.max, negate=True), reads=[l_b], writes=[s_b])
            e_t, e_b = ex.nxt()
            S.op("act", lambda e: e.activation(out=e_t[:, :], in_=l_t[:, :NE], func=AF.Exp, bias=s_t[:, 0:1], accum_out=s_t[:, 1:2]),
                 reads=[l_b, s_b], writes=[e_b, s_b])
            S.op("dve", lambda e: e.reciprocal(out=s_t[:, 2:3], in_=s_t[:, 1:2]), reads=[s_b], writes=[s_b])
            a_t, a_b = af.nxt()
            S.op("dve", lambda e: e.tensor_scalar(out=a_t[:, :], in0=e_t[:, :], scalar1=s_t[:, 2:3], scalar2=None, op0=ALU.mult), reads=[e_b, s_b], writes=[a_b])
            q_t, q_b = pt2.nxt()
            S.op("pe", lambda e: e.transpose(out=q_t[:NE, :128], in_=a_t[:, :], identity=idf[:]), reads=[a_b, idfb], writes=[q_b])
            S.op("dve", lambda e: e.tensor_copy(out=affT[:, tt * 128:(tt + 1) * 128], in_=q_t[:NE, :128]), reads=[q_b], writes=[affT_b])

        NT = T // 128
        SKR = 2
        for tt in range(NT + SKR):
            if tt < NT:
                front(tt)
            if tt - SKR >= 0:
                back(tt - SKR)
        cur, cur_b, oth, oth_b = affT, affT_b, affT2, affT2_b
        for r in range(CAP // 8):
            S.op("dve", lambda e, cur=cur, r=r: e.max(out=vals[:, r * 8:(r + 1) * 8], in_=cur[:, :]), reads=[cur_b], writes=[vals_b])
            S.op("dve", lambda e, cur=cur, r=r: e.max_index(out=idx[:, r * 8:(r + 1) * 8], in_max=vals[:, r * 8:(r + 1) * 8], in_values=cur[:, :]), reads=[cur_b, vals_b], writes=[idx_b])
            if r < CAP // 8 - 1:
                S.op("dve", lambda e, cur=cur, oth=oth, r=r: e.match_replace(out=oth[:, :], in_to_replace=vals[:, r * 8:(r + 1) * 8], in_values=cur[:, :], imm_value=-1.0),
                     reads=[cur_b, vals_b], writes=[oth_b])
                cur, cur_b, oth, oth_b = oth, oth_b, cur, cur_b
        S.op("sp", lambda e: e.dma_start(out=idx_d[:, :], in_=idx[:, :]), reads=[idx_b], writes=[C.buf("idx_d")], dma=True)
        S.op("sp", lambda e: e.dma_start(out=gate_d[:, :], in_=vals[:, :]), reads=[vals_b], writes=[C.buf("gate_d")], dma=True)
        C.end_stage()


def stage_experts(C, h_ap, h_b, xn2, idx_d, gate_d, w_g, w_u, w_d, ident_f, NE=16, CAP=512):
    nc, S = C.nc, C.S
    NJ = CAP // 128
    with contextlib.ExitStack() as st:
        idf, idfb, idb, idbb = load_consts(C, st, ident_f)
        idxc = st.enter_context(nc.sbuf_tensor(U("idxc"), [128, NE * NJ], U32))
        gatec = st.enter_context(nc.sbuf_tensor(U("gatec"), [128, NE * NJ], F32))
        idxc_b, gatec_b = Buf("idxc"), Buf("gatec")
        S.op("sp", lambda e: e.dma_start(out=idxc[:, :].rearrange("p (e j) -> p e j", j=NJ), in_=idx_d.rearrange("e (j p) -> p e j", p=128), allow_slow_non_contiguous=True),
             reads=[C.buf("idx_d")], writes=[idxc_b], dma=True)
        S.op("sp", lambda e: e.dma_start(out=gatec[:, :].rearrange("p (e j) -> p e j", j=NJ), in_=gate_d.rearrange("e (j p) -> p e j", p=128), allow_slow_non_contiguous=True),
             reads=[C.buf("gate_d")], writes=[gatec_b], dma=True)
        xg = Rot(nc, st, "xg", 4, [128, D], BF16)
        xeTp = Rot(nc, st, "xeT", 2, [128, 16, CAP], BF16)
        hT = st.enter_context(nc.sbuf_tensor(U("hT"), [128, 16, CAP], BF16))
        hT_b = Buf("hT")
        ye = st.enter_context(nc.sbuf_tensor(U("ye"), [128, NJ, D], F32))
        ye_b = [Buf("ye%d" % j) for j in range(NJ)]
        wst = Rot(nc, st, "wst", 3, [128, 16, 128], F32)
        wbf = Rot(nc, st, "wbf", 3, [128, 16, 128], BF16)
        sg = Rot(nc, st, "sg", 2, [128, CAP], F32)
        wdw = Rot(nc, st, "wdw", 2, [128, 16, 512], BF16)
        wd_cur = {}
        ptr = Rot(nc, st, "ptr", 2, [128, 8, 128], BF16, psum=True)
        pg = Rot(nc, st, "pg", 2, [128, 512], F32, psum=True)
        pu = Rot(nc, st, "pu", 2, [128, 512], F32, psum=True)
        pd = Rot(nc, st, "pd", 2, [128, 512], F32, psum=True)
        xn2_b = C.buf("xn2")

        def load_w(src):
            ws_t, ws_b = wst.nxt()
            S.op("sp", lambda e: e.dma_start(out=ws_t[:, :, :], in_=src.rearrange("(kc p) n -> p kc n", p=128)), writes=[ws_b], dma=True)
            wb_t, wb_b = wbf.nxt()
            cast_op(C, wb_t[:, :, :], ws_t[:, :, :], [ws_b], [wb_b])
            return wb_t, wb_b

        xe_of = {}

        def gather(ex):
            xeT, xeT_b = xeTp.nxt()
            xe_of[ex] = (xeT, xeT_b)
            for j in range(NJ):
                col = ex * NJ + j
                x_t, x_b = xg.nxt()
                S.op("pool", lambda e, x_t=x_t, col=col: e.indirect_dma_start(out=x_t[:, :], out_offset=None, in_=xn2[:, :], in_offset=bass.IndirectOffsetOnAxis(ap=idxc[:, col:col + 1], axis=0)),
                     reads=[xn2_b, idxc_b], writes=[x_b], dma=True)
                for g in range(2):
                    p_t, p_b = ptr.nxt()
                    for jj in range(8):
                        kc = g * 8 + jj
                        S.op("pe", lambda e, p_t=p_t, jj=jj, kc=kc, x_t=x_t: e.transpose(out=p_t[:, jj, :], in_=x_t[:, kc * 128:(kc + 1) * 128], identity=idb[:]),
                             reads=[x_b, idbb], writes=[p_b])
                    S.op("dve", lambda e, p_t=p_t, g=g, j=j: e.tensor_copy(out=xeT[:, g * 8:(g + 1) * 8, j * 128:(j + 1) * 128], in_=p_t[:]), reads=[p_b], writes=[xeT_b])

        jobs = []
        for ex in range(NE):
            for fc in range(16):
                jobs += [(ex, "g", fc), (ex, "u", fc)]
            jobs += [(ex, "d", dc) for dc in range(16)]
        wjob = {}
        sil = {}

        def prep(i):
            ex, kind, c = jobs[i]
            if kind == "d":
                q = c % 4
                if q == 0:
                    wd_cur["t"] = wdw.nxt()
                wt, wt_b = wd_cur["t"]
                ws_t, ws_b = wst.nxt()
                S.op("sp", lambda e: e.dma_start(out=ws_t[:, :, :], in_=w_d[ex, :, c * 128:(c + 1) * 128].rearrange("(kc p) n -> p kc n", p=128)), writes=[ws_b], dma=True)
                cast_op(C, wt[:, :, q * 128:(q + 1) * 128], ws_t[:, :, :], [ws_b], [wt_b])
                wjob[i] = (wt, wt_b)
                return
            src = {"g": w_g, "u": w_u}[kind]
            wjob[i] = load_w(src[ex, :, c * 128:(c + 1) * 128])

        def compute(i):
            ex, kind, c = jobs[i]
            if kind == "g":
                xeT, xeT_b = xe_of[ex]
                wg_t, wg_b = wjob.pop(i)
                g_t, g_b = pg.nxt()
                for kc in range(16):
                    S.op("pe", lambda e, kc=kc: e.matmul(g_t[:, :CAP], lhsT=wg_t[:, kc, :], rhs=xeT[:, kc, :], start=(kc == 0), stop=(kc == 15)),
                         reads=[wg_b, xeT_b], writes=[g_b])
                s_t, s_b = sg.nxt()
                S.op("act", lambda e: e.activation(out=s_t[:, :], in_=g_t[:, :CAP], func=AF.Silu), reads=[g_b], writes=[s_b])
                sil[(ex, c)] = (s_t, s_b)
            elif kind == "u":
                xeT, xeT_b = xe_of[ex]
                wu_t, wu_b = wjob.pop(i)
                u_t, u_b = pu.nxt()
                for kc in range(16):
                    S.op("pe", lambda e, kc=kc: e.matmul(u_t[:, :CAP], lhsT=wu_t[:, kc, :], rhs=xeT[:, kc, :], start=(kc == 0), stop=(kc == 15)),
                         reads=[wu_b, xeT_b], writes=[u_b])
                s_t, s_b = sil.pop((ex, c))
                S.op("dve", lambda e: e.tensor_tensor(out=hT[:, c, :], in0=u_t[:, :CAP], in1=s_t[:, :], op=ALU.mult), reads=[u_b, s_b], writes=[hT_b])
            else:
                dc = c
                if dc == 0 and ex + 1 < NE:
                    gather(ex + 1)
                wd_t, wd_b = wjob.pop(i)
                if dc % 4 == 3:
                    d4 = dc // 4
                    for j in range(NJ):
                        col = ex * NJ + j
                        p_t, p_b = pd.nxt()
                        for fc in range(16):
                            S.op("pe", lambda e, p_t=p_t, j=j, fc=fc: e.matmul(p_t[:, :], lhsT=hT[:, fc, j * 128:(j + 1) * 128], rhs=wd_t[:, fc, :], start=(fc == 0), stop=(fc == 15)),
                                 reads=[wd_b, hT_b], writes=[p_b])
                        S.op("act", lambda e, p_t=p_t, j=j, col=col: e.activation(out=ye[:, j, d4 * 512:(d4 + 1) * 512], in_=p_t[:, :], func=AF.Copy, scale=gatec[:, col:col + 1]),
                             reads=[p_b, gatec_b], writes=[ye_b[j]])
                if dc == 15:
                    for j in range(NJ):
                        col = ex * NJ + j
                        S.op("pool", lambda e, j=j, col=col: e.indirect_dma_start(out=h_ap[:, :], out_offset=bass.IndirectOffsetOnAxis(ap=idxc[:, col:col + 1], axis=0), in_=ye[:, j, :], in_offset=None, compute_op=ALU.add),
                             reads=[ye_b[j], idxc_b], writes=[h_b], dma=True)

        gather(0)
        pipeline(len(jobs), prep, compute, depth=2)
        C.end_stage()


def stage_final(C, h_ap, h_b, gain_ap, y_ap, y_b):
    nc, S = C.nc, C.S
    with contextlib.ExitStack() as st:
        pools = norm_pools(C, st)
        gain_t, gain_b = load_gain(C, st, "gainf", gain_ap, D)
        of = Rot(nc, st, "of", 2, [128, D], F32)
        for tt in range(T // 128):
            o_t, o_b = of.nxt()
            rms_tile(C, pools, h_ap[tt * 128:(tt + 1) * 128, :], gain_t, gain_b, o_t[:], o_b, D, [h_b])
            S.op("sp", lambda e, o_t=o_t, tt=tt: e.dma_start(out=y_ap[tt * 128:(tt + 1) * 128, :], in_=o_t[:, :]), reads=[o_b], writes=[y_b], dma=True)
        C.end_stage()


NCORES = 4
NB = 4 // NCORES
DEPTH = 2
_CACHE = {}


def build_program():
    nc = bass.Bass("TRN2", target_bir_lowering=False)
    def inp(name, shape, dt=F32):
        return nc.dram_tensor(name, list(shape), dt, kind="ExternalInput").ap()
    x = inp("x", [NB * T, D])
    w_in = inp("w_in", [DEPTH, D, INC])
    b_gate = inp("b_gate", [DEPTH, 6144])
    w_uq = inp("w_uq", [DEPTH, 448, 1536])
    q_norm = inp("q_norm", [DEPTH, 448])
    w_ukv = inp("w_ukv", [DEPTH, 160, 2048])
    kv_norm = inp("kv_norm", [DEPTH, 160])
    rpbT = inp("rpbT", [DEPTH, 16, 128, 14, 64])
    maskc = inp("maskc", [128, 14, 64])
    w_branch = inp("w_branch", [DEPTH, 3, 1024, 2048])
    w_o = inp("w_o", [DEPTH, D, D])
    norm_mix = inp("norm_mix", [DEPTH, D])
    norm_moe = inp("norm_moe", [DEPTH, D])
    w_router = inp("w_router", [DEPTH, D, 16])
    w_g = inp("w_exp_gate", [DEPTH, 16, D, D])
    w_u = inp("w_exp_up", [DEPTH, 16, D, D])
    w_d = inp("w_exp_down", [DEPTH, 16, D, D])
    norm_final = inp("norm_final", [D])
    ident = inp("ident", [128, 128])
    cos2T = inp("cos2T", [64, T])
    sin2T = inp("sin2T", [64, T])
    ccs = inp("ccs", [2, 256, 256])
    csT = inp("csT", [T, T], BF16)
    ssT = inp("ssT", [T, T], BF16)
    y = nc.dram_tensor("y", [NB * T, D], F32, kind="ExternalOutput").ap()
    with contextlib.ExitStack() as es:
        C = Ctx(nc, es)
        sc = {"qkT": C.dram("qkT", [2048, T], BF16), "vna": C.dram("vna", [T, 1024], BF16), "cT": C.dram("cT", [672, T], F32),
              "ufT": C.dram("ufT", [1024, T], BF16), "gT": C.dram("gT", [6144, T], BF16),
              "ynaT": C.dram("ynaT", [1024, T], BF16), "ymlaT": C.dram("ymlaT", [1024, T], BF16), "yfT": C.dram("yfT", [1024, T], BF16)}
        mergedT = C.dram("mergedT", [2048, T], BF16)
        wbr_c = C.dram("wbr_c", [16, 128, 24 * 128], BF16)
        wo_c = C.dram("wo_c", [16, 128, 16 * 128], BF16)
        hA = C.dram("hA", [T, D], F32)
        xn2 = C.dram("xn2", [T, D], BF16)
        idx_d = C.dram("idx_d", [16, 512], U32)
        gate_d = C.dram("gate_d", [16, 512], F32)
        hA_b = C.buf("hA")
        x_b = Buf("x")
        y_b = C.buf("y")
        for b in range(NB):
            xb = x[b * T:(b + 1) * T, :]
            for l in range(DEPTH):
                h_in, h_in_b = (xb, x_b) if l == 0 else (hA, hA_b)
                stage_inproj(C, h_in, h_in_b, norm_mix[l], w_in[l], b_gate[l], ident, sc)
                stage_na(C, sc, rpbT[l], maskc, sc["ynaT"], wprep=(w_branch[l], w_o[l], wbr_c, wo_c))
                stage_mla(C, sc, w_uq[l], q_norm[l], w_ukv[l], kv_norm[l], cos2T, sin2T, sc["ymlaT"])
                stage_fnet(C, sc, ccs, csT, ssT, sc["yfT"])
                stage_merge(C, sc, w_branch[l], mergedT, wbr_c=wbr_c)
                stage_outproj(C, mergedT, w_o[l], h_in, h_in_b, hA, hA_b, wo_c=wo_c)
                stage_route(C, hA, hA_b, norm_moe[l], w_router[l], ident, xn2, idx_d, gate_d)
                stage_experts(C, hA, hA_b, xn2, idx_d, gate_d, w_g[l], w_u[l], w_d[l], ident)
            stage_final(C, hA, hA_b, norm_final, y[b * T:(b + 1) * T, :], y_b)
    return nc


def rope_consts():
    pos = np.arange(T, dtype=np.float32)
    inv = (1.0 / (10000.0 ** (np.arange(0, 64, 2, dtype=np.float32) / 64))).astype(np.float32)
    ang = pos[:, None] * inv[None, :]
    cos, sin = np.cos(ang).astype(np.float32), np.sin(ang).astype(np.float32)
    cos2T = np.ascontiguousarray(np.concatenate([cos, cos], 1).T)
    sin2T = np.ascontiguousarray(np.concatenate([-sin, sin], 1).T)
    return cos2T, sin2T


def kernel(x, w_in, b_gate, w_uq, q_norm, w_ukv, kv_norm, na_rpb, w_branch, w_o,
           norm_mix, norm_moe, w_router, w_exp_gate, w_exp_up, w_exp_down, norm_final):
    f = lambda a: np.ascontiguousarray(np.asarray(a, dtype=np.float32))
    if "nc" not in _CACHE:
        _CACHE["nc"] = build_program()
        cos2T, sin2T = rope_consts()
        ccs, csT, ssT = fnet_host_consts()
        _CACHE["consts"] = dict(cos2T=cos2T, sin2T=sin2T, ccs=ccs, csT=csT, ssT=ssT, ident=np.eye(128, dtype=np.float32))
    nc = _CACHE["nc"]
    na_rpb = f(na_rpb)
    tabs = [na_host_tables(na_rpb[l]) for l in range(DEPTH)]
    rpbT = np.stack([t[0] for t in tabs])
    maskc = tabs[0][1]
    shared = dict(w_in=f(w_in), b_gate=f(b_gate), w_uq=f(w_uq), q_norm=f(q_norm), w_ukv=f(w_ukv), kv_norm=f(kv_norm),
                  rpbT=rpbT, maskc=maskc, w_branch=f(w_branch), w_o=f(w_o), norm_mix=f(norm_mix), norm_moe=f(norm_moe),
                  w_router=f(w_router), w_exp_gate=f(w_exp_gate), w_exp_up=f(w_exp_up), w_exp_down=f(w_exp_down),
                  norm_final=f(norm_final), **_CACHE["consts"])
    xf = f(x).reshape(4 * T, D)
    in_maps = []
    for c in range(NCORES):
        m = dict(shared)
        m["x"] = xf[c * NB * T:(c + 1) * NB * T]
        in_maps.append(m)
    res = run_bass_kernel_spmd(nc, in_maps, core_ids=list(range(NCORES)))
    out = np.concatenate([np.asarray(r["y"], dtype=np.float32) for r in res.results], axis=0)
    return out.reshape(4, T, D)
```

```python
import numpy as np
import concourse.bass as bass
import concourse.mybir as mybir
from concourse.bass_utils import run_bass_kernel_spmd

F32 = mybir.dt.float32
BF16 = mybir.dt.bfloat16
I32 = mybir.dt.int32
U32 = mybir.dt.uint32
U16 = mybir.dt.uint16
AF = mybir.ActivationFunctionType
ALU = mybir.AluOpType
AX = mybir.AxisListType


class Buf:
    __slots__ = ("name", "w", "r")

    def __init__(self, name=""):
        self.name = name
        self.w = []
        self.r = []


def _merge(tokens):
    d = {}
    for s, v in tokens:
        if d.get(s, 0) < v:
            d[s] = v
    return d


class Sched:
    ENG = ("pe", "act", "dve", "pool", "sp")

    def __init__(self, nc, es, n_dma_sems=40, rot=30000):
        self.nc = nc
        self.es = es
        self.rot = rot
        self.lists = {e: [] for e in self.ENG}
        self.sems = []
        self.cur = {}
        self.known = {e: {} for e in self.ENG}
        for e in self.ENG:
            self.cur[e] = [self._new_sem("e_" + e), 0]
        self.dma_pool = [[self._new_sem("d%d" % i), 0] for i in range(n_dma_sems)]
        self.dma_next = 0
        self.n_ops = 0

    def _new_sem(self, name):
        h = self.es.enter_context(self.nc.semaphore(name + "_%d" % len(self.sems)))
        self.sems.append(h)
        return len(self.sems) - 1

    def op(self, eng, fn, reads=(), writes=(), dma=False):
        deps = []
        for b in reads:
            deps += b.w
        for b in writes:
            deps += b.w
            deps += b.r
        tok_extra = None
        if dma:
            slot = self.dma_pool[self.dma_next]
            self.dma_next = (self.dma_next + 1) % len(self.dma_pool)
            if slot[1] > 0:
                deps.append((slot[0], 16 * slot[1]))
            slot[1] += 1
            token = (slot[0], 16 * slot[1])
            inc = (slot[0], 16)
        else:
            c = self.cur[eng]
            if c[1] >= self.rot:
                c[0] = self._new_sem("e_" + eng)
                c[1] = 0
            c[1] += 1
            token = (c[0], c[1])
            inc = (c[0], 1)
        need = _merge(deps)
        kn = self.known[eng]
        waits = []
        own = self.cur[eng][0]
        for s, v in need.items():
            if eng == "pe" and s == own and not dma:
                continue
            if kn.get(s, 0) >= v:
                continue
            kn[s] = v
            waits.append((s, v))
        self.lists[eng].append((waits, fn, inc))
        for b in writes:
            b.w = [token]
            b.r = []
        for b in reads:
            if b in writes:
                continue
            m = _merge(b.r + [token])
            b.r = list(m.items())
        self.n_ops += 1
        return token

    def wait_all(self, eng, bufs):
        deps = []
        for b in bufs:
            deps += b.w
        need = _merge(deps)
        waits = [(s, v) for s, v in need.items()]
        self.lists[eng].append((waits, None, None))

    def emit(self):
        nc = self.nc
        sems = self.sems
        lists = self.lists

        def run(engname, e):
            for waits, fn, inc in lists[engname]:
                for s, v in waits:
                    e.wait_ge(sems[s], v)
                if fn is not None:
                    ins = fn(e)
                    ins.then_inc(sems[inc[0]], inc[1])

        with nc.Block() as block:
            @block.tensor
            def _(e):
                run("pe", e)

            @block.scalar
            def _(e):
                run("act", e)

            @block.vector
            def _(e):
                run("dve", e)

            @block.gpsimd
            def _(e):
                run("pool", e)

            @block.sync
            def _(e):
                run("sp", e)


import contextlib

D = 2048
T = 4096
INC = 10912
EPS = 1e-6


_UID = [0]


def U(name):
    _UID[0] += 1
    return "%s_u%d" % (name, _UID[0])


class Rot:
    def __init__(self, nc, st, name, n, shape, dtype, psum=False):
        self.slots = []
        for i in range(n):
            if psum:
                t = st.enter_context(nc.psum_tensor(U("%s%d" % (name, i)), shape, dtype))
            else:
                t = st.enter_context(nc.sbuf_tensor(U("%s%d") % (name, i), shape, dtype))
            self.slots.append((t, Buf(name + str(i))))
        self.i = 0

    def nxt(self):
        s = self.slots[self.i]
        self.i = (self.i + 1) % len(self.slots)
        return s


class Ctx:
    def __init__(self, nc, es, debug_outs=()):
        self.nc = nc
        self.es = es
        self.S = Sched(nc, es)
        self.debug_outs = set(debug_outs)
        self.dbufs = {}
        self.cast_i = 0

    def dram(self, name, shape, dtype):
        kind = "ExternalOutput" if name in self.debug_outs else "Internal"
        t = self.nc.dram_tensor(name, list(shape), dtype, kind=kind).ap()
        return t

    def buf(self, key):
        if key not in self.dbufs:
            self.dbufs[key] = Buf(str(key))
        return self.dbufs[key]

    def end_stage(self):
        S = self.S
        waits = [(s[0], 16 * s[1]) for s in S.dma_pool if s[1] > 0]
        for e in ("sp", "pool", "act"):
            S.lists[e].append((list(waits), None, None))
        S.emit()
        S.lists = {e: [] for e in S.ENG}


def load_consts(C, st, ident_f):
    nc, S = C.nc, C.S
    idf = st.enter_context(nc.sbuf_tensor(U("idf"), [128, 128], F32))
    idb = st.enter_context(nc.sbuf_tensor(U("idb"), [128, 128], BF16))
    bf = Buf("idf")
    bb = Buf("idb")
    S.op("sp", lambda e: e.dma_start(out=idf[:], in_=ident_f[:, :]), writes=[bf], dma=True)
    S.op("dve", lambda e: e.tensor_copy(out=idb[:], in_=idf[:]), reads=[bf], writes=[bb])
    return idf, bf, idb, bb


def rms_tile(C, pools, src_ap, gain_t, gain_b, out_t, out_b, width, src_reads):
    nc, S = C.nc, C.S
    ht, hb = pools["h"].nxt()
    S.op("sp", lambda e: e.dma_start(out=ht[:, :width], in_=src_ap), reads=src_reads, writes=[hb], dma=True)
    jt, jb = pools["junk"].nxt()
    st_, sb_ = pools["stat"].nxt()
    S.op("act", lambda e: e.activation(out=jt[:, :width], in_=ht[:, :width], func=AF.Square, accum_out=st_[:, 0:1]),
         reads=[hb], writes=[jb, sb_])
    S.op("act", lambda e: e.activation(out=st_[:, 1:2], in_=st_[:, 0:1], func=AF.Sqrt, scale=1.0 / width, bias=pools["eps"][0][:, 0:1]),
         reads=[sb_, pools["eps"][1]], writes=[sb_])
    S.op("dve", lambda e: e.reciprocal(out=st_[:, 2:3], in_=st_[:, 1:2]), reads=[sb_], writes=[sb_])
    S.op("dve", lambda e: e.scalar_tensor_tensor(out=out_t, in0=ht[:, :width], scalar=st_[:, 2:3], in1=gain_t[:, :width],
                                                op0=ALU.mult, op1=ALU.mult),
         reads=[hb, sb_, gain_b], writes=[out_b])
    return ht, hb


def norm_pools(C, st, width=D, nh=2):
    nc, S = C.nc, C.S
    pools = {
        "h": Rot(nc, st, "nh", nh, [128, width], F32),
        "junk": Rot(nc, st, "nj", 1, [128, width], BF16),
        "stat": Rot(nc, st, "ns", 4, [128, 4], F32),
    }
    eps_t = st.enter_context(nc.sbuf_tensor(U("epsT"), [128, 1], F32))
    eb = Buf("eps")
    S.op("dve", lambda e: e.memset(eps_t[:], EPS), writes=[eb])
    pools["eps"] = (eps_t, eb)
    return pools


def load_gain(C, st, name, vec_ap, width):
    nc, S = C.nc, C.S
    g = st.enter_context(nc.sbuf_tensor(U(name), [128, width], F32))
    gb = Buf(name)
    S.op("sp", lambda e: e.dma_start(out=g[:], in_=vec_ap.partition_broadcast(128)), writes=[gb], dma=True)
    return g, gb


def pipeline(n, prep, compute, depth=1):
    for i in range(min(depth, n)):
        prep(i)
    for i in range(n):
        if i + depth < n:
            prep(i + depth)
        compute(i)


CAST_PATTERN = {"default": ("act", "dve"), "moe": ("act", "dve", "act", "dve", "pool")}


def cast_op(C, out_ap, in_ap, reads, writes, pattern="default"):
    S = C.S
    pat = CAST_PATTERN[pattern]
    eng = pat[C.cast_i % len(pat)]
    C.cast_i += 1
    if eng == "dve":
        S.op("dve", lambda e: e.tensor_copy(out=out_ap, in_=in_ap), reads=reads, writes=writes)
    elif eng == "act":
        S.op("act", lambda e: e.copy(out=out_ap, in_=in_ap), reads=reads, writes=writes)
    else:
        S.op("pool", lambda e: e.tensor_copy(out=out_ap, in_=in_ap), reads=reads, writes=writes)


def stage_inproj(C, h_ap, h_buf, gain_ap, w_in, b_gate, ident_f, sc):
    nc, S = C.nc, C.S
    HALF = 2048
    with contextlib.ExitStack() as st:
        idf, idfb, idb, idbb = load_consts(C, st, ident_f)
        pools = norm_pools(C, st)
        gain_t, gain_b = load_gain(C, st, "gain1", gain_ap, D)
        xs_pool = Rot(nc, st, "xs", 2, [128, D], BF16)
        xnT = st.enter_context(nc.sbuf_tensor(U("xnT"), [128, 16, HALF], BF16))
        xnT_b = [Buf("xnT%d" % i) for i in range(16)]
        wst = Rot(nc, st, "wst", 3, [128, 16, 128], F32)
        wbf = Rot(nc, st, "wbf", 3, [128, 16, 128], BF16)
        ost_b = Rot(nc, st, "ostb", 2, [128, HALF], BF16)
        ost_f = Rot(nc, st, "ostf", 2, [128, HALF], F32)
        bg = st.enter_context(nc.sbuf_tensor(U("bg"), [128, 48], F32))
        bgb = Buf("bg")
        S.op("sp", lambda e: e.dma_start(out=bg[:], in_=b_gate.rearrange("(c p) -> p c", p=128), allow_slow_non_contiguous=True), writes=[bgb], dma=True)
        pmm = Rot(nc, st, "pmm", 6, [128, 512], F32, psum=True)
        ptr = Rot(nc, st, "ptr", 2, [128, 8, 128], BF16, psum=True)

        chunks = []
        for i in range(16):
            chunks.append((i * 128, 128, "F", "qkT", i * 128))
        for i in range(8):
            chunks.append((2048 + i * 128, 128, "T", "vna", i * 128))
        c0 = 3072
        r0 = 0
        while r0 < 672:
            m = min(128, 672 - r0)
            chunks.append((c0 + r0, m, "C", "cT", r0))
            r0 += m
        for i in range(8):
            chunks.append((3744 + i * 128, 128, "F", "ufT", i * 128))
        for i in range(48):
            chunks.append((4768 + i * 128, 128, "G", "gT", i * 128))

        w_v = w_in
        for half in range(T // HALF):
            t0 = half * HALF
            for tt in range(16):
                xs_t, xs_b = xs_pool.nxt()
                rms_tile(C, pools, h_ap[t0 + tt * 128: t0 + (tt + 1) * 128, :], gain_t, gain_b, xs_t[:], xs_b, D, [h_buf])
                for g in range(2):
                    pt, pb = ptr.nxt()
                    for j in range(8):
                        kc = g * 8 + j
                        S.op("pe", lambda e, pt=pt, j=j, kc=kc, xs_t=xs_t: e.transpose(out=pt[:, j, :], in_=xs_t[:, kc * 128:(kc + 1) * 128], identity=idb[:]),
                             reads=[xs_b, idbb], writes=[pb])
                    S.op("dve", lambda e, pt=pt, g=g, tt=tt: e.tensor_copy(out=xnT[:, g * 8:(g + 1) * 8, tt * 128:(tt + 1) * 128], in_=pt[:]),
                         reads=[pb], writes=[xnT_b[tt]])
            wjob = {}

            def prep(i):
                (c0, m, mode, dst, dr0) = chunks[i]
                ws_t, ws_b = wst.nxt()
                S.op("sp", lambda e: e.dma_start(out=ws_t[:, :, :m], in_=w_v[:, c0:c0 + m].rearrange("(kc p) n -> p kc n", p=128)),
                     writes=[ws_b], dma=True)
                wb_t, wb_b = wbf.nxt()
                cast_op(C, wb_t[:, :, :m], ws_t[:, :, :m], [ws_b], [wb_b])
                wjob[i] = (wb_t, wb_b)

            def compute(i, t0=t0):
                (c0, m, mode, dst, dr0) = chunks[i]
                wb_t, wb_b = wjob.pop(i)
                if mode != "T":
                    if mode == "C":
                        o_t, o_b = ost_f.nxt()
                    else:
                        o_t, o_b = ost_b.nxt()
                    for tb in range(4):
                        p_t, p_b = pmm.nxt()
                        for kc in range(16):
                            S.op("pe", lambda e, p_t=p_t, kc=kc, tb=tb: e.matmul(p_t[:m, :], lhsT=wb_t[:, kc, :m], rhs=xnT[:, kc, tb * 512:(tb + 1) * 512], start=(kc == 0), stop=(kc == 15)),
                                 reads=[wb_b] + xnT_b[tb * 4:(tb + 1) * 4], writes=[p_b])
                        if mode == "G":
                            gi = dr0 // 128
                            S.op("act", lambda e, p_t=p_t, tb=tb, gi=gi: e.activation(out=o_t[:, tb * 512:(tb + 1) * 512], in_=p_t[:, :], func=AF.Sigmoid, bias=bg[:, gi:gi + 1]),
                                 reads=[p_b, bgb], writes=[o_b])
                        else:
                            S.op("dve", lambda e, p_t=p_t, tb=tb: e.tensor_copy(out=o_t[:m, tb * 512:(tb + 1) * 512], in_=p_t[:m, :]),
                                 reads=[p_b], writes=[o_b])
                    S.op("sp", lambda e: e.dma_start(out=sc[dst][dr0:dr0 + m, t0:t0 + HALF], in_=o_t[:m, :]),
                         reads=[o_b], writes=[C.buf(dst)], dma=True)
                else:
                    o_t, o_b = ost_b.nxt()
                    for g4 in range(4):
                        p_t, p_b = pmm.nxt()
                        for q in range(4):
                            tt = g4 * 4 + q
                            for kc in range(16):
                                S.op("pe", lambda e, p_t=p_t, kc=kc, tt=tt, q=q: e.matmul(p_t[:, q * 128:(q + 1) * 128], lhsT=xnT[:, kc, tt * 128:(tt + 1) * 128], rhs=wb_t[:, kc, :], start=(kc == 0), stop=(kc == 15)),
                                     reads=[wb_b, xnT_b[tt]], writes=[p_b])
                        S.op("dve", lambda e, p_t=p_t, g4=g4: e.tensor_copy(out=o_t[:, g4 * 512:(g4 + 1) * 512], in_=p_t[:, :]),
                             reads=[p_b], writes=[o_b])
                    S.op("sp", lambda e: e.dma_start(
                        out=sc["vna"][t0:t0 + HALF, dr0:dr0 + 128].rearrange("(tt p) c -> p tt c", p=128),
                        in_=o_t[:, :].rearrange("p (tt c) -> p tt c", c=128)),
                        reads=[o_b], writes=[C.buf("vna")], dma=True)

            pipeline(len(chunks), prep, compute, depth=2)
        C.end_stage()


def fm_rmsnorm(C, st, name, src, src_buf, row0, nrows, gain_ap, onesf, onesf_b, eps, pmm, out_t, out_b, cin, csq, rs):
    nc, S = C.nc, C.S
    nch = (nrows + 127) // 128
    gcol = st.enter_context(nc.sbuf_tensor(U(name + "g"), [128, nch], F32))
    gb = Buf(name + "g")
    for c in range(nch):
        ksz = min(128, nrows - c * 128)
        S.op("sp", lambda e, c=c, ksz=ksz: e.dma_start(out=gcol[:ksz, c:c + 1], in_=gain_ap[c * 128:c * 128 + ksz].rearrange("(p o) -> p o", o=1)),
             writes=[gb], dma=True)
    for tb in range(T // 512):
        ci, cib = cin.nxt()
        cs, csb = csq.nxt()
        for c in range(nch):
            ksz = min(128, nrows - c * 128)
            S.op("sp", lambda e, ci=ci, c=c, ksz=ksz, tb=tb: e.dma_start(out=ci[:ksz, c, :], in_=src[row0 + c * 128: row0 + c * 128 + ksz, tb * 512:(tb + 1) * 512]),
                 reads=[src_buf], writes=[cib], dma=True)
        p_t, p_b = pmm.nxt()
        for c in range(nch):
            ksz = min(128, nrows - c * 128)
            S.op("act", lambda e, ci=ci, cs=cs, c=c, ksz=ksz: e.activation(out=cs[:ksz, c, :], in_=ci[:ksz, c, :], func=AF.Square),
                 reads=[cib], writes=[csb])
            S.op("pe", lambda e, p_t=p_t, cs=cs, c=c, ksz=ksz: e.matmul(p_t[:, :], lhsT=onesf[:ksz, :], rhs=cs[:ksz, c, :], start=(c == 0), stop=(c == nch - 1)),
                 reads=[csb, onesf_b], writes=[p_b])
        r_t, r_b = rs.nxt()
        S.op("act", lambda e, r_t=r_t, p_t=p_t: e.activation(out=r_t[:, :], in_=p_t[:, :], func=AF.Sqrt, scale=1.0 / nrows, bias=eps[0][:, 0:1]),
             reads=[p_b, eps[1]], writes=[r_b])
        S.op("dve", lambda e, r_t=r_t: e.reciprocal(out=r_t[:, :], in_=r_t[:, :]), reads=[r_b], writes=[r_b])
        for c in range(nch):
            ksz = min(128, nrows - c * 128)
            S.op("dve", lambda e, ci=ci, r_t=r_t, c=c, ksz=ksz, tb=tb: e.scalar_tensor_tensor(
                out=out_t[:ksz, c, tb * 512:(tb + 1) * 512], in0=ci[:ksz, c, :], scalar=gcol[:ksz, c:c + 1], in1=r_t[:ksz, :], op0=ALU.mult, op1=ALU.mult),
                reads=[cib, r_b, gb], writes=[out_b])


def stage_mla(C, sc, w_uq, q_norm, w_ukv, kv_norm, cos2T, sin2T, ymlaT):
    nc, S = C.nc, C.S
    cT = sc["cT"]
    cTb = C.buf("cT")
    scale = 192.0 ** -0.5
    QCH = [(0, 128), (128, 128), (256, 128), (384, 64)]
    KCH = [(0, 128), (128, 32)]
    with contextlib.ExitStack() as st:
        onesf = st.enter_context(nc.sbuf_tensor(U("onesf"), [128, 128], F32))
        onesb = st.enter_context(nc.sbuf_tensor(U("onesb"), [128, 128], BF16))
        of_b, ob_b = Buf("onesf"), Buf("onesb")
        S.op("dve", lambda e: e.memset(onesf[:], 1.0), writes=[of_b])
        S.op("dve", lambda e: e.memset(onesb[:], 1.0), writes=[ob_b])
        eps_t = st.enter_context(nc.sbuf_tensor(U("epsT"), [128, 1], F32))
        eb = Buf("eps")
        S.op("dve", lambda e: e.memset(eps_t[:], EPS), writes=[eb])
        pmm = Rot(nc, st, "pmm", 4, [128, 512], F32, psum=True)
        pO = Rot(nc, st, "pO", 2, [128, 512], F32, psum=True)
        pD = Rot(nc, st, "pD", 2, [128, 512], F32, psum=True)
        cqn = st.enter_context(nc.sbuf_tensor(U("cqn"), [128, 4, T], BF16))
        ckvn = st.enter_context(nc.sbuf_tensor(U("ckvn"), [128, 2, T], BF16))
        cqn_b, ckvn_b = Buf("cqn"), Buf("ckvn")
        cin = Rot(nc, st, "fci", 2, [128, 4, 512], F32)
        csq = Rot(nc, st, "fcs", 1, [128, 4, 512], F32)
        rs = Rot(nc, st, "frs", 2, [128, 512], F32)
        fm_rmsnorm(C, st, "nq", cT, cTb, 0, 448, q_norm, onesf, of_b, (eps_t, eb), pmm, cqn, cqn_b, cin, csq, rs)
        fm_rmsnorm(C, st, "nk", cT, cTb, 448, 160, kv_norm, onesf, of_b, (eps_t, eb), pmm, ckvn, ckvn_b, cin, csq, rs)
        kpe = st.enter_context(nc.sbuf_tensor(U("kpe"), [128, T], BF16))
        kpe_b = Buf("kpe")
        S.op("pool", lambda e: e.memset(kpe[64:128, :], 0.0), writes=[kpe_b])
        tmpA = Rot(nc, st, "tmpA", 2, [64, 512], F32)
        tmpB = Rot(nc, st, "tmpB", 2, [64, 512], F32)
        tmpC = Rot(nc, st, "tmpC", 2, [64, 512], F32)
        tmpD = Rot(nc, st, "tmpD", 2, [64, 512], F32)
        cosr = Rot(nc, st, "cosr", 2, [64, 512], F32)
        sinr = Rot(nc, st, "sinr", 2, [64, 512], F32)

        def rope(src_a, src_a_b, src_r, src_r_b, tb, out_ap, out_b):
            ct, cb = cosr.nxt()
            s_t, s_b = sinr.nxt()
            S.op("sp", lambda e: e.dma_start(out=ct[:, :], in_=cos2T[:, tb * 512:(tb + 1) * 512]), writes=[cb], dma=True)
            S.op("sp", lambda e: e.dma_start(out=s_t[:, :], in_=sin2T[:, tb * 512:(tb + 1) * 512]), writes=[s_b], dma=True)
            a_t, a_b = tmpC.nxt()
            b_t, b_b = tmpD.nxt()
            S.op("dve", lambda e: e.tensor_tensor(out=a_t[:, :], in0=src_a, in1=ct[:, :], op=ALU.mult), reads=[src_a_b, cb], writes=[a_b])
            S.op("dve", lambda e: e.tensor_tensor(out=b_t[:, :], in0=src_r, in1=s_t[:, :], op=ALU.mult), reads=[src_r_b, s_b], writes=[b_b])
            S.op("dve", lambda e: e.tensor_tensor(out=out_ap, in0=a_t[:, :], in1=b_t[:, :], op=ALU.add), reads=[a_b, b_b], writes=[out_b])

        for tb in range(T // 512):
            a_t, a_b = tmpA.nxt()
            r_t, r_b = tmpB.nxt()
            S.op("sp", lambda e, a_t=a_t, tb=tb: e.dma_start(out=a_t[:, :], in_=cT[608:672, tb * 512:(tb + 1) * 512]), reads=[cTb], writes=[a_b], dma=True)
            S.op("sp", lambda e, r_t=r_t, tb=tb: e.dma_start(out=r_t[0:32, :], in_=cT[640:672, tb * 512:(tb + 1) * 512]), reads=[cTb], writes=[r_b], dma=True)
            S.op("sp", lambda e, r_t=r_t, tb=tb: e.dma_start(out=r_t[32:64, :], in_=cT[608:640, tb * 512:(tb + 1) * 512]), reads=[cTb], writes=[r_b], dma=True)
            rope(a_t[:, :], a_b, r_t[:, :], r_b, tb, kpe[0:64, tb * 512:(tb + 1) * 512], kpe_b)

        qn = st.enter_context(nc.sbuf_tensor(U("qn"), [128, T], BF16))
        qp = st.enter_context(nc.sbuf_tensor(U("qp"), [128, T], BF16))
        kn = st.enter_context(nc.sbuf_tensor(U("kn"), [128, T], BF16))
        vv = st.enter_context(nc.sbuf_tensor(U("vv"), [128, 32, 128], BF16))
        qn_b, qp_b, kn_b, vv_b = Buf("qn"), Buf("qp"), Buf("kn"), Buf("vv")
        S.op("pool", lambda e: e.memset(qp[64:128, :], 0.0), writes=[qp_b])
        wq_s = st.enter_context(nc.sbuf_tensor(U("wq_s"), [128, 4, 256], F32))
        wq_bf = st.enter_context(nc.sbuf_tensor(U("wq_bf"), [128, 4, 256], BF16))
        wk_s = st.enter_context(nc.sbuf_tensor(U("wk_s"), [128, 2, 256], F32))
        wk_bf = st.enter_context(nc.sbuf_tensor(U("wk_bf"), [128, 2, 256], BF16))
        wq_sb, wq_bb, wk_sb, wk_bb = Buf("wqs"), Buf("wqb"), Buf("wks"), Buf("wkb")
        Et = Rot(nc, st, "Et", 6, [128, 512], BF16)
        accA = Rot(nc, st, "accA", 2, [128, 512], F32)
        accB = Rot(nc, st, "accB", 2, [128, 512], F32)
        rden = Rot(nc, st, "rden", 2, [128, 512], F32)
        yst = Rot(nc, st, "yst", 2, [128, 512], BF16)
        qa = Rot(nc, st, "qa", 2, [64, 512], F32)
        qr = Rot(nc, st, "qr", 2, [64, 512], F32)

        for h in range(8):
            for c, (k0, ksz) in enumerate(QCH):
                S.op("sp", lambda e, c=c, k0=k0, ksz=ksz, h=h: e.dma_start(out=wq_s[:ksz, c, 0:192], in_=w_uq[k0:k0 + ksz, h * 192:(h + 1) * 192]), writes=[wq_sb], dma=True)
                S.op("sp", lambda e, c=c, k0=k0, ksz=ksz, h=h: e.dma_start(out=wq_s[:ksz, c, 192:224], in_=w_uq[k0:k0 + ksz, h * 192 + 160:h * 192 + 192]), writes=[wq_sb], dma=True)
                S.op("sp", lambda e, c=c, k0=k0, ksz=ksz, h=h: e.dma_start(out=wq_s[:ksz, c, 224:256], in_=w_uq[k0:k0 + ksz, h * 192 + 128:h * 192 + 160]), writes=[wq_sb], dma=True)
            for c, (k0, ksz) in enumerate(KCH):
                S.op("sp", lambda e, c=c, k0=k0, ksz=ksz, h=h: e.dma_start(out=wk_s[:ksz, c, :], in_=w_ukv[k0:k0 + ksz, h * 256:(h + 1) * 256]), writes=[wk_sb], dma=True)
            for c, (k0, ksz) in enumerate(QCH):
                S.op("dve", lambda e, c=c, ksz=ksz: e.tensor_copy(out=wq_bf[:ksz, c, :], in_=wq_s[:ksz, c, :]), reads=[wq_sb], writes=[wq_bb])
            for c, (k0, ksz) in enumerate(KCH):
                S.op("dve", lambda e, c=c, ksz=ksz: e.tensor_copy(out=wk_bf[:ksz, c, :], in_=wk_s[:ksz, c, :]), reads=[wk_sb], writes=[wk_bb])
            for tb in range(T // 512):
                tsl = slice(tb * 512, (tb + 1) * 512)
                p_t, p_b = pmm.nxt()
                for c, (k0, ksz) in enumerate(QCH):
                    S.op("pe", lambda e, p_t=p_t, c=c, ksz=ksz, tsl=tsl: e.matmul(p_t[:, :], lhsT=wq_bf[:ksz, c, 0:128], rhs=cqn[:ksz, c, tsl], start=(c == 0), stop=(c == 3)),
                         reads=[wq_bb, cqn_b], writes=[p_b])
                S.op("act", lambda e, p_t=p_t, tsl=tsl: e.copy(out=qn[:, tsl], in_=p_t[:, :]), reads=[p_b], writes=[qn_b])
                p1, p1b = pmm.nxt()
                for c, (k0, ksz) in enumerate(QCH):
                    S.op("pe", lambda e, p1=p1, c=c, ksz=ksz, tsl=tsl: e.matmul(p1[0:64, :], lhsT=wq_bf[:ksz, c, 128:192], rhs=cqn[:ksz, c, tsl], start=(c == 0), stop=(c == 3)),
                         reads=[wq_bb, cqn_b], writes=[p1b])
                p2, p2b = pmm.nxt()
                for c, (k0, ksz) in enumerate(QCH):
                    S.op("pe", lambda e, p2=p2, c=c, ksz=ksz, tsl=tsl: e.matmul(p2[0:64, :], lhsT=wq_bf[:ksz, c, 192:256], rhs=cqn[:ksz, c, tsl], start=(c == 0), stop=(c == 3)),
                         reads=[wq_bb, cqn_b], writes=[p2b])
                qa_t, qa_b = qa.nxt()
                qr_t, qr_b = qr.nxt()
                S.op("act", lambda e, qa_t=qa_t, p1=p1: e.copy(out=qa_t[:, :], in_=p1[0:64, :]), reads=[p1b], writes=[qa_b])
                S.op("act", lambda e, qr_t=qr_t, p2=p2: e.copy(out=qr_t[:, :], in_=p2[0:64, :]), reads=[p2b], writes=[qr_b])
                rope(qa_t[:, :], qa_b, qr_t[:, :], qr_b, tb, qp[0:64, tsl], qp_b)
                p3, p3b = pmm.nxt()
                for c, (k0, ksz) in enumerate(KCH):
                    S.op("pe", lambda e, p3=p3, c=c, ksz=ksz, tsl=tsl: e.matmul(p3[:, :], lhsT=wk_bf[:ksz, c, 0:128], rhs=ckvn[:ksz, c, tsl], start=(c == 0), stop=(c == 1)),
                         reads=[wk_bb, ckvn_b], writes=[p3b])
                S.op("act", lambda e, p3=p3, tsl=tsl: e.copy(out=kn[:, tsl], in_=p3[:, :]), reads=[p3b], writes=[kn_b])
                p4, p4b = pmm.nxt()
                for q4 in range(4):
                    tt = tb * 4 + q4
                    for c, (k0, ksz) in enumerate(KCH):
                        S.op("pe", lambda e, p4=p4, c=c, ksz=ksz, tt=tt, q4=q4: e.matmul(p4[:, q4 * 128:(q4 + 1) * 128], lhsT=ckvn[:ksz, c, tt * 128:(tt + 1) * 128], rhs=wk_bf[:ksz, c, 128:256], start=(c == 0), stop=(c == 1)),
                             reads=[wk_bb, ckvn_b], writes=[p4b])
                S.op("dve", lambda e, p4=p4, tb=tb: e.tensor_copy(out=vv[:, tb * 4:(tb + 1) * 4, :], in_=p4[:, :].rearrange("p (a b) -> p a b", b=128)),
                     reads=[p4b], writes=[vv_b])
            SK = 2
            fr = {}
            acc = {}

            def front(it):
                qb, kc = divmod(it, 32)
                qsl = slice(qb * 512, (qb + 1) * 512)
                ksl = slice(kc * 128, (kc + 1) * 128)
                s_t, s_b = pmm.nxt()
                S.op("pe", lambda e: e.matmul(s_t[:, :], lhsT=kn[:, ksl], rhs=qn[:, qsl], start=True, stop=False), reads=[kn_b, qn_b], writes=[s_b])
                S.op("pe", lambda e: e.matmul(s_t[:, :], lhsT=kpe[:, ksl], rhs=qp[:, qsl], start=False, stop=True), reads=[kpe_b, qp_b], writes=[s_b])
                e_t, e_b = Et.nxt()
                S.op("act", lambda e: e.activation(out=e_t[:, :], in_=s_t[:, :], func=AF.Exp, scale=scale), reads=[s_b], writes=[e_b])
                fr[it] = (e_t, e_b)

            def back(it, h=h):
                qb, kc = divmod(it, 32)
                qsl = slice(qb * 512, (qb + 1) * 512)
                e_t, e_b = fr.pop(it)
                if kc == 0:
                    acc["o"] = pO.nxt()
                    acc["a0"] = accA.nxt()
                    acc["a1"] = accB.nxt()
                o_t, o_b = acc["o"]
                S.op("pe", lambda e: e.matmul(o_t[:, :], lhsT=vv[:, kc, :], rhs=e_t[:, :], start=(kc == 0), stop=(kc == 31)), reads=[vv_b, e_b], writes=[o_b])
                if kc == 0:
                    acc["d"] = pD.nxt()
                d_t, d_b = acc["d"]
                S.op("pe", lambda e: e.matmul(d_t[:, :], lhsT=onesb[:, :], rhs=e_t[:, :], start=(kc == 0), stop=(kc == 31)), reads=[ob_b, e_b], writes=[d_b])
                if kc == 31:
                    rd_t, rd_b = rden.nxt()
                    S.op("dve", lambda e: e.reciprocal(out=rd_t[:, :], in_=d_t[:, :]), reads=[d_b], writes=[rd_b])
                    y_t, y_b = yst.nxt()
                    S.op("dve", lambda e: e.tensor_tensor(out=y_t[:, :], in0=o_t[:, :], in1=rd_t[:, :], op=ALU.mult), reads=[o_b, rd_b], writes=[y_b])
                    S.op("sp", lambda e: e.dma_start(out=ymlaT[h * 128:(h + 1) * 128, qsl], in_=y_t[:, :]), reads=[y_b], writes=[C.buf("ymlaT")], dma=True)

            NIT = (T // 512) * 32
            for it in range(NIT + SK):
                if it < NIT:
                    front(it)
                if it - SK >= 0:
                    back(it - SK)
        C.end_stage()


def stage_na(C, sc, rpbT, maskc, ynaT, wprep=None):
    nc, S = C.nc, C.S
    qkT, vna = sc["qkT"], sc["vna"]
    qk_b, v_b = C.buf("qkT"), C.buf("vna")
    scale = 64.0 ** -0.5
    with contextlib.ExitStack() as st:
        ones = st.enter_context(nc.sbuf_tensor(U("ones"), [128, 64], BF16))
        ones_b = Buf("ones")
        S.op("dve", lambda e: e.memset(ones[:], 1.0), writes=[ones_b])
        mk = st.enter_context(nc.sbuf_tensor(U("mk"), [128, 14 * 64], F32))
        mk_b = Buf("mk")
        S.op("sp", lambda e: e.dma_start(out=mk[:, :], in_=maskc.rearrange("j d q -> j (d q)")), writes=[mk_b], dma=True)
        khp = Rot(nc, st, "kh", 2, [128, T], BF16)
        qhp = Rot(nc, st, "qh", 2, [128, T], BF16)
        vhp = Rot(nc, st, "vh", 2, [128, 63, 128], BF16)
        btp = Rot(nc, st, "bt", 2, [128, 14 * 64], F32)
        btq = Rot(nc, st, "btq", 2, [128, 512], F32)
        tmp = Rot(nc, st, "tmp", 4, [128, 512], F32)
        Ep = Rot(nc, st, "E", 6, [128, 512], BF16)
        rdp = Rot(nc, st, "rd", 2, [128, 512], F32)
        yp = Rot(nc, st, "y", 2, [64, 512], BF16)
        ps = Rot(nc, st, "ps", 5, [128, 512], F32, psum=True)
        po = Rot(nc, st, "po", 3, [128, 512], F32, psum=True)
        SK = 3
        state = {}
        if wprep is not None:
            w_branch_, w_o_, wbr_c, wo_c = wprep
            pw_st = Rot(nc, st, "pwst", 2, [128, 24, 128], F32)
            pw_bf = Rot(nc, st, "pwbf", 2, [128, 24, 128], BF16)
            po_st = Rot(nc, st, "post", 2, [128, 16, 128], F32)
            po_bf = Rot(nc, st, "pobf", 2, [128, 16, 128], BF16)

        pend = []

        def wprep_flush():
            while pend:
                pend.pop(0)()

        def wprep_step(dc):
            wprep_flush()
            ws_t, ws_b = pw_st.nxt()
            for b in range(3):
                S.op("sp", lambda e, b=b: e.dma_start(out=ws_t[:, b * 8:(b + 1) * 8, :], in_=w_branch_[b, :, dc * 128:(dc + 1) * 128].rearrange("(kc p) n -> p kc n", p=128)),
                     writes=[ws_b], dma=True)
            wb_t, wb_b = pw_bf.nxt()
            S.op("pool", lambda e: e.tensor_copy(out=wb_t[:, :, :], in_=ws_t[:, :, :]), reads=[ws_b], writes=[wb_b])
            pend.append(lambda: S.op("sp", lambda e: e.dma_start(out=wbr_c[dc], in_=wb_t[:, :, :].rearrange("p a b -> p (a b)")), reads=[wb_b], writes=[C.buf("wbr_c")], dma=True))
            os_t, os_b = po_st.nxt()
            S.op("sp", lambda e: e.dma_start(out=os_t[:, :, :], in_=w_o_[:, dc * 128:(dc + 1) * 128].rearrange("(kc p) n -> p kc n", p=128)), writes=[os_b], dma=True)
            ob_t, ob_b = po_bf.nxt()
            S.op("pool", lambda e: e.tensor_copy(out=ob_t[:, :, :], in_=os_t[:, :, :]), reads=[os_b], writes=[ob_b])
            pend.append(lambda: S.op("sp", lambda e: e.dma_start(out=wo_c[dc], in_=ob_t[:, :, :].rearrange("p a b -> p (a b)")), reads=[ob_b], writes=[C.buf("wo_c")], dma=True))

        for (kh_, khb_) in khp.slots:
            S.op("pool", lambda e, kh_=kh_: e.memset(kh_[64:128, :], 0.0), writes=[khb_])
        for (qh_, qhb_) in qhp.slots:
            S.op("pool", lambda e, qh_=qh_: e.memset(qh_[64:128, :], 0.0), writes=[qhb_])
        for (vh_, vhb_) in vhp.slots:
            S.op("pool", lambda e, vh_=vh_: e.memset(vh_[:, :, 64:128], 1.0), writes=[vhb_])
        def head_load(h):
            kh, kh_b = khp.nxt()
            qh, qh_b = qhp.nxt()
            vh, vh_b = vhp.nxt()
            bt, bt_b = btp.nxt()
            S.op("sp", lambda e: e.dma_start(out=qh[0:64, :], in_=qkT[h * 64:(h + 1) * 64, :]), reads=[qk_b], writes=[qh_b], dma=True)
            S.op("sp", lambda e: e.dma_start(out=kh[0:64, :], in_=qkT[1024 + h * 64:1024 + (h + 1) * 64, :]), reads=[qk_b], writes=[kh_b], dma=True)
            for g in range(2):
                S.op("sp", lambda e, g=g: e.dma_start(out=vh[:, g * 16:(g + 1) * 16, 0:64], in_=vna[g * 2048:(g + 1) * 2048, h * 64:(h + 1) * 64].rearrange("(t p) c -> p t c", p=128)),
                     reads=[v_b], writes=[vh_b], dma=True)
            S.op("sp", lambda e: e.dma_start(out=vh[:, 32:48, 0:64], in_=vna[64:64 + 2048, h * 64:(h + 1) * 64].rearrange("(t p) c -> p t c", p=128)),
                 reads=[v_b], writes=[vh_b], dma=True)
            S.op("sp", lambda e: e.dma_start(out=vh[:, 48:63, 0:64], in_=vna[64 + 2048:64 + 2048 + 15 * 128, h * 64:(h + 1) * 64].rearrange("(t p) c -> p t c", p=128)),
                 reads=[v_b], writes=[vh_b], dma=True)
            S.op("sp", lambda e: e.dma_start(out=bt[:, :], in_=rpbT[h].rearrange("j d q -> j (d q)")), writes=[bt_b], dma=True)
            S.op("pool", lambda e: e.tensor_tensor(out=bt[:, :], in0=bt[:, :], in1=mk[:, :], op=ALU.add), reads=[mk_b], writes=[bt_b])
            bq, bq_b = btq.nxt()
            for a_ in range(2):
                S.op("pool", lambda e, a_=a_: e.tensor_copy(out=bq[:, a_ * 256:(a_ + 1) * 256].rearrange("p (c q) -> p c q", q=64), in_=bt[:, :].rearrange("p (d q) -> p d q", q=64)[:, 3:10:2, :]),
                     reads=[bt_b], writes=[bq_b])
            state[h] = (kh, kh_b, qh, qh_b, vh, vh_b, bt, bt_b, bq, bq_b)
            if wprep is not None:
                wprep_step(h)

        fr = {}
        units = []
        for h_ in range(16):
            for r_ in (0, 1, 2, 3):
                units.append((h_, [r_]))
            for r_ in range(4, 60, 2):
                units.append((h_, [r_, r_ + 1]))
            for r_ in (60, 61, 62, 63):
                units.append((h_, [r_]))

        def front(it):
            h, rows = units[it]
            if rows[0] == 0:
                head_load(h)
            kh, kh_b, qh, qh_b, vh, vh_b, bt, bt_b, bq, bq_b = state[h]
            s_t, s_b = ps.nxt()
            starts = []
            for ri, r in enumerate(rows):
                start = min(max(r - 4, 0), 56)
                starts.append(start)
                for c in range(4):
                    S.op("pe", lambda e, c=c, ri=ri, r=r, start=start: e.matmul(s_t[:, (ri * 4 + c) * 64:(ri * 4 + c + 1) * 64], lhsT=kh[:, (start + 2 * c) * 64:(start + 2 * c + 2) * 64], rhs=qh[:, r * 64:(r + 1) * 64], start=True, stop=True),
                         reads=[kh_b, qh_b], writes=[s_b])
            t_t, t_b = tmp.nxt()
            e_t, e_b = Ep.nxt()
            if len(rows) == 2:
                S.op("dve", lambda e: e.scalar_tensor_tensor(out=t_t[:, :], in0=s_t[:, :], scalar=scale, in1=bq[:, :], op0=ALU.mult, op1=ALU.add),
                     reads=[s_b, bq_b], writes=[t_b])
                S.op("act", lambda e: e.activation(out=e_t[:, :], in_=t_t[:, :], func=AF.Exp), reads=[t_b], writes=[e_b])
            else:
                base = starts[0] - rows[0] + 7
                S.op("dve", lambda e: e.scalar_tensor_tensor(out=t_t[:, 0:256].rearrange("p (c q) -> p c q", q=64), in0=s_t[:, 0:256].rearrange("p (c q) -> p c q", q=64), scalar=scale,
                                                            in1=bt[:, :].rearrange("p (d q) -> p d q", q=64)[:, base:base + 7:2, :], op0=ALU.mult, op1=ALU.add),
                     reads=[s_b, bt_b], writes=[t_b])
                S.op("act", lambda e: e.activation(out=e_t[:, 0:256], in_=t_t[:, 0:256], func=AF.Exp), reads=[t_b], writes=[e_b])
            fr[it] = (e_t, e_b, starts)

        acc = {}

        def back(it):
            h, rows = units[it]
            kh, kh_b, qh, qh_b, vh, vh_b, bt, bt_b, bq, bq_b = state[h]
            e_t, e_b, starts = fr.pop(it)
            for ri, r in enumerate(rows):
                back_row(h, r, ri, starts[ri], e_t, e_b, vh, vh_b)

        def back_row(h, r, ri, start, e_t, e_b, vh, vh_b):
            rr = r % 8
            r8 = r // 8
            if rr == 0:
                acc["o"] = po.nxt()
            o_t, o_b = acc["o"]
            for c in range(4):
                g = start + 2 * c
                vi = g // 2 if g % 2 == 0 else 32 + (g - 1) // 2
                S.op("pe", lambda e, c=c, vi=vi: e.matmul(o_t[:, rr * 64:(rr + 1) * 64], lhsT=vh[:, vi, :], rhs=e_t[:, (ri * 4 + c) * 64:(ri * 4 + c + 1) * 64], start=(c == 0), stop=(c == 3)),
                     reads=[vh_b, e_b], writes=[o_b])
            if rr == 7:
                rd_t, rd_b = rdp.nxt()
                S.op("dve", lambda e: e.reciprocal(out=rd_t[64:128, :], in_=o_t[64:128, :]), reads=[o_b], writes=[rd_b])
                y_t, y_b = yp.nxt()
                S.op("dve", lambda e: e.tensor_tensor(out=y_t[:, :], in0=o_t[0:64, :], in1=rd_t[64:128, :], op=ALU.mult), reads=[o_b, rd_b], writes=[y_b])
                S.op("sp", lambda e: e.dma_start(out=ynaT[h * 64:(h + 1) * 64, r8 * 512:(r8 + 1) * 512], in_=y_t[:, :]), reads=[y_b], writes=[C.buf("ynaT")], dma=True)

        NIT = len(units)
        for it in range(NIT + SK):
            if it < NIT:
                front(it)
            if it - SK >= 0:
                back(it - SK)
        if wprep is not None:
            wprep_flush()
        C.end_stage()


def na_host_tables(rpb):
    cols = np.arange(64)
    col_start = np.clip(cols - 8, 0, 64 - 16)
    valid = (cols[None, :] >= col_start[:, None]) & (cols[None, :] < col_start[:, None] + 16)
    col_idx = np.clip(cols[None, :] - cols[:, None] + 15, 0, 30)
    t = rpb[:, :, col_idx]
    t = np.transpose(t, (0, 3, 1, 2))
    rpbT = np.ascontiguousarray(np.concatenate([t[:, :, 0:14, :], t[:, :, 1:15, :]], axis=1)).astype(np.float32)
    m = np.where(valid.T, 0.0, -30000.0).astype(np.float32)
    m2 = np.concatenate([m, m], axis=0)
    maskc = np.ascontiguousarray(np.broadcast_to(m2[:, None, :], (128, 14, 64))).astype(np.float32)
    return rpbT, maskc


def stage_fnet(C, sc, ccs, csT, ssT, yfT):
    nc, S = C.nc, C.S
    ufT = sc["ufT"]
    uf_b = C.buf("ufT")
    SB = 256
    HN = T // 2
    NT2 = HN // 128
    with contextlib.ExitStack() as st:
        ccf = st.enter_context(nc.sbuf_tensor(U("ccf"), [128, 2, 2, 256], F32))
        ccb = st.enter_context(nc.sbuf_tensor(U("ccb"), [128, 2, 2, 256], BF16))
        ccf_b, ccb_b = Buf("ccf"), Buf("ccb")
        for m in range(2):
            S.op("sp", lambda e, m=m: e.dma_start(out=ccf[:, m, :, :], in_=ccs[m].rearrange("(kc p) n -> p kc n", p=128)), writes=[ccf_b], dma=True)
        S.op("dve", lambda e: e.tensor_copy(out=ccb[:], in_=ccf[:]), reads=[ccf_b], writes=[ccb_b])
        ugp = Rot(nc, st, "ug", 2, [128, 2, T], BF16)
        upm = st.enter_context(nc.sbuf_tensor(U("upm"), [128, 2, 2, HN], BF16))
        upm_b = Buf("upm")
        gcs = st.enter_context(nc.sbuf_tensor(U("gcs"), [128, 2, NT2, 256], BF16))
        gcs_b = Buf("gcs")
        e2 = st.enter_context(nc.sbuf_tensor(U("e2"), [128, 256], BF16))
        e2_b = Buf("e2")
        S.op("pool", lambda e: e.memset(e2[:, :], 0.0), writes=[e2_b])
        csp = Rot(nc, st, "csb", 3, [128, NT2, SB], BF16)
        ssp = Rot(nc, st, "ssb", 3, [128, NT2, SB], BF16)
        cxp = Rot(nc, st, "cxb", 3, [128, SB], BF16)
        yst = Rot(nc, st, "yst", 2, [128, SB], BF16)
        pa = Rot(nc, st, "pa", 3, [128, 512], F32, psum=True)
        pb = Rot(nc, st, "pb", 4, [128, 512], F32, psum=True)
        px = Rot(nc, st, "px", 1, [128, 512], F32, psum=True)
        for g in range(4):
            ug, ug_b = ugp.nxt()
            S.op("sp", lambda e, g=g, ug=ug: e.dma_start(out=ug[:, :, :], in_=ufT[g * 256:(g + 1) * 256, :].rearrange("(kc p) t -> p kc t", p=128)), reads=[uf_b], writes=[ug_b], dma=True)
            for kc in range(2):
                S.op("dve", lambda e, kc=kc, ug=ug: e.tensor_tensor(out=upm[:, 0, kc, 1:HN], in0=ug[:, kc, 1:HN], in1=ug[:, kc, T - 1:HN:-1], op=ALU.add), reads=[ug_b], writes=[upm_b])
                S.op("pool", lambda e, kc=kc, ug=ug: e.tensor_tensor(out=upm[:, 1, kc, 1:HN], in0=ug[:, kc, 1:HN], in1=ug[:, kc, T - 1:HN:-1], op=ALU.subtract), reads=[ug_b], writes=[upm_b])
                S.op("dve", lambda e, kc=kc, ug=ug: e.tensor_copy(out=upm[:, 0, kc, 0:1], in_=ug[:, kc, 0:1]), reads=[ug_b], writes=[upm_b])
                S.op("pool", lambda e, kc=kc: e.memset(upm[:, 1, kc, 0:1], 0.0), writes=[upm_b])
            x_t, x_b = px.nxt()
            for kc in range(2):
                S.op("pe", lambda e, kc=kc, ug=ug: e.matmul(x_t[0:1, 0:256], lhsT=ug[:, kc, HN:HN + 1], rhs=ccb[:, 0, kc, :], start=(kc == 0), stop=(kc == 1)),
                     reads=[ug_b, ccb_b], writes=[x_b])
            S.op("act", lambda e: e.copy(out=e2[0:1, :], in_=x_t[0:1, 0:256]), reads=[x_b], writes=[e2_b])
            for tt in range(NT2):
                p_t, p_b = pa.nxt()
                for m in range(2):
                    for kc in range(2):
                        S.op("pe", lambda e, p_t=p_t, m=m, kc=kc, tt=tt: e.matmul(p_t[:, m * 256:(m + 1) * 256], lhsT=upm[:, m, kc, tt * 128:(tt + 1) * 128], rhs=ccb[:, m, kc, :], start=(kc == 0), stop=(kc == 1)),
                             reads=[upm_b, ccb_b], writes=[p_b])
                S.op("act", lambda e, p_t=p_t, tt=tt: e.copy(out=gcs[:, :, tt, :], in_=p_t[:, :].rearrange("p (m c) -> p m c", m=2)), reads=[p_b], writes=[gcs_b])
            blk = {}

            def prep(sb):
                cs_t, cs_b = csp.nxt()
                ss_t, ss_b = ssp.nxt()
                cx_t, cx_b = cxp.nxt()
                S.op("sp", lambda e: e.dma_start(out=cs_t[:, :, :], in_=csT[0:HN, sb * SB:(sb + 1) * SB].rearrange("(tt p) n -> p tt n", p=128)), writes=[cs_b], dma=True)
                S.op("sp", lambda e: e.dma_start(out=ss_t[:, :, :], in_=ssT[0:HN, sb * SB:(sb + 1) * SB].rearrange("(tt p) n -> p tt n", p=128)), writes=[ss_b], dma=True)
                S.op("sp", lambda e: e.dma_start(out=cx_t[:, :], in_=csT[HN:HN + 128, sb * SB:(sb + 1) * SB]), writes=[cx_b], dma=True)
                blk[sb] = (cs_t, cs_b, ss_t, ss_b, cx_t, cx_b)

            def compute(sb, g=g):
                cs_t, cs_b, ss_t, ss_b, cx_t, cx_b = blk.pop(sb)
                for half in range(2):
                    p_t, p_b = pb.nxt()
                    for tt in range(NT2):
                        S.op("pe", lambda e, p_t=p_t, tt=tt, half=half: e.matmul(p_t[:, :SB], lhsT=gcs[:, 0, tt, half * 128:(half + 1) * 128], rhs=cs_t[:, tt, :], start=(tt == 0), stop=False),
                             reads=[gcs_b, cs_b], writes=[p_b])
                        S.op("pe", lambda e, p_t=p_t, tt=tt, half=half: e.matmul(p_t[:, :SB], lhsT=gcs[:, 1, tt, half * 128:(half + 1) * 128], rhs=ss_t[:, tt, :], start=False, stop=False),
                             reads=[gcs_b, ss_b], writes=[p_b])
                    S.op("pe", lambda e, p_t=p_t, half=half: e.matmul(p_t[:, :SB], lhsT=e2[:, half * 128:(half + 1) * 128], rhs=cx_t[:, :], start=False, stop=True),
                         reads=[e2_b, cx_b], writes=[p_b])
                    y_t, y_b = yst.nxt()
                    S.op("dve", lambda e, y_t=y_t, p_t=p_t: e.tensor_copy(out=y_t[:, :], in_=p_t[:, :SB]), reads=[p_b], writes=[y_b])
                    S.op("sp", lambda e, y_t=y_t, half=half: e.dma_start(out=yfT[g * 256 + half * 128: g * 256 + (half + 1) * 128, sb * SB:(sb + 1) * SB], in_=y_t[:, :]),
                         reads=[y_b], writes=[C.buf("yfT")], dma=True)

            pipeline(T // SB, prep, compute, depth=2)
        C.end_stage()


def fnet_host_consts():
    import ml_dtypes
    n = np.arange(256, dtype=np.float64)
    a = 2 * np.pi * np.outer(n, n) / 256.0
    ccs = np.stack([np.cos(a) / 16.0, -np.sin(a) / 16.0]).astype(np.float32)
    s = np.arange(T, dtype=np.int64)
    ph = (np.outer(s, s) % T).astype(np.float64) * (2 * np.pi / T)
    csT = (np.cos(ph) / 64.0).astype(np.float32).astype(ml_dtypes.bfloat16)
    ssT = (np.sin(ph) / 64.0).astype(np.float32).astype(ml_dtypes.bfloat16)
    return ccs, csT, ssT


def stage_merge(C, sc, w_branch, mergedT, wbr_c=None):
    nc, S = C.nc, C.S
    TQ = 1024
    ysrc = [(sc["ynaT"], C.buf("ynaT")), (sc["ymlaT"], C.buf("ymlaT")), (sc["yfT"], C.buf("yfT"))]
    gT, gT_b = sc["gT"], C.buf("gT")
    with contextlib.ExitStack() as st:
        yb = st.enter_context(nc.sbuf_tensor(U("yb"), [128, 24, TQ], BF16))
        yb_b = Buf("yb")
        wst = Rot(nc, st, "wst", 3, [128, 24, 128], F32)
        wbf = Rot(nc, st, "wbf", 3, [128, 24, 128], BF16)
        gtp = Rot(nc, st, "gt", 6, [128, 3, 512], BF16)
        m0p = Rot(nc, st, "m0", 2, [128, 512], F32)
        m1p = Rot(nc, st, "m1", 2, [128, 512], F32)
        m2p = Rot(nc, st, "m2", 2, [128, 512], F32)
        mo = Rot(nc, st, "mo", 2, [128, TQ], BF16)
        pp = Rot(nc, st, "pp", 6, [128, 512], F32, psum=True)
        jobs = [(tq, dc) for tq in range(T // TQ) for dc in range(16)]
        wjob = {}

        def prep(i):
            tq, dc = jobs[i]
            wb_t, wb_b = wbf.nxt()
            if wbr_c is not None:
                S.op("sp", lambda e: e.dma_start(out=wb_t[:, :, :].rearrange("p a b -> p (a b)"), in_=wbr_c[dc]), reads=[C.buf("wbr_c")], writes=[wb_b], dma=True)
            else:
                ws_t, ws_b = wst.nxt()
                for b in range(3):
                    S.op("sp", lambda e, b=b: e.dma_start(out=ws_t[:, b * 8:(b + 1) * 8, :], in_=w_branch[b, :, dc * 128:(dc + 1) * 128].rearrange("(kc p) n -> p kc n", p=128)),
                         writes=[ws_b], dma=True)
                cast_op(C, wb_t[:, :, :], ws_t[:, :, :], [ws_b], [wb_b])
            gts = []
            for tb in range(TQ // 512):
                g_t, g_b = gtp.nxt()
                t0 = tq * TQ
                S.op("sp", lambda e, g_t=g_t, tb=tb, t0=t0: e.dma_start(
                    out=g_t[:, :, :], in_=gT[:, t0 + tb * 512:t0 + (tb + 1) * 512].rearrange("(b c p) t -> c p b t", b=3, p=128)[dc]),
                    reads=[gT_b], writes=[g_b], dma=True)
                gts.append((g_t, g_b))
            wjob[i] = (wb_t, wb_b, gts)

        def compute(i):
            tq, dc = jobs[i]
            t0 = tq * TQ
            if dc == 0:
                for b in range(3):
                    S.op("sp", lambda e, b=b: e.dma_start(out=yb[:, b * 8:(b + 1) * 8, :], in_=ysrc[b][0][:, t0:t0 + TQ].rearrange("(kc p) t -> p kc t", p=128)),
                         reads=[ysrc[b][1]], writes=[yb_b], dma=True)
            wb_t, wb_b, gts = wjob.pop(i)
            o_t, o_b = mo.nxt()
            for tb in range(TQ // 512):
                g_t, g_b = gts[tb]
                ps = []
                for b in range(3):
                    p_t, p_b = pp.nxt()
                    for kc in range(8):
                        S.op("pe", lambda e, p_t=p_t, b=b, kc=kc, tb=tb: e.matmul(p_t[:, :], lhsT=wb_t[:, b * 8 + kc, :], rhs=yb[:, b * 8 + kc, tb * 512:(tb + 1) * 512], start=(kc == 0), stop=(kc == 7)),
                             reads=[wb_b, yb_b], writes=[p_b])
                    ps.append((p_t, p_b))
                ms = []
                for b, mp in enumerate((m0p, m1p, m2p)):
                    m_t, m_b = mp.nxt()
                    S.op("dve", lambda e, m_t=m_t, b=b, g_t=g_t, p_t=ps[b][0]: e.tensor_tensor(out=m_t[:, :], in0=p_t[:, :], in1=g_t[:, b, :], op=ALU.mult),
                         reads=[ps[b][1], g_b], writes=[m_b])
                    ms.append((m_t, m_b))
                S.op("pool", lambda e, a=ms[0][0], b_=ms[1][0]: e.tensor_tensor(out=a[:, :], in0=a[:, :], in1=b_[:, :], op=ALU.add),
                     reads=[ms[1][1]], writes=[ms[0][1]])
                S.op("pool", lambda e, a=ms[0][0], c_=ms[2][0], tb=tb: e.tensor_tensor(out=o_t[:, tb * 512:(tb + 1) * 512], in0=a[:, :], in1=c_[:, :], op=ALU.add),
                     reads=[ms[0][1], ms[2][1]], writes=[o_b])
            S.op("sp", lambda e: e.dma_start(out=mergedT[dc * 128:(dc + 1) * 128, t0:t0 + TQ], in_=o_t[:, :]), reads=[o_b], writes=[C.buf("mergedT")], dma=True)

        pipeline(len(jobs), prep, compute, depth=2)
        C.end_stage()


def stage_outproj(C, mergedT, w_o, h_in, h_in_b, h_out, h_out_b, wo_c=None):
    nc, S = C.nc, C.S
    TQ = 1024
    mg_b = C.buf("mergedT")
    with contextlib.ExitStack() as st:
        mt = st.enter_context(nc.sbuf_tensor(U("mt"), [128, 16, TQ], BF16))
        mt_b = Buf("mt")
        hacc = st.enter_context(nc.sbuf_tensor(U("hacc"), [128, 8, D], F32))
        hacc_b = [Buf("hacc%d" % i) for i in range(8)]
        wst = Rot(nc, st, "wst", 3, [128, 16, 128], F32)
        wow = Rot(nc, st, "wow", 2, [128, 16, 512], BF16)
        wo_cur = {}
        pp = Rot(nc, st, "pp", 4, [128, 512], F32, psum=True)
        jobs = [(tq, cc) for tq in range(T // TQ) for cc in range(16)]
        wjob = {}

        def prep(i):
            tq, cc = jobs[i]
            q = cc % 4
            if q == 0:
                wo_cur["t"] = wow.nxt()
            wb_t, wb_b = wo_cur["t"]
            if wo_c is not None:
                S.op("sp", lambda e: e.dma_start(out=wb_t[:, :, q * 128:(q + 1) * 128], in_=wo_c[cc].rearrange("p (a b) -> p a b", b=128)), reads=[C.buf("wo_c")], writes=[wb_b], dma=True)
            else:
                ws_t, ws_b = wst.nxt()
                S.op("sp", lambda e: e.dma_start(out=ws_t[:, :, :], in_=w_o[:, cc * 128:(cc + 1) * 128].rearrange("(kc p) n -> p kc n", p=128)), writes=[ws_b], dma=True)
                cast_op(C, wb_t[:, :, q * 128:(q + 1) * 128], ws_t[:, :, :], [ws_b], [wb_b])
            wjob[i] = (wb_t, wb_b)

        def compute(i):
            tq, cc = jobs[i]
            t0 = tq * TQ
            if cc == 0:
                S.op("sp", lambda e: e.dma_start(out=mt[:, :, :], in_=mergedT[:, t0:t0 + TQ].rearrange("(kc p) t -> p kc t", p=128)), reads=[mg_b], writes=[mt_b], dma=True)
                for tt in range(8):
                    S.op("sp", lambda e, tt=tt: e.dma_start(out=hacc[:, tt, :], in_=h_in[t0 + tt * 128:t0 + (tt + 1) * 128, :]), reads=[h_in_b], writes=[hacc_b[tt]], dma=True)
            wb_t, wb_b = wjob.pop(i)
            if cc % 4 == 3:
                c4 = cc // 4
                for tt in range(8):
                    p_t, p_b = pp.nxt()
                    for kc in range(16):
                        S.op("pe", lambda e, p_t=p_t, kc=kc, tt=tt: e.matmul(p_t[:, :], lhsT=mt[:, kc, tt * 128:(tt + 1) * 128], rhs=wb_t[:, kc, :], start=(kc == 0), stop=(kc == 15)),
                             reads=[wb_b, mt_b], writes=[p_b])
                    S.op("dve", lambda e, p_t=p_t, tt=tt: e.tensor_tensor(out=hacc[:, tt, c4 * 512:(c4 + 1) * 512], in0=hacc[:, tt, c4 * 512:(c4 + 1) * 512], in1=p_t[:, :], op=ALU.add),
                         reads=[p_b], writes=[hacc_b[tt]])
            if cc == 15:
                for tt in range(8):
                    S.op("sp", lambda e, tt=tt: e.dma_start(out=h_out[t0 + tt * 128:t0 + (tt + 1) * 128, :], in_=hacc[:, tt, :]), reads=[hacc_b[tt]], writes=[h_out_b], dma=True)

        pipeline(len(jobs), prep, compute, depth=2)
        C.end_stage()


def stage_route(C, h_ap, h_b, gain_ap, w_router, ident_f, xn2, idx_d, gate_d, NE=16, CAP=512):
    nc, S = C.nc, C.S
    with contextlib.ExitStack() as st:
        idf, idfb, idb, idbb = load_consts(C, st, ident_f)
        pools = norm_pools(C, st, nh=3)
        gain_t, gain_b = load_gain(C, st, "gain2", gain_ap, D)
        wr = st.enter_context(nc.sbuf_tensor(U("wr"), [128, 16, NE], F32))
        wr_b = Buf("wr")
        S.op("sp", lambda e: e.dma_start(out=wr[:, :, :], in_=w_router.rearrange("(kc p) n -> p kc n", p=128)), writes=[wr_b], dma=True)
        xf = Rot(nc, st, "xf", 3, [128, D], F32)
        xb = Rot(nc, st, "xb", 4, [128, D], BF16)
        xT = Rot(nc, st, "xT", 3, [128, 16, 128], F32)
        sm = Rot(nc, st, "sm", 4, [128, 8], F32)
        ex = Rot(nc, st, "ex", 4, [128, NE], F32)
        af = Rot(nc, st, "af", 4, [128, NE], F32)
        affT = st.enter_context(nc.sbuf_tensor(U("affT"), [NE, T], F32))
        affT2 = st.enter_context(nc.sbuf_tensor(U("affT2"), [NE, T], F32))
        affT_b, affT2_b = Buf("affT"), Buf("affT2")
        vals = st.enter_context(nc.sbuf_tensor(U("vals"), [NE, CAP], F32))
        idx = st.enter_context(nc.sbuf_tensor(U("idx"), [NE, CAP], U32))
        vals_b, idx_b = Buf("vals"), Buf("idx")
        ptr = Rot(nc, st, "ptr", 3, [128, 4, 128], F32, psum=True)
        pl = Rot(nc, st, "pl", 3, [128, 512], F32, psum=True)
        pt2 = Rot(nc, st, "pt2", 2, [128, 512], F32, psum=True)
        lg = {}

        def front(tt):
            x_t, x_b = xf.nxt()
            rms_tile(C, pools, h_ap[tt * 128:(tt + 1) * 128, :], gain_t, gain_b, x_t[:], x_b, D, [h_b])
            xb_t, xb_b = xb.nxt()
            S.op("act", lambda e: e.copy(out=xb_t[:, :], in_=x_t[:, :]), reads=[x_b], writes=[xb_b])
            xT_t, xT_b = xT.nxt()
            for g in range(4):
                p_t, p_b = ptr.nxt()
                for j in range(4):
                    kc = g * 4 + j
                    S.op("pe", lambda e, p_t=p_t, j=j, kc=kc: e.transpose(out=p_t[:, j, :], in_=x_t[:, kc * 128:(kc + 1) * 128], identity=idf[:]),
                         reads=[x_b, idfb], writes=[p_b])
                S.op("dve", lambda e, p_t=p_t, g=g: e.tensor_copy(out=xT_t[:, g * 4:(g + 1) * 4, :], in_=p_t[:, :, :]), reads=[p_b], writes=[xT_b])
            l_t, l_b = pl.nxt()
            for kc in range(16):
                S.op("pe", lambda e, kc=kc: e.matmul(l_t[:, :NE], lhsT=xT_t[:, kc, :], rhs=wr[:, kc, :], start=(kc == 0), stop=(kc == 15)),
                     reads=[xT_b, wr_b], writes=[l_b])
            lg[tt] = (l_t, l_b, xb_t, xb_b)

        def back(tt):
            l_t, l_b, xb_t, xb_b = lg.pop(tt)
            S.op("sp", lambda e: e.dma_start(out=xn2[tt * 128:(tt + 1) * 128, :], in_=xb_t[:, :]), reads=[xb_b], writes=[C.buf("xn2")], dma=True)
            s_t, s_b = sm.nxt()
            S.op("dve", lambda e: e.tensor_reduce(out=s_t[:, 0:1], in_=l_t[:, :NE], axis=AX.X, op=ALU.max, negate=True), reads=[l_b], writes=[s_b])
            e_t, e_b = ex.nxt()
            S.op("act", lambda e: e.activation(out=e_t[:, :], in_=l_t[:, :NE], func=AF.Exp, bias=s_t[:, 0:1], accum_out=s_t[:, 1:2]),
                 reads=[l_b, s_b], writes=[e_b, s_b])
            S.op("dve", lambda e: e.reciprocal(out=s_t[:, 2:3], in_=s_t[:, 1:2]), reads=[s_b], writes=[s_b])
            a_t, a_b = af.nxt()
            S.op("dve", lambda e: e.tensor_scalar(out=a_t[:, :], in0=e_t[:, :], scalar1=s_t[:, 2:3], scalar2=None, op0=ALU.mult), reads=[e_b, s_b], writes=[a_b])
            q_t, q_b = pt2.nxt()
            S.op("pe", lambda e: e.transpose(out=q_t[:NE, :128], in_=a_t[:, :], identity=idf[:]), reads=[a_b, idfb], writes=[q_b])
            S.op("dve", lambda e: e.tensor_copy(out=affT[:, tt * 128:(tt + 1) * 128], in_=q_t[:NE, :128]), reads=[q_b], writes=[affT_b])

        NT = T // 128
        SKR = 2
        for tt in range(NT + SKR):
            if tt < NT:
                front(tt)
            if tt - SKR >= 0:
                back(tt - SKR)
        cur, cur_b, oth, oth_b = affT, affT_b, affT2, affT2_b
        for r in range(CAP // 8):
            S.op("dve", lambda e, cur=cur, r=r: e.max(out=vals[:, r * 8:(r + 1) * 8], in_=cur[:, :]), reads=[cur_b], writes=[vals_b])
            S.op("dve", lambda e, cur=cur, r=r: e.max_index(out=idx[:, r * 8:(r + 1) * 8], in_max=vals[:, r * 8:(r + 1) * 8], in_values=cur[:, :]), reads=[cur_b, vals_b], writes=[idx_b])
            if r < CAP // 8 - 1:
                S.op("dve", lambda e, cur=cur, oth=oth, r=r: e.match_replace(out=oth[:, :], in_to_replace=vals[:, r * 8:(r + 1) * 8], in_values=cur[:, :], imm_value=-1.0),
                     reads=[cur_b, vals_b], writes=[oth_b])
                cur, cur_b, oth, oth_b = oth, oth_b, cur, cur_b
        S.op("sp", lambda e: e.dma_start(out=idx_d[:, :], in_=idx[:, :]), reads=[idx_b], writes=[C.buf("idx_d")], dma=True)
        S.op("sp", lambda e: e.dma_start(out=gate_d[:, :], in_=vals[:, :]), reads=[vals_b], writes=[C.buf("gate_d")], dma=True)
        C.end_stage()


def stage_experts(C, h_ap, h_b, xn2, idx_d, gate_d, w_g, w_u, w_d, ident_f, NE=16, CAP=512):
    nc, S = C.nc, C.S
    NJ = CAP // 128
    with contextlib.ExitStack() as st:
        idf, idfb, idb, idbb = load_consts(C, st, ident_f)
        idxc = st.enter_context(nc.sbuf_tensor(U("idxc"), [128, NE * NJ], U32))
        gatec = st.enter_context(nc.sbuf_tensor(U("gatec"), [128, NE * NJ], F32))
        idxc_b, gatec_b = Buf("idxc"), Buf("gatec")
        S.op("sp", lambda e: e.dma_start(out=idxc[:, :].rearrange("p (e j) -> p e j", j=NJ), in_=idx_d.rearrange("e (j p) -> p e j", p=128), allow_slow_non_contiguous=True),
             reads=[C.buf("idx_d")], writes=[idxc_b], dma=True)
        S.op("sp", lambda e: e.dma_start(out=gatec[:, :].rearrange("p (e j) -> p e j", j=NJ), in_=gate_d.rearrange("e (j p) -> p e j", p=128), allow_slow_non_contiguous=True),
             reads=[C.buf("gate_d")], writes=[gatec_b], dma=True)
        xg = Rot(nc, st, "xg", 4, [128, D], BF16)
        xeTp = Rot(nc, st, "xeT", 2, [128, 16, CAP], BF16)
        hT = st.enter_context(nc.sbuf_tensor(U("hT"), [128, 16, CAP], BF16))
        hT_b = Buf("hT")
        ye = st.enter_context(nc.sbuf_tensor(U("ye"), [128, NJ, D], F32))
        ye_b = [Buf("ye%d" % j) for j in range(NJ)]
        wst = Rot(nc, st, "wst", 3, [128, 16, 128], F32)
        wbf = Rot(nc, st, "wbf", 3, [128, 16, 128], BF16)
        sg = Rot(nc, st, "sg", 2, [128, CAP], F32)
        wdw = Rot(nc, st, "wdw", 2, [128, 16, 512], BF16)
        wd_cur = {}
        ptr = Rot(nc, st, "ptr", 2, [128, 8, 128], BF16, psum=True)
        pg = Rot(nc, st, "pg", 2, [128, 512], F32, psum=True)
        pu = Rot(nc, st, "pu", 2, [128, 512], F32, psum=True)
        pd = Rot(nc, st, "pd", 2, [128, 512], F32, psum=True)
        xn2_b = C.buf("xn2")

        def load_w(src):
            ws_t, ws_b = wst.nxt()
            S.op("sp", lambda e: e.dma_start(out=ws_t[:, :, :], in_=src.rearrange("(kc p) n -> p kc n", p=128)), writes=[ws_b], dma=True)
            wb_t, wb_b = wbf.nxt()
            cast_op(C, wb_t[:, :, :], ws_t[:, :, :], [ws_b], [wb_b])
            return wb_t, wb_b

        xe_of = {}

        def gather(ex):
            xeT, xeT_b = xeTp.nxt()
            xe_of[ex] = (xeT, xeT_b)
            for j in range(NJ):
                col = ex * NJ + j
                x_t, x_b = xg.nxt()
                S.op("pool", lambda e, x_t=x_t, col=col: e.indirect_dma_start(out=x_t[:, :], out_offset=None, in_=xn2[:, :], in_offset=bass.IndirectOffsetOnAxis(ap=idxc[:, col:col + 1], axis=0)),
                     reads=[xn2_b, idxc_b], writes=[x_b], dma=True)
                for g in range(2):
                    p_t, p_b = ptr.nxt()
                    for jj in range(8):
                        kc = g * 8 + jj
                        S.op("pe", lambda e, p_t=p_t, jj=jj, kc=kc, x_t=x_t: e.transpose(out=p_t[:, jj, :], in_=x_t[:, kc * 128:(kc + 1) * 128], identity=idb[:]),
                             reads=[x_b, idbb], writes=[p_b])
                    S.op("dve", lambda e, p_t=p_t, g=g, j=j: e.tensor_copy(out=xeT[:, g * 8:(g + 1) * 8, j * 128:(j + 1) * 128], in_=p_t[:]), reads=[p_b], writes=[xeT_b])

        jobs = []
        for ex in range(NE):
            for fc in range(16):
                jobs += [(ex, "g", fc), (ex, "u", fc)]
            jobs += [(ex, "d", dc) for dc in range(16)]
        wjob = {}
        sil = {}

        def prep(i):
            ex, kind, c = jobs[i]
            if kind == "d":
                q = c % 4
                if q == 0:
                    wd_cur["t"] = wdw.nxt()
                wt, wt_b = wd_cur["t"]
                ws_t, ws_b = wst.nxt()
                S.op("sp", lambda e: e.dma_start(out=ws_t[:, :, :], in_=w_d[ex, :, c * 128:(c + 1) * 128].rearrange("(kc p) n -> p kc n", p=128)), writes=[ws_b], dma=True)
                cast_op(C, wt[:, :, q * 128:(q + 1) * 128], ws_t[:, :, :], [ws_b], [wt_b])
                wjob[i] = (wt, wt_b)
                return
            src = {"g": w_g, "u": w_u}[kind]
            wjob[i] = load_w(src[ex, :, c * 128:(c + 1) * 128])

        def compute(i):
            ex, kind, c = jobs[i]
            if kind == "g":
                xeT, xeT_b = xe_of[ex]
                wg_t, wg_b = wjob.pop(i)
                g_t, g_b = pg.nxt()
                for kc in range(16):
                    S.op("pe", lambda e, kc=kc: e.matmul(g_t[:, :CAP], lhsT=wg_t[:, kc, :], rhs=xeT[:, kc, :], start=(kc == 0), stop=(kc == 15)),
                         reads=[wg_b, xeT_b], writes=[g_b])
                s_t, s_b = sg.nxt()
                S.op("act", lambda e: e.activation(out=s_t[:, :], in_=g_t[:, :CAP], func=AF.Silu), reads=[g_b], writes=[s_b])
                sil[(ex, c)] = (s_t, s_b)
            elif kind == "u":
                xeT, xeT_b = xe_of[ex]
                wu_t, wu_b = wjob.pop(i)
                u_t, u_b = pu.nxt()
                for kc in range(16):
                    S.op("pe", lambda e, kc=kc: e.matmul(u_t[:, :CAP], lhsT=wu_t[:, kc, :], rhs=xeT[:, kc, :], start=(kc == 0), stop=(kc == 15)),
                         reads=[wu_b, xeT_b], writes=[u_b])
                s_t, s_b = sil.pop((ex, c))
                S.op("dve", lambda e: e.tensor_tensor(out=hT[:, c, :], in0=u_t[:, :CAP], in1=s_t[:, :], op=ALU.mult), reads=[u_b, s_b], writes=[hT_b])
            else:
                dc = c
                if dc == 0 and ex + 1 < NE:
                    gather(ex + 1)
                wd_t, wd_b = wjob.pop(i)
                if dc % 4 == 3:
                    d4 = dc // 4
                    for j in range(NJ):
                        col = ex * NJ + j
                        p_t, p_b = pd.nxt()
                        for fc in range(16):
                            S.op("pe", lambda e, p_t=p_t, j=j, fc=fc: e.matmul(p_t[:, :], lhsT=hT[:, fc, j * 128:(j + 1) * 128], rhs=wd_t[:, fc, :], start=(fc == 0), stop=(fc == 15)),
                                 reads=[wd_b, hT_b], writes=[p_b])
                        S.op("act", lambda e, p_t=p_t, j=j, col=col: e.activation(out=ye[:, j, d4 * 512:(d4 + 1) * 512], in_=p_t[:, :], func=AF.Copy, scale=gatec[:, col:col + 1]),
                             reads=[p_b, gatec_b], writes=[ye_b[j]])
                if dc == 15:
                    for j in range(NJ):
                        col = ex * NJ + j
                        S.op("pool", lambda e, j=j, col=col: e.indirect_dma_start(out=h_ap[:, :], out_offset=bass.IndirectOffsetOnAxis(ap=idxc[:, col:col + 1], axis=0), in_=ye[:, j, :], in_offset=None, compute_op=ALU.add),
                             reads=[ye_b[j], idxc_b], writes=[h_b], dma=True)

        gather(0)
        pipeline(len(jobs), prep, compute, depth=2)
        C.end_stage()


def stage_final(C, h_ap, h_b, gain_ap, y_ap, y_b):
    nc, S = C.nc, C.S
    with contextlib.ExitStack() as st:
        pools = norm_pools(C, st)
        gain_t, gain_b = load_gain(C, st, "gainf", gain_ap, D)
        of = Rot(nc, st, "of", 2, [128, D], F32)
        for tt in range(T // 128):
            o_t, o_b = of.nxt()
            rms_tile(C, pools, h_ap[tt * 128:(tt + 1) * 128, :], gain_t, gain_b, o_t[:], o_b, D, [h_b])
            S.op("sp", lambda e, o_t=o_t, tt=tt: e.dma_start(out=y_ap[tt * 128:(tt + 1) * 128, :], in_=o_t[:, :]), reads=[o_b], writes=[y_b], dma=True)
        C.end_stage()


NCORES = 4
NB = 4 // NCORES
DEPTH = 2
_CACHE = {}


def build_program():
    nc = bass.Bass("TRN2", target_bir_lowering=False)
    def inp(name, shape, dt=F32):
        return nc.dram_tensor(name, list(shape), dt, kind="ExternalInput").ap()
    x = inp("x", [NB * T, D])
    w_in = inp("w_in", [DEPTH, D, INC])
    b_gate = inp("b_gate", [DEPTH, 6144])
    w_uq = inp("w_uq", [DEPTH, 448, 1536])
    q_norm = inp("q_norm", [DEPTH, 448])
    w_ukv = inp("w_ukv", [DEPTH, 160, 2048])
    kv_norm = inp("kv_norm", [DEPTH, 160])
    rpbT = inp("rpbT", [DEPTH, 16, 128, 14, 64])
    maskc = inp("maskc", [128, 14, 64])
    w_branch = inp("w_branch", [DEPTH, 3, 1024, 2048])
    w_o = inp("w_o", [DEPTH, D, D])
    norm_mix = inp("norm_mix", [DEPTH, D])
    norm_moe = inp("norm_moe", [DEPTH, D])
    w_router = inp("w_router", [DEPTH, D, 16])
    w_g = inp("w_exp_gate", [DEPTH, 16, D, D])
    w_u = inp("w_exp_up", [DEPTH, 16, D, D])
    w_d = inp("w_exp_down", [DEPTH, 16, D, D])
    norm_final = inp("norm_final", [D])
    ident = inp("ident", [128, 128])
    cos2T = inp("cos2T", [64, T])
    sin2T = inp("sin2T", [64, T])
    ccs = inp("ccs", [2, 256, 256])
    csT = inp("csT", [T, T], BF16)
    ssT = inp("ssT", [T, T], BF16)
    y = nc.dram_tensor("y", [NB * T, D], F32, kind="ExternalOutput").ap()
    with contextlib.ExitStack() as es:
        C = Ctx(nc, es)
        sc = {"qkT": C.dram("qkT", [2048, T], BF16), "vna": C.dram("vna", [T, 1024], BF16), "cT": C.dram("cT", [672, T], F32),
              "ufT": C.dram("ufT", [1024, T], BF16), "gT": C.dram("gT", [6144, T], BF16),
              "ynaT": C.dram("ynaT", [1024, T], BF16), "ymlaT": C.dram("ymlaT", [1024, T], BF16), "yfT": C.dram("yfT", [1024, T], BF16)}
        mergedT = C.dram("mergedT", [2048, T], BF16)
        wbr_c = C.dram("wbr_c", [16, 128, 24 * 128], BF16)
        wo_c = C.dram("wo_c", [16, 128, 16 * 128], BF16)
        hA = C.dram("hA", [T, D], F32)
        xn2 = C.dram("xn2", [T, D], BF16)
        idx_d = C.dram("idx_d", [16, 512], U32)
        gate_d = C.dram("gate_d", [16, 512], F32)
        hA_b = C.buf("hA")
        x_b = Buf("x")
        y_b = C.buf("y")
        for b in range(NB):
            xb = x[b * T:(b + 1) * T, :]
            for l in range(DEPTH):
                h_in, h_in_b = (xb, x_b) if l == 0 else (hA, hA_b)
                stage_inproj(C, h_in, h_in_b, norm_mix[l], w_in[l], b_gate[l], ident, sc)
                stage_na(C, sc, rpbT[l], maskc, sc["ynaT"], wprep=(w_branch[l], w_o[l], wbr_c, wo_c))
                stage_mla(C, sc, w_uq[l], q_norm[l], w_ukv[l], kv_norm[l], cos2T, sin2T, sc["ymlaT"])
                stage_fnet(C, sc, ccs, csT, ssT, sc["yfT"])
                stage_merge(C, sc, w_branch[l], mergedT, wbr_c=wbr_c)
                stage_outproj(C, mergedT, w_o[l], h_in, h_in_b, hA, hA_b, wo_c=wo_c)
                stage_route(C, hA, hA_b, norm_moe[l], w_router[l], ident, xn2, idx_d, gate_d)
                stage_experts(C, hA, hA_b, xn2, idx_d, gate_d, w_g[l], w_u[l], w_d[l], ident)
            stage_final(C, hA, hA_b, norm_final, y[b * T:(b + 1) * T, :], y_b)
    return nc


def rope_consts():
    pos = np.arange(T, dtype=np.float32)
    inv = (1.0 / (10000.0 ** (np.arange(0, 64, 2, dtype=np.float32) / 64))).astype(np.float32)
    ang = pos[:, None] * inv[None, :]
    cos, sin = np.cos(ang).astype(np.float32), np.sin(ang).astype(np.float32)
    cos2T = np.ascontiguousarray(np.concatenate([cos, cos], 1).T)
    sin2T = np.ascontiguousarray(np.concatenate([-sin, sin], 1).T)
    return cos2T, sin2T


def kernel(x, w_in, b_gate, w_uq, q_norm, w_ukv, kv_norm, na_rpb, w_branch, w_o,
           norm_mix, norm_moe, w_router, w_exp_gate, w_exp_up, w_exp_down, norm_final):
    f = lambda a: np.ascontiguousarray(np.asarray(a, dtype=np.float32))
    if "nc" not in _CACHE:
        _CACHE["nc"] = build_program()
        cos2T, sin2T = rope_consts()
        ccs, csT, ssT = fnet_host_consts()
        _CACHE["consts"] = dict(cos2T=cos2T, sin2T=sin2T, ccs=ccs, csT=csT, ssT=ssT, ident=np.eye(128, dtype=np.float32))
    nc = _CACHE["nc"]
    na_rpb = f(na_rpb)
    tabs = [na_host_tables(na_rpb[l]) for l in range(DEPTH)]
    rpbT = np.stack([t[0] for t in tabs])
    maskc = tabs[0][1]
    shared = dict(w_in=f(w_in), b_gate=f(b_gate), w_uq=f(w_uq), q_norm=f(q_norm), w_ukv=f(w_ukv), kv_norm=f(kv_norm),
                  rpbT=rpbT, maskc=maskc, w_branch=f(w_branch), w_o=f(w_o), norm_mix=f(norm_mix), norm_moe=f(norm_moe),
                  w_router=f(w_router), w_exp_gate=f(w_exp_gate), w_exp_up=f(w_exp_up), w_exp_down=f(w_exp_down),
                  norm_final=f(norm_final), **_CACHE["consts"])
    xf = f(x).reshape(4 * T, D)
    in_maps = []
    for c in range(NCORES):
        m = dict(shared)
        m["x"] = xf[c * NB * T:(c + 1) * NB * T]
        in_maps.append(m)
    res = run_bass_kernel_spmd(nc, in_maps, core_ids=list(range(NCORES)))
    out = np.concatenate([np.asarray(r["y"], dtype=np.float32) for r in res.results], axis=0)
    return out.reshape(4, T, D)
```

```python
import numpy as np
import concourse.bass as bass
import concourse.mybir as mybir
from concourse.bass_utils import run_bass_kernel_spmd

F32 = mybir.dt.float32
BF16 = mybir.dt.bfloat16
I32 = mybir.dt.int32
U32 = mybir.dt.uint32
U16 = mybir.dt.uint16
AF = mybir.ActivationFunctionType
ALU = mybir.AluOpType
AX = mybir.AxisListType


class Buf:
    __slots__ = ("name", "w", "r")

    def __init__(self, name=""):
        self.name = name
        self.w = []
        self.r = []


def _merge(tokens):
    d = {}
    for s, v in tokens:
        if d.get(s, 0) < v:
            d[s] = v
    return d


class Sched:
    ENG = ("pe", "act", "dve", "pool", "sp")

    def __init__(self, nc, es, n_dma_sems=40, rot=30000):
        self.nc = nc
        self.es = es
        self.rot = rot
        self.lists = {e: [] for e in self.ENG}
        self.sems = []
        self.cur = {}
        self.known = {e: {} for e in self.ENG}
        for e in self.ENG:
            self.cur[e] = [self._new_sem("e_" + e), 0]
        self.dma_pool = [[self._new_sem("d%d" % i), 0] for i in range(n_dma_sems)]
        self.dma_next = 0
        self.n_ops = 0

    def _new_sem(self, name):
        h = self.es.enter_context(self.nc.semaphore(name + "_%d" % len(self.sems)))
        self.sems.append(h)
        return len(self.sems) - 1

    def op(self, eng, fn, reads=(), writes=(), dma=False):
        deps = []
        for b in reads:
            deps += b.w
        for b in writes:
            deps += b.w
            deps += b.r
        tok_extra = None
        if dma:
            slot = self.dma_pool[self.dma_next]
            self.dma_next = (self.dma_next + 1) % len(self.dma_pool)
            if slot[1] > 0:
                deps.append((slot[0], 16 * slot[1]))
            slot[1] += 1
            token = (slot[0], 16 * slot[1])
            inc = (slot[0], 16)
        else:
            c = self.cur[eng]
            if c[1] >= self.rot:
                c[0] = self._new_sem("e_" + eng)
                c[1] = 0
            c[1] += 1
            token = (c[0], c[1])
            inc = (c[0], 1)
        need = _merge(deps)
        kn = self.known[eng]
        waits = []
        own = self.cur[eng][0]
        for s, v in need.items():
            if eng == "pe" and s == own and not dma:
                continue
            if kn.get(s, 0) >= v:
                continue
            kn[s] = v
            waits.append((s, v))
        self.lists[eng].append((waits, fn, inc))
        for b in writes:
            b.w = [token]
            b.r = []
        for b in reads:
            if b in writes:
                continue
            m = _merge(b.r + [token])
            b.r = list(m.items())
        self.n_ops += 1
        return token

    def wait_all(self, eng, bufs):
        deps = []
        for b in bufs:
            deps += b.w
        need = _merge(deps)
        waits = [(s, v) for s, v in need.items()]
        self.lists[eng].append((waits, None, None))

    def emit(self):
        nc = self.nc
        sems = self.sems
        lists = self.lists

        def run(engname, e):
            for waits, fn, inc in lists[engname]:
                for s, v in waits:
                    e.wait_ge(sems[s], v)
                if fn is not None:
                    ins = fn(e)
                    ins.then_inc(sems[inc[0]], inc[1])

        with nc.Block() as block:
            @block.tensor
            def _(e):
                run("pe", e)

            @block.scalar
            def _(e):
                run("act", e)

            @block.vector
            def _(e):
                run("dve", e)

            @block.gpsimd
            def _(e):
                run("pool", e)

            @block.sync
            def _(e):
                run("sp", e)


import contextlib

D = 2048
T = 4096
INC = 10912
EPS = 1e-6


_UID = [0]


def U(name):
    _UID[0] += 1
    return "%s_u%d" % (name, _UID[0])


class Rot:
    def __init__(self, nc, st, name, n, shape, dtype, psum=False):
        self.slots = []
        for i in range(n):
            if psum:
                t = st.enter_context(nc.psum_tensor(U("%s%d" % (name, i)), shape, dtype))
            else:
                t = st.enter_context(nc.sbuf_tensor(U("%s%d") % (name, i), shape, dtype))
            self.slots.append((t, Buf(name + str(i))))
        self.i = 0

    def nxt(self):
        s = self.slots[self.i]
        self.i = (self.i + 1) % len(self.slots)
        return s


class Ctx:
    def __init__(self, nc, es, debug_outs=()):
        self.nc = nc
        self.es = es
        self.S = Sched(nc, es)
        self.debug_outs = set(debug_outs)
        self.dbufs = {}
        self.cast_i = 0

    def dram(self, name, shape, dtype):
        kind = "ExternalOutput" if name in self.debug_outs else "Internal"
        t = self.nc.dram_tensor(name, list(shape), dtype, kind=kind).ap()
        return t

    def buf(self, key):
        if key not in self.dbufs:
            self.dbufs[key] = Buf(str(key))
        return self.dbufs[key]

    def end_stage(self):
        S = self.S
        waits = [(s[0], 16 * s[1]) for s in S.dma_pool if s[1] > 0]
        for e in ("sp", "pool", "act"):
            S.lists[e].append((list(waits), None, None))
        S.emit()
        S.lists = {e: [] for e in S.ENG}


def load_consts(C, st, ident_f):
    nc, S = C.nc, C.S
    idf = st.enter_context(nc.sbuf_tensor(U("idf"), [128, 128], F32))
    idb = st.enter_context(nc.sbuf_tensor(U("idb"), [128, 128], BF16))
    bf = Buf("idf")
    bb = Buf("idb")
    S.op("sp", lambda e: e.dma_start(out=idf[:], in_=ident_f[:, :]), writes=[bf], dma=True)
    S.op("dve", lambda e: e.tensor_copy(out=idb[:], in_=idf[:]), reads=[bf], writes=[bb])
    return idf, bf, idb, bb


def rms_tile(C, pools, src_ap, gain_t, gain_b, out_t, out_b, width, src_reads):
    nc, S = C.nc, C.S
    ht, hb = pools["h"].nxt()
    S.op("sp", lambda e: e.dma_start(out=ht[:, :width], in_=src_ap), reads=src_reads, writes=[hb], dma=True)
    jt, jb = pools["junk"].nxt()
    st_, sb_ = pools["stat"].nxt()
    S.op("act", lambda e: e.activation(out=jt[:, :width], in_=ht[:, :width], func=AF.Square, accum_out=st_[:, 0:1]),
         reads=[hb], writes=[jb, sb_])
    S.op("act", lambda e: e.activation(out=st_[:, 1:2], in_=st_[:, 0:1], func=AF.Sqrt, scale=1.0 / width, bias=pools["eps"][0][:, 0:1]),
         reads=[sb_, pools["eps"][1]], writes=[sb_])
    S.op("dve", lambda e: e.reciprocal(out=st_[:, 2:3], in_=st_[:, 1:2]), reads=[sb_], writes=[sb_])
    S.op("dve", lambda e: e.scalar_tensor_tensor(out=out_t, in0=ht[:, :width], scalar=st_[:, 2:3], in1=gain_t[:, :width],
                                                op0=ALU.mult, op1=ALU.mult),
         reads=[hb, sb_, gain_b], writes=[out_b])
    return ht, hb


def norm_pools(C, st, width=D, nh=2):
    nc, S = C.nc, C.S
    pools = {
        "h": Rot(nc, st, "nh", nh, [128, width], F32),
        "junk": Rot(nc, st, "nj", 1, [128, width], BF16),
        "stat": Rot(nc, st, "ns", 4, [128, 4], F32),
    }
    eps_t = st.enter_context(nc.sbuf_tensor(U("epsT"), [128, 1], F32))
    eb = Buf("eps")
    S.op("dve", lambda e: e.memset(eps_t[:], EPS), writes=[eb])
    pools["eps"] = (eps_t, eb)
    return pools


def load_gain(C, st, name, vec_ap, width):
    nc, S = C.nc, C.S
    g = st.enter_context(nc.sbuf_tensor(U(name), [128, width], F32))
    gb = Buf(name)
    S.op("sp", lambda e: e.dma_start(out=g[:], in_=vec_ap.partition_broadcast(128)), writes=[gb], dma=True)
    return g, gb


def pipeline(n, prep, compute, depth=1):
    for i in range(min(depth, n)):
        prep(i)
    for i in range(n):
        if i + depth < n:
            prep(i + depth)
        compute(i)


CAST_PATTERN = {"default": ("act", "dve"), "moe": ("act", "dve", "act", "dve", "pool")}


def cast_op(C, out_ap, in_ap, reads, writes, pattern="default"):
    S = C.S
    pat = CAST_PATTERN[pattern]
    eng = pat[C.cast_i % len(pat)]
    C.cast_i += 1
    if eng == "dve":
        S.op("dve", lambda e: e.tensor_copy(out=out_ap, in_=in_ap), reads=reads, writes=writes)
    elif eng == "act":
        S.op("act", lambda e: e.copy(out=out_ap, in_=in_ap), reads=reads, writes=writes)
    else:
        S.op("pool", lambda e: e.tensor_copy(out=out_ap, in_=in_ap), reads=reads, writes=writes)


def stage_inproj(C, h_ap, h_buf, gain_ap, w_in, b_gate, ident_f, sc):
    nc, S = C.nc, C.S
    HALF = 2048
    with contextlib.ExitStack() as st:
        idf, idfb, idb, idbb = load_consts(C, st, ident_f)
        pools = norm_pools(C, st)
        gain_t, gain_b = load_gain(C, st, "gain1", gain_ap, D)
        xs_pool = Rot(nc, st, "xs", 2, [128, D], BF16)
        xnT = st.enter_context(nc.sbuf_tensor(U("xnT"), [128, 16, HALF], BF16))
        xnT_b = [Buf("xnT%d" % i) for i in range(16)]
        wst = Rot(nc, st, "wst", 3, [128, 16, 128], F32)
        wbf = Rot(nc, st, "wbf", 3, [128, 16, 128], BF16)
        ost_b = Rot(nc, st, "ostb", 2, [128, HALF], BF16)
        ost_f = Rot(nc, st, "ostf", 2, [128, HALF], F32)
        bg = st.enter_context(nc.sbuf_tensor(U("bg"), [128, 48], F32))
        bgb = Buf("bg")
        S.op("sp", lambda e: e.dma_start(out=bg[:], in_=b_gate.rearrange("(c p) -> p c", p=128), allow_slow_non_contiguous=True), writes=[bgb], dma=True)
        pmm = Rot(nc, st, "pmm", 6, [128, 512], F32, psum=True)
        ptr = Rot(nc, st, "ptr", 2, [128, 8, 128], BF16, psum=True)

        chunks = []
        for i in range(16):
            chunks.append((i * 128, 128, "F", "qkT", i * 128))
        for i in range(8):
            chunks.append((2048 + i * 128, 128, "T", "vna", i * 128))
        c0 = 3072
        r0 = 0
        while r0 < 672:
            m = min(128, 672 - r0)
            chunks.append((c0 + r0, m, "C", "cT", r0))
            r0 += m
        for i in range(8):
            chunks.append((3744 + i * 128, 128, "F", "ufT", i * 128))
        for i in range(48):
            chunks.append((4768 + i * 128, 128, "G", "gT", i * 128))

        w_v = w_in
        for half in range(T // HALF):
            t0 = half * HALF
            for tt in range(16):
                xs_t, xs_b = xs_pool.nxt()
                rms_tile(C, pools, h_ap[t0 + tt * 128: t0 + (tt + 1) * 128, :], gain_t, gain_b, xs_t[:], xs_b, D, [h_buf])
                for g in range(2):
                    pt, pb = ptr.nxt()
                    for j in range(8):
                        kc = g * 8 + j
                        S.op("pe", lambda e, pt=pt, j=j, kc=kc, xs_t=xs_t: e.transpose(out=pt[:, j, :], in_=xs_t[:, kc * 128:(kc + 1) * 128], identity=idb[:]),
                             reads=[xs_b, idbb], writes=[pb])
                    S.op("dve", lambda e, pt=pt, g=g, tt=tt: e.tensor_copy(out=xnT[:, g * 8:(g + 1) * 8, tt * 128:(tt + 1) * 128], in_=pt[:]),
                         reads=[pb], writes=[xnT_b[tt]])
            wjob = {}

            def prep(i):
                (c0, m, mode, dst, dr0) = chunks[i]
                ws_t, ws_b = wst.nxt()
                S.op("sp", lambda e: e.dma_start(out=ws_t[:, :, :m], in_=w_v[:, c0:c0 + m].rearrange("(kc p) n -> p kc n", p=128)),
                     writes=[ws_b], dma=True)
                wb_t, wb_b = wbf.nxt()
                cast_op(C, wb_t[:, :, :m], ws_t[:, :, :m], [ws_b], [wb_b])
                wjob[i] = (wb_t, wb_b)

            def compute(i, t0=t0):
                (c0, m, mode, dst, dr0) = chunks[i]
                wb_t, wb_b = wjob.pop(i)
                if mode != "T":
                    if mode == "C":
                        o_t, o_b = ost_f.nxt()
                    else:
                        o_t, o_b = ost_b.nxt()
                    for tb in range(4):
                        p_t, p_b = pmm.nxt()
                        for kc in range(16):
                            S.op("pe", lambda e, p_t=p_t, kc=kc, tb=tb: e.matmul(p_t[:m, :], lhsT=wb_t[:, kc, :m], rhs=xnT[:, kc, tb * 512:(tb + 1) * 512], start=(kc == 0), stop=(kc == 15)),
                                 reads=[wb_b] + xnT_b[tb * 4:(tb + 1) * 4], writes=[p_b])
                        if mode == "G":
                            gi = dr0 // 128
                            S.op("act", lambda e, p_t=p_t, tb=tb, gi=gi: e.activation(out=o_t[:, tb * 512:(tb + 1) * 512], in_=p_t[:, :], func=AF.Sigmoid, bias=bg[:, gi:gi + 1]),
                                 reads=[p_b, bgb], writes=[o_b])
                        else:
                            S.op("dve", lambda e, p_t=p_t, tb=tb: e.tensor_copy(out=o_t[:m, tb * 512:(tb + 1) * 512], in_=p_t[:m, :]),
                                 reads=[p_b], writes=[o_b])
                    S.op("sp", lambda e: e.dma_start(out=sc[dst][dr0:dr0 + m, t0:t0 + HALF], in_=o_t[:m, :]),
                         reads=[o_b], writes=[C.buf(dst)], dma=True)
                else:
                    o_t, o_b = ost_b.nxt()
                    for g4 in range(4):
                        p_t, p_b = pmm.nxt()
                        for q in range(4):
                            tt = g4 * 4 + q
                            for kc in range(16):
                                S.op("pe", lambda e, p_t=p_t, kc=kc, tt=tt, q=q: e.matmul(p_t[:, q * 128:(q + 1) * 128], lhsT=xnT[:, kc, tt * 128:(tt + 1) * 128], rhs=wb_t[:, kc, :], start=(kc == 0), stop=(kc == 15)),
                                     reads=[wb_b, xnT_b[tt]], writes=[p_b])
                        S.op("dve", lambda e, p_t=p_t, g4=g4: e.tensor_copy(out=o_t[:, g4 * 512:(g4 + 1) * 512], in_=p_t[:, :]),
                             reads=[p_b], writes=[o_b])
                    S.op("sp", lambda e: e.dma_start(
                        out=sc["vna"][t0:t0 + HALF, dr0:dr0 + 128].rearrange("(tt p) c -> p tt c", p=128),
                        in_=o_t[:, :].rearrange("p (tt c) -> p tt c", c=128)),
                        reads=[o_b], writes=[C.buf("vna")], dma=True)

            pipeline(len(chunks), prep, compute, depth=2)
        C.end_stage()


def fm_rmsnorm(C, st, name, src, src_buf, row0, nrows, gain_ap, onesf, onesf_b, eps, pmm, out_t, out_b, cin, csq, rs):
    nc, S = C.nc, C.S
    nch = (nrows + 127) // 128
    gcol = st.enter_context(nc.sbuf_tensor(U(name + "g"), [128, nch], F32))
    gb = Buf(name + "g")
    for c in range(nch):
        ksz = min(128, nrows - c * 128)
        S.op("sp", lambda e, c=c, ksz=ksz: e.dma_start(out=gcol[:ksz, c:c + 1], in_=gain_ap[c * 128:c * 128 + ksz].rearrange("(p o) -> p o", o=1)),
             writes=[gb], dma=True)
    for tb in range(T // 512):
        ci, cib = cin.nxt()
        cs, csb = csq.nxt()
        for c in range(nch):
            ksz = min(128, nrows - c * 128)
            S.op("sp", lambda e, ci=ci, c=c, ksz=ksz, tb=tb: e.dma_start(out=ci[:ksz, c, :], in_=src[row0 + c * 128: row0 + c * 128 + ksz, tb * 512:(tb + 1) * 512]),
                 reads=[src_buf], writes=[cib], dma=True)
        p_t, p_b = pmm.nxt()
        for c in range(nch):
            ksz = min(128, nrows - c * 128)
            S.op("act", lambda e, ci=ci, cs=cs, c=c, ksz=ksz: e.activation(out=cs[:ksz, c, :], in_=ci[:ksz, c, :], func=AF.Square),
                 reads=[cib], writes=[csb])
            S.op("pe", lambda e, p_t=p_t, cs=cs, c=c, ksz=ksz: e.matmul(p_t[:, :], lhsT=onesf[:ksz, :], rhs=cs[:ksz, c, :], start=(c == 0), stop=(c == nch - 1)),
                 reads=[csb, onesf_b], writes=[p_b])
        r_t, r_b = rs.nxt()
        S.op("act", lambda e, r_t=r_t, p_t=p_t: e.activation(out=r_t[:, :], in_=p_t[:, :], func=AF.Sqrt, scale=1.0 / nrows, bias=eps[0][:, 0:1]),
             reads=[p_b, eps[1]], writes=[r_b])
        S.op("dve", lambda e, r_t=r_t: e.reciprocal(out=r_t[:, :], in_=r_t[:, :]), reads=[r_b], writes=[r_b])
        for c in range(nch):
            ksz = min(128, nrows - c * 128)
            S.op("dve", lambda e, ci=ci, r_t=r_t, c=c, ksz=ksz, tb=tb: e.scalar_tensor_tensor(
                out=out_t[:ksz, c, tb * 512:(tb + 1) * 512], in0=ci[:ksz, c, :], scalar=gcol[:ksz, c:c + 1], in1=r_t[:ksz, :], op0=ALU.mult, op1=ALU.mult),
                reads=[cib, r_b, gb], writes=[out_b])


def stage_mla(C, sc, w_uq, q_norm, w_ukv, kv_norm, cos2T, sin2T, ymlaT, ident_f=None):
    nc, S = C.nc, C.S
    cT = sc["cT"]
    cTb = C.buf("cT")
    scale = 192.0 ** -0.5
    QCH = [(0, 128), (128, 128), (256, 128), (384, 64)]
    KCH = [(0, 128), (128, 32)]
    with contextlib.ExitStack() as st:
        idf, idfb, idb_, idbb_ = load_consts(C, st, ident_f)
        onesf = st.enter_context(nc.sbuf_tensor(U("onesf"), [128, 128], F32))
        onesb = st.enter_context(nc.sbuf_tensor(U("onesb"), [128, 128], BF16))
        of_b, ob_b = Buf("onesf"), Buf("onesb")
        S.op("dve", lambda e: e.memset(onesf[:], 1.0), writes=[of_b])
        S.op("dve", lambda e: e.memset(onesb[:], 1.0), writes=[ob_b])
        eps_t = st.enter_context(nc.sbuf_tensor(U("epsT"), [128, 1], F32))
        eb = Buf("eps")
        S.op("dve", lambda e: e.memset(eps_t[:], EPS), writes=[eb])
        pmm = Rot(nc, st, "pmm", 4, [128, 512], F32, psum=True)
        pO = Rot(nc, st, "pO", 4, [128, 512], F32, psum=True)
        cqn = st.enter_context(nc.sbuf_tensor(U("cqn"), [128, 4, T], BF16))
        ckvn = st.enter_context(nc.sbuf_tensor(U("ckvn"), [128, 2, T], BF16))
        cqn_b, ckvn_b = Buf("cqn"), Buf("ckvn")
        cin = Rot(nc, st, "fci", 2, [128, 4, 512], F32)
        csq = Rot(nc, st, "fcs", 1, [128, 4, 512], F32)
        rs = Rot(nc, st, "frs", 2, [128, 512], F32)
        fm_rmsnorm(C, st, "nq", cT, cTb, 0, 448, q_norm, onesf, of_b, (eps_t, eb), pmm, cqn, cqn_b, cin, csq, rs)
        fm_rmsnorm(C, st, "nk", cT, cTb, 448, 160, kv_norm, onesf, of_b, (eps_t, eb), pmm, ckvn, ckvn_b, cin, csq, rs)
        kpe = st.enter_context(nc.sbuf_tensor(U("kpe"), [128, T], BF16))
        kpe_b = Buf("kpe")
        S.op("pool", lambda e: e.memset(kpe[64:128, :], 0.0), writes=[kpe_b])
        tmpA = Rot(nc, st, "tmpA", 2, [64, 512], F32)
        tmpB = Rot(nc, st, "tmpB", 2, [64, 512], F32)
        tmpC = Rot(nc, st, "tmpC", 2, [64, 512], F32)
        tmpD = Rot(nc, st, "tmpD", 2, [64, 512], F32)
        cosr = Rot(nc, st, "cosr", 2, [64, 512], F32)
        sinr = Rot(nc, st, "sinr", 2, [64, 512], F32)

        def rope(src_a, src_a_b, src_r, src_r_b, tb, out_ap, out_b):
            ct, cb = cosr.nxt()
            s_t, s_b = sinr.nxt()
            S.op("sp", lambda e: e.dma_start(out=ct[:, :], in_=cos2T[:, tb * 512:(tb + 1) * 512]), writes=[cb], dma=True)
            S.op("sp", lambda e: e.dma_start(out=s_t[:, :], in_=sin2T[:, tb * 512:(tb + 1) * 512]), writes=[s_b], dma=True)
            a_t, a_b = tmpC.nxt()
            b_t, b_b = tmpD.nxt()
            S.op("dve", lambda e: e.tensor_tensor(out=a_t[:, :], in0=src_a, in1=ct[:, :], op=ALU.mult), reads=[src_a_b, cb], writes=[a_b])
            S.op("dve", lambda e: e.tensor_tensor(out=b_t[:, :], in0=src_r, in1=s_t[:, :], op=ALU.mult), reads=[src_r_b, s_b], writes=[b_b])
            S.op("dve", lambda e: e.tensor_tensor(out=out_ap, in0=a_t[:, :], in1=b_t[:, :], op=ALU.add), reads=[a_b, b_b], writes=[out_b])

        for tb in range(T // 512):
            a_t, a_b = tmpA.nxt()
            r_t, r_b = tmpB.nxt()
            S.op("sp", lambda e, a_t=a_t, tb=tb: e.dma_start(out=a_t[:, :], in_=cT[608:672, tb * 512:(tb + 1) * 512]), reads=[cTb], writes=[a_b], dma=True)
            S.op("sp", lambda e, r_t=r_t, tb=tb: e.dma_start(out=r_t[0:32, :], in_=cT[640:672, tb * 512:(tb + 1) * 512]), reads=[cTb], writes=[r_b], dma=True)
            S.op("sp", lambda e, r_t=r_t, tb=tb: e.dma_start(out=r_t[32:64, :], in_=cT[608:640, tb * 512:(tb + 1) * 512]), reads=[cTb], writes=[r_b], dma=True)
            rope(a_t[:, :], a_b, r_t[:, :], r_b, tb, kpe[0:64, tb * 512:(tb + 1) * 512], kpe_b)

        qn = st.enter_context(nc.sbuf_tensor(U("qn"), [128, T], BF16))
        qp = st.enter_context(nc.sbuf_tensor(U("qp"), [128, T], BF16))
        kn = st.enter_context(nc.sbuf_tensor(U("kn"), [128, T], BF16))
        vv = st.enter_context(nc.sbuf_tensor(U("vv"), [128, 32, 129], BF16))
        qn_b, qp_b, kn_b, vv_b = Buf("qn"), Buf("qp"), Buf("kn"), Buf("vv")
        S.op("pool", lambda e: e.memset(vv[:, :, 128:129], 1.0), writes=[vv_b])
        onp = Rot(nc, st, "onp", 2, [128, 4, 128], F32)
        rcp = Rot(nc, st, "rcp", 2, [128, 4], F32)
        S.op("pool", lambda e: e.memset(qp[64:128, :], 0.0), writes=[qp_b])
        wq_s = st.enter_context(nc.sbuf_tensor(U("wq_s"), [128, 4, 256], F32))
        wq_bf = st.enter_context(nc.sbuf_tensor(U("wq_bf"), [128, 4, 256], BF16))
        wk_s = st.enter_context(nc.sbuf_tensor(U("wk_s"), [128, 2, 256], F32))
        wk_bf = st.enter_context(nc.sbuf_tensor(U("wk_bf"), [128, 2, 256], BF16))
        wq_sb, wq_bb, wk_sb, wk_bb = Buf("wqs"), Buf("wqb"), Buf("wks"), Buf("wkb")
        Et = Rot(nc, st, "Et", 6, [128, 512], BF16)
        accA = Rot(nc, st, "accA", 2, [128, 512], F32)
        accB = Rot(nc, st, "accB", 2, [128, 512], F32)
        rden = Rot(nc, st, "rden", 2, [128, 512], F32)
        yst = Rot(nc, st, "yst", 2, [128, 512], BF16)
        qa = Rot(nc, st, "qa", 2, [64, 512], F32)
        qr = Rot(nc, st, "qr", 2, [64, 512], F32)

        for h in range(8):
            for c, (k0, ksz) in enumerate(QCH):
                S.op("sp", lambda e, c=c, k0=k0, ksz=ksz, h=h: e.dma_start(out=wq_s[:ksz, c, 0:192], in_=w_uq[k0:k0 + ksz, h * 192:(h + 1) * 192]), writes=[wq_sb], dma=True)
                S.op("sp", lambda e, c=c, k0=k0, ksz=ksz, h=h: e.dma_start(out=wq_s[:ksz, c, 192:224], in_=w_uq[k0:k0 + ksz, h * 192 + 160:h * 192 + 192]), writes=[wq_sb], dma=True)
                S.op("sp", lambda e, c=c, k0=k0, ksz=ksz, h=h: e.dma_start(out=wq_s[:ksz, c, 224:256], in_=w_uq[k0:k0 + ksz, h * 192 + 128:h * 192 + 160]), writes=[wq_sb], dma=True)
            for c, (k0, ksz) in enumerate(KCH):
                S.op("sp", lambda e, c=c, k0=k0, ksz=ksz, h=h: e.dma_start(out=wk_s[:ksz, c, :], in_=w_ukv[k0:k0 + ksz, h * 256:(h + 1) * 256]), writes=[wk_sb], dma=True)
            for c, (k0, ksz) in enumerate(QCH):
                S.op("dve", lambda e, c=c, ksz=ksz: e.tensor_copy(out=wq_bf[:ksz, c, :], in_=wq_s[:ksz, c, :]), reads=[wq_sb], writes=[wq_bb])
            for c, (k0, ksz) in enumerate(KCH):
                S.op("dve", lambda e, c=c, ksz=ksz: e.tensor_copy(out=wk_bf[:ksz, c, :], in_=wk_s[:ksz, c, :]), reads=[wk_sb], writes=[wk_bb])
            for tb in range(T // 512):
                tsl = slice(tb * 512, (tb + 1) * 512)
                p_t, p_b = pmm.nxt()
                for c, (k0, ksz) in enumerate(QCH):
                    S.op("pe", lambda e, p_t=p_t, c=c, ksz=ksz, tsl=tsl: e.matmul(p_t[:, :], lhsT=wq_bf[:ksz, c, 0:128], rhs=cqn[:ksz, c, tsl], start=(c == 0), stop=(c == 3)),
                         reads=[wq_bb, cqn_b], writes=[p_b])
                S.op("act", lambda e, p_t=p_t, tsl=tsl: e.copy(out=qn[:, tsl], in_=p_t[:, :]), reads=[p_b], writes=[qn_b])
                p1, p1b = pmm.nxt()
                for c, (k0, ksz) in enumerate(QCH):
                    S.op("pe", lambda e, p1=p1, c=c, ksz=ksz, tsl=tsl: e.matmul(p1[0:64, :], lhsT=wq_bf[:ksz, c, 128:192], rhs=cqn[:ksz, c, tsl], start=(c == 0), stop=(c == 3)),
                         reads=[wq_bb, cqn_b], writes=[p1b])
                p2, p2b = pmm.nxt()
                for c, (k0, ksz) in enumerate(QCH):
                    S.op("pe", lambda e, p2=p2, c=c, ksz=ksz, tsl=tsl: e.matmul(p2[0:64, :], lhsT=wq_bf[:ksz, c, 192:256], rhs=cqn[:ksz, c, tsl], start=(c == 0), stop=(c == 3)),
                         reads=[wq_bb, cqn_b], writes=[p2b])
                qa_t, qa_b = qa.nxt()
                qr_t, qr_b = qr.nxt()
                S.op("act", lambda e, qa_t=qa_t, p1=p1: e.copy(out=qa_t[:, :], in_=p1[0:64, :]), reads=[p1b], writes=[qa_b])
                S.op("act", lambda e, qr_t=qr_t, p2=p2: e.copy(out=qr_t[:, :], in_=p2[0:64, :]), reads=[p2b], writes=[qr_b])
                rope(qa_t[:, :], qa_b, qr_t[:, :], qr_b, tb, qp[0:64, tsl], qp_b)
                p3, p3b = pmm.nxt()
                for c, (k0, ksz) in enumerate(KCH):
                    S.op("pe", lambda e, p3=p3, c=c, ksz=ksz, tsl=tsl: e.matmul(p3[:, :], lhsT=wk_bf[:ksz, c, 0:128], rhs=ckvn[:ksz, c, tsl], start=(c == 0), stop=(c == 1)),
                         reads=[wk_bb, ckvn_b], writes=[p3b])
                S.op("act", lambda e, p3=p3, tsl=tsl: e.copy(out=kn[:, tsl], in_=p3[:, :]), reads=[p3b], writes=[kn_b])
                p4, p4b = pmm.nxt()
                for q4 in range(4):
                    tt = tb * 4 + q4
                    for c, (k0, ksz) in enumerate(KCH):
                        S.op("pe", lambda e, p4=p4, c=c, ksz=ksz, tt=tt, q4=q4: e.matmul(p4[:, q4 * 128:(q4 + 1) * 128], lhsT=ckvn[:ksz, c, tt * 128:(tt + 1) * 128], rhs=wk_bf[:ksz, c, 128:256], start=(c == 0), stop=(c == 1)),
                             reads=[wk_bb, ckvn_b], writes=[p4b])
                S.op("dve", lambda e, p4=p4, tb=tb: e.tensor_copy(out=vv[:, tb * 4:(tb + 1) * 4, 0:128], in_=p4[:, :].rearrange("p (a b) -> p a b", b=128)),
                     reads=[p4b], writes=[vv_b])
            SK = 2
            fr = {}
            acc = {}

            def front(it):
                qb, kc = divmod(it, 32)
                qsl = slice(qb * 512, (qb + 1) * 512)
                ksl = slice(kc * 128, (kc + 1) * 128)
                s_t, s_b = pmm.nxt()
                S.op("pe", lambda e: e.matmul(s_t[:, :], lhsT=kn[:, ksl], rhs=qn[:, qsl], start=True, stop=False), reads=[kn_b, qn_b], writes=[s_b])
                S.op("pe", lambda e: e.matmul(s_t[:, :], lhsT=kpe[:, ksl], rhs=qp[:, qsl], start=False, stop=True), reads=[kpe_b, qp_b], writes=[s_b])
                e_t, e_b = Et.nxt()
                S.op("act", lambda e: e.activation(out=e_t[:, :], in_=s_t[:, :], func=AF.Exp, scale=scale), reads=[s_b], writes=[e_b])
                fr[it] = (e_t, e_b)

            def back(it, h=h):
                qb, kc = divmod(it, 32)
                qsl = slice(qb * 512, (qb + 1) * 512)
                e_t, e_b = fr.pop(it)
                if kc == 0:
                    acc["o"] = (pO.nxt(), pO.nxt())
                banks = acc["o"]
                for sl in range(4):
                    o_t, o_b = banks[sl // 2]
                    c0 = (sl % 2) * 256
                    S.op("pe", lambda e, o_t=o_t, c0=c0, sl=sl: e.matmul(o_t[:, c0:c0 + 129], lhsT=e_t[:, sl * 128:(sl + 1) * 128], rhs=vv[:, kc, :], start=(kc == 0 and sl % 2 == 0), stop=(kc == 31), skip_group_check=True),
                         reads=[vv_b, e_b], writes=[o_b])
                if kc == 31:
                    rc_t, rc_b = rcp.nxt()
                    on_t, on_b = onp.nxt()
                    for sl in range(4):
                        o_t, o_b = banks[sl // 2]
                        c0 = (sl % 2) * 256
                        S.op("dve", lambda e, o_t=o_t, c0=c0, sl=sl: e.reciprocal(out=rc_t[:, sl:sl + 1], in_=o_t[:, c0 + 128:c0 + 129]), reads=[o_b], writes=[rc_b])
                        if sl % 2 == 0:
                            S.op("act", lambda e, o_t=o_t, c0=c0, sl=sl: e.activation(out=on_t[:, sl, :], in_=o_t[:, c0:c0 + 128], func=AF.Copy, scale=rc_t[:, sl:sl + 1]), reads=[o_b, rc_b], writes=[on_b])
                        else:
                            S.op("dve", lambda e, o_t=o_t, c0=c0, sl=sl: e.tensor_scalar(out=on_t[:, sl, :], in0=o_t[:, c0:c0 + 128], scalar1=rc_t[:, sl:sl + 1], scalar2=None, op0=ALU.mult), reads=[o_b, rc_b], writes=[on_b])
                    t_t, t_b = pmm.nxt()
                    for sl in range(4):
                        S.op("pe", lambda e, sl=sl: e.transpose(out=t_t[:, sl * 128:(sl + 1) * 128], in_=on_t[:, sl, :], identity=idf[:]), reads=[on_b, idfb], writes=[t_b])
                    y_t, y_b = yst.nxt()
                    S.op("dve", lambda e: e.tensor_copy(out=y_t[:, :], in_=t_t[:, :]), reads=[t_b], writes=[y_b])
                    S.op("sp", lambda e: e.dma_start(out=ymlaT[h * 128:(h + 1) * 128, qsl], in_=y_t[:, :]), reads=[y_b], writes=[C.buf("ymlaT")], dma=True)

            NIT = (T // 512) * 32
            for it in range(NIT + SK):
                if it < NIT:
                    front(it)
                if it - SK >= 0:
                    back(it - SK)
        C.end_stage()


def stage_na(C, sc, rpbT, maskc, ynaT, wprep=None):
    nc, S = C.nc, C.S
    qkT, vna = sc["qkT"], sc["vna"]
    qk_b, v_b = C.buf("qkT"), C.buf("vna")
    scale = 64.0 ** -0.5
    with contextlib.ExitStack() as st:
        ones = st.enter_context(nc.sbuf_tensor(U("ones"), [128, 64], BF16))
        ones_b = Buf("ones")
        S.op("dve", lambda e: e.memset(ones[:], 1.0), writes=[ones_b])
        mk = st.enter_context(nc.sbuf_tensor(U("mk"), [128, 14 * 64], F32))
        mk_b = Buf("mk")
        S.op("sp", lambda e: e.dma_start(out=mk[:, :], in_=maskc.rearrange("j d q -> j (d q)")), writes=[mk_b], dma=True)
        khp = Rot(nc, st, "kh", 2, [128, T], BF16)
        qhp = Rot(nc, st, "qh", 2, [128, T], BF16)
        vhp = Rot(nc, st, "vh", 2, [128, 63, 128], BF16)
        btp = Rot(nc, st, "bt", 2, [128, 14 * 64], F32)
        btq = Rot(nc, st, "btq", 2, [128, 512], F32)
        tmp = Rot(nc, st, "tmp", 4, [128, 512], F32)
        Ep = Rot(nc, st, "E", 6, [128, 512], BF16)
        rdp = Rot(nc, st, "rd", 2, [128, 512], F32)
        yp = Rot(nc, st, "y", 2, [64, 512], BF16)
        ps = Rot(nc, st, "ps", 5, [128, 512], F32, psum=True)
        po = Rot(nc, st, "po", 3, [128, 512], F32, psum=True)
        SK = 3
        state = {}
        if wprep is not None:
            w_branch_, w_o_, wbr_c, wo_c = wprep
            pw_st = Rot(nc, st, "pwst", 2, [128, 24, 128], F32)
            pw_bf = Rot(nc, st, "pwbf", 2, [128, 24, 128], BF16)
            po_st = Rot(nc, st, "post", 2, [128, 16, 128], F32)
            po_bf = Rot(nc, st, "pobf", 2, [128, 16, 128], BF16)

        pend = []

        def wprep_flush():
            while pend:
                pend.pop(0)()

        def wprep_step(dc):
            wprep_flush()
            ws_t, ws_b = pw_st.nxt()
            for b in range(3):
                S.op("sp", lambda e, b=b: e.dma_start(out=ws_t[:, b * 8:(b + 1) * 8, :], in_=w_branch_[b, :, dc * 128:(dc + 1) * 128].rearrange("(kc p) n -> p kc n", p=128)),
                     writes=[ws_b], dma=True)
            wb_t, wb_b = pw_bf.nxt()
            S.op("pool", lambda e: e.tensor_copy(out=wb_t[:, :, :], in_=ws_t[:, :, :]), reads=[ws_b], writes=[wb_b])
            pend.append(lambda: S.op("sp", lambda e: e.dma_start(out=wbr_c[dc], in_=wb_t[:, :, :].rearrange("p a b -> p (a b)")), reads=[wb_b], writes=[C.buf("wbr_c")], dma=True))
            os_t, os_b = po_st.nxt()
            S.op("sp", lambda e: e.dma_start(out=os_t[:, :, :], in_=w_o_[:, dc * 128:(dc + 1) * 128].rearrange("(kc p) n -> p kc n", p=128)), writes=[os_b], dma=True)
            ob_t, ob_b = po_bf.nxt()
            S.op("pool", lambda e: e.tensor_copy(out=ob_t[:, :, :], in_=os_t[:, :, :]), reads=[os_b], writes=[ob_b])
            pend.append(lambda: S.op("sp", lambda e: e.dma_start(out=wo_c[dc], in_=ob_t[:, :, :].rearrange("p a b -> p (a b)")), reads=[ob_b], writes=[C.buf("wo_c")], dma=True))

        for (kh_, khb_) in khp.slots:
            S.op("pool", lambda e, kh_=kh_: e.memset(kh_[64:128, :], 0.0), writes=[khb_])
        for (qh_, qhb_) in qhp.slots:
            S.op("pool", lambda e, qh_=qh_: e.memset(qh_[64:128, :], 0.0), writes=[qhb_])
        for (vh_, vhb_) in vhp.slots:
            S.op("pool", lambda e, vh_=vh_: e.memset(vh_[:, :, 64:128], 1.0), writes=[vhb_])
        def head_load(h):
            kh, kh_b = khp.nxt()
            qh, qh_b = qhp.nxt()
            vh, vh_b = vhp.nxt()
            bt, bt_b = btp.nxt()
            S.op("sp", lambda e: e.dma_start(out=qh[0:64, :], in_=qkT[h * 64:(h + 1) * 64, :]), reads=[qk_b], writes=[qh_b], dma=True)
            S.op("sp", lambda e: e.dma_start(out=kh[0:64, :], in_=qkT[1024 + h * 64:1024 + (h + 1) * 64, :]), reads=[qk_b], writes=[kh_b], dma=True)
            for g in range(2):
                S.op("sp", lambda e, g=g: e.dma_start(out=vh[:, g * 16:(g + 1) * 16, 0:64], in_=vna[g * 2048:(g + 1) * 2048, h * 64:(h + 1) * 64].rearrange("(t p) c -> p t c", p=128)),
                     reads=[v_b], writes=[vh_b], dma=True)
            S.op("sp", lambda e: e.dma_start(out=vh[:, 32:48, 0:64], in_=vna[64:64 + 2048, h * 64:(h + 1) * 64].rearrange("(t p) c -> p t c", p=128)),
                 reads=[v_b], writes=[vh_b], dma=True)
            S.op("sp", lambda e: e.dma_start(out=vh[:, 48:63, 0:64], in_=vna[64 + 2048:64 + 2048 + 15 * 128, h * 64:(h + 1) * 64].rearrange("(t p) c -> p t c", p=128)),
                 reads=[v_b], writes=[vh_b], dma=True)
            S.op("sp", lambda e: e.dma_start(out=bt[:, :], in_=rpbT[h].rearrange("j d q -> j (d q)")), writes=[bt_b], dma=True)
            S.op("pool", lambda e: e.tensor_tensor(out=bt[:, :], in0=bt[:, :], in1=mk[:, :], op=ALU.add), reads=[mk_b], writes=[bt_b])
            bq, bq_b = btq.nxt()
            for a_ in range(2):
                S.op("pool", lambda e, a_=a_: e.tensor_copy(out=bq[:, a_ * 256:(a_ + 1) * 256].rearrange("p (c q) -> p c q", q=64), in_=bt[:, :].rearrange("p (d q) -> p d q", q=64)[:, 3:10:2, :]),
                     reads=[bt_b], writes=[bq_b])
            state[h] = (kh, kh_b, qh, qh_b, vh, vh_b, bt, bt_b, bq, bq_b)
            if wprep is not None:
                wprep_step(h)

        fr = {}
        units = []
        for h_ in range(16):
            for r_ in (0, 1, 2, 3):
                units.append((h_, [r_]))
            for r_ in range(4, 60, 2):
                units.append((h_, [r_, r_ + 1]))
            for r_ in (60, 61, 62, 63):
                units.append((h_, [r_]))

        def front(it):
            h, rows = units[it]
            if rows[0] == 0:
                head_load(h)
            kh, kh_b, qh, qh_b, vh, vh_b, bt, bt_b, bq, bq_b = state[h]
            s_t, s_b = ps.nxt()
            starts = []
            for ri, r in enumerate(rows):
                start = min(max(r - 4, 0), 56)
                starts.append(start)
                for c in range(4):
                    S.op("pe", lambda e, c=c, ri=ri, r=r, start=start: e.matmul(s_t[:, (ri * 4 + c) * 64:(ri * 4 + c + 1) * 64], lhsT=kh[:, (start + 2 * c) * 64:(start + 2 * c + 2) * 64], rhs=qh[:, r * 64:(r + 1) * 64], start=True, stop=True),
                         reads=[kh_b, qh_b], writes=[s_b])
            t_t, t_b = tmp.nxt()
            e_t, e_b = Ep.nxt()
            if len(rows) == 2:
                S.op("dve", lambda e: e.scalar_tensor_tensor(out=t_t[:, :], in0=s_t[:, :], scalar=scale, in1=bq[:, :], op0=ALU.mult, op1=ALU.add),
                     reads=[s_b, bq_b], writes=[t_b])
                S.op("act", lambda e: e.activation(out=e_t[:, :], in_=t_t[:, :], func=AF.Exp), reads=[t_b], writes=[e_b])
            else:
                base = starts[0] - rows[0] + 7
                S.op("dve", lambda e: e.scalar_tensor_tensor(out=t_t[:, 0:256].rearrange("p (c q) -> p c q", q=64), in0=s_t[:, 0:256].rearrange("p (c q) -> p c q", q=64), scalar=scale,
                                                            in1=bt[:, :].rearrange("p (d q) -> p d q", q=64)[:, base:base + 7:2, :], op0=ALU.mult, op1=ALU.add),
                     reads=[s_b, bt_b], writes=[t_b])
                S.op("act", lambda e: e.activation(out=e_t[:, 0:256], in_=t_t[:, 0:256], func=AF.Exp), reads=[t_b], writes=[e_b])
            fr[it] = (e_t, e_b, starts)

        acc = {}

        def back(it):
            h, rows = units[it]
            kh, kh_b, qh, qh_b, vh, vh_b, bt, bt_b, bq, bq_b = state[h]
            e_t, e_b, starts = fr.pop(it)
            for ri, r in enumerate(rows):
                back_row(h, r, ri, starts[ri], e_t, e_b, vh, vh_b)

        def back_row(h, r, ri, start, e_t, e_b, vh, vh_b):
            rr = r % 8
            r8 = r // 8
            if rr == 0:
                acc["o"] = po.nxt()
            o_t, o_b = acc["o"]
            for c in range(4):
                g = start + 2 * c
                vi = g // 2 if g % 2 == 0 else 32 + (g - 1) // 2
                S.op("pe", lambda e, c=c, vi=vi: e.matmul(o_t[:, rr * 64:(rr + 1) * 64], lhsT=vh[:, vi, :], rhs=e_t[:, (ri * 4 + c) * 64:(ri * 4 + c + 1) * 64], start=(c == 0), stop=(c == 3)),
                     reads=[vh_b, e_b], writes=[o_b])
            if rr == 7:
                rd_t, rd_b = rdp.nxt()
                S.op("dve", lambda e: e.reciprocal(out=rd_t[64:128, :], in_=o_t[64:128, :]), reads=[o_b], writes=[rd_b])
                y_t, y_b = yp.nxt()
                S.op("dve", lambda e: e.tensor_tensor(out=y_t[:, :], in0=o_t[0:64, :], in1=rd_t[64:128, :], op=ALU.mult), reads=[o_b, rd_b], writes=[y_b])
                S.op("sp", lambda e: e.dma_start(out=ynaT[h * 64:(h + 1) * 64, r8 * 512:(r8 + 1) * 512], in_=y_t[:, :]), reads=[y_b], writes=[C.buf("ynaT")], dma=True)

        NIT = len(units)
        for it in range(NIT + SK):
            if it < NIT:
                front(it)
            if it - SK >= 0:
                back(it - SK)
        if wprep is not None:
            wprep_flush()
        C.end_stage()


def na_host_tables(rpb):
    cols = np.arange(64)
    col_start = np.clip(cols - 8, 0, 64 - 16)
    valid = (cols[None, :] >= col_start[:, None]) & (cols[None, :] < col_start[:, None] + 16)
    col_idx = np.clip(cols[None, :] - cols[:, None] + 15, 0, 30)
    t = rpb[:, :, col_idx]
    t = np.transpose(t, (0, 3, 1, 2))
    rpbT = np.ascontiguousarray(np.concatenate([t[:, :, 0:14, :], t[:, :, 1:15, :]], axis=1)).astype(np.float32)
    m = np.where(valid.T, 0.0, -30000.0).astype(np.float32)
    m2 = np.concatenate([m, m], axis=0)
    maskc = np.ascontiguousarray(np.broadcast_to(m2[:, None, :], (128, 14, 64))).astype(np.float32)
    return rpbT, maskc


def stage_fnet(C, sc, ccs, csT, ssT, yfT):
    nc, S = C.nc, C.S
    ufT = sc["ufT"]
    uf_b = C.buf("ufT")
    SB = 256
    HN = T // 2
    NT2 = HN // 128
    with contextlib.ExitStack() as st:
        ccf = st.enter_context(nc.sbuf_tensor(U("ccf"), [128, 2, 2, 256], F32))
        ccb = st.enter_context(nc.sbuf_tensor(U("ccb"), [128, 2, 2, 256], BF16))
        ccf_b, ccb_b = Buf("ccf"), Buf("ccb")
        for m in range(2):
            S.op("sp", lambda e, m=m: e.dma_start(out=ccf[:, m, :, :], in_=ccs[m].rearrange("(kc p) n -> p kc n", p=128)), writes=[ccf_b], dma=True)
        S.op("dve", lambda e: e.tensor_copy(out=ccb[:], in_=ccf[:]), reads=[ccf_b], writes=[ccb_b])
        ugp = Rot(nc, st, "ug", 2, [128, 2, T], BF16)
        upm = st.enter_context(nc.sbuf_tensor(U("upm"), [128, 2, 2, HN], BF16))
        upm_b = Buf("upm")
        gcs = st.enter_context(nc.sbuf_tensor(U("gcs"), [128, 2, NT2, 256], BF16))
        gcs_b = Buf("gcs")
        e2 = st.enter_context(nc.sbuf_tensor(U("e2"), [128, 256], BF16))
        e2_b = Buf("e2")
        S.op("pool", lambda e: e.memset(e2[:, :], 0.0), writes=[e2_b])
        csp = Rot(nc, st, "csb", 3, [128, NT2, SB], BF16)
        ssp = Rot(nc, st, "ssb", 3, [128, NT2, SB], BF16)
        cxp = Rot(nc, st, "cxb", 3, [128, SB], BF16)
        yst = Rot(nc, st, "yst", 2, [128, SB], BF16)
        pa = Rot(nc, st, "pa", 3, [128, 512], F32, psum=True)
        pb = Rot(nc, st, "pb", 4, [128, 512], F32, psum=True)
        px = Rot(nc, st, "px", 1, [128, 512], F32, psum=True)
        for g in range(4):
            ug, ug_b = ugp.nxt()
            S.op("sp", lambda e, g=g, ug=ug: e.dma_start(out=ug[:, :, :], in_=ufT[g * 256:(g + 1) * 256, :].rearrange("(kc p) t -> p kc t", p=128)), reads=[uf_b], writes=[ug_b], dma=True)
            for kc in range(2):
                S.op("dve", lambda e, kc=kc, ug=ug: e.tensor_tensor(out=upm[:, 0, kc, 1:HN], in0=ug[:, kc, 1:HN], in1=ug[:, kc, T - 1:HN:-1], op=ALU.add), reads=[ug_b], writes=[upm_b])
                S.op("pool", lambda e, kc=kc, ug=ug: e.tensor_tensor(out=upm[:, 1, kc, 1:HN], in0=ug[:, kc, 1:HN], in1=ug[:, kc, T - 1:HN:-1], op=ALU.subtract), reads=[ug_b], writes=[upm_b])
                S.op("dve", lambda e, kc=kc, ug=ug: e.tensor_copy(out=upm[:, 0, kc, 0:1], in_=ug[:, kc, 0:1]), reads=[ug_b], writes=[upm_b])
                S.op("pool", lambda e, kc=kc: e.memset(upm[:, 1, kc, 0:1], 0.0), writes=[upm_b])
            x_t, x_b = px.nxt()
            for kc in range(2):
                S.op("pe", lambda e, kc=kc, ug=ug: e.matmul(x_t[0:1, 0:256], lhsT=ug[:, kc, HN:HN + 1], rhs=ccb[:, 0, kc, :], start=(kc == 0), stop=(kc == 1)),
                     reads=[ug_b, ccb_b], writes=[x_b])
            S.op("act", lambda e: e.copy(out=e2[0:1, :], in_=x_t[0:1, 0:256]), reads=[x_b], writes=[e2_b])
            for tt in range(NT2):
                p_t, p_b = pa.nxt()
                for m in range(2):
                    for kc in range(2):
                        S.op("pe", lambda e, p_t=p_t, m=m, kc=kc, tt=tt: e.matmul(p_t[:, m * 256:(m + 1) * 256], lhsT=upm[:, m, kc, tt * 128:(tt + 1) * 128], rhs=ccb[:, m, kc, :], start=(kc == 0), stop=(kc == 1)),
                             reads=[upm_b, ccb_b], writes=[p_b])
                S.op("act", lambda e, p_t=p_t, tt=tt: e.copy(out=gcs[:, :, tt, :], in_=p_t[:, :].rearrange("p (m c) -> p m c", m=2)), reads=[p_b], writes=[gcs_b])
            blk = {}

            def prep(sb):
                cs_t, cs_b = csp.nxt()
                ss_t, ss_b = ssp.nxt()
                cx_t, cx_b = cxp.nxt()
                S.op("sp", lambda e: e.dma_start(out=cs_t[:, :, :], in_=csT[0:HN, sb * SB:(sb + 1) * SB].rearrange("(tt p) n -> p tt n", p=128)), writes=[cs_b], dma=True)
                S.op("sp", lambda e: e.dma_start(out=ss_t[:, :, :], in_=ssT[0:HN, sb * SB:(sb + 1) * SB].rearrange("(tt p) n -> p tt n", p=128)), writes=[ss_b], dma=True)
                S.op("sp", lambda e: e.dma_start(out=cx_t[:, :], in_=csT[HN:HN + 128, sb * SB:(sb + 1) * SB]), writes=[cx_b], dma=True)
                blk[sb] = (cs_t, cs_b, ss_t, ss_b, cx_t, cx_b)

            def compute(sb, g=g):
                cs_t, cs_b, ss_t, ss_b, cx_t, cx_b = blk.pop(sb)
                for half in range(2):
                    p_t, p_b = pb.nxt()
                    for tt in range(NT2):
                        S.op("pe", lambda e, p_t=p_t, tt=tt, half=half: e.matmul(p_t[:, :SB], lhsT=gcs[:, 0, tt, half * 128:(half + 1) * 128], rhs=cs_t[:, tt, :], start=(tt == 0), stop=False),
                             reads=[gcs_b, cs_b], writes=[p_b])
                        S.op("pe", lambda e, p_t=p_t, tt=tt, half=half: e.matmul(p_t[:, :SB], lhsT=gcs[:, 1, tt, half * 128:(half + 1) * 128], rhs=ss_t[:, tt, :], start=False, stop=False),
                             reads=[gcs_b, ss_b], writes=[p_b])
                    S.op("pe", lambda e, p_t=p_t, half=half: e.matmul(p_t[:, :SB], lhsT=e2[:, half * 128:(half + 1) * 128], rhs=cx_t[:, :], start=False, stop=True),
                         reads=[e2_b, cx_b], writes=[p_b])
                    y_t, y_b = yst.nxt()
                    S.op("dve", lambda e, y_t=y_t, p_t=p_t: e.tensor_copy(out=y_t[:, :], in_=p_t[:, :SB]), reads=[p_b], writes=[y_b])
                    S.op("sp", lambda e, y_t=y_t, half=half: e.dma_start(out=yfT[g * 256 + half * 128: g * 256 + (half + 1) * 128, sb * SB:(sb + 1) * SB], in_=y_t[:, :]),
                         reads=[y_b], writes=[C.buf("yfT")], dma=True)

            pipeline(T // SB, prep, compute, depth=2)
        C.end_stage()


def fnet_host_consts():
    import ml_dtypes
    n = np.arange(256, dtype=np.float64)
    a = 2 * np.pi * np.outer(n, n) / 256.0
    ccs = np.stack([np.cos(a) / 16.0, -np.sin(a) / 16.0]).astype(np.float32)
    s = np.arange(T, dtype=np.int64)
    ph = (np.outer(s, s) % T).astype(np.float64) * (2 * np.pi / T)
    csT = (np.cos(ph) / 64.0).astype(np.float32).astype(ml_dtypes.bfloat16)
    ssT = (np.sin(ph) / 64.0).astype(np.float32).astype(ml_dtypes.bfloat16)
    return ccs, csT, ssT


def stage_merge(C, sc, w_branch, mergedT, wbr_c=None):
    nc, S = C.nc, C.S
    TQ = 1024
    ysrc = [(sc["ynaT"], C.buf("ynaT")), (sc["ymlaT"], C.buf("ymlaT")), (sc["yfT"], C.buf("yfT"))]
    gT, gT_b = sc["gT"], C.buf("gT")
    with contextlib.ExitStack() as st:
        yb = st.enter_context(nc.sbuf_tensor(U("yb"), [128, 24, TQ], BF16))
        yb_b = Buf("yb")
        wst = Rot(nc, st, "wst", 3, [128, 24, 128], F32)
        wbf = Rot(nc, st, "wbf", 3, [128, 24, 128], BF16)
        gtp = Rot(nc, st, "gt", 6, [128, 3, 512], BF16)
        m0p = Rot(nc, st, "m0", 2, [128, 512], F32)
        m1p = Rot(nc, st, "m1", 2, [128, 512], F32)
        m2p = Rot(nc, st, "m2", 2, [128, 512], F32)
        mo = Rot(nc, st, "mo", 2, [128, TQ], BF16)
        pp = Rot(nc, st, "pp", 6, [128, 512], F32, psum=True)
        jobs = [(tq, dc) for tq in range(T // TQ) for dc in range(16)]
        wjob = {}

        def prep(i):
            tq, dc = jobs[i]
            wb_t, wb_b = wbf.nxt()
            if wbr_c is not None:
                S.op("sp", lambda e: e.dma_start(out=wb_t[:, :, :].rearrange("p a b -> p (a b)"), in_=wbr_c[dc]), reads=[C.buf("wbr_c")], writes=[wb_b], dma=True)
            else:
                ws_t, ws_b = wst.nxt()
                for b in range(3):
                    S.op("sp", lambda e, b=b: e.dma_start(out=ws_t[:, b * 8:(b + 1) * 8, :], in_=w_branch[b, :, dc * 128:(dc + 1) * 128].rearrange("(kc p) n -> p kc n", p=128)),
                         writes=[ws_b], dma=True)
                cast_op(C, wb_t[:, :, :], ws_t[:, :, :], [ws_b], [wb_b])
            gts = []
            for tb in range(TQ // 512):
                g_t, g_b = gtp.nxt()
                t0 = tq * TQ
                S.op("sp", lambda e, g_t=g_t, tb=tb, t0=t0: e.dma_start(
                    out=g_t[:, :, :], in_=gT[:, t0 + tb * 512:t0 + (tb + 1) * 512].rearrange("(b c p) t -> c p b t", b=3, p=128)[dc]),
                    reads=[gT_b], writes=[g_b], dma=True)
                gts.append((g_t, g_b))
            wjob[i] = (wb_t, wb_b, gts)

        def compute(i):
            tq, dc = jobs[i]
            t0 = tq * TQ
            if dc == 0:
                for b in range(3):
                    S.op("sp", lambda e, b=b: e.dma_start(out=yb[:, b * 8:(b + 1) * 8, :], in_=ysrc[b][0][:, t0:t0 + TQ].rearrange("(kc p) t -> p kc t", p=128)),
                         reads=[ysrc[b][1]], writes=[yb_b], dma=True)
            wb_t, wb_b, gts = wjob.pop(i)
            o_t, o_b = mo.nxt()
            for tb in range(TQ // 512):
                g_t, g_b = gts[tb]
                ps = []
                for b in range(3):
                    p_t, p_b = pp.nxt()
                    for kc in range(8):
                        S.op("pe", lambda e, p_t=p_t, b=b, kc=kc, tb=tb: e.matmul(p_t[:, :], lhsT=wb_t[:, b * 8 + kc, :], rhs=yb[:, b * 8 + kc, tb * 512:(tb + 1) * 512], start=(kc == 0), stop=(kc == 7)),
                             reads=[wb_b, yb_b], writes=[p_b])
                    ps.append((p_t, p_b))
                ms = []
                for b, mp in enumerate((m0p, m1p, m2p)):
                    m_t, m_b = mp.nxt()
                    S.op("dve", lambda e, m_t=m_t, b=b, g_t=g_t, p_t=ps[b][0]: e.tensor_tensor(out=m_t[:, :], in0=p_t[:, :], in1=g_t[:, b, :], op=ALU.mult),
                         reads=[ps[b][1], g_b], writes=[m_b])
                    ms.append((m_t, m_b))
                S.op("pool", lambda e, a=ms[0][0], b_=ms[1][0]: e.tensor_tensor(out=a[:, :], in0=a[:, :], in1=b_[:, :], op=ALU.add),
                     reads=[ms[1][1]], writes=[ms[0][1]])
                S.op("pool", lambda e, a=ms[0][0], c_=ms[2][0], tb=tb: e.tensor_tensor(out=o_t[:, tb * 512:(tb + 1) * 512], in0=a[:, :], in1=c_[:, :], op=ALU.add),
                     reads=[ms[0][1], ms[2][1]], writes=[o_b])
            S.op("sp", lambda e: e.dma_start(out=mergedT[dc * 128:(dc + 1) * 128, t0:t0 + TQ], in_=o_t[:, :]), reads=[o_b], writes=[C.buf("mergedT")], dma=True)

        pipeline(len(jobs), prep, compute, depth=2)
        C.end_stage()


def stage_outproj(C, mergedT, w_o, h_in, h_in_b, h_out, h_out_b, wo_c=None):
    nc, S = C.nc, C.S
    TQ = 1024
    mg_b = C.buf("mergedT")
    with contextlib.ExitStack() as st:
        mt = st.enter_context(nc.sbuf_tensor(U("mt"), [128, 16, TQ], BF16))
        mt_b = Buf("mt")
        hacc = st.enter_context(nc.sbuf_tensor(U("hacc"), [128, 8, D], F32))
        hacc_b = [Buf("hacc%d" % i) for i in range(8)]
        wst = Rot(nc, st, "wst", 3, [128, 16, 128], F32)
        wow = Rot(nc, st, "wow", 2, [128, 16, 512], BF16)
        wo_cur = {}
        pp = Rot(nc, st, "pp", 4, [128, 512], F32, psum=True)
        jobs = [(tq, cc) for tq in range(T // TQ) for cc in range(16)]
        wjob = {}

        def prep(i):
            tq, cc = jobs[i]
            q = cc % 4
            if q == 0:
                wo_cur["t"] = wow.nxt()
            wb_t, wb_b = wo_cur["t"]
            if wo_c is not None:
                S.op("sp", lambda e: e.dma_start(out=wb_t[:, :, q * 128:(q + 1) * 128], in_=wo_c[cc].rearrange("p (a b) -> p a b", b=128)), reads=[C.buf("wo_c")], writes=[wb_b], dma=True)
            else:
                ws_t, ws_b = wst.nxt()
                S.op("sp", lambda e: e.dma_start(out=ws_t[:, :, :], in_=w_o[:, cc * 128:(cc + 1) * 128].rearrange("(kc p) n -> p kc n", p=128)), writes=[ws_b], dma=True)
                cast_op(C, wb_t[:, :, q * 128:(q + 1) * 128], ws_t[:, :, :], [ws_b], [wb_b])
            wjob[i] = (wb_t, wb_b)

        def compute(i):
            tq, cc = jobs[i]
            t0 = tq * TQ
            if cc == 0:
                S.op("sp", lambda e: e.dma_start(out=mt[:, :, :], in_=mergedT[:, t0:t0 + TQ].rearrange("(kc p) t -> p kc t", p=128)), reads=[mg_b], writes=[mt_b], dma=True)
                for tt in range(8):
                    S.op("sp", lambda e, tt=tt: e.dma_start(out=hacc[:, tt, :], in_=h_in[t0 + tt * 128:t0 + (tt + 1) * 128, :]), reads=[h_in_b], writes=[hacc_b[tt]], dma=True)
            wb_t, wb_b = wjob.pop(i)
            if cc % 4 == 3:
                c4 = cc // 4
                for tt in range(8):
                    p_t, p_b = pp.nxt()
                    for kc in range(16):
                        S.op("pe", lambda e, p_t=p_t, kc=kc, tt=tt: e.matmul(p_t[:, :], lhsT=mt[:, kc, tt * 128:(tt + 1) * 128], rhs=wb_t[:, kc, :], start=(kc == 0), stop=(kc == 15)),
                             reads=[wb_b, mt_b], writes=[p_b])
                    S.op("dve", lambda e, p_t=p_t, tt=tt: e.tensor_tensor(out=hacc[:, tt, c4 * 512:(c4 + 1) * 512], in0=hacc[:, tt, c4 * 512:(c4 + 1) * 512], in1=p_t[:, :], op=ALU.add),
                         reads=[p_b], writes=[hacc_b[tt]])
            if cc == 15:
                for tt in range(8):
                    S.op("sp", lambda e, tt=tt: e.dma_start(out=h_out[t0 + tt * 128:t0 + (tt + 1) * 128, :], in_=hacc[:, tt, :]), reads=[hacc_b[tt]], writes=[h_out_b], dma=True)

        pipeline(len(jobs), prep, compute, depth=2)
        C.end_stage()


def stage_route(C, h_ap, h_b, gain_ap, w_router, ident_f, xn2, idx_d, gate_d, NE=16, CAP=512):
    nc, S = C.nc, C.S
    with contextlib.ExitStack() as st:
        idf, idfb, idb, idbb = load_consts(C, st, ident_f)
        pools = norm_pools(C, st, nh=3)
        gain_t, gain_b = load_gain(C, st, "gain2", gain_ap, D)
        wr = st.enter_context(nc.sbuf_tensor(U("wr"), [128, 16, NE], F32))
        wr_b = Buf("wr")
        S.op("sp", lambda e: e.dma_start(out=wr[:, :, :], in_=w_router.rearrange("(kc p) n -> p kc n", p=128)), writes=[wr_b], dma=True)
        xf = Rot(nc, st, "xf", 3, [128, D], F32)
        xb = Rot(nc, st, "xb", 4, [128, D], BF16)
        xT = Rot(nc, st, "xT", 3, [128, 16, 128], F32)
        sm = Rot(nc, st, "sm", 4, [128, 8], F32)
        ex = Rot(nc, st, "ex", 4, [128, NE], F32)
        af = Rot(nc, st, "af", 4, [128, NE], F32)
        affT = st.enter_context(nc.sbuf_tensor(U("affT"), [NE, T], F32))
        affT2 = st.enter_context(nc.sbuf_tensor(U("affT2"), [NE, T], F32))
        affT_b, affT2_b = Buf("affT"), Buf("affT2")
        vals = st.enter_context(nc.sbuf_tensor(U("vals"), [NE, CAP], F32))
        idx = st.enter_context(nc.sbuf_tensor(U("idx"), [NE, CAP], U32))
        vals_b, idx_b = Buf("vals"), Buf("idx")
        ptr = Rot(nc, st, "ptr", 3, [128, 4, 128], F32, psum=True)
        pl = Rot(nc, st, "pl", 3, [128, 512], F32, psum=True)
        pt2 = Rot(nc, st, "pt2", 2, [128, 512], F32, psum=True)
        lg = {}

        def front(tt):
            x_t, x_b = xf.nxt()
            rms_tile(C, pools, h_ap[tt * 128:(tt + 1) * 128, :], gain_t, gain_b, x_t[:], x_b, D, [h_b])
            xb_t, xb_b = xb.nxt()
            S.op("act", lambda e: e.copy(out=xb_t[:, :], in_=x_t[:, :]), reads=[x_b], writes=[xb_b])
            xT_t, xT_b = xT.nxt()
            for g in range(4):
                p_t, p_b = ptr.nxt()
                for j in range(4):
                    kc = g * 4 + j
                    S.op("pe", lambda e, p_t=p_t, j=j, kc=kc: e.transpose(out=p_t[:, j, :], in_=x_t[:, kc * 128:(kc + 1) * 128], identity=idf[:]),
                         reads=[x_b, idfb], writes=[p_b])
                S.op("dve", lambda e, p_t=p_t, g=g: e.tensor_copy(out=xT_t[:, g * 4:(g + 1) * 4, :], in_=p_t[:, :, :]), reads=[p_b], writes=[xT_b])
            l_t, l_b = pl.nxt()
            for kc in range(16):
                S.op("pe", lambda e, kc=kc: e.matmul(l_t[:, :NE], lhsT=xT_t[:, kc, :], rhs=wr[:, kc, :], start=(kc == 0), stop=(kc == 15)),
                     reads=[xT_b, wr_b], writes=[l_b])
            lg[tt] = (l_t, l_b, xb_t, xb_b)

        def back(tt):
            l_t, l_b, xb_t, xb_b = lg.pop(tt)
            S.op("sp", lambda e: e.dma_start(out=xn2[tt * 128:(tt + 1) * 128, :], in_=xb_t[:, :]), reads=[xb_b], writes=[C.buf("xn2")], dma=True)
            s_t, s_b = sm.nxt()
            S.op("dve", lambda e: e.tensor_reduce(out=s_t[:, 0:1], in_=l_t[:, :NE], axis=AX.X, op=ALU.max, negate=True), reads=[l_b], writes=[s_b])
            e_t, e_b = ex.nxt()
            S.op("act", lambda e: e.activation(out=e_t[:, :], in_=l_t[:, :NE], func=AF.Exp, bias=s_t[:, 0:1], accum_out=s_t[:, 1:2]),
                 reads=[l_b, s_b], writes=[e_b, s_b])
            S.op("dve", lambda e: e.reciprocal(out=s_t[:, 2:3], in_=s_t[:, 1:2]), reads=[s_b], writes=[s_b])
            a_t, a_b = af.nxt()
            S.op("dve", lambda e: e.tensor_scalar(out=a_t[:, :], in0=e_t[:, :], scalar1=s_t[:, 2:3], scalar2=None, op0=ALU.mult), reads=[e_b, s_b], writes=[a_b])
            q_t, q_b = pt2.nxt()
            S.op("pe", lambda e: e.transpose(out=q_t[:NE, :128], in_=a_t[:, :], identity=idf[:]), reads=[a_b, idfb], writes=[q_b])
            S.op("dve", lambda e: e.tensor_copy(out=affT[:, tt * 128:(tt + 1) * 128], in_=q_t[:NE, :128]), reads=[q_b], writes=[affT_b])

        NT = T // 128
        SKR = 2
        for tt in range(NT + SKR):
            if tt < NT:
                front(tt)
            if tt - SKR >= 0:
                back(tt - SKR)
        cur, cur_b, oth, oth_b = affT, affT_b, affT2, affT2_b
        for r in range(CAP // 8):
            S.op("dve", lambda e, cur=cur, r=r: e.max(out=vals[:, r * 8:(r + 1) * 8], in_=cur[:, :]), reads=[cur_b], writes=[vals_b])
            S.op("dve", lambda e, cur=cur, r=r: e.max_index(out=idx[:, r * 8:(r + 1) * 8], in_max=vals[:, r * 8:(r + 1) * 8], in_values=cur[:, :]), reads=[cur_b, vals_b], writes=[idx_b])
            if r < CAP // 8 - 1:
                S.op("dve", lambda e, cur=cur, oth=oth, r=r: e.match_replace(out=oth[:, :], in_to_replace=vals[:, r * 8:(r + 1) * 8], in_values=cur[:, :], imm_value=-1.0),
                     reads=[cur_b, vals_b], writes=[oth_b])
                cur, cur_b, oth, oth_b = oth, oth_b, cur, cur_b
        S.op("sp", lambda e: e.dma_start(out=idx_d[:, :], in_=idx[:, :]), reads=[idx_b], writes=[C.buf("idx_d")], dma=True)
        S.op("sp", lambda e: e.dma_start(out=gate_d[:, :], in_=vals[:, :]), reads=[vals_b], writes=[C.buf("gate_d")], dma=True)
        C.end_stage()


def stage_experts(C, h_ap, h_b, xn2, idx_d, gate_d, w_g, w_u, w_d, ident_f, NE=16, CAP=512):
    nc, S = C.nc, C.S
    NJ = CAP // 128
    with contextlib.ExitStack() as st:
        idf, idfb, idb, idbb = load_consts(C, st, ident_f)
        idxc = st.enter_context(nc.sbuf_tensor(U("idxc"), [128, NE * NJ], U32))
        gatec = st.enter_context(nc.sbuf_tensor(U("gatec"), [128, NE * NJ], F32))
        idxc_b, gatec_b = Buf("idxc"), Buf("gatec")
        S.op("sp", lambda e: e.dma_start(out=idxc[:, :].rearrange("p (e j) -> p e j", j=NJ), in_=idx_d.rearrange("e (j p) -> p e j", p=128), allow_slow_non_contiguous=True),
             reads=[C.buf("idx_d")], writes=[idxc_b], dma=True)
        S.op("sp", lambda e: e.dma_start(out=gatec[:, :].rearrange("p (e j) -> p e j", j=NJ), in_=gate_d.rearrange("e (j p) -> p e j", p=128), allow_slow_non_contiguous=True),
             reads=[C.buf("gate_d")], writes=[gatec_b], dma=True)
        xg = Rot(nc, st, "xg", 4, [128, D], BF16)
        xeTp = Rot(nc, st, "xeT", 2, [128, 16, CAP], BF16)
        hT = st.enter_context(nc.sbuf_tensor(U("hT"), [128, 16, CAP], BF16))
        hT_b = Buf("hT")
        ye = st.enter_context(nc.sbuf_tensor(U("ye"), [128, NJ, D], F32))
        ye_b = [Buf("ye%d" % j) for j in range(NJ)]
        wst = Rot(nc, st, "wst", 3, [128, 16, 128], F32)
        wbf = Rot(nc, st, "wbf", 3, [128, 16, 128], BF16)
        sg = Rot(nc, st, "sg", 2, [128, CAP], F32)
        wdw = Rot(nc, st, "wdw", 2, [128, 16, 512], BF16)
        wd_cur = {}
        ptr = Rot(nc, st, "ptr", 2, [128, 8, 128], BF16, psum=True)
        pg = Rot(nc, st, "pg", 2, [128, 512], F32, psum=True)
        pu = Rot(nc, st, "pu", 2, [128, 512], F32, psum=True)
        pd = Rot(nc, st, "pd", 2, [128, 512], F32, psum=True)
        xn2_b = C.buf("xn2")

        def load_w(src):
            ws_t, ws_b = wst.nxt()
            S.op("sp", lambda e: e.dma_start(out=ws_t[:, :, :], in_=src.rearrange("(kc p) n -> p kc n", p=128)), writes=[ws_b], dma=True)
            wb_t, wb_b = wbf.nxt()
            cast_op(C, wb_t[:, :, :], ws_t[:, :, :], [ws_b], [wb_b])
            return wb_t, wb_b

        xe_of = {}

        def gather(ex):
            xeT, xeT_b = xeTp.nxt()
            xe_of[ex] = (xeT, xeT_b)
            for j in range(NJ):
                col = ex * NJ + j
                x_t, x_b = xg.nxt()
                S.op("pool", lambda e, x_t=x_t, col=col: e.indirect_dma_start(out=x_t[:, :], out_offset=None, in_=xn2[:, :], in_offset=bass.IndirectOffsetOnAxis(ap=idxc[:, col:col + 1], axis=0)),
                     reads=[xn2_b, idxc_b], writes=[x_b], dma=True)
                for g in range(2):
                    p_t, p_b = ptr.nxt()
                    for jj in range(8):
                        kc = g * 8 + jj
                        S.op("pe", lambda e, p_t=p_t, jj=jj, kc=kc, x_t=x_t: e.transpose(out=p_t[:, jj, :], in_=x_t[:, kc * 128:(kc + 1) * 128], identity=idb[:]),
                             reads=[x_b, idbb], writes=[p_b])
                    S.op("dve", lambda e, p_t=p_t, g=g, j=j: e.tensor_copy(out=xeT[:, g * 8:(g + 1) * 8, j * 128:(j + 1) * 128], in_=p_t[:]), reads=[p_b], writes=[xeT_b])

        jobs = []
        for ex in range(NE):
            for fc in range(16):
                jobs += [(ex, "g", fc), (ex, "u", fc)]
            jobs += [(ex, "d", dc) for dc in range(16)]
        wjob = {}
        sil = {}

        def prep(i):
            ex, kind, c = jobs[i]
            if kind == "d":
                q = c % 4
                if q == 0:
                    wd_cur["t"] = wdw.nxt()
                wt, wt_b = wd_cur["t"]
                ws_t, ws_b = wst.nxt()
                S.op("sp", lambda e: e.dma_start(out=ws_t[:, :, :], in_=w_d[ex, :, c * 128:(c + 1) * 128].rearrange("(kc p) n -> p kc n", p=128)), writes=[ws_b], dma=True)
                cast_op(C, wt[:, :, q * 128:(q + 1) * 128], ws_t[:, :, :], [ws_b], [wt_b])
                wjob[i] = (wt, wt_b)
                return
            src = {"g": w_g, "u": w_u}[kind]
            wjob[i] = load_w(src[ex, :, c * 128:(c + 1) * 128])

        deferred = []

        def run_deferred(i):
            while deferred and deferred[0][0] <= i:
                deferred.pop(0)[1]()

        def compute(i):
            run_deferred(i)
            ex, kind, c = jobs[i]
            if kind == "g":
                xeT, xeT_b = xe_of[ex]
                wg_t, wg_b = wjob.pop(i)
                g_t, g_b = pg.nxt()
                for kc in range(16):
                    S.op("pe", lambda e, kc=kc: e.matmul(g_t[:, :CAP], lhsT=wg_t[:, kc, :], rhs=xeT[:, kc, :], start=(kc == 0), stop=(kc == 15)),
                         reads=[wg_b, xeT_b], writes=[g_b])
                s_t, s_b = sg.nxt()
                S.op("act", lambda e: e.activation(out=s_t[:, :], in_=g_t[:, :CAP], func=AF.Silu), reads=[g_b], writes=[s_b])
                sil[(ex, c)] = (s_t, s_b)
            elif kind == "u":
                xeT, xeT_b = xe_of[ex]
                wu_t, wu_b = wjob.pop(i)
                u_t, u_b = pu.nxt()
                for kc in range(16):
                    S.op("pe", lambda e, kc=kc: e.matmul(u_t[:, :CAP], lhsT=wu_t[:, kc, :], rhs=xeT[:, kc, :], start=(kc == 0), stop=(kc == 15)),
                         reads=[wu_b, xeT_b], writes=[u_b])
                s_t, s_b = sil.pop((ex, c))
                S.op("dve", lambda e: e.tensor_tensor(out=hT[:, c, :], in0=u_t[:, :CAP], in1=s_t[:, :], op=ALU.mult), reads=[u_b, s_b], writes=[hT_b])
            else:
                dc = c
                if dc == 0 and ex + 1 < NE:
                    gather(ex + 1)
                wd_t, wd_b = wjob.pop(i)
                if dc % 4 == 3:
                    def wide(ex=ex, dc=dc, wd_t=wd_t, wd_b=wd_b):
                        d4 = dc // 4
                        for j in range(NJ):
                            col = ex * NJ + j
                            p_t, p_b = pd.nxt()
                            for fc in range(16):
                                S.op("pe", lambda e, p_t=p_t, j=j, fc=fc: e.matmul(p_t[:, :], lhsT=hT[:, fc, j * 128:(j + 1) * 128], rhs=wd_t[:, fc, :], start=(fc == 0), stop=(fc == 15)),
                                     reads=[wd_b, hT_b], writes=[p_b])
                            S.op("act", lambda e, p_t=p_t, j=j, col=col: e.activation(out=ye[:, j, d4 * 512:(d4 + 1) * 512], in_=p_t[:, :], func=AF.Copy, scale=gatec[:, col:col + 1]),
                                 reads=[p_b, gatec_b], writes=[ye_b[j]])
                        if dc == 15:
                            for j in range(NJ):
                                col = ex * NJ + j
                                S.op("pool", lambda e, j=j, col=col: e.indirect_dma_start(out=h_ap[:, :], out_offset=bass.IndirectOffsetOnAxis(ap=idxc[:, col:col + 1], axis=0), in_=ye[:, j, :], in_offset=None, compute_op=ALU.add),
                                     reads=[ye_b[j], idxc_b], writes=[h_b], dma=True)
                    deferred.append((i + 2, wide))

        gather(0)
        pipeline(len(jobs), prep, compute, depth=2)
        run_deferred(len(jobs) + 10)
        C.end_stage()


def stage_final(C, h_ap, h_b, gain_ap, y_ap, y_b):
    nc, S = C.nc, C.S
    with contextlib.ExitStack() as st:
        pools = norm_pools(C, st)
        gain_t, gain_b = load_gain(C, st, "gainf", gain_ap, D)
        of = Rot(nc, st, "of", 2, [128, D], F32)
        for tt in range(T // 128):
            o_t, o_b = of.nxt()
            rms_tile(C, pools, h_ap[tt * 128:(tt + 1) * 128, :], gain_t, gain_b, o_t[:], o_b, D, [h_b])
            S.op("sp", lambda e, o_t=o_t, tt=tt: e.dma_start(out=y_ap[tt * 128:(tt + 1) * 128, :], in_=o_t[:, :]), reads=[o_b], writes=[y_b], dma=True)
        C.end_stage()


NCORES = 4
NB = 4 // NCORES
DEPTH = 2
_CACHE = {}


def build_program():
    nc = bass.Bass("TRN2", target_bir_lowering=False)
    def inp(name, shape, dt=F32):
        return nc.dram_tensor(name, list(shape), dt, kind="ExternalInput").ap()
    x = inp("x", [NB * T, D])
    w_in = inp("w_in", [DEPTH, D, INC])
    b_gate = inp("b_gate", [DEPTH, 6144])
    w_uq = inp("w_uq", [DEPTH, 448, 1536])
    q_norm = inp("q_norm", [DEPTH, 448])
    w_ukv = inp("w_ukv", [DEPTH, 160, 2048])
    kv_norm = inp("kv_norm", [DEPTH, 160])
    rpbT = inp("rpbT", [DEPTH, 16, 128, 14, 64])
    maskc = inp("maskc", [128, 14, 64])
    w_branch = inp("w_branch", [DEPTH, 3, 1024, 2048])
    w_o = inp("w_o", [DEPTH, D, D])
    norm_mix = inp("norm_mix", [DEPTH, D])
    norm_moe = inp("norm_moe", [DEPTH, D])
    w_router = inp("w_router", [DEPTH, D, 16])
    w_g = inp("w_exp_gate", [DEPTH, 16, D, D])
    w_u = inp("w_exp_up", [DEPTH, 16, D, D])
    w_d = inp("w_exp_down", [DEPTH, 16, D, D])
    norm_final = inp("norm_final", [D])
    ident = inp("ident", [128, 128])
    cos2T = inp("cos2T", [64, T])
    sin2T = inp("sin2T", [64, T])
    ccs = inp("ccs", [2, 256, 256])
    csT = inp("csT", [T, T], BF16)
    ssT = inp("ssT", [T, T], BF16)
    y = nc.dram_tensor("y", [NB * T, D], F32, kind="ExternalOutput").ap()
    with contextlib.ExitStack() as es:
        C = Ctx(nc, es)
        sc = {"qkT": C.dram("qkT", [2048, T], BF16), "vna": C.dram("vna", [T, 1024], BF16), "cT": C.dram("cT", [672, T], F32),
              "ufT": C.dram("ufT", [1024, T], BF16), "gT": C.dram("gT", [6144, T], BF16),
              "ynaT": C.dram("ynaT", [1024, T], BF16), "ymlaT": C.dram("ymlaT", [1024, T], BF16), "yfT": C.dram("yfT", [1024, T], BF16)}
        mergedT = C.dram("mergedT", [2048, T], BF16)
        wbr_c = C.dram("wbr_c", [16, 128, 24 * 128], BF16)
        wo_c = C.dram("wo_c", [16, 128, 16 * 128], BF16)
        hA = C.dram("hA", [T, D], F32)
        xn2 = C.dram("xn2", [T, D], BF16)
        idx_d = C.dram("idx_d", [16, 512], U32)
        gate_d = C.dram("gate_d", [16, 512], F32)
        hA_b = C.buf("hA")
        x_b = Buf("x")
        y_b = C.buf("y")
        for b in range(NB):
            xb = x[b * T:(b + 1) * T, :]
            for l in range(DEPTH):
                h_in, h_in_b = (xb, x_b) if l == 0 else (hA, hA_b)
                stage_inproj(C, h_in, h_in_b, norm_mix[l], w_in[l], b_gate[l], ident, sc)
                stage_na(C, sc, rpbT[l], maskc, sc["ynaT"], wprep=(w_branch[l], w_o[l], wbr_c, wo_c))
                stage_mla(C, sc, w_uq[l], q_norm[l], w_ukv[l], kv_norm[l], cos2T, sin2T, sc["ymlaT"], ident_f=ident)
                stage_fnet(C, sc, ccs, csT, ssT, sc["yfT"])
                stage_merge(C, sc, w_branch[l], mergedT, wbr_c=wbr_c)
                stage_outproj(C, mergedT, w_o[l], h_in, h_in_b, hA, hA_b, wo_c=wo_c)
                stage_route(C, hA, hA_b, norm_moe[l], w_router[l], ident, xn2, idx_d, gate_d)
                stage_experts(C, hA, hA_b, xn2, idx_d, gate_d, w_g[l], w_u[l], w_d[l], ident)
            stage_final(C, hA, hA_b, norm_final, y[b * T:(b + 1) * T, :], y_b)
    return nc


def rope_consts():
    pos = np.arange(T, dtype=np.float32)
    inv = (1.0 / (10000.0 ** (np.arange(0, 64, 2, dtype=np.float32) / 64))).astype(np.float32)
    ang = pos[:, None] * inv[None, :]
    cos, sin = np.cos(ang).astype(np.float32), np.sin(ang).astype(np.float32)
    cos2T = np.ascontiguousarray(np.concatenate([cos, cos], 1).T)
    sin2T = np.ascontiguousarray(np.concatenate([-sin, sin], 1).T)
    return cos2T, sin2T


def kernel(x, w_in, b_gate, w_uq, q_norm, w_ukv, kv_norm, na_rpb, w_branch, w_o,
           norm_mix, norm_moe, w_router, w_exp_gate, w_exp_up, w_exp_down, norm_final):
    f = lambda a: np.ascontiguousarray(np.asarray(a, dtype=np.float32))
    if "nc" not in _CACHE:
        _CACHE["nc"] = build_program()
        cos2T, sin2T = rope_consts()
        ccs, csT, ssT = fnet_host_consts()
        _CACHE["consts"] = dict(cos2T=cos2T, sin2T=sin2T, ccs=ccs, csT=csT, ssT=ssT, ident=np.eye(128, dtype=np.float32))
    nc = _CACHE["nc"]
    na_rpb = f(na_rpb)
    tabs = [na_host_tables(na_rpb[l]) for l in range(DEPTH)]
    rpbT = np.stack([t[0] for t in tabs])
    maskc = tabs[0][1]
    shared = dict(w_in=f(w_in), b_gate=f(b_gate), w_uq=f(w_uq), q_norm=f(q_norm), w_ukv=f(w_ukv), kv_norm=f(kv_norm),
                  rpbT=rpbT, maskc=maskc, w_branch=f(w_branch), w_o=f(w_o), norm_mix=f(norm_mix), norm_moe=f(norm_moe),
                  w_router=f(w_router), w_exp_gate=f(w_exp_gate), w_exp_up=f(w_exp_up), w_exp_down=f(w_exp_down),
                  norm_final=f(norm_final), **_CACHE["consts"])
    xf = f(x).reshape(4 * T, D)
    in_maps = []
    for c in range(NCORES):
        m = dict(shared)
        m["x"] = xf[c * NB * T:(c + 1) * NB * T]
        in_maps.append(m)
    res = run_bass_kernel_spmd(nc, in_maps, core_ids=list(range(NCORES)))
    out = np.concatenate([np.asarray(r["y"], dtype=np.float32) for r in res.results], axis=0)
    return out.reshape(4, T, D)
```
